# Optimizing a Trainium2 kernel written in Bass

```python
import math
import jax, jax.numpy as jnp
from jax import lax
import numpy as np

D_MODEL = 2048
BATCH = 8
SEQ = 2048
DEPTH = 1

HEAD_DIM = 128
N_GDN_HEADS = 8
N_MOBA_HEADS = 8
GDN_WIDTH = N_GDN_HEADS * HEAD_DIM
MOBA_WIDTH = N_MOBA_HEADS * HEAD_DIM
MIX_WIDTH = GDN_WIDTH + MOBA_WIDTH
IN_PROJ_WIDTH = 4 * GDN_WIDTH + 2 * N_GDN_HEADS + 3 * MOBA_WIDTH
GDN_CONV = 4
GDN_CHUNK = 64
MOBA_BLOCK = 256
MOBA_TOPK = 3
MOBA_Q_CHUNK = 64
REL_BUCKETS = 32
REL_MAX_DIST = 128
MEM_LEN = 256
N_XATTN_HEADS = 4
XATTN_WIDTH = N_XATTN_HEADS * HEAD_DIM
D_FF = 5632
FFN_CONV = 3
EPS = 1e-6
NEG = -1e30

kernel_name = "hybrid_gdn_moba_parallel_heads"


def rmsnorm(x, g):
    xf = x.astype(jnp.float32)
    y = xf * lax.rsqrt(jnp.mean(xf * xf, axis=-1, keepdims=True) + EPS)
    return (y * g.astype(jnp.float32)).astype(x.dtype)


def l2norm(x):
    xf = x.astype(jnp.float32)
    return (xf * lax.rsqrt(jnp.sum(xf * xf, axis=-1, keepdims=True) + EPS)).astype(x.dtype)


def causal_dwconv(x, w):
    K = w.shape[0]
    S = x.shape[1]
    xp = jnp.pad(x, ((0, 0), (K - 1, 0), (0, 0)))
    return sum(xp[:, j:j + S] * w[j] for j in range(K))


def rel_bucket(rel):
    n = jnp.maximum(-rel, 0)
    max_exact = REL_BUCKETS // 2
    nf = jnp.maximum(n, 1).astype(jnp.float32)
    large = max_exact + (jnp.log(nf / max_exact) / math.log(REL_MAX_DIST / max_exact)
                         * (REL_BUCKETS - max_exact)).astype(jnp.int32)
    large = jnp.minimum(large, REL_BUCKETS - 1)
    return jnp.where(n < max_exact, n, large)


def gated_delta_rule(q, k, v, g, beta):
    out_dtype = v.dtype
    B, H, S, Dk = q.shape
    Dv = v.shape[-1]
    C = GDN_CHUNK
    N = S // C
    f32 = jnp.float32
    q = (q.astype(f32) * Dk ** -0.5).reshape(B, H, N, C, Dk)
    k = k.astype(f32).reshape(B, H, N, C, Dk)
    v = v.astype(f32).reshape(B, H, N, C, Dv)
    g = g.astype(f32).reshape(B, H, N, C)
    beta = beta.astype(f32).reshape(B, H, N, C)
    G = jnp.cumsum(g, axis=-1)
    tri_incl = jnp.tril(jnp.ones((C, C), dtype=bool))
    tri_strict = jnp.tril(jnp.ones((C, C), dtype=bool), -1)
    decay = jnp.exp(jnp.where(tri_incl, G[..., :, None] - G[..., None, :], NEG))
    kb = k * beta[..., None]
    A = jnp.where(tri_strict, jnp.einsum('bhnid,bhnjd->bhnij', kb, k) * decay, 0.0)
    eye = jnp.eye(C, dtype=f32)
    T = lax.linalg.triangular_solve(eye + A, jnp.broadcast_to(eye, A.shape),
                                    left_side=True, lower=True)
    u = jnp.einsum('bhnij,bhnjd->bhnid', T, v * beta[..., None])
    w = jnp.einsum('bhnij,bhnjd->bhnid', T, kb * jnp.exp(G)[..., None])
    qk = jnp.einsum('bhnid,bhnjd->bhnij', q, k) * decay
    q_dec = q * jnp.exp(G)[..., None]
    k_dec = k * jnp.exp(G[..., -1:] - G)[..., None]
    g_last = jnp.exp(G[..., -1])

    def step(state, xs):
        q_i, k_i, u_i, w_i, qk_i, gl_i = xs
        v_new = u_i - jnp.einsum('bhck,bhkv->bhcv', w_i, state)
        o_i = (jnp.einsum('bhck,bhkv->bhcv', q_i, state)
               + jnp.einsum('bhij,bhjv->bhiv', qk_i, v_new))
        state = state * gl_i[..., None, None] + jnp.einsum('bhck,bhcv->bhkv', k_i, v_new)
        return state, o_i

    xs = tuple(jnp.moveaxis(t, 2, 0) for t in (q_dec, k_dec, u, w, qk, g_last))
    state0 = jnp.zeros((B, H, Dk, Dv), f32)
    _, o = lax.scan(step, state0, xs)
    o = jnp.moveaxis(o, 0, 2).reshape(B, H, S, Dv)
    return o.astype(out_dtype)


def moba_attention(q, k, v, rel_bias):
    B, H, S, Dh = q.shape
    nb = -(-S // MOBA_BLOCK)
    s_pad = nb * MOBA_BLOCK
    pad = ((0, 0), (0, 0), (0, s_pad - S), (0, 0))
    k_blocks = jnp.pad(k, pad).reshape(B, H, nb, MOBA_BLOCK, Dh)
    v_blocks = jnp.pad(v, pad).reshape(B, H, nb, MOBA_BLOCK, Dh)
    k_mean = jnp.mean(k_blocks.astype(jnp.float32), axis=3).astype(k.dtype)
    n_sel = min(MOBA_TOPK, nb)
    scale = Dh ** -0.5
    n_chunks = S // MOBA_Q_CHUNK
    offs = jnp.arange(MOBA_BLOCK)
    rb_t = rel_bias.T
    head_idx = jnp.arange(H)[:, None, None, None]

    def per_batch(args):
        qb, kb, vb, kmb = args

        def per_chunk(c):
            q0 = c * MOBA_Q_CHUNK
            qc = lax.dynamic_slice_in_dim(qb, q0, MOBA_Q_CHUNK, axis=1)
            q_pos = q0 + jnp.arange(MOBA_Q_CHUNK)
            own = q0 // MOBA_BLOCK
            gate = jnp.einsum('hqd,hnd->hqn', qc, kmb).astype(jnp.float32)
            gate = jnp.where(jnp.arange(nb) < own, gate, NEG)
            _, idx = lax.top_k(gate, n_sel)
            valid = jnp.arange(n_sel) < own
            k_sel = jax.vmap(lambda blk, ix: blk[ix])(kb, idx)
            v_sel = jax.vmap(lambda blk, ix: blk[ix])(vb, idx)
            pos_sel = idx[..., None] * MOBA_BLOCK + offs
            bias_sel = rb_t[head_idx, rel_bucket(pos_sel - q_pos[None, :, None, None])]
            logit_sel = (jnp.einsum('hqd,hqnkd->hqnk', qc, k_sel).astype(jnp.float32) * scale
                         + bias_sel.astype(jnp.float32))
            logit_sel = jnp.where(valid[None, None, :, None], logit_sel, NEG)
            k_own = lax.dynamic_index_in_dim(kb, own, axis=1, keepdims=False)
            v_own = lax.dynamic_index_in_dim(vb, own, axis=1, keepdims=False)
            pos_own = own * MOBA_BLOCK + offs
            rel_own = pos_own[None, :] - q_pos[:, None]
            logit_own = (jnp.einsum('hqd,hkd->hqk', qc, k_own).astype(jnp.float32) * scale
                         + rb_t[:, rel_bucket(rel_own)].astype(jnp.float32))
            logit_own = jnp.where((rel_own <= 0)[None], logit_own, NEG)
            logits = jnp.concatenate(
                [logit_sel.reshape(H, MOBA_Q_CHUNK, n_sel * MOBA_BLOCK), logit_own], axis=-1)
            p = jax.nn.softmax(logits, axis=-1).astype(v.dtype)
            p_sel = p[..., :n_sel * MOBA_BLOCK].reshape(H, MOBA_Q_CHUNK, n_sel, MOBA_BLOCK)
            p_own = p[..., n_sel * MOBA_BLOCK:]
            return (jnp.einsum('hqnk,hqnkd->hqd', p_sel, v_sel)
                    + jnp.einsum('hqk,hkd->hqd', p_own, v_own))

        o = lax.map(per_chunk, jnp.arange(n_chunks))
        return o.transpose(1, 0, 2, 3).reshape(H, S, Dh)

    return lax.map(per_batch, (q, k_blocks, v_blocks, k_mean))


def to_heads(t, n_heads):
    B, S, _ = t.shape
    return t.reshape(B, S, n_heads, HEAD_DIM).transpose(0, 2, 1, 3)


def hybrid_mixer(x, norm_g, w_in, gdn_conv_w, a_log, dt_bias, gdn_norm_g,
                 moba_norm_g, rel_bias, w_out):
    B, S, _ = x.shape
    h = rmsnorm(x, norm_g)
    proj = h @ w_in
    i0 = 3 * GDN_WIDTH
    i1 = i0 + GDN_WIDTH
    i2 = i1 + N_GDN_HEADS
    i3 = i2 + N_GDN_HEADS
    qkv_a, z_a, b_a, a_a, qkv_b = (proj[..., :i0], proj[..., i0:i1], proj[..., i1:i2],
                                   proj[..., i2:i3], proj[..., i3:])
    qkv_a = jax.nn.silu(causal_dwconv(qkv_a, gdn_conv_w))
    q_a, k_a, v_a = [to_heads(t, N_GDN_HEADS) for t in jnp.split(qkv_a, 3, axis=-1)]
    beta = jax.nn.sigmoid(b_a).transpose(0, 2, 1)
    g = (-jnp.exp(a_log) * jax.nn.softplus(a_a + dt_bias)).transpose(0, 2, 1)
    o_a = gated_delta_rule(l2norm(q_a), l2norm(k_a), v_a, g, beta).transpose(0, 2, 1, 3)
    o_a = rmsnorm(o_a, gdn_norm_g) * jax.nn.silu(z_a.reshape(B, S, N_GDN_HEADS, HEAD_DIM))
    q_b, k_b, v_b = [to_heads(t, N_MOBA_HEADS) for t in jnp.split(qkv_b, 3, axis=-1)]
    o_b = moba_attention(q_b, k_b, v_b, rel_bias).transpose(0, 2, 1, 3)
    o_b = rmsnorm(o_b, moba_norm_g)
    o = jnp.concatenate([o_a.reshape(B, S, GDN_WIDTH), o_b.reshape(B, S, MOBA_WIDTH)], axis=-1)
    return o @ w_out


def cross_attention(x, mem, norm_g, mem_norm_g, w_xq, w_xkv, w_xo):
    B, S, _ = x.shape
    M = mem.shape[1]
    q = (rmsnorm(x, norm_g) @ w_xq).reshape(B, S, N_XATTN_HEADS, HEAD_DIM)
    kv = rmsnorm(mem, mem_norm_g) @ w_xkv
    k, v = [t.reshape(B, M, N_XATTN_HEADS, HEAD_DIM) for t in jnp.split(kv, 2, axis=-1)]
    logits = jnp.einsum('bshd,bmhd->bhsm', q, k).astype(jnp.float32) * HEAD_DIM ** -0.5
    p = jax.nn.softmax(logits, axis=-1).astype(v.dtype)
    o = jnp.einsum('bhsm,bmhd->bshd', p, v).reshape(B, S, XATTN_WIDTH)
    return o @ w_xo


def conv_ffn(x, norm_g, w_gate, w_up, conv_w, conv_b, w_down):
    h = rmsnorm(x, norm_g)
    gate = causal_dwconv(h @ w_gate, conv_w) + conv_b
    return (jax.nn.silu(gate) * (h @ w_up)) @ w_down


def setup_inputs(seed: int = 0) -> dict:
    key = jax.random.key(seed)
    ks = jax.random.split(key, 24)
    f32 = jnp.float32
    L = DEPTH

    def nrm(k, shape, scale):
        return jax.random.normal(k, shape, f32) * scale

    def gain(k, shape):
        return 1.0 + 0.02 * jax.random.normal(k, shape, f32)

    dt = jnp.exp(jax.random.uniform(ks[6], (L, N_GDN_HEADS), f32, math.log(1e-3), math.log(1e-1)))
    return {
        "x": nrm(ks[0], (BATCH, SEQ, D_MODEL), 1.0),
        "mem": nrm(ks[1], (BATCH, MEM_LEN, D_MODEL), 1.0),
        "mix_norm_g": gain(ks[2], (L, D_MODEL)),
        "w_in": nrm(ks[3], (L, D_MODEL, IN_PROJ_WIDTH), D_MODEL ** -0.5),
        "gdn_conv_w": nrm(ks[4], (L, GDN_CONV, 3 * GDN_WIDTH), GDN_CONV ** -0.5),
        "gdn_a_log": jnp.log(jax.random.uniform(ks[5], (L, N_GDN_HEADS), f32, 1.0, 16.0)),
        "gdn_dt_bias": dt + jnp.log(-jnp.expm1(-dt)),
        "gdn_norm_g": gain(ks[7], (L, HEAD_DIM)),
        "moba_norm_g": gain(ks[8], (L, HEAD_DIM)),
        "rel_bias": nrm(ks[9], (REL_BUCKETS, N_MOBA_HEADS), 0.5),
        "w_out": nrm(ks[10], (L, MIX_WIDTH, D_MODEL), MIX_WIDTH ** -0.5),
        "xattn_norm_g": gain(ks[11], (L, D_MODEL)),
        "mem_norm_g": gain(ks[12], (L, D_MODEL)),
        "w_xq": nrm(ks[13], (L, D_MODEL, XATTN_WIDTH), D_MODEL ** -0.5),
        "w_xkv": nrm(ks[14], (L, D_MODEL, 2 * XATTN_WIDTH), D_MODEL ** -0.5),
        "w_xo": nrm(ks[15], (L, XATTN_WIDTH, D_MODEL), XATTN_WIDTH ** -0.5),
        "ffn_norm_g": gain(ks[16], (L, D_MODEL)),
        "w_gate": nrm(ks[17], (L, D_MODEL, D_FF), D_MODEL ** -0.5),
        "w_up": nrm(ks[18], (L, D_MODEL, D_FF), D_MODEL ** -0.5),
        "ffn_conv_w": nrm(ks[19], (L, FFN_CONV, D_FF), FFN_CONV ** -0.5),
        "ffn_conv_b": nrm(ks[20], (L, D_FF), 0.02),
        "w_down": nrm(ks[21], (L, D_FF, D_MODEL), D_FF ** -0.5),
        "final_norm_g": gain(ks[22], (D_MODEL,)),
    }


def reference(x, mem, mix_norm_g, w_in, gdn_conv_w, gdn_a_log, gdn_dt_bias, gdn_norm_g,
              moba_norm_g, rel_bias, w_out, xattn_norm_g, mem_norm_g, w_xq, w_xkv, w_xo,
              ffn_norm_g, w_gate, w_up, ffn_conv_w, ffn_conv_b, w_down, final_norm_g):
    for l in range(DEPTH):
        x = x + hybrid_mixer(x, mix_norm_g[l], w_in[l], gdn_conv_w[l], gdn_a_log[l],
                             gdn_dt_bias[l], gdn_norm_g[l], moba_norm_g[l], rel_bias, w_out[l])
        x = x + cross_attention(x, mem, xattn_norm_g[l], mem_norm_g[l], w_xq[l], w_xkv[l], w_xo[l])
        x = x + conv_ffn(x, ffn_norm_g[l], w_gate[l], w_up[l], ffn_conv_w[l], ffn_conv_b[l], w_down[l])
    return rmsnorm(x, final_norm_g)
```

```python
import bisect
import math
from contextlib import ExitStack

import numpy as np
import concourse.bass as bass
import concourse.mybir as mybir
from concourse.bass_utils import run_bass_kernel_spmd

F32 = mybir.dt.float32
BF16 = mybir.dt.bfloat16
ALU = mybir.AluOpType
AF = mybir.ActivationFunctionType
AX = mybir.AxisListType

S = 2048
D = 2048
NT = 16
H = 8
HD = 128
DFF = 5632
NFF = 44
MEM = 256
INW = 7184
EPS = 1e-6
BIG = 30000.0
SQ128 = math.sqrt(128.0)

ENGS = ("pe", "act", "dve", "pool", "sp")
SEM_LIMIT = 2000
N_ENG_SEMS = {"pe": 16, "act": 16, "dve": 16, "pool": 8, "sp": 1}
N_DMA_SEMS = 40


class Buf:
    __slots__ = ("name", "last_w", "creads", "dreads")

    def __init__(self, name=""):
        self.name = name
        self.last_w = None
        self.creads = {}
        self.dreads = []


class Op:
    __slots__ = ("eng", "name", "kw", "waits", "signal", "pos")

    def __init__(self, eng, name, kw, pos):
        self.eng = eng
        self.name = name
        self.kw = kw
        self.waits = []
        self.signal = None
        self.pos = pos


class Prog:
    def __init__(self, nc, stack):
        self.nc = nc
        self.stack = stack
        self.ops = {e: [] for e in ENGS}
        self.nsig = {e: 0 for e in ENGS}
        self.sigpos = {e: [] for e in ENGS}
        self.sigtok = {e: [] for e in ENGS}
        self.waited = {e: {} for e in ENGS}
        self.esems = {e: [stack.enter_context(nc.semaphore(f"s_{e}_{i}")) for i in range(N_ENG_SEMS[e])]
                      for e in ENGS}
        self.dsems = [stack.enter_context(nc.semaphore(f"s_dma_{i}")) for i in range(N_DMA_SEMS)]
        self.ndma = 0
        self.dma_toks = []

    def sbuf(self, name, shape, dtype):
        return self.stack.enter_context(self.nc.sbuf_tensor(name, list(shape), dtype))

    def psum(self, name, shape, dtype):
        return self.stack.enter_context(self.nc.psum_tensor(name, list(shape), dtype))

    def buf(self, name=""):
        return Buf(name)

    def bufs(self, n):
        return [Buf() for _ in range(n)]

    def _force_signal(self, eng):
        lst = self.ops[eng]
        if not lst:
            return None
        last = lst[-1]
        if last.signal is None:
            n = self.nsig[eng]
            self.nsig[eng] += 1
            sem = self.esems[eng][n // SEM_LIMIT]
            val = n % SEM_LIMIT + 1
            last.signal = (sem, 1)
            self.sigpos[eng].append(last.pos)
            self.sigtok[eng].append((sem, val))
        return last

    def _resolve(self, tok):
        if tok[0] == "d":
            return tok[1], tok[2]
        _, eng, pos = tok
        sp = self.sigpos[eng]
        i = bisect.bisect_left(sp, pos)
        if i < len(sp):
            return self.sigtok[eng][i]
        last = self._force_signal(eng)
        assert last.pos >= pos
        i = bisect.bisect_left(sp, pos)
        return self.sigtok[eng][i]

    def _add_wait(self, op, tok):
        sem, val = self._resolve(tok)
        w = self.waited[op.eng]
        key = id(sem)
        if w.get(key, 0) >= val:
            return
        w[key] = val
        for i, (s, v) in enumerate(op.waits):
            if s is sem:
                op.waits[i] = (sem, max(v, val))
                return
        op.waits.append((sem, val))

    def _deps(self, op, reads, writes, is_dma):
        eng = op.eng
        for b in reads:
            if b.last_w is not None:
                self._add_wait(op, b.last_w)
        for b in writes:
            t = b.last_w
            if t is not None and not (t[0] == "c" and t[1] == eng and not is_dma):
                self._add_wait(op, t)
            for e, t in b.creads.items():
                if e == eng and not is_dma:
                    continue
                self._add_wait(op, t)
            for t in b.dreads:
                self._add_wait(op, t)

    def _compute_pos_check(self, eng):
        lst = self.ops[eng]
        if lst and lst[-1].signal is None and lst[-1].name not in ("nop", "dma_start"):
            self._force_signal(eng)

    def op(self, eng, name, reads=(), writes=(), **kw):
        lst = self.ops[eng]
        o = Op(eng, name, kw, len(lst))
        self._deps(o, reads, writes, False)
        lst.append(o)
        tok = ("c", eng, o.pos)
        for b in reads:
            b.creads[eng] = tok
        for b in writes:
            b.last_w = tok
            b.creads = {}
            b.dreads = []
        return o

    def dma(self, out, in_, reads=(), writes=(), q="sp", **kw):
        if q != "sp":
            self._compute_pos_check(q)
        lst = self.ops[q]
        o = Op(q, "dma_start", dict(out=out, in_=in_, **kw), len(lst))
        self._deps(o, reads, writes, True)
        i = self.ndma % N_DMA_SEMS
        r = self.ndma // N_DMA_SEMS
        self.ndma += 1
        sem = self.dsems[i]
        if r > 0:
            self._add_wait(o, ("d", sem, 16 * r))
        o.signal = (sem, 16)
        lst.append(o)
        tok = ("d", sem, 16 * (r + 1))
        self.dma_toks.append(tok)
        for b in reads:
            b.dreads.append(tok)
        for b in writes:
            b.last_w = tok
            b.creads = {}
            b.dreads = []
        return tok

    def barrier(self):
        toks = list(self.dma_toks[-N_DMA_SEMS:])
        for e in ENGS:
            lst = self.ops[e]
            if lst and lst[-1].name not in ("dma_start", "nop"):
                last = self._force_signal(e)
                toks.append(("c", e, last.pos))
            else:
                for o in reversed(lst):
                    if o.name not in ("dma_start", "nop"):
                        assert o.signal is not None
                        toks.append(("c", e, o.pos))
                        break
        for e in ENGS:
            o = Op(e, "nop", {}, len(self.ops[e]))
            for t in toks:
                if t[0] == "c" and t[1] == e:
                    continue
                self._add_wait(o, t)
            self.ops[e].append(o)

    def final_wait(self, eng, toks):
        o = Op(eng, "nop", {}, len(self.ops[eng]))
        for t in toks:
            self._add_wait(o, t)
        self.ops[eng].append(o)

    def emit(self):
        nc = self.nc
        prog = self
        with nc.Block() as block:
            def run(engname):
                def body(e):
                    for o in prog.ops[engname]:
                        for (sem, val) in o.waits:
                            e.wait_ge(sem, val)
                        if o.name == "nop":
                            continue
                        ins = getattr(e, o.name)(**o.kw)
                        if o.signal is not None:
                            ins.then_inc(o.signal[0], o.signal[1])
                return body
            block.tensor(run("pe"))
            block.scalar(run("act"))
            block.vector(run("dve"))
            block.gpsimd(run("pool"))
            block.sync(run("sp"))

    def stats(self):
        return {e: (len(self.ops[e]), self.nsig[e]) for e in ENGS}, self.ndma


class Arena:
    def __init__(self, p, nbytes):
        self.t = p.sbuf("arena", [128, nbytes // 2], BF16)
        self.n = nbytes
        self.off = 0

    def reset(self):
        self.off = 0

    def alloc(self, shape, dtype):
        esz = 2 if dtype == BF16 else 4
        n = 1
        for s in shape:
            n *= s
        nb = n * esz
        a = self.t[:, self.off // 2:(self.off + nb) // 2]
        self.off += (nb + 63) // 64 * 64
        assert self.off <= self.n, f"arena overflow {self.off} > {self.n}"
        if dtype != BF16:
            a = a.bitcast(dtype)
        if len(shape) == 2:
            a = a.rearrange("p (a b) -> p a b", a=shape[0])
        elif len(shape) == 3:
            a = a.rearrange("p (a b c) -> p a b c", a=shape[0], b=shape[1])
        return a


class Ring:
    def __init__(self, items):
        self.items = items
        self.i = 0

    def next(self):
        it = self.items[self.i % len(self.items)]
        self.i += 1
        return it


NCF = 576
NCB = 768 + 9 * 128


def host_consts():
    cf = np.zeros((128, NCF), np.float32)
    ii = np.arange(128)
    cf[:, 0:128] = np.eye(128, dtype=np.float32)
    cf[:, 128:256] = (ii[:, None] <= ii[None, :]).astype(np.float32)
    cf[:, 256:384] = (ii[:, None] > ii[None, :]).astype(np.float32)
    cf[:, 384:512] = 1.0
    gm = np.zeros((128, 8, 8), np.float32)
    for qi in range(8, 16):
        own = qi // 2
        gm[:, qi - 8, own:] = -1e30
    cf[:, 512:576] = gm.reshape(128, 64)
    cb = np.zeros((128, NCB), np.float32)
    cb[:, 0:128] = np.eye(128, dtype=np.float32)
    cb[:, 128:256] = 1.0
    cb[:, 256:384] = (ii[None, :] > ii[:, None]).astype(np.float32) * BIG
    cb[:, 384:512] = (ii[None, :] >= ii[:, None]).astype(np.float32) * BIG
    rr = np.arange(256)
    cb[:, 512:768] = (rr[None, :] < ii[:, None]).astype(np.float32) * (-BIG * SQ128)
    e = np.zeros((9, 9, 128), np.float32)
    for v in range(9):
        e[8, v, :] = 1.0
        if v < 8:
            e[v, v, :] = 1.0
    cb[0:9, 768:768 + 9 * 128] = e.reshape(9, 9 * 128)
    return cf, cb


def rel_bucket_np(n):
    n = np.maximum(n, 0)
    max_exact = 16
    nf = np.maximum(n, 1).astype(np.float32)
    large = max_exact + (np.log(nf / max_exact) / math.log(128 / max_exact) * (32 - max_exact)).astype(np.int32)
    large = np.minimum(large, 31)
    return np.where(n < max_exact, n, large)


def toeplitz_index():
    kk = np.arange(128)[:, None]
    r = np.arange(256)[None, :]
    return rel_bucket_np(r - kk)


class K:
    pass


def build_program(debug_phase=None):
    nc = bass.Bass("TRN2", target_bir_lowering=False)
    k = K()
    k.nc = nc
    dbg = debug_phase is not None

    order = ["inproj", "gdn", "moba", "wout", "xattn", "ffn"]
    need_from = {"mem": "xattn", "w_out": "wout", "w_xq": "xattn", "w_xkv": "xattn", "w_xo": "xattn",
                 "w_gate": "ffn", "w_up": "ffn", "w_down": "ffn"}
    k.skip = set()
    if dbg:
        for nm, ph in need_from.items():
            if order.index(ph) > order.index(debug_phase):
                k.skip.add(nm)

    def din(name, shape, dt=F32):
        if name in k.skip:
            shape = [1, 1]
        return nc.dram_tensor(name, list(shape), dt, kind="ExternalInput").ap()

    DUMP = {"inproj": ("QA", "KA", "KT", "VA", "ZS", "QB", "KB", "VB", "DBG"), "moba": ("OT",), "gdn": ("OT",),
            "wout": ("X1",), "xattn": ("X2",), "ffn": ("OT", "X1", "X2")}.get(debug_phase, ())

    def dscr(name, shape, dt=BF16):
        kind = "ExternalOutput" if name in DUMP else "Internal"
        return nc.dram_tensor(name, list(shape), dt, kind=kind).ap()

    I = {}
    I["x"] = din("x", [S, D])
    I["mem"] = din("mem", [MEM, D])
    for nm in ("mix_norm_g", "xattn_norm_g", "mem_norm_g", "ffn_norm_g", "final_norm_g"):
        I[nm] = din(nm, [1, D])
    I["w_in"] = din("w_in", [D, INW])
    I["gdn_conv_w"] = din("gdn_conv_w", [4, 3072])
    I["gdn_a_log"] = din("gdn_a_log", [1, 8])
    I["gdn_dt_bias"] = din("gdn_dt_bias", [1, 8])
    I["gdn_norm_g"] = din("gdn_norm_g", [1, 128])
    I["moba_norm_g"] = din("moba_norm_g", [1, 128])
    I["rel_bias"] = din("rel_bias", [1, 256])
    I["rb_toep"] = din("rb_toep", [128, 8, 256])
    I["w_out"] = din("w_out", [D, D])
    I["w_xq"] = din("w_xq", [D, 512])
    I["w_xkv"] = din("w_xkv", [D, 1024])
    I["w_xo"] = din("w_xo", [512, D])
    I["w_gate"] = din("w_gate", [D, DFF])
    I["w_up"] = din("w_up", [D, DFF])
    I["ffn_conv_w"] = din("ffn_conv_w", [3, DFF])
    I["ffn_conv_b"] = din("ffn_conv_b", [1, DFF])
    I["w_down"] = din("w_down", [DFF, D])
    I["cst_f"] = din("cst_f", [128, NCF])
    I["cst_b"] = din("cst_b", [128, NCB])
    out = nc.dram_tensor("out", [S, D], F32, kind="ExternalOutput").ap()
    k.I = I
    k.out = out

    k.QA = dscr("QA", [128, 16, 8, 128])
    k.KA = dscr("KA", [128, 16, 8, 128])
    k.KT = dscr("KT", [128, 16, 8, 128])
    k.VA = dscr("VA", [128, 16, 8, 128])
    k.ZS = dscr("ZS", [128, 16, 8, 128])
    k.QB = dscr("QB", [8, 128, 2048])
    k.KB = dscr("KB", [8, 128, 2048])
    k.VB = dscr("VB", [8, 128, 16, 128])
    k.X1 = dscr("X1", [S, D], F32)
    k.X2 = dscr("X2", [S, D], F32)
    if "DBG" in DUMP:
        k.DBG = dscr("DBG", [128, 4096], F32)
    if "OT" in DUMP:
        k.OT = dscr("OT", [128, 16, S], BF16)
    k.b_scr = {nm: Buf(nm) for nm in ("QA", "KA", "KT", "VA", "ZS", "QB", "KB", "VB", "X1", "X2", "DBG")}

    with ExitStack() as st:
        p = Prog(nc, st)
        k.p = p
        k.cf = p.sbuf("cf", [128, NCF], F32)
        k.cb = p.sbuf("cb", [128, NCB], BF16)
        k.b_c = p.buf("consts")
        p.dma(k.cf[:], I["cst_f"], writes=[k.b_c])
        p.dma(k.cb[:], I["cst_b"], writes=[k.b_c], q="pool")
        k.identf = k.cf[:, 0:128]
        k.trif = k.cf[:, 128:256]
        k.suf = k.cf[:, 256:384]
        k.onesf = k.cf[:, 384:512]
        k.gm = k.cf[:, 512:576]
        k.ident = k.cb[:, 0:128]
        k.ones = k.cb[:, 128:256]
        k.umq = k.cb[:, 256:384]
        k.uma = k.cb[:, 384:512]
        k.cm = k.cb[:, 512:768]
        k.emat = k.cb[0:9, 768:768 + 9 * 128].rearrange("p (v n) -> p v n", v=9)
        k.BA = p.sbuf("BA", [128, 16, 16], F32)
        k.b_BA = p.buf("BA")
        k.BETA = p.sbuf("BETA", [128, 16, 8], F32)
        k.GG = p.sbuf("GG", [128, 16, 8], F32)
        k.b_bg = p.buf("betag")
        banks = [(p.psum(f"ps{i}", [128, 512], F32), p.buf(f"ps{i}")) for i in range(8)]
        k.ps = Ring(banks[0:6])
        k.psx = banks[6:8]
        k.banks = banks
        k.ar = Arena(p, 176 * 1024)

        phases = [phase_inproj, phase_gdn, phase_moba, phase_wout, phase_xattn, phase_ffn]
        names = ["inproj", "gdn", "moba", "wout", "xattn", "ffn"]
        out_toks = []
        for fn, nm in zip(phases, names):
            r = fn(k)
            if r:
                out_toks += r
            p.barrier()
            if debug_phase == nm:
                break
        p.final_wait("sp", p.dma_toks[-N_DMA_SEMS:])
        k.stats = p.stats()
        p.emit()
    return nc, k


def MM(k, out, lhsT, rhs, start, stop, r, w):
    k.p.op("pe", "matmul", reads=r, writes=w, out=out, lhsT=lhsT, rhs=rhs, start=start, stop=stop)


def TR(k, out, in_, ident, r, w):
    k.p.op("pe", "transpose", reads=r + [k.b_c], writes=w, out=out, in_=in_, identity=ident)


def ACT(k, out, in_, func, r, w, **kw):
    k.p.op("act", "activation", reads=r, writes=w, out=out, in_=in_, func=func, **kw)


def CP(k, eng, out, in_, r, w):
    if eng == "act":
        k.p.op("act", "copy", reads=r, writes=w, out=out, in_=in_)
    else:
        k.p.op(eng, "tensor_copy", reads=r, writes=w, out=out, in_=in_)


def TT(k, eng, out, in0, in1, op, r, w):
    k.p.op(eng, "tensor_tensor", reads=r, writes=w, out=out, in0=in0, in1=in1, op=op)


def TS(k, eng, out, in0, s1, s2, op0, op1, r, w):
    if s2 is None:
        k.p.op(eng, "tensor_scalar", reads=r, writes=w, out=out, in0=in0, scalar1=s1, scalar2=None, op0=op0)
    else:
        k.p.op(eng, "tensor_scalar", reads=r, writes=w, out=out, in0=in0, scalar1=s1, scalar2=s2, op0=op0, op1=op1)


def STT(k, eng, out, in0, scalar, in1, op0, op1, r, w):
    k.p.op(eng, "scalar_tensor_tensor", reads=r, writes=w, out=out, in0=in0, scalar=scalar, in1=in1,
           op0=op0, op1=op1)


def norm_T(k, tiles, gain, dstT, b_dst, col0=0):
    p = k.p
    ar = k.ar
    gb = ar.alloc((D,), F32)
    b_gb = p.buf()
    p.dma(gb, gain.partition_broadcast(128), writes=[b_gb])
    xt = [ar.alloc((D,), F32) for _ in range(2)]
    b_xt = p.bufs(2)
    junk = ar.alloc((D,), BF16)
    b_junk = p.buf()
    hb = [ar.alloc((D,), BF16) for _ in range(2)]
    b_hb = p.bufs(2)
    ss = ar.alloc((len(tiles),), F32)
    b_ss = p.buf()
    for t, (src, rb) in enumerate(tiles):
        i = t % 2
        p.dma(xt[i], src, reads=rb, writes=[b_xt[i]])
        ACT(k, junk, xt[i], AF.Square, [b_xt[i]], [b_junk, b_ss], accum_out=ss[:, t:t + 1])
        ACT(k, ss[:, t:t + 1], ss[:, t:t + 1], AF.Sqrt, [b_ss], [b_ss], scale=1.0 / D, bias=EPS)
        p.op("dve", "reciprocal", reads=[b_ss], writes=[b_ss], out=ss[:, t:t + 1], in_=ss[:, t:t + 1])
        STT(k, "dve", hb[i], xt[i], ss[:, t:t + 1], gb, ALU.mult, ALU.mult, [b_xt[i], b_ss, b_gb], [b_hb[i]])
        for k4 in range(4):
            pb, bpb = k.ps.next()
            pbv = pb[:].bitcast(BF16)
            for j in range(4):
                kt = k4 * 4 + j
                TR(k, pbv[:, j * 128:(j + 1) * 128], hb[i][:, kt * 128:(kt + 1) * 128], k.ident, [b_hb[i]], [bpb])
            CP(k, "act" if k4 % 2 else "dve",
               dstT[:, k4 * 4:(k4 + 1) * 4, col0 + t * 128:col0 + (t + 1) * 128],
               pbv[:, 0:512].rearrange("p (a b) -> p a b", a=4), [bpb], [b_dst])


def load_w_block(k, ring, src):
    ap, b = ring.next()
    k.p.dma(ap, src, writes=[b], q="pool")
    return ap, b


def phase_inproj(k):
    p = k.p
    ar = k.ar
    I = k.I
    ar.reset()
    hT = ar.alloc((16, S), BF16)
    b_hT = p.buf("hT")
    k.hT = hT
    mark = ar.off
    tiles = [(I["x"][t * 128:(t + 1) * 128, :], []) for t in range(NT)]
    norm_T(k, tiles, I["mix_norm_g"], hT, b_hT)
    p.barrier()
    ar.off = mark
    wring = Ring([(ar.alloc((16, 512), BF16), p.buf()) for _ in range(2)])
    wba = ar.alloc((16, 16), BF16)
    b_wba = p.buf()
    raw = [ar.alloc((S + 4,), F32) for _ in range(2)]
    b_raw = p.bufs(2)
    y = ar.alloc((S,), F32)
    b_y = p.buf()
    sq = ar.alloc((S,), BF16)
    b_sq = p.buf()
    rs = ar.alloc((S,), F32)
    b_rs = p.buf()
    obf = [ar.alloc((S,), BF16) for _ in range(2)]
    b_obf = p.bufs(2)
    tst = [ar.alloc((16, 128), BF16) for _ in range(2)]
    b_tst = p.bufs(2)
    zst = [ar.alloc((512,), BF16) for _ in range(3)]
    b_zst = p.bufs(3)
    cw = ar.alloc((96,), F32)
    cwl = ar.alloc((128,), F32)
    b_cw = p.buf()
    small = ar.alloc((64,), F32)
    b_small = p.buf()
    p.dma(cwl[0:96, :], I["gdn_conv_w"].rearrange("j (b q) -> (j b) q", q=128), writes=[b_cw])
    pb, bpb = k.ps.next()
    TR(k, pb[:, 0:96], cwl[0:96, :], k.identf[0:96, 0:96], [b_cw], [bpb])
    CP(k, "dve", cw, pb[:, 0:96], [bpb], [b_cw])
    for i in range(2):
        p.op("pool", "memset", writes=[b_raw[i]], ap=raw[i][:, 0:4], constant=0.0)

    wsrc = I["w_in"].rearrange("(kt q) n -> q kt n", q=128)
    ri = 0
    oi = 0
    ti = 0

    def fblock(wap, wb, f, evac):
        for c in range(4):
            pb, bpb = k.ps.next()
            for kt in range(16):
                MM(k, pb[:], wap[:, kt, f * 128:(f + 1) * 128], hT[:, kt, c * 512:(c + 1) * 512],
                   kt == 0, kt == 15, [wb, b_hT], [bpb])
            evac(pb, bpb, c * 512)

    for blk in range(6):
        wap, wb = load_w_block(k, wring, wsrc[:, :, blk * 512:(blk + 1) * 512])
        for f in range(4):
            fb = blk * 4 + f
            kind = fb // 8
            hd = fb % 8
            r_ap, r_b = raw[ri % 2], b_raw[ri % 2]
            ri += 1

            def evac_raw(pb, bpb, tok0, r_ap=r_ap, r_b=r_b):
                CP(k, "act", r_ap[:, 3 + tok0:3 + tok0 + 512], pb[:], [bpb], [r_b])
            fblock(wap, wb, f, evac_raw)
            TS(k, "dve", y, r_ap[:, 0:S], cw[:, 0 * 24 + fb:0 * 24 + fb + 1], None, ALU.mult, ALU.bypass,
               [r_b, b_cw], [b_y])
            for j in range(1, 4):
                STT(k, "dve", y, r_ap[:, j:j + S], cw[:, j * 24 + fb:j * 24 + fb + 1], y, ALU.mult, ALU.add,
                    [r_b, b_cw, b_y], [b_y])
            o_ap, o_b = obf[oi % 2], b_obf[oi % 2]
            oi += 1
            if kind == 2:
                ACT(k, o_ap, y, AF.Silu, [b_y], [o_b])
            else:
                ACT(k, y, y, AF.Silu, [b_y], [b_y])
                ACT(k, sq, y, AF.Square, [b_y], [b_sq])
                for c in range(4):
                    pb, bpb = k.ps.next()
                    MM(k, pb[:], k.ones, sq[:, c * 512:(c + 1) * 512], True, True, [b_sq, k.b_c], [bpb])
                    if kind == 0:
                        ACT(k, rs[:, c * 512:(c + 1) * 512], pb[:], AF.Sqrt, [bpb], [b_rs], scale=128.0,
                            bias=128.0 * EPS)
                    else:
                        ACT(k, rs[:, c * 512:(c + 1) * 512], pb[:], AF.Sqrt, [bpb], [b_rs], scale=1.0, bias=EPS)
                p.op("dve", "reciprocal", reads=[b_rs], writes=[b_rs], out=rs, in_=rs)
                TT(k, "dve", o_ap, y, rs, ALU.mult, [b_y, b_rs], [o_b])
            if kind == 0:
                p.dma(k.QA[:, :, hd, :], o_ap.rearrange("p (c t) -> p c t", c=16), reads=[o_b],
                      writes=[k.b_scr["QA"]])
            if kind == 1:
                p.dma(k.KA[:, :, hd, :], o_ap.rearrange("p (c t) -> p c t", c=16), reads=[o_b],
                      writes=[k.b_scr["KA"]])
            if kind >= 1:
                t_ap, t_b = tst[ti % 2], b_tst[ti % 2]
                ti += 1
                for c4 in range(4):
                    pb, bpb = k.ps.next()
                    pbv = pb[:].bitcast(BF16)
                    for j in range(4):
                        c = c4 * 4 + j
                        TR(k, pbv[:, j * 128:(j + 1) * 128], o_ap[:, c * 128:(c + 1) * 128], k.ident, [o_b], [bpb])
                    CP(k, "act" if c4 % 2 else "dve", t_ap[:, c4 * 4:(c4 + 1) * 4, :],
                       pbv[:, 0:512].rearrange("p (a b) -> p a b", a=4), [bpb], [t_b])
                dst = k.KT if kind == 1 else k.VA
                p.dma(dst[:, :, hd, :], t_ap, reads=[t_b], writes=[k.b_scr["KT" if kind == 1 else "VA"]])

    zi = 0
    for blk in range(2):
        wap, wb = load_w_block(k, wring, wsrc[:, :, 3072 + blk * 512:3072 + (blk + 1) * 512])
        for t in range(NT):
            pb, bpb = k.ps.next()
            for kt in range(16):
                MM(k, pb[:], hT[:, kt, t * 128:(t + 1) * 128], wap[:, kt, :], kt == 0, kt == 15, [wb, b_hT], [bpb])
            z_ap, z_b = zst[zi % 3], b_zst[zi % 3]
            zi += 1
            ACT(k, z_ap, pb[:], AF.Silu, [bpb], [z_b])
            p.dma(k.ZS[:, t, blk * 4:(blk + 1) * 4, :], z_ap.rearrange("p (h d) -> p h d", h=4), reads=[z_b],
                  writes=[k.b_scr["ZS"]])
    p.dma(wba, wsrc[:, :, 4096:4112], writes=[b_wba], q="pool")
    for t in range(NT):
        pb, bpb = k.ps.next()
        for kt in range(16):
            MM(k, pb[:, 0:16], hT[:, kt, t * 128:(t + 1) * 128], wba[:, kt, :], kt == 0, kt == 15, [b_wba, b_hT], [bpb])
        CP(k, "dve", k.BA[:, t, :], pb[:, 0:16], [bpb], [k.b_BA])
    albc = small[:, 0:8]
    dtbc = small[:, 8:16]
    nea = small[:, 16:24]
    p.dma(albc, I["gdn_a_log"].partition_broadcast(128), writes=[b_small])
    p.dma(dtbc, I["gdn_dt_bias"].partition_broadcast(128), writes=[b_small])
    ACT(k, nea, albc, AF.Exp, [b_small], [b_small])
    TS(k, "dve", nea, nea, -1.0, None, ALU.mult, ALU.bypass, [b_small], [b_small])
    ACT(k, k.BETA[:], k.BA[:, :, 0:8], AF.Sigmoid, [k.b_BA], [k.b_bg])
    xa = ar.alloc((16, 8), F32)
    xb = ar.alloc((16, 8), F32)
    b_xa = p.buf()
    TT(k, "dve", xa, k.BA[:, :, 8:16], dtbc.unsqueeze(1).broadcast_to([128, 16, 8]), ALU.add, [k.b_BA, b_small], [b_xa])
    ACT(k, xb, xa, AF.Abs, [b_xa], [b_xa])
    ACT(k, xb, xb, AF.Exp, [b_xa], [b_xa], scale=-1.0)
    ACT(k, xb, xb, AF.Ln, [b_xa], [b_xa], bias=1.0)
    TS(k, "dve", xa, xa, 0.0, None, ALU.max, ALU.bypass, [b_xa], [b_xa])
    TT(k, "dve", xa, xa, xb, ALU.add, [b_xa], [b_xa])
    TT(k, "dve", k.GG[:], xa, nea.unsqueeze(1).broadcast_to([128, 16, 8]), ALU.mult, [b_xa, b_small], [k.b_bg])

    for blk in range(4):
        wap, wb = load_w_block(k, wring, wsrc[:, :, 4112 + blk * 512:4112 + (blk + 1) * 512])
        for f in range(4):
            fb = blk * 4 + f
            kind = fb // 8
            hd = fb % 8
            o_ap, o_b = obf[oi % 2], b_obf[oi % 2]
            oi += 1

            def evac_o(pb, bpb, tok0, o_ap=o_ap, o_b=o_b, c=[0]):
                CP(k, "act" if (tok0 // 512) % 2 else "dve", o_ap[:, tok0:tok0 + 512], pb[:], [bpb], [o_b])
            fblock(wap, wb, f, evac_o)
            dst = k.QB if kind == 0 else k.KB
            p.dma(dst[hd], o_ap, reads=[o_b], writes=[k.b_scr["QB" if kind == 0 else "KB"]])
    for blk in range(2):
        wap, wb = load_w_block(k, wring, wsrc[:, :, 6160 + blk * 512:6160 + (blk + 1) * 512])
        for t in range(NT):
            pb, bpb = k.ps.next()
            for kt in range(16):
                MM(k, pb[:], hT[:, kt, t * 128:(t + 1) * 128], wap[:, kt, :], kt == 0, kt == 15, [wb, b_hT], [bpb])
            z_ap, z_b = zst[zi % 3], b_zst[zi % 3]
            zi += 1
            CP(k, "act" if t % 2 else "dve", z_ap, pb[:], [bpb], [z_b])
            p.dma(k.VB[blk * 4:(blk + 1) * 4, :, t, :].rearrange("h p d -> p h d"),
                  z_ap.rearrange("p (h d) -> p h d", h=4), reads=[z_b], writes=[k.b_scr["VB"]])
    if hasattr(k, "DBG"):
        p.dma(k.DBG[:, 0:128], k.BETA[:].rearrange("p a b -> p (a b)"), reads=[k.b_bg], writes=[k.b_scr["DBG"]])
        p.dma(k.DBG[:, 128:256], k.GG[:].rearrange("p a b -> p (a b)"), reads=[k.b_bg], writes=[k.b_scr["DBG"]])


def phase_gdn(k):
    p = k.p
    ar = k.ar
    I = k.I
    ar.reset()
    oT = ar.alloc((16, S), BF16)
    k.oT = oT
    k.b_oT = p.buf("oT")
    k.oT_mark = ar.off
    S32 = ar.alloc((8, 128), F32)
    S16 = ar.alloc((8, 128), BF16)
    b_S32 = p.bufs(2)
    b_S16 = p.bufs(2)
    p.op("pool", "memset", writes=b_S32, ap=S32, constant=0.0)
    p.op("pool", "memset", writes=b_S16, ap=S16, constant=0.0)
    gnb = ar.alloc((128,), F32)
    b_gnb = p.buf()
    p.dma(gnb, I["gdn_norm_g"].partition_broadcast(128), writes=[b_gnb])
    names = ("QA", "KA", "KT", "VA", "ZS")
    inr = Ring([([ar.alloc((8, 128), BF16) for _ in names], p.bufs(5)) for _ in range(2)])
    sm = Ring([(ar.alloc((128,), F32), p.buf()) for _ in range(2)])
    kbg_, vb_, kdec_, zg_ = [ar.alloc((8, 128), BF16) for _ in range(4)]
    b_kbg, b_vb, b_kdec, b_zg = p.bufs(4)

    class G:
        pass
    grp = []
    for g in range(2):
        G_ = G()
        G_.diagG = ar.alloc((4, 128), F32)
        G_.decQ = ar.alloc((4, 128), BF16)
        G_.decA = ar.alloc((4, 128), BF16)
        G_.A = [ar.alloc((4, 128), BF16) for _ in range(2)]
        G_.B = [ar.alloc((4, 128), BF16) for _ in range(2)]
        G_.P = [ar.alloc((4, 128), BF16) for _ in range(2)]
        G_.qk = ar.alloc((4, 128), BF16)
        G_.qkT = ar.alloc((4, 128), BF16)
        G_.wT = ar.alloc((4, 128), BF16)
        G_.u = ar.alloc((4, 128), F32)
        G_.vnew = ar.alloc((4, 128), BF16)
        G_.o1 = ar.alloc((4, 128), F32)
        G_.o = ar.alloc((4, 128), F32)
        G_.sq = ar.alloc((4, 128), F32)
        G_.fin = ar.alloc((4, 128), BF16)
        G_.ss = ar.alloc((4,), F32)
        for nm in ("diagG", "decQ", "decA", "qk", "qkT", "wT", "u", "vnew", "o1", "o", "sq", "fin", "ss"):
            setattr(G_, "b_" + nm, p.buf())
        G_.b_A = p.bufs(2)
        G_.b_B = p.bufs(2)
        G_.b_P = p.bufs(2)
        grp.append(G_)

    def v512(ap):
        return ap.rearrange("p a b -> p (a b)")

    for c in range(16):
        (qT, kT, ktok, vtok, zs), bi = inr.next()
        b_qT, b_kT, b_ktok, b_vtok, b_zs = bi
        for ap, b, nm in zip((qT, kT, ktok, vtok, zs), bi, names):
            p.dma(ap, getattr(k, nm)[:, c, :, :], reads=[k.b_scr[nm]], writes=[b])
        smt, b_sm = sm.next()
        gc = smt[:, 0:8]
        e24 = smt[:, 8:32]
        eg = smt[:, 8:16]
        egrev = smt[:, 16:24]
        gl = smt[:, 24:32]
        bexp = smt[:, 32:40]
        gcl = smt[:, 40:48]
        pbA, bpA = k.ps.next()
        gsrc = k.GG[:, c, :]
        MM(k, pbA[:, 0:8], k.trif, gsrc, True, True, [k.b_c, k.b_bg], [bpA])
        MM(k, pbA[:, 8:16], k.suf, gsrc, True, True, [k.b_c, k.b_bg], [bpA])
        MM(k, pbA[:, 16:24], k.onesf, gsrc, True, True, [k.b_c, k.b_bg], [bpA])
        CP(k, "dve", gc, pbA[:, 0:8], [bpA], [b_sm])
        ACT(k, e24, pbA[:, 0:24], AF.Exp, [bpA], [b_sm])
        TT(k, "dve", bexp, k.BETA[:, c, :], eg, ALU.mult, [k.b_bg, b_sm], [b_sm])
        ACT(k, gcl, k.BETA[:, c, :], AF.Ln, [k.b_bg], [b_sm])
        TT(k, "dve", gcl, gcl, gc, ALU.add, [b_sm], [b_sm])
        bc = lambda a: a.unsqueeze(2).broadcast_to([128, 8, 128])
        TT(k, "pool", kbg_, ktok, bc(bexp), ALU.mult, [b_ktok, b_sm], [b_kbg])
        TT(k, "pool", vb_, vtok, bc(k.BETA[:, c, :]), ALU.mult, [b_vtok, k.b_bg], [b_vb])
        TT(k, "pool", kdec_, ktok, bc(egrev), ALU.mult, [b_ktok, b_sm], [b_kdec])
        TT(k, "pool", zg_, zs, gnb.unsqueeze(1).broadcast_to([128, 8, 128]), ALU.mult, [b_zs, b_gnb], [b_zg])
        bc4 = lambda a: a.unsqueeze(2).broadcast_to([128, 4, 128])
        for g, G_ in enumerate(grp):
            hs = slice(4 * g, 4 * g + 4)
            TT(k, "dve", G_.diagG, k.identf.unsqueeze(1).broadcast_to([128, 4, 128]), bc4(gc[:, hs]), ALU.mult,
               [k.b_c, b_sm], [G_.b_diagG])
        for g, G_ in enumerate(grp):
            for which, um, dec, b_dec, bias in ((0, k.umq, G_.decQ, G_.b_decQ, gc), (1, k.uma, G_.decA, G_.b_decA, gcl)):
                pR, bpR = k.ps.next()
                for hh in range(4):
                    MM(k, pR[:, hh * 128:(hh + 1) * 128], k.onesf, G_.diagG[:, hh, :], True, False,
                       [k.b_c, G_.b_diagG], [bpR])
                    MM(k, pR[:, hh * 128:(hh + 1) * 128], k.ident, um, False, True, [k.b_c], [bpR])
                for hh in range(4):
                    h = 4 * g + hh
                    ACT(k, dec[:, hh, :], pR[:, hh * 128:(hh + 1) * 128], AF.Exp, [bpR, b_sm], [b_dec],
                        scale=-1.0, bias=bias[:, h:h + 1])
        for g, G_ in enumerate(grp):
            pKK, bKK = k.ps.next()
            pQK, bQK = k.ps.next()
            for hh in range(4):
                h = 4 * g + hh
                MM(k, pKK[:, hh * 128:(hh + 1) * 128], kT[:, h, :], kT[:, h, :], True, True, [b_kT], [bKK])
                MM(k, pQK[:, hh * 128:(hh + 1) * 128], qT[:, h, :], kT[:, h, :], True, True, [b_qT, b_kT], [bQK])
            TT(k, "dve", v512(G_.A[0]), pKK[:], v512(G_.decA), ALU.mult, [bKK, G_.b_decA], [G_.b_A[0]])
            TT(k, "dve", v512(G_.qk), pQK[:], v512(G_.decQ), ALU.mult, [bQK, G_.b_decQ], [G_.b_qk])
        for g, G_ in enumerate(grp):
            for src, bsrc, dst, bdst, eng in ((G_.A[0], G_.b_A[0], G_.B[0], G_.b_B[0], "act"),
                                              (G_.qk, G_.b_qk, G_.qkT, G_.b_qkT, "dve")):
                pT, bpT = k.ps.next()
                pTv = pT[:].bitcast(BF16)
                for hh in range(4):
                    TR(k, pTv[:, hh * 128:(hh + 1) * 128], src[:, hh, :], k.ident, [bsrc], [bpT])
                CP(k, eng, v512(dst), pTv[:, 0:512], [bpT], [bdst])
            TT(k, "pool", G_.P[0], k.ident.unsqueeze(1).broadcast_to([128, 4, 128]), G_.B[0], ALU.subtract,
               [k.b_c, G_.b_B[0]], [G_.b_P[0]])
        for lvl in range(1, 7):
            cur = (lvl - 1) % 2
            nxt = lvl % 2
            for g, G_ in enumerate(grp):
                pA, bA = k.ps.next()
                for hh in range(4):
                    MM(k, pA[:, hh * 128:(hh + 1) * 128], G_.B[cur][:, hh, :], G_.A[cur][:, hh, :], True, True,
                       [G_.b_A[cur], G_.b_B[cur]], [bA])
                if lvl <= 5:
                    pB, bB = k.ps.next()
                    for hh in range(4):
                        MM(k, pB[:, hh * 128:(hh + 1) * 128], G_.A[cur][:, hh, :], G_.B[cur][:, hh, :], True, True,
                           [G_.b_A[cur], G_.b_B[cur]], [bB])
                CP(k, "act", v512(G_.A[nxt]), pA[:], [bA], [G_.b_A[nxt]])
                if lvl <= 5:
                    CP(k, "dve", v512(G_.B[nxt]), pB[:], [bB], [G_.b_B[nxt]])
            for g, G_ in enumerate(grp):
                pP, bP = k.ps.next()
                for hh in range(4):
                    MM(k, pP[:, hh * 128:(hh + 1) * 128], k.ident, G_.P[cur][:, hh, :], True, False,
                       [k.b_c, G_.b_P[cur]], [bP])
                    MM(k, pP[:, hh * 128:(hh + 1) * 128], G_.A[nxt][:, hh, :], G_.P[cur][:, hh, :], False, True,
                       [G_.b_A[nxt], G_.b_P[cur]], [bP])
                CP(k, "dve" if g else "act", v512(G_.P[nxt]), pP[:], [bP], [G_.b_P[nxt]])
        for g, G_ in enumerate(grp):
            TTm = G_.P[0]
            b_TT = G_.b_P[0]
            pW, bW = k.ps.next()
            pU, bU = k.ps.next()
            for hh in range(4):
                h = 4 * g + hh
                MM(k, pW[:, hh * 128:(hh + 1) * 128], kbg_[:, h, :], TTm[:, hh, :], True, True, [b_kbg, b_TT], [bW])
                MM(k, pU[:, hh * 128:(hh + 1) * 128], TTm[:, hh, :], vb_[:, h, :], True, True, [b_vb, b_TT], [bU])
            CP(k, "act", v512(G_.wT), pW[:], [bW], [G_.b_wT])
            CP(k, "dve", v512(G_.u), pU[:], [bU], [G_.b_u])
        for g, G_ in enumerate(grp):
            pVN, bVN = k.ps.next()
            pO1, bO1 = k.ps.next()
            for hh in range(4):
                h = 4 * g + hh
                MM(k, pVN[:, hh * 128:(hh + 1) * 128], G_.wT[:, hh, :], S16[:, h, :], True, True,
                   [G_.b_wT, b_S16[g]], [bVN])
                MM(k, pO1[:, hh * 128:(hh + 1) * 128], qT[:, h, :], S16[:, h, :], True, True, [b_qT, b_S16[g]], [bO1])
            TT(k, "dve", v512(G_.vnew), v512(G_.u), pVN[:], ALU.subtract, [G_.b_u, bVN], [G_.b_vnew])
            TT(k, "dve", G_.o1, pO1[:].rearrange("p (a b) -> p a b", a=4), bc4(eg[:, 4 * g:4 * g + 4]), ALU.mult,
               [bO1, b_sm], [G_.b_o1])
            TT(k, "pool", S32[:, 4 * g:4 * g + 4, :], S32[:, 4 * g:4 * g + 4, :], bc4(gl[:, 4 * g:4 * g + 4]), ALU.mult,
               [b_sm, b_S32[g]], [b_S32[g]])
        for g, G_ in enumerate(grp):
            pO2, bO2 = k.ps.next()
            pSU, bSU = k.ps.next()
            for hh in range(4):
                h = 4 * g + hh
                MM(k, pO2[:, hh * 128:(hh + 1) * 128], G_.qkT[:, hh, :], G_.vnew[:, hh, :], True, True,
                   [G_.b_qkT, G_.b_vnew], [bO2])
                MM(k, pSU[:, hh * 128:(hh + 1) * 128], kdec_[:, h, :], G_.vnew[:, hh, :], True, True,
                   [b_kdec, G_.b_vnew], [bSU])
            TT(k, "dve", v512(S32[:, 4 * g:4 * g + 4, :]), v512(S32[:, 4 * g:4 * g + 4, :]), pSU[:], ALU.add,
               [b_S32[g], bSU], [b_S32[g]])
            CP(k, "act", S16[:, 4 * g:4 * g + 4, :], S32[:, 4 * g:4 * g + 4, :], [b_S32[g]], [b_S16[g]])
            TT(k, "dve", v512(G_.o), pO2[:], v512(G_.o1), ALU.add, [bO2, G_.b_o1], [G_.b_o])
        for g, G_ in enumerate(grp):
            TT(k, "pool", G_.sq, G_.o, G_.o, ALU.mult, [G_.b_o], [G_.b_sq])
            p.op("dve", "tensor_reduce", reads=[G_.b_sq], writes=[G_.b_ss], out=G_.ss, in_=G_.sq, axis=AX.X, op=ALU.add)
            ACT(k, G_.ss, G_.ss, AF.Sqrt, [G_.b_ss], [G_.b_ss], scale=1.0 / 128, bias=EPS)
            p.op("dve", "reciprocal", reads=[G_.b_ss], writes=[G_.b_ss], out=G_.ss, in_=G_.ss)
            TT(k, "dve", G_.sq, G_.o, bc4(G_.ss), ALU.mult, [G_.b_o, G_.b_ss, G_.b_sq], [G_.b_sq])
            TT(k, "pool", G_.fin, G_.sq, zg_[:, 4 * g:4 * g + 4, :], ALU.mult, [G_.b_sq, b_zg], [G_.b_fin])
            pT, bpT = k.ps.next()
            pTv = pT[:].bitcast(BF16)
            for hh in range(4):
                TR(k, pTv[:, hh * 128:(hh + 1) * 128], G_.fin[:, hh, :], k.ident, [G_.b_fin], [bpT])
            CP(k, "act", oT[:, 4 * g:4 * g + 4, c * 128:(c + 1) * 128],
               pTv[:, 0:512].rearrange("p (a b) -> p a b", a=4), [bpT], [k.b_oT])


def phase_moba(k):
    p = k.p
    ar = k.ar
    I = k.I
    ar.off = k.oT_mark
    oT = k.oT
    hr = Ring([([ar.alloc((S,), BF16), ar.alloc((S,), BF16), ar.alloc((16, 132), BF16)], p.bufs(3)) for _ in range(2)])
    for (aps, bs) in hr.items:
        p.op("pool", "memset", writes=[bs[2]], ap=aps[2][:, :, 128:132], constant=1.0)
    tg = ar.alloc((8, 256), F32)
    Tp = ar.alloc((8, 256), BF16)
    rbb = ar.alloc((256,), F32)
    bmax = ar.alloc((8,), F32)
    mgb = ar.alloc((128,), F32)
    b_tp = p.buf()
    p.dma(tg, I["rb_toep"], writes=[b_tp])
    p.dma(rbb, I["rel_bias"].partition_broadcast(128), writes=[b_tp])
    p.dma(mgb, I["moba_norm_g"].partition_broadcast(128), writes=[b_tp])
    rb31 = rbb[:, 31 * 8:32 * 8]
    TT(k, "dve", tg, tg, rb31.unsqueeze(2).broadcast_to([128, 8, 256]), ALU.subtract, [b_tp], [b_tp])
    STT(k, "dve", Tp, tg, SQ128, k.cm.unsqueeze(1).broadcast_to([128, 8, 256]), ALU.mult, ALU.add, [b_tp, k.b_c], [b_tp])
    p.op("dve", "tensor_reduce", reads=[b_tp], writes=[b_tp], out=bmax, in_=rbb.rearrange("p (b h) -> p h b", h=8),
         axis=AX.X, op=ALU.max)
    TT(k, "dve", bmax, bmax, rb31, ALU.subtract, [b_tp], [b_tp])
    sq = ar.alloc((S,), BF16)
    ksq = ar.alloc((S,), BF16)
    b_sq, b_ksq = p.bufs(2)
    sm = ar.alloc((256,), F32)
    b_sm = p.buf()
    kms = sm[:, 0:8]
    km4 = sm[:, 8:12]
    ksc = sm[:, 12:13]
    nm = sm[:, 16:32]
    gsb = sm[:, 32:96].rearrange("p (a b) -> p a b", a=8)
    g2 = sm[:, 96:160].rearrange("p (a b) -> p a b", a=8)
    eq = sm[:, 160:224].rearrange("p (a b) -> p a b", a=8)
    mx = sm[:, 224:232]
    kmT = ar.alloc((8,), BF16)
    MBq = ar.alloc((16, 16), BF16)
    b_MBq = p.buf()
    p.op("pool", "memset", writes=[b_MBq], ap=MBq, constant=0.0)
    MB = ar.alloc((S,), BF16)
    b_MB = p.buf()
    PTr = Ring([(ar.alloc((256,), BF16), p.buf()) for _ in range(4)])
    yq = [ar.alloc((128,), F32) for _ in range(2)]
    b_yq = p.bufs(2)
    junk = ar.alloc((128,), BF16)
    b_junk = p.buf()
    fin = [ar.alloc((256,), BF16) for _ in range(2)]
    b_fin = p.bufs(2)
    sm2 = Ring([(ar.alloc((8,), F32), p.buf()) for _ in range(4)])
    bc8 = lambda a: a.unsqueeze(2).broadcast_to([128, 8, 8])

    for h in range(8):
        (qbT, kbT, vb1), (b_q, b_k, b_v) = hr.next()
        p.dma(qbT, k.QB[h], reads=[k.b_scr["QB"]], writes=[b_q])
        p.dma(kbT, k.KB[h], reads=[k.b_scr["KB"]], writes=[b_k])
        p.dma(vb1[:, :, 0:128], k.VB[h], reads=[k.b_scr["VB"]], writes=[b_v])
        ACT(k, sq, qbT, AF.Square, [b_q], [b_sq])
        ACT(k, ksq, kbT, AF.Square, [b_k], [b_ksq])
        p.op("dve", "tensor_reduce", reads=[b_k], writes=[b_sm], out=kms, in_=kbT.rearrange("p (n t) -> p n t", n=8),
             axis=AX.X, op=ALU.add)
        TS(k, "dve", kmT, kms, 1.0 / 256, None, ALU.mult, None, [b_sm], [b_sm])
        pG, bG = k.ps.next()
        for qi in range(8, 16):
            MM(k, pG[:, (qi - 8) * 8:(qi - 7) * 8], qbT[:, qi * 128:(qi + 1) * 128], kmT, True, True, [b_q, b_sm], [bG])
        pN, bN = k.ps.next()
        for qi in range(16):
            MM(k, pN[:, qi:qi + 1], sq[:, qi * 128:(qi + 1) * 128], k.ones[:, 0:1], True, True, [b_sq, k.b_c], [bN])
        for c in range(4):
            pK, bK = k.ps.next()
            MM(k, pK[:], k.ones, ksq[:, c * 512:(c + 1) * 512], True, True, [b_ksq, k.b_c], [bK])
            p.op("dve", "tensor_reduce", reads=[bK], writes=[b_sm], out=km4[:, c:c + 1], in_=pK[:], axis=AX.X, op=ALU.max)
        p.op("dve", "tensor_reduce", reads=[b_sm], writes=[b_sm], out=ksc, in_=km4, axis=AX.X, op=ALU.max)
        TS(k, "dve", ksc, ksc, 1.0 / 128, None, ALU.mult, None, [b_sm], [b_sm])
        ACT(k, nm, pN[:, 0:16], AF.Sqrt, [bN, b_sm], [b_sm], scale=ksc)
        TS(k, "dve", MBq[:, :, 8], nm, bmax[:, h:h + 1], -SQ128, ALU.add, ALU.mult, [b_sm, b_tp], [b_MBq])
        TT(k, "dve", gsb, pG[:, 0:64].rearrange("p (a b) -> p a b", a=8), k.gm.rearrange("p (a b) -> p a b", a=8),
           ALU.add, [bG, k.b_c], [b_sm])
        src = gsb
        for it in range(2):
            p.op("dve", "tensor_reduce", reads=[b_sm], writes=[b_sm], out=mx, in_=src, axis=AX.X, op=ALU.max)
            TT(k, "dve", eq, src, bc8(mx), ALU.is_equal, [b_sm], [b_sm])
            STT(k, "dve", g2, eq, -1e30, src, ALU.mult, ALU.add, [b_sm], [b_sm])
            src = g2
        p.op("dve", "tensor_reduce", reads=[b_sm], writes=[b_sm], out=mx, in_=g2, axis=AX.X, op=ALU.max)
        TT(k, "dve", eq, gsb, bc8(mx), ALU.is_ge, [b_sm], [b_sm])
        TS(k, "dve", MBq[:, 8:16, 0:8], eq, BIG * SQ128, -BIG * SQ128, ALU.mult, ALU.add, [b_sm], [b_MBq])
        for half in range(2):
            pM, bM = k.ps.next()
            pMv = pM[:].bitcast(BF16)
            for j in range(8):
                qi = half * 8 + j
                TR(k, pMv[0:9, j * 128:(j + 1) * 128], MBq[:, qi, 0:9], k.ident, [b_MBq], [bM])
            CP(k, "act" if half else "dve", MB[0:9, half * 1024:(half + 1) * 1024], pMv[0:9, 0:1024], [bM], [b_MB])
        for qb in range(8):
            for kj in range(2 * qb + 2):
                n = kj // 2
                pS, bS = k.ps.next()
                q0 = qb * 256
                v = n if (qb >= 4 and n < qb) else 8
                if n == qb and kj % 2 == 0:
                    segs = [(0, 256, Tp[:, h, 0:256])]
                elif n == qb:
                    segs = [(128, 256, Tp[:, h, 0:128])]
                elif n == qb - 1 and kj % 2 == 1:
                    segs = [(0, 128, Tp[:, h, 128:256]), (128, 256, None)]
                else:
                    segs = [(0, 256, None)]
                c0 = segs[0][0]
                for (a, b, bias) in segs:
                    MM(k, pS[:, a:b], kbT[:, kj * 128:(kj + 1) * 128], qbT[:, q0 + a:q0 + b], True, False, [b_k, b_q], [bS])
                    MM(k, pS[:, a:b], k.emat[:, v, :], MB[0:9, q0 + a:q0 + b], False, bias is None, [k.b_c, b_MB], [bS])
                    if bias is not None:
                        MM(k, pS[:, a:b], k.ident, bias, False, True, [k.b_c, b_tp], [bS])
                PT, b_PT = PTr.next()
                ACT(k, PT[:, c0:256], pS[:, c0:256], AF.Exp, [bS], [b_PT], scale=1.0 / SQ128)
                for qt in range(2):
                    if kj <= 2 * qb + qt:
                        pO, bO = k.psx[qt]
                        MM(k, pO[:, 0:129], PT[:, qt * 128:(qt + 1) * 128], vb1[:, kj, 0:129],
                           kj == 0, kj == 2 * qb + qt, [b_PT, b_v], [bO])
            f_ap, f_b = fin[qb % 2], b_fin[qb % 2]
            for qt in range(2):
                s2, b_s2 = sm2.next()
                y_ap, y_b = yq[qt], b_yq[qt]
                pO, bO = k.psx[qt]
                p.op("dve", "reciprocal", reads=[bO], writes=[b_s2], out=s2[:, 0:1], in_=pO[:, 128:129])
                TS(k, "dve", y_ap, pO[:, 0:128], s2[:, 0:1], None, ALU.mult, None, [bO, b_s2], [y_b])
                ACT(k, junk, y_ap, AF.Square, [y_b], [b_junk, b_s2], accum_out=s2[:, 1:2])
                ACT(k, s2[:, 1:2], s2[:, 1:2], AF.Sqrt, [b_s2], [b_s2], scale=1.0 / 128, bias=EPS)
                p.op("dve", "reciprocal", reads=[b_s2], writes=[b_s2], out=s2[:, 1:2], in_=s2[:, 1:2])
                STT(k, "dve", f_ap[:, qt * 128:(qt + 1) * 128], y_ap, s2[:, 1:2], mgb, ALU.mult, ALU.mult,
                    [y_b, b_s2, b_tp], [f_b])
            pT, bpT = k.ps.next()
            pTv = pT[:].bitcast(BF16)
            for qt in range(2):
                TR(k, pTv[:, qt * 128:(qt + 1) * 128], f_ap[:, qt * 128:(qt + 1) * 128], k.ident, [f_b], [bpT])
            CP(k, "act", oT[:, 8 + h, qb * 256:(qb + 1) * 256], pTv[:, 0:256], [bpT], [k.b_oT])
    if hasattr(k, "OT"):
        p.dma(k.OT, oT, reads=[k.b_oT], writes=[k.b_scr["DBG"]])


def phase_wout(k):
    p = k.p
    ar = k.ar
    I = k.I
    oT = k.oT
    ar.off = k.oT_mark
    h2T = ar.alloc((16, S), BF16)
    k.h2T = h2T
    k.b_h2T = p.buf("h2T")
    k.ws_mark = ar.off
    wring = Ring([(ar.alloc((16, 512), BF16), p.buf()) for _ in range(2)])
    xr = Ring([(ar.alloc((512,), F32), p.buf()) for _ in range(3)])
    wsrc = I["w_out"].rearrange("(kt q) n -> q kt n", q=128)
    for c in range(4):
        wap, wb = load_w_block(k, wring, wsrc[:, :, c * 512:(c + 1) * 512])
        for t in range(NT):
            x_ap, x_b = xr.next()
            p.dma(x_ap, I["x"][t * 128:(t + 1) * 128, c * 512:(c + 1) * 512], writes=[x_b])
            pb, bpb = k.ps.next()
            for kt in range(16):
                MM(k, pb[:], oT[:, kt, t * 128:(t + 1) * 128], wap[:, kt, :], kt == 0, kt == 15, [wb, k.b_oT], [bpb])
            TT(k, "dve", x_ap, pb[:], x_ap, ALU.add, [bpb, x_b], [x_b])
            p.dma(k.X1[t * 128:(t + 1) * 128, c * 512:(c + 1) * 512], x_ap, reads=[x_b], writes=[k.b_scr["X1"]])
    p.barrier()
    ar.off = k.ws_mark
    tiles = [(k.X1[t * 128:(t + 1) * 128, :], [k.b_scr["X1"]]) for t in range(NT)]
    norm_T(k, tiles, I["xattn_norm_g"], h2T, k.b_h2T)


def phase_xattn(k):
    p = k.p
    ar = k.ar
    I = k.I
    h2T = k.h2T
    ar.off = 0
    memT = ar.alloc((16, MEM), BF16)
    b_memT = p.buf()
    kxT = ar.alloc((4, MEM), BF16)
    vx = ar.alloc((2, 512), BF16)
    b_kv = p.buf()
    qxT = ar.alloc((4, S), BF16)
    b_qx = p.buf()
    oxT = ar.alloc((4, S), BF16)
    b_ox = p.buf()
    wxo = ar.alloc((4, D), BF16)
    b_wxo = p.buf()
    assert ar.off <= k.oT_mark
    ar.off = k.ws_mark
    tiles = [(I["mem"][t * 128:(t + 1) * 128, :], []) for t in range(2)]
    norm_T(k, tiles, I["mem_norm_g"], memT, b_memT)
    p.barrier()
    ar.off = k.ws_mark
    wring = Ring([(ar.alloc((16, 512), BF16), p.buf()) for _ in range(2)])
    Pf = Ring([(ar.alloc((256,), F32), p.buf()) for _ in range(2)])
    Pn = Ring([(ar.alloc((256,), BF16), p.buf()) for _ in range(2)])
    PnT = Ring([(ar.alloc((2, 128), BF16), p.buf()) for _ in range(2)])
    sm = Ring([(ar.alloc((8,), F32), p.buf()) for _ in range(4)])
    xr = Ring([(ar.alloc((512,), F32), p.buf()) for _ in range(3)])
    wkv = I["w_xkv"].rearrange("(kt q) n -> q kt n", q=128)
    wap, wb = load_w_block(k, wring, wkv[:, :, 0:512])
    for hx in range(4):
        pb, bpb = k.ps.next()
        for kt in range(16):
            MM(k, pb[:, 0:MEM], wap[:, kt, hx * 128:(hx + 1) * 128], memT[:, kt, :], kt == 0, kt == 15, [wb, b_memT], [bpb])
        CP(k, "act" if hx % 2 else "dve", kxT[:, hx, :], pb[:, 0:MEM], [bpb], [b_kv])
    wap, wb = load_w_block(k, wring, wkv[:, :, 512:1024])
    for mt in range(2):
        pb, bpb = k.ps.next()
        for kt in range(16):
            MM(k, pb[:], memT[:, kt, mt * 128:(mt + 1) * 128], wap[:, kt, :], kt == 0, kt == 15, [wb, b_memT], [bpb])
        CP(k, "act" if mt else "dve", vx[:, mt, :], pb[:], [bpb], [b_kv])
    wq = I["w_xq"].rearrange("(kt q) n -> q kt n", q=128)
    wap, wb = load_w_block(k, wring, wq)
    for hx in range(4):
        for c in range(4):
            pb, bpb = k.ps.next()
            for kt in range(16):
                MM(k, pb[:], wap[:, kt, hx * 128:(hx + 1) * 128], h2T[:, kt, c * 512:(c + 1) * 512], kt == 0, kt == 15,
                   [wb, k.b_h2T], [bpb])
            CP(k, "act" if c % 2 else "dve", qxT[:, hx, c * 512:(c + 1) * 512], pb[:], [bpb], [b_qx])
    for hx in range(4):
        for t in range(NT):
            pS, bS = k.ps.next()
            MM(k, pS[:, 0:MEM], qxT[:, hx, t * 128:(t + 1) * 128], kxT[:, hx, :], True, True, [b_qx, b_kv], [bS])
            s_ap, s_b = sm.next()
            p.op("dve", "tensor_reduce", reads=[bS], writes=[s_b], out=s_ap[:, 0:1], in_=pS[:, 0:MEM], axis=AX.X, op=ALU.max)
            TS(k, "dve", s_ap[:, 1:2], s_ap[:, 0:1], -1.0 / SQ128, None, ALU.mult, None, [s_b], [s_b])
            pf, b_pf = Pf.next()
            ACT(k, pf, pS[:, 0:MEM], AF.Exp, [bS, s_b], [b_pf, s_b], scale=1.0 / SQ128, bias=s_ap[:, 1:2],
                accum_out=s_ap[:, 2:3])
            p.op("dve", "reciprocal", reads=[s_b], writes=[s_b], out=s_ap[:, 3:4], in_=s_ap[:, 2:3])
            pn, b_pn = Pn.next()
            TS(k, "dve", pn, pf, s_ap[:, 3:4], None, ALU.mult, None, [b_pf, s_b], [b_pn])
            pT, bpT = k.ps.next()
            pTv = pT[:].bitcast(BF16)
            for mt in range(2):
                TR(k, pTv[:, mt * 128:(mt + 1) * 128], pn[:, mt * 128:(mt + 1) * 128], k.ident, [b_pn], [bpT])
            pnt, b_pnt = PnT.next()
            CP(k, "act", pnt, pTv[:, 0:256].rearrange("p (a b) -> p a b", a=2), [bpT], [b_pnt])
            pO, bO = k.ps.next()
            for mt in range(2):
                MM(k, pO[:, 0:128], vx[:, mt, hx * 128:(hx + 1) * 128], pnt[:, mt, :], mt == 0, mt == 1, [b_kv, b_pnt], [bO])
            CP(k, "dve" if t % 2 else "act", oxT[:, hx, t * 128:(t + 1) * 128], pO[:, 0:128], [bO], [b_ox])
    p.dma(wxo, I["w_xo"].rearrange("(kt q) n -> q kt n", q=128), writes=[b_wxo], q="pool")
    for t in range(NT):
        for c in range(4):
            x_ap, x_b = xr.next()
            p.dma(x_ap, k.X1[t * 128:(t + 1) * 128, c * 512:(c + 1) * 512], reads=[k.b_scr["X1"]], writes=[x_b])
            pb, bpb = k.ps.next()
            for kt in range(4):
                MM(k, pb[:], oxT[:, kt, t * 128:(t + 1) * 128], wxo[:, kt, c * 512:(c + 1) * 512], kt == 0, kt == 3,
                   [b_wxo, b_ox], [bpb])
            TT(k, "dve", x_ap, pb[:], x_ap, ALU.add, [bpb, x_b], [x_b])
            p.dma(k.X2[t * 128:(t + 1) * 128, c * 512:(c + 1) * 512], x_ap, reads=[x_b], writes=[k.b_scr["X2"]])


def phase_ffn(k):
    p = k.p
    ar = k.ar
    I = k.I
    HT = 1024
    halo = p.sbuf("halo", [128, NFF, 2], F32)
    b_halo = p.buf()
    p.op("pool", "memset", writes=[b_halo], ap=halo[:], constant=0.0)
    cwf = p.sbuf("cwf", [128, 4 * NFF], F32)
    b_cwf = p.buf()
    for hf in range(2):
        ar.reset()
        aT = ar.alloc((NFF, HT), BF16)
        b_aT = p.buf()
        mark0 = ar.off
        h3T = ar.alloc((16, HT), BF16)
        b_h3T = p.buf()
        mark1 = ar.off
        tiles = [(k.X2[(hf * 8 + t) * 128:(hf * 8 + t + 1) * 128, :], [k.b_scr["X2"]]) for t in range(8)]
        norm_T(k, tiles, I["ffn_norm_g"], h3T, b_h3T)
        p.barrier()
        ar.off = mark1
        if hf == 0:
            stg = ar.alloc((128,), F32)
            b_stg = p.buf()
            for j in range(4):
                src = (I["ffn_conv_w"][j] if j < 3 else I["ffn_conv_b"][0]).rearrange("(b q) -> b q", q=128)
                p.dma(stg[0:NFF, :], src, writes=[b_stg])
                pb, bpb = k.ps.next()
                TR(k, pb[:, 0:NFF], stg[0:NFF, :], k.identf[0:NFF, 0:NFF], [b_stg], [bpb])
                CP(k, "dve", cwf[:, j * NFF:(j + 1) * NFF], pb[:, 0:NFF], [bpb], [b_cwf])
        wg = Ring([(ar.alloc((16, 128), BF16), p.buf()) for _ in range(3)])
        wu = Ring([(ar.alloc((16, 128), BF16), p.buf()) for _ in range(3)])
        graw = Ring([(ar.alloc((HT + 2,), F32), p.buf()) for _ in range(2)])
        gy = Ring([(ar.alloc((HT,), F32), p.buf()) for _ in range(2)])
        wgs = I["w_gate"].rearrange("(kt q) n -> q kt n", q=128)
        wus = I["w_up"].rearrange("(kt q) n -> q kt n", q=128)
        for j in range(NFF):
            g_ap, g_b = wg.next()
            u_ap, u_b = wu.next()
            p.dma(g_ap, wgs[:, :, j * 128:(j + 1) * 128], writes=[g_b], q="pool")
            p.dma(u_ap, wus[:, :, j * 128:(j + 1) * 128], writes=[u_b], q="pool")
            r_ap, r_b = graw.next()
            y_ap, y_b = gy.next()
            pus = []
            for c in range(2):
                pg, bg = k.ps.next()
                pu, bu = k.ps.next()
                for kt in range(16):
                    MM(k, pg[:], g_ap[:, kt, :], h3T[:, kt, c * 512:(c + 1) * 512], kt == 0, kt == 15, [g_b, b_h3T], [bg])
                for kt in range(16):
                    MM(k, pu[:], u_ap[:, kt, :], h3T[:, kt, c * 512:(c + 1) * 512], kt == 0, kt == 15, [u_b, b_h3T], [bu])
                CP(k, "act", r_ap[:, 2 + c * 512:2 + (c + 1) * 512], pg[:], [bg], [r_b])
                pus.append((pu, bu))
            CP(k, "pool", r_ap[:, 0:2], halo[:, j, :], [b_halo], [r_b])
            if hf == 0:
                CP(k, "pool", halo[:, j, :], r_ap[:, HT:HT + 2], [r_b], [b_halo])
            TS(k, "dve", y_ap, r_ap[:, 0:HT], cwf[:, j:j + 1], None, ALU.mult, None, [r_b, b_cwf], [y_b])
            for tap in range(1, 3):
                STT(k, "dve", y_ap, r_ap[:, tap:tap + HT], cwf[:, tap * NFF + j:tap * NFF + j + 1], y_ap, ALU.mult, ALU.add,
                    [r_b, b_cwf, y_b], [y_b])
            ACT(k, y_ap, y_ap, AF.Silu, [y_b, b_cwf], [y_b], bias=cwf[:, 3 * NFF + j:3 * NFF + j + 1])
            for c in range(2):
                pu, bu = pus[c]
                TT(k, "dve", aT[:, j, c * 512:(c + 1) * 512], y_ap[:, c * 512:(c + 1) * 512], pu[:], ALU.mult,
                   [y_b, bu], [b_aT])
        p.barrier()
        ar.off = mark0
        x3 = ar.alloc((8, D), F32)
        b_x3 = p.bufs(8)
        fgb = ar.alloc((D,), F32)
        b_fgb = p.buf()
        p.dma(fgb, I["final_norm_g"].partition_broadcast(128), writes=[b_fgb])
        junk = ar.alloc((D,), BF16)
        b_junk = p.buf()
        wd = Ring([(ar.alloc((512,), BF16), p.buf()) for _ in range(4)])
        xr = Ring([(ar.alloc((512,), F32), p.buf()) for _ in range(2)])
        ssf = ar.alloc((8,), F32)
        b_ssf = p.buf()
        for cc in range(4):
            for j in range(NFF):
                w_ap, w_b = wd.next()
                p.dma(w_ap, I["w_down"][j * 128:(j + 1) * 128, cc * 512:(cc + 1) * 512], writes=[w_b], q="pool")
                for tt in range(8):
                    pb, bpb = k.banks[tt]
                    MM(k, pb[:], aT[:, j, tt * 128:(tt + 1) * 128], w_ap, j == 0, j == NFF - 1, [w_b, b_aT], [bpb])
            for tt in range(8):
                pb, bpb = k.banks[tt]
                x_ap, x_b = xr.next()
                row = (hf * 8 + tt) * 128
                p.dma(x_ap, k.X2[row:row + 128, cc * 512:(cc + 1) * 512], reads=[k.b_scr["X2"]], writes=[x_b])
                TT(k, "dve", x3[:, tt, cc * 512:(cc + 1) * 512], pb[:], x_ap, ALU.add, [bpb, x_b], [b_x3[tt]])
        outs = []
        for tt in range(8):
            row = (hf * 8 + tt) * 128
            ACT(k, junk, x3[:, tt, :], AF.Square, [b_x3[tt]], [b_junk, b_ssf], accum_out=ssf[:, tt:tt + 1])
            ACT(k, ssf[:, tt:tt + 1], ssf[:, tt:tt + 1], AF.Sqrt, [b_ssf], [b_ssf], scale=1.0 / D, bias=EPS)
            p.op("dve", "reciprocal", reads=[b_ssf], writes=[b_ssf], out=ssf[:, tt:tt + 1], in_=ssf[:, tt:tt + 1])
            STT(k, "dve", x3[:, tt, :], x3[:, tt, :], ssf[:, tt:tt + 1], fgb, ALU.mult, ALU.mult,
                [b_x3[tt], b_ssf, b_fgb], [b_x3[tt]])
            outs.append(p.dma(k.out[row:row + 128, :], x3[:, tt, :], reads=[b_x3[tt]]))
        p.barrier()
    return []


_CACHE = {}


def make_in_maps(inputs, n=8, skip=()):
    cf, cb = host_consts()
    tidx = toeplitz_index()
    rb = np.asarray(inputs["rel_bias"], np.float32)
    rb_toep = np.ascontiguousarray(rb[tidx].transpose(0, 2, 1))
    shared = {
        "mix_norm_g": inputs["mix_norm_g"].reshape(1, D), "xattn_norm_g": inputs["xattn_norm_g"].reshape(1, D),
        "mem_norm_g": inputs["mem_norm_g"].reshape(1, D), "ffn_norm_g": inputs["ffn_norm_g"].reshape(1, D),
        "final_norm_g": inputs["final_norm_g"].reshape(1, D),
        "w_in": inputs["w_in"][0], "gdn_conv_w": inputs["gdn_conv_w"][0], "gdn_a_log": inputs["gdn_a_log"].reshape(1, 8),
        "gdn_dt_bias": inputs["gdn_dt_bias"].reshape(1, 8), "gdn_norm_g": inputs["gdn_norm_g"].reshape(1, 128),
        "moba_norm_g": inputs["moba_norm_g"].reshape(1, 128), "rel_bias": rb.reshape(1, 256), "rb_toep": rb_toep,
        "w_out": inputs["w_out"][0], "w_xq": inputs["w_xq"][0], "w_xkv": inputs["w_xkv"][0], "w_xo": inputs["w_xo"][0],
        "w_gate": inputs["w_gate"][0], "w_up": inputs["w_up"][0], "ffn_conv_w": inputs["ffn_conv_w"][0],
        "ffn_conv_b": inputs["ffn_conv_b"].reshape(1, DFF), "w_down": inputs["w_down"][0],
        "cst_f": cf, "cst_b": cb,
    }
    shared = {kk: (np.zeros((1, 1), np.float32) if kk in skip else np.ascontiguousarray(np.asarray(v, np.float32)))
              for kk, v in shared.items()}
    maps = []
    for b in range(n):
        m = dict(shared)
        m["x"] = np.ascontiguousarray(np.asarray(inputs["x"][b], np.float32))
        m["mem"] = (np.zeros((1, 1), np.float32) if "mem" in skip
                    else np.ascontiguousarray(np.asarray(inputs["mem"][b], np.float32)))
        maps.append(m)
    return maps


def kernel(**inputs):
    if "nc" not in _CACHE:
        _CACHE["nc"] = build_program()[0]
    nc = _CACHE["nc"]
    maps = make_in_maps(inputs, 8)
    res = run_bass_kernel_spmd(nc, maps, core_ids=list(range(8)))
    return np.stack([np.asarray(r["out"], np.float32) for r in res.results], axis=0)
```

```python
import bisect
import math
from contextlib import ExitStack

import numpy as np
import concourse.bass as bass
import concourse.mybir as mybir
from concourse.bass_utils import run_bass_kernel_spmd

F32 = mybir.dt.float32
BF16 = mybir.dt.bfloat16
ALU = mybir.AluOpType
AF = mybir.ActivationFunctionType
AX = mybir.AxisListType

S = 2048
D = 2048
NT = 16
H = 8
HD = 128
DFF = 5632
NFF = 44
MEM = 256
INW = 7184
EPS = 1e-6
BIG = 30000.0
SQ128 = math.sqrt(128.0)

ENGS = ("pe", "act", "dve", "pool", "sp")
SEM_LIMIT = 2000
N_ENG_SEMS = {"pe": 4, "act": 4, "dve": 4, "pool": 2, "sp": 1}
N_DMA_SEMS = 32
N_SWDMA_SEMS = 16


class Buf:
    __slots__ = ("name", "last_w", "creads", "dreads")

    def __init__(self, name=""):
        self.name = name
        self.last_w = None
        self.creads = {}
        self.dreads = []


class Op:
    __slots__ = ("eng", "name", "kw", "waits", "signal", "pos")

    def __init__(self, eng, name, kw, pos):
        self.eng = eng
        self.name = name
        self.kw = kw
        self.waits = []
        self.signal = None
        self.pos = pos


class Prog:
    def __init__(self, nc, stack):
        self.nc = nc
        self.stack = stack
        self.ops = {e: [] for e in ENGS}
        self.nsig = {e: 0 for e in ENGS}
        self.sigpos = {e: [] for e in ENGS}
        self.sigtok = {e: [] for e in ENGS}
        self.waited = {e: {} for e in ENGS}
        self.esems = {e: [stack.enter_context(nc.semaphore(f"s_{e}_{i}")) for i in range(N_ENG_SEMS[e])]
                      for e in ENGS}
        self.dsems = {"hw": [stack.enter_context(nc.semaphore(f"s_dma_{i}")) for i in range(N_DMA_SEMS)],
                      "sw": [stack.enter_context(nc.semaphore(f"s_swdma_{i}")) for i in range(N_SWDMA_SEMS)]}
        self.ndma_q = {"hw": 0, "sw": 0}
        self.ndma = 0
        self.dma_toks = []

    def sbuf(self, name, shape, dtype):
        return self.stack.enter_context(self.nc.sbuf_tensor(name, list(shape), dtype))

    def psum(self, name, shape, dtype):
        return self.stack.enter_context(self.nc.psum_tensor(name, list(shape), dtype))

    def buf(self, name=""):
        return Buf(name)

    def bufs(self, n):
        return [Buf() for _ in range(n)]

    def _force_signal(self, eng):
        lst = self.ops[eng]
        if not lst:
            return None
        last = lst[-1]
        if last.signal is None:
            n = self.nsig[eng]
            self.nsig[eng] += 1
            sem = self.esems[eng][n // SEM_LIMIT]
            val = n % SEM_LIMIT + 1
            last.signal = (sem, 1)
            self.sigpos[eng].append(last.pos)
            self.sigtok[eng].append((sem, val))
        return last

    def _resolve(self, tok):
        if tok[0] == "d":
            return tok[1], tok[2]
        _, eng, pos = tok
        sp = self.sigpos[eng]
        i = bisect.bisect_left(sp, pos)
        if i < len(sp):
            return self.sigtok[eng][i]
        last = self._force_signal(eng)
        assert last.pos >= pos
        i = bisect.bisect_left(sp, pos)
        return self.sigtok[eng][i]

    def _add_wait(self, op, tok):
        sem, val = self._resolve(tok)
        w = self.waited[op.eng]
        key = id(sem)
        if w.get(key, 0) >= val:
            return
        w[key] = val
        for i, (s, v) in enumerate(op.waits):
            if s is sem:
                op.waits[i] = (sem, max(v, val))
                return
        op.waits.append((sem, val))

    def _deps(self, op, reads, writes, is_dma):
        eng = op.eng
        for b in reads:
            if b.last_w is not None:
                self._add_wait(op, b.last_w)
        for b in writes:
            t = b.last_w
            if t is not None and not (t[0] == "c" and t[1] == eng == "pe" and not is_dma):
                self._add_wait(op, t)
            for e, t in b.creads.items():
                if e == eng == "pe" and not is_dma:
                    continue
                self._add_wait(op, t)
            for t in b.dreads:
                self._add_wait(op, t)

    def _compute_pos_check(self, eng):
        lst = self.ops[eng]
        if lst and lst[-1].signal is None and lst[-1].name not in ("nop", "dma_start"):
            self._force_signal(eng)

    def op(self, eng, name, reads=(), writes=(), **kw):
        lst = self.ops[eng]
        o = Op(eng, name, kw, len(lst))
        self._deps(o, reads, writes, False)
        lst.append(o)
        tok = ("c", eng, o.pos)
        for b in reads:
            b.creads[eng] = tok
        for b in writes:
            b.last_w = tok
            b.creads = {}
            b.dreads = []
        return o

    def dma(self, out, in_, reads=(), writes=(), q="sp", **kw):
        if q != "sp":
            self._compute_pos_check(q)
        lst = self.ops[q]
        o = Op(q, "dma_start", dict(out=out, in_=in_, **kw), len(lst))
        self._deps(o, reads, writes, True)
        kind = "sw" if q == "pool" else "hw"
        pool_ = self.dsems[kind]
        i = self.ndma_q[kind] % len(pool_)
        r = self.ndma_q[kind] // len(pool_)
        self.ndma_q[kind] += 1
        self.ndma += 1
        sem = pool_[i]
        if r > 0:
            self._add_wait(o, ("d", sem, 16 * r))
        o.signal = (sem, 16)
        lst.append(o)
        tok = ("d", sem, 16 * (r + 1))
        self.dma_toks.append(tok)
        for b in reads:
            b.dreads.append(tok)
        for b in writes:
            b.last_w = tok
            b.creads = {}
            b.dreads = []
        return tok

    def barrier(self):
        toks = list(self.dma_toks[-(N_DMA_SEMS + N_SWDMA_SEMS) * 2:])
        for e in ENGS:
            lst = self.ops[e]
            if lst and lst[-1].name not in ("dma_start", "nop"):
                last = self._force_signal(e)
                toks.append(("c", e, last.pos))
            else:
                for o in reversed(lst):
                    if o.name not in ("dma_start", "nop"):
                        assert o.signal is not None
                        toks.append(("c", e, o.pos))
                        break
        for e in ENGS:
            o = Op(e, "nop", {}, len(self.ops[e]))
            for t in toks:
                if t[0] == "c" and t[1] == e:
                    continue
                self._add_wait(o, t)
            self.ops[e].append(o)

    def final_wait(self, eng, toks):
        o = Op(eng, "nop", {}, len(self.ops[eng]))
        for t in toks:
            self._add_wait(o, t)
        self.ops[eng].append(o)

    def emit(self):
        nc = self.nc
        prog = self
        with nc.Block() as block:
            def run(engname):
                def body(e):
                    for o in prog.ops[engname]:
                        for (sem, val) in o.waits:
                            e.wait_ge(sem, val)
                        if o.name == "nop":
                            continue
                        ins = getattr(e, o.name)(**o.kw)
                        if o.signal is not None:
                            ins.then_inc(o.signal[0], o.signal[1])
                return body
            block.tensor(run("pe"))
            block.scalar(run("act"))
            block.vector(run("dve"))
            block.gpsimd(run("pool"))
            block.sync(run("sp"))

    def stats(self):
        return {e: (len(self.ops[e]), self.nsig[e]) for e in ENGS}, self.ndma


class Arena:
    def __init__(self, p, nbytes):
        self.t = p.sbuf("arena", [128, nbytes // 2], BF16)
        self.n = nbytes
        self.off = 0

    def reset(self):
        self.off = 0

    def alloc(self, shape, dtype):
        esz = 2 if dtype == BF16 else 4
        n = 1
        for s in shape:
            n *= s
        nb = n * esz
        a = self.t[:, self.off // 2:(self.off + nb) // 2]
        self.off += (nb + 63) // 64 * 64
        assert self.off <= self.n, f"arena overflow {self.off} > {self.n}"
        if dtype != BF16:
            a = a.bitcast(dtype)
        if len(shape) == 2:
            a = a.rearrange("p (a b) -> p a b", a=shape[0])
        elif len(shape) == 3:
            a = a.rearrange("p (a b c) -> p a b c", a=shape[0], b=shape[1])
        return a


class Ring:
    def __init__(self, items):
        self.items = items
        self.i = 0

    def next(self):
        it = self.items[self.i % len(self.items)]
        self.i += 1
        return it


NCF = 576
NCB = 768 + 9 * 128


def host_consts():
    cf = np.zeros((128, NCF), np.float32)
    ii = np.arange(128)
    cf[:, 0:128] = np.eye(128, dtype=np.float32)
    cf[:, 128:256] = (ii[:, None] <= ii[None, :]).astype(np.float32)
    cf[:, 256:384] = (ii[:, None] > ii[None, :]).astype(np.float32)
    cf[:, 384:512] = 1.0
    gm = np.zeros((128, 8, 8), np.float32)
    for qi in range(8, 16):
        own = qi // 2
        gm[:, qi - 8, own:] = -1e30
    cf[:, 512:576] = gm.reshape(128, 64)
    cb = np.zeros((128, NCB), np.float32)
    cb[:, 0:128] = np.eye(128, dtype=np.float32)
    cb[:, 128:256] = 1.0
    cb[:, 256:384] = (ii[None, :] > ii[:, None]).astype(np.float32) * BIG
    cb[:, 384:512] = (ii[None, :] >= ii[:, None]).astype(np.float32) * BIG
    rr = np.arange(256)
    cb[:, 512:768] = (rr[None, :] < ii[:, None]).astype(np.float32) * (-BIG * SQ128)
    e = np.zeros((9, 9, 128), np.float32)
    for v in range(9):
        e[8, v, :] = 1.0
        if v < 8:
            e[v, v, :] = 1.0
    cb[0:9, 768:768 + 9 * 128] = e.reshape(9, 9 * 128)
    return cf, cb


def rel_bucket_np(n):
    n = np.maximum(n, 0)
    max_exact = 16
    nf = np.maximum(n, 1).astype(np.float32)
    large = max_exact + (np.log(nf / max_exact) / math.log(128 / max_exact) * (32 - max_exact)).astype(np.int32)
    large = np.minimum(large, 31)
    return np.where(n < max_exact, n, large)


def toeplitz_index():
    kk = np.arange(128)[:, None]
    r = np.arange(256)[None, :]
    return rel_bucket_np(r - kk)


class K:
    pass


def build_program(debug_phase=None):
    nc = bass.Bass("TRN2", target_bir_lowering=False)
    k = K()
    k.nc = nc
    dbg = debug_phase is not None

    order = ["inproj", "gdn", "moba", "wout", "xattn", "ffn"]
    need_from = {"mem": "xattn", "w_out": "wout", "w_xq": "xattn", "w_xkv": "xattn", "w_xo": "xattn",
                 "w_gate": "ffn", "w_up": "ffn", "w_down": "ffn"}
    k.skip = set()
    if dbg:
        for nm, ph in need_from.items():
            if order.index(ph) > order.index(debug_phase):
                k.skip.add(nm)

    def din(name, shape, dt=F32):
        if name in k.skip:
            shape = [1, 1]
        return nc.dram_tensor(name, list(shape), dt, kind="ExternalInput").ap()

    DUMP = {"inproj": ("QA", "KA", "KT", "VA", "ZS", "QB", "KB", "VB", "DBG"), "moba": ("OT",), "gdn": ("OT",),
            "wout": ("X1",), "xattn": ("X2",), "ffn": ("OT", "X1", "X2")}.get(debug_phase, ())

    def dscr(name, shape, dt=BF16):
        kind = "ExternalOutput" if name in DUMP else "Internal"
        return nc.dram_tensor(name, list(shape), dt, kind=kind).ap()

    I = {}
    I["x"] = din("x", [S, D])
    I["mem"] = din("mem", [MEM, D])
    for nm in ("mix_norm_g", "xattn_norm_g", "mem_norm_g", "ffn_norm_g", "final_norm_g"):
        I[nm] = din(nm, [1, D])
    I["w_in"] = din("w_in", [D, INW])
    I["gdn_conv_w"] = din("gdn_conv_w", [4, 3072])
    I["gdn_a_log"] = din("gdn_a_log", [1, 8])
    I["gdn_dt_bias"] = din("gdn_dt_bias", [1, 8])
    I["gdn_norm_g"] = din("gdn_norm_g", [1, 128])
    I["moba_norm_g"] = din("moba_norm_g", [1, 128])
    I["rel_bias"] = din("rel_bias", [1, 256])
    I["rb_toep"] = din("rb_toep", [128, 8, 256])
    I["w_out"] = din("w_out", [D, D])
    I["w_xq"] = din("w_xq", [D, 512])
    I["w_xkv"] = din("w_xkv", [D, 1024])
    I["w_xo"] = din("w_xo", [512, D])
    I["w_gate"] = din("w_gate", [D, DFF])
    I["w_up"] = din("w_up", [D, DFF])
    I["ffn_conv_w"] = din("ffn_conv_w", [3, DFF])
    I["ffn_conv_b"] = din("ffn_conv_b", [1, DFF])
    I["w_down"] = din("w_down", [DFF, D])
    I["cst_f"] = din("cst_f", [128, NCF])
    I["cst_b"] = din("cst_b", [128, NCB])
    out = nc.dram_tensor("out", [S, D], F32, kind="ExternalOutput").ap()
    k.I = I
    k.out = out

    k.QA = dscr("QA", [128, 16, 8, 128])
    k.KA = dscr("KA", [128, 16, 8, 128])
    k.KT = dscr("KT", [128, 16, 8, 128])
    k.VA = dscr("VA", [128, 16, 8, 128])
    k.ZS = dscr("ZS", [128, 16, 8, 128])
    k.QB = dscr("QB", [8, 128, 2048])
    k.KB = dscr("KB", [8, 128, 2048])
    k.VB = dscr("VB", [8, 128, 16, 128])
    k.X1 = dscr("X1", [S, D], F32)
    k.X2 = dscr("X2", [S, D], F32)
    if "DBG" in DUMP:
        k.DBG = dscr("DBG", [128, 4096], F32)
    if "OT" in DUMP:
        k.OT = dscr("OT", [128, 16, S], BF16)
    k.b_scr = {nm: Buf(nm) for nm in ("QA", "KA", "KT", "VA", "ZS", "QB", "KB", "VB", "X1", "X2", "DBG")}

    with ExitStack() as st:
        p = Prog(nc, st)
        k.p = p
        k.cf = p.sbuf("cf", [128, NCF], F32)
        k.cb = p.sbuf("cb", [128, NCB], BF16)
        k.b_c = p.buf("consts")
        p.dma(k.cf[:], I["cst_f"], writes=[k.b_c])
        p.dma(k.cb[:], I["cst_b"], writes=[k.b_c], q="pool")
        k.identf = k.cf[:, 0:128]
        k.trif = k.cf[:, 128:256]
        k.suf = k.cf[:, 256:384]
        k.onesf = k.cf[:, 384:512]
        k.gm = k.cf[:, 512:576]
        k.ident = k.cb[:, 0:128]
        k.ones = k.cb[:, 128:256]
        k.umq = k.cb[:, 256:384]
        k.uma = k.cb[:, 384:512]
        k.cm = k.cb[:, 512:768]
        k.emat = k.cb[0:9, 768:768 + 9 * 128].rearrange("p (v n) -> p v n", v=9)
        k.BA = p.sbuf("BA", [128, 16, 16], F32)
        k.b_BA = p.buf("BA")
        k.BETA = p.sbuf("BETA", [128, 16, 8], F32)
        k.GG = p.sbuf("GG", [128, 16, 8], F32)
        k.b_bg = p.buf("betag")
        banks = [(p.psum(f"ps{i}", [128, 512], F32), p.buf(f"ps{i}")) for i in range(8)]
        k.ps = Ring(banks[0:6])
        k.psx = banks[6:8]
        k.banks = banks
        k.ar = Arena(p, 176 * 1024)

        phases = [phase_inproj, phase_gdn, phase_moba, phase_wout, phase_xattn, phase_ffn]
        names = ["inproj", "gdn", "moba", "wout", "xattn", "ffn"]
        out_toks = []
        for fn, nm in zip(phases, names):
            r = fn(k)
            if r:
                out_toks += r
            p.barrier()
            if debug_phase == nm:
                break
        p.final_wait("sp", p.dma_toks[-(N_DMA_SEMS + N_SWDMA_SEMS) * 2:])
        k.stats = p.stats()
        p.emit()
    return nc, k


def MM(k, out, lhsT, rhs, start, stop, r, w):
    k.p.op("pe", "matmul", reads=r, writes=w, out=out, lhsT=lhsT, rhs=rhs, start=start, stop=stop)


def TR(k, out, in_, ident, r, w):
    k.p.op("pe", "transpose", reads=r + [k.b_c], writes=w, out=out, in_=in_, identity=ident)


def ACT(k, out, in_, func, r, w, **kw):
    k.p.op("act", "activation", reads=r, writes=w, out=out, in_=in_, func=func, **kw)


def CP(k, eng, out, in_, r, w):
    if eng == "act":
        k.p.op("act", "copy", reads=r, writes=w, out=out, in_=in_)
    else:
        k.p.op(eng, "tensor_copy", reads=r, writes=w, out=out, in_=in_)


def TT(k, eng, out, in0, in1, op, r, w):
    k.p.op(eng, "tensor_tensor", reads=r, writes=w, out=out, in0=in0, in1=in1, op=op)


def TS(k, eng, out, in0, s1, s2, op0, op1, r, w):
    if s2 is None:
        k.p.op(eng, "tensor_scalar", reads=r, writes=w, out=out, in0=in0, scalar1=s1, scalar2=None, op0=op0)
    else:
        k.p.op(eng, "tensor_scalar", reads=r, writes=w, out=out, in0=in0, scalar1=s1, scalar2=s2, op0=op0, op1=op1)


def STT(k, eng, out, in0, scalar, in1, op0, op1, r, w):
    k.p.op(eng, "scalar_tensor_tensor", reads=r, writes=w, out=out, in0=in0, scalar=scalar, in1=in1,
           op0=op0, op1=op1)


def norm_T(k, tiles, gain, dstT, b_dst, col0=0):
    p = k.p
    ar = k.ar
    gb = ar.alloc((D,), F32)
    b_gb = p.buf()
    p.dma(gb, gain.partition_broadcast(128), writes=[b_gb])
    xt = [ar.alloc((D,), F32) for _ in range(2)]
    b_xt = p.bufs(2)
    junk = ar.alloc((D,), BF16)
    b_junk = p.buf()
    hb = [ar.alloc((D,), BF16) for _ in range(2)]
    b_hb = p.bufs(2)
    ss = ar.alloc((len(tiles),), F32)
    b_ss = p.buf()
    for t, (src, rb) in enumerate(tiles):
        i = t % 2
        p.dma(xt[i], src, reads=rb, writes=[b_xt[i]])
        ACT(k, junk, xt[i], AF.Square, [b_xt[i]], [b_junk, b_ss], accum_out=ss[:, t:t + 1])
        ACT(k, ss[:, t:t + 1], ss[:, t:t + 1], AF.Sqrt, [b_ss], [b_ss], scale=1.0 / D, bias=EPS)
        p.op("dve", "reciprocal", reads=[b_ss], writes=[b_ss], out=ss[:, t:t + 1], in_=ss[:, t:t + 1])
        STT(k, "dve", hb[i], xt[i], ss[:, t:t + 1], gb, ALU.mult, ALU.mult, [b_xt[i], b_ss, b_gb], [b_hb[i]])
        for k4 in range(4):
            pb, bpb = k.ps.next()
            pbv = pb[:].bitcast(BF16)
            for j in range(4):
                kt = k4 * 4 + j
                TR(k, pbv[:, j * 128:(j + 1) * 128], hb[i][:, kt * 128:(kt + 1) * 128], k.ident, [b_hb[i]], [bpb])
            CP(k, "act" if k4 % 2 else "dve",
               dstT[:, k4 * 4:(k4 + 1) * 4, col0 + t * 128:col0 + (t + 1) * 128],
               pbv[:, 0:512].rearrange("p (a b) -> p a b", a=4), [bpb], [b_dst])


def load_w_block(k, ring, src):
    ap, b = ring.next()
    k.p.dma(ap, src, writes=[b], q="pool")
    return ap, b


def phase_inproj(k):
    p = k.p
    ar = k.ar
    I = k.I
    ar.reset()
    hT = ar.alloc((16, S), BF16)
    b_hT = p.buf("hT")
    k.hT = hT
    mark = ar.off
    tiles = [(I["x"][t * 128:(t + 1) * 128, :], []) for t in range(NT)]
    norm_T(k, tiles, I["mix_norm_g"], hT, b_hT)
    p.barrier()
    ar.off = mark
    wring = Ring([(ar.alloc((16, 512), BF16), p.buf()) for _ in range(2)])
    wba = ar.alloc((16, 16), BF16)
    b_wba = p.buf()
    raw = [ar.alloc((S + 4,), F32) for _ in range(2)]
    b_raw = p.bufs(2)
    y = ar.alloc((S,), F32)
    b_y = p.buf()
    y1 = ar.alloc((S,), F32)
    b_y1 = p.buf()
    sq = ar.alloc((S,), BF16)
    b_sq = p.buf()
    rs = ar.alloc((S,), F32)
    b_rs = p.buf()
    obf = [ar.alloc((S,), BF16) for _ in range(2)]
    b_obf = p.bufs(2)
    tst = [ar.alloc((16, 128), BF16) for _ in range(2)]
    b_tst = p.bufs(2)
    zst = [ar.alloc((512,), BF16) for _ in range(3)]
    b_zst = p.bufs(3)
    cw = ar.alloc((96,), F32)
    cwl = ar.alloc((128,), F32)
    b_cw = p.buf()
    small = ar.alloc((64,), F32)
    b_small = p.buf()
    p.dma(cwl[0:96, :], I["gdn_conv_w"].rearrange("j (b q) -> (j b) q", q=128), writes=[b_cw])
    pb, bpb = k.ps.next()
    TR(k, pb[:, 0:96], cwl[0:96, :], k.identf[0:96, 0:96], [b_cw], [bpb])
    CP(k, "dve", cw, pb[:, 0:96], [bpb], [b_cw])
    for i in range(2):
        p.op("pool", "memset", writes=[b_raw[i]], ap=raw[i][:, 0:4], constant=0.0)

    wsrc = I["w_in"].rearrange("(kt q) n -> q kt n", q=128)
    ri = 0
    oi = 0
    ti = 0

    def fblock(wap, wb, f, evac):
        for c in range(4):
            pb, bpb = k.ps.next()
            for kt in range(16):
                MM(k, pb[:], wap[:, kt, f * 128:(f + 1) * 128], hT[:, kt, c * 512:(c + 1) * 512],
                   kt == 0, kt == 15, [wb, b_hT], [bpb])
            evac(pb, bpb, c * 512)

    for blk in range(6):
        wap, wb = load_w_block(k, wring, wsrc[:, :, blk * 512:(blk + 1) * 512])
        for f in range(4):
            fb = blk * 4 + f
            kind = fb // 8
            hd = fb % 8
            r_ap, r_b = raw[ri % 2], b_raw[ri % 2]
            ri += 1

            def evac_raw(pb, bpb, tok0, r_ap=r_ap, r_b=r_b):
                CP(k, "act", r_ap[:, 3 + tok0:3 + tok0 + 512], pb[:], [bpb], [r_b])
            fblock(wap, wb, f, evac_raw)
            TS(k, "pool", y1, r_ap[:, 0:S], cw[:, 0 * 24 + fb:0 * 24 + fb + 1], None, ALU.mult, None, [r_b, b_cw], [b_y1])
            STT(k, "dve", y, r_ap[:, 1:1 + S], cw[:, 1 * 24 + fb:1 * 24 + fb + 1], y1, ALU.mult, ALU.add,
                [r_b, b_cw, b_y1], [b_y])
            for j in range(2, 4):
                STT(k, "dve", y, r_ap[:, j:j + S], cw[:, j * 24 + fb:j * 24 + fb + 1], y, ALU.mult, ALU.add,
                    [r_b, b_cw, b_y], [b_y])
            o_ap, o_b = obf[oi % 2], b_obf[oi % 2]
            oi += 1
            if kind == 2:
                ACT(k, o_ap, y, AF.Silu, [b_y], [o_b])
            else:
                ACT(k, y, y, AF.Silu, [b_y], [b_y])
                ACT(k, sq, y, AF.Square, [b_y], [b_sq])
                for c in range(4):
                    pb, bpb = k.ps.next()
                    MM(k, pb[:], k.ones, sq[:, c * 512:(c + 1) * 512], True, True, [b_sq, k.b_c], [bpb])
                    if kind == 0:
                        ACT(k, rs[:, c * 512:(c + 1) * 512], pb[:], AF.Sqrt, [bpb], [b_rs], scale=128.0,
                            bias=128.0 * EPS)
                    else:
                        ACT(k, rs[:, c * 512:(c + 1) * 512], pb[:], AF.Sqrt, [bpb], [b_rs], scale=1.0, bias=EPS)
                p.op("dve", "reciprocal", reads=[b_rs], writes=[b_rs], out=rs, in_=rs)
                TT(k, "dve", o_ap, y, rs, ALU.mult, [b_y, b_rs], [o_b])
            if kind == 0:
                p.dma(k.QA[:, :, hd, :], o_ap.rearrange("p (c t) -> p c t", c=16), reads=[o_b],
                      writes=[k.b_scr["QA"]])
            if kind == 1:
                p.dma(k.KA[:, :, hd, :], o_ap.rearrange("p (c t) -> p c t", c=16), reads=[o_b],
                      writes=[k.b_scr["KA"]])
            if kind >= 1:
                t_ap, t_b = tst[ti % 2], b_tst[ti % 2]
                ti += 1
                for c4 in range(4):
                    pb, bpb = k.ps.next()
                    pbv = pb[:].bitcast(BF16)
                    for j in range(4):
                        c = c4 * 4 + j
                        TR(k, pbv[:, j * 128:(j + 1) * 128], o_ap[:, c * 128:(c + 1) * 128], k.ident, [o_b], [bpb])
                    CP(k, "act" if c4 % 2 else "dve", t_ap[:, c4 * 4:(c4 + 1) * 4, :],
                       pbv[:, 0:512].rearrange("p (a b) -> p a b", a=4), [bpb], [t_b])
                dst = k.KT if kind == 1 else k.VA
                p.dma(dst[:, :, hd, :], t_ap, reads=[t_b], writes=[k.b_scr["KT" if kind == 1 else "VA"]])

    zi = 0
    for blk in range(2):
        wap, wb = load_w_block(k, wring, wsrc[:, :, 3072 + blk * 512:3072 + (blk + 1) * 512])
        for t in range(NT):
            pb, bpb = k.ps.next()
            for kt in range(16):
                MM(k, pb[:], hT[:, kt, t * 128:(t + 1) * 128], wap[:, kt, :], kt == 0, kt == 15, [wb, b_hT], [bpb])
            z_ap, z_b = zst[zi % 3], b_zst[zi % 3]
            zi += 1
            ACT(k, z_ap, pb[:], AF.Silu, [bpb], [z_b])
            p.dma(k.ZS[:, t, blk * 4:(blk + 1) * 4, :], z_ap.rearrange("p (h d) -> p h d", h=4), reads=[z_b],
                  writes=[k.b_scr["ZS"]])
    p.dma(wba, wsrc[:, :, 4096:4112], writes=[b_wba], q="pool")
    for t in range(NT):
        pb, bpb = k.ps.next()
        for kt in range(16):
            MM(k, pb[:, 0:16], hT[:, kt, t * 128:(t + 1) * 128], wba[:, kt, :], kt == 0, kt == 15, [b_wba, b_hT], [bpb])
        CP(k, "dve", k.BA[:, t, :], pb[:, 0:16], [bpb], [k.b_BA])
    albc = small[:, 0:8]
    dtbc = small[:, 8:16]
    nea = small[:, 16:24]
    p.dma(albc, I["gdn_a_log"].partition_broadcast(128), writes=[b_small])
    p.dma(dtbc, I["gdn_dt_bias"].partition_broadcast(128), writes=[b_small])
    ACT(k, nea, albc, AF.Exp, [b_small], [b_small])
    TS(k, "dve", nea, nea, -1.0, None, ALU.mult, ALU.bypass, [b_small], [b_small])
    ACT(k, k.BETA[:], k.BA[:, :, 0:8], AF.Sigmoid, [k.b_BA], [k.b_bg])
    xa = ar.alloc((16, 8), F32)
    xb = ar.alloc((16, 8), F32)
    b_xa = p.buf()
    TT(k, "dve", xa, k.BA[:, :, 8:16], dtbc.unsqueeze(1).broadcast_to([128, 16, 8]), ALU.add, [k.b_BA, b_small], [b_xa])
    ACT(k, xb, xa, AF.Abs, [b_xa], [b_xa])
    ACT(k, xb, xb, AF.Exp, [b_xa], [b_xa], scale=-1.0)
    ACT(k, xb, xb, AF.Ln, [b_xa], [b_xa], bias=1.0)
    TS(k, "dve", xa, xa, 0.0, None, ALU.max, ALU.bypass, [b_xa], [b_xa])
    TT(k, "dve", xa, xa, xb, ALU.add, [b_xa], [b_xa])
    TT(k, "dve", k.GG[:], xa, nea.unsqueeze(1).broadcast_to([128, 16, 8]), ALU.mult, [b_xa, b_small], [k.b_bg])

    for blk in range(4):
        wap, wb = load_w_block(k, wring, wsrc[:, :, 4112 + blk * 512:4112 + (blk + 1) * 512])
        for f in range(4):
            fb = blk * 4 + f
            kind = fb // 8
            hd = fb % 8
            o_ap, o_b = obf[oi % 2], b_obf[oi % 2]
            oi += 1

            def evac_o(pb, bpb, tok0, o_ap=o_ap, o_b=o_b, c=[0]):
                CP(k, "act" if (tok0 // 512) % 2 else "dve", o_ap[:, tok0:tok0 + 512], pb[:], [bpb], [o_b])
            fblock(wap, wb, f, evac_o)
            dst = k.QB if kind == 0 else k.KB
            p.dma(dst[hd], o_ap, reads=[o_b], writes=[k.b_scr["QB" if kind == 0 else "KB"]])
    for blk in range(2):
        wap, wb = load_w_block(k, wring, wsrc[:, :, 6160 + blk * 512:6160 + (blk + 1) * 512])
        for t in range(NT):
            pb, bpb = k.ps.next()
            for kt in range(16):
                MM(k, pb[:], hT[:, kt, t * 128:(t + 1) * 128], wap[:, kt, :], kt == 0, kt == 15, [wb, b_hT], [bpb])
            z_ap, z_b = zst[zi % 3], b_zst[zi % 3]
            zi += 1
            CP(k, "act" if t % 2 else "dve", z_ap, pb[:], [bpb], [z_b])
            p.dma(k.VB[blk * 4:(blk + 1) * 4, :, t, :].rearrange("h p d -> p h d"),
                  z_ap.rearrange("p (h d) -> p h d", h=4), reads=[z_b], writes=[k.b_scr["VB"]])
    if hasattr(k, "DBG"):
        p.dma(k.DBG[:, 0:128], k.BETA[:].rearrange("p a b -> p (a b)"), reads=[k.b_bg], writes=[k.b_scr["DBG"]])
        p.dma(k.DBG[:, 128:256], k.GG[:].rearrange("p a b -> p (a b)"), reads=[k.b_bg], writes=[k.b_scr["DBG"]])


def phase_gdn(k):
    p = k.p
    ar = k.ar
    I = k.I
    ar.reset()
    oT = ar.alloc((16, S), BF16)
    k.oT = oT
    k.b_oT = p.buf("oT")
    k.oT_mark = ar.off
    S32 = ar.alloc((8, 128), F32)
    S16 = ar.alloc((8, 128), BF16)
    b_S32 = p.bufs(2)
    b_S16 = p.bufs(2)
    p.op("pool", "memset", writes=b_S32, ap=S32, constant=0.0)
    p.op("pool", "memset", writes=b_S16, ap=S16, constant=0.0)
    gnb = ar.alloc((128,), F32)
    b_gnb = p.buf()
    p.dma(gnb, I["gdn_norm_g"].partition_broadcast(128), writes=[b_gnb])
    names = ("QA", "KA", "KT", "VA", "ZS")
    inr = Ring([([ar.alloc((8, 128), BF16) for _ in names], p.bufs(5)) for _ in range(2)])
    sm = Ring([(ar.alloc((128,), F32), p.buf()) for _ in range(2)])
    kbg_, vb_, kdec_, zg_ = [ar.alloc((8, 128), BF16) for _ in range(4)]
    b_kbg, b_vb, b_kdec, b_zg = p.bufs(4)

    class G:
        pass
    grp = []
    for g in range(2):
        G_ = G()
        G_.diagG = ar.alloc((4, 128), F32)
        G_.decQ = ar.alloc((4, 128), BF16)
        G_.decA = ar.alloc((4, 128), BF16)
        G_.A = [ar.alloc((4, 128), BF16) for _ in range(2)]
        G_.B = [ar.alloc((4, 128), BF16) for _ in range(2)]
        G_.P = [ar.alloc((4, 128), BF16) for _ in range(2)]
        G_.qk = ar.alloc((4, 128), BF16)
        G_.qkT = ar.alloc((4, 128), BF16)
        G_.wT = ar.alloc((4, 128), BF16)
        G_.u = ar.alloc((4, 128), F32)
        G_.vnew = ar.alloc((4, 128), BF16)
        G_.o1 = ar.alloc((4, 128), F32)
        G_.o = ar.alloc((4, 128), F32)
        G_.sq = ar.alloc((4, 128), F32)
        G_.fin = ar.alloc((4, 128), BF16)
        G_.ss = ar.alloc((4,), F32)
        for nm in ("diagG", "decQ", "decA", "qk", "qkT", "wT", "u", "vnew", "o1", "o", "sq", "fin", "ss"):
            setattr(G_, "b_" + nm, p.buf())
        G_.b_A = p.bufs(2)
        G_.b_B = p.bufs(2)
        G_.b_P = p.bufs(2)
        grp.append(G_)

    def v512(ap):
        return ap.rearrange("p a b -> p (a b)")

    for c in range(16):
        (qT, kT, ktok, vtok, zs), bi = inr.next()
        b_qT, b_kT, b_ktok, b_vtok, b_zs = bi
        for ap, b, nm in zip((qT, kT, ktok, vtok, zs), bi, names):
            p.dma(ap, getattr(k, nm)[:, c, :, :], reads=[k.b_scr[nm]], writes=[b])
        smt, b_sm = sm.next()
        gc = smt[:, 0:8]
        e24 = smt[:, 8:32]
        eg = smt[:, 8:16]
        egrev = smt[:, 16:24]
        gl = smt[:, 24:32]
        bexp = smt[:, 32:40]
        gcl = smt[:, 40:48]
        pbA, bpA = k.ps.next()
        gsrc = k.GG[:, c, :]
        MM(k, pbA[:, 0:8], k.trif, gsrc, True, True, [k.b_c, k.b_bg], [bpA])
        MM(k, pbA[:, 8:16], k.suf, gsrc, True, True, [k.b_c, k.b_bg], [bpA])
        MM(k, pbA[:, 16:24], k.onesf, gsrc, True, True, [k.b_c, k.b_bg], [bpA])
        CP(k, "dve", gc, pbA[:, 0:8], [bpA], [b_sm])
        ACT(k, e24, pbA[:, 0:24], AF.Exp, [bpA], [b_sm])
        TT(k, "dve", bexp, k.BETA[:, c, :], eg, ALU.mult, [k.b_bg, b_sm], [b_sm])
        ACT(k, gcl, k.BETA[:, c, :], AF.Ln, [k.b_bg], [b_sm])
        TT(k, "dve", gcl, gcl, gc, ALU.add, [b_sm], [b_sm])
        bc = lambda a: a.unsqueeze(2).broadcast_to([128, 8, 128])
        TT(k, "pool", kbg_, ktok, bc(bexp), ALU.mult, [b_ktok, b_sm], [b_kbg])
        TT(k, "pool", vb_, vtok, bc(k.BETA[:, c, :]), ALU.mult, [b_vtok, k.b_bg], [b_vb])
        TT(k, "pool", kdec_, ktok, bc(egrev), ALU.mult, [b_ktok, b_sm], [b_kdec])
        TT(k, "pool", zg_, zs, gnb.unsqueeze(1).broadcast_to([128, 8, 128]), ALU.mult, [b_zs, b_gnb], [b_zg])
        bc4 = lambda a: a.unsqueeze(2).broadcast_to([128, 4, 128])
        for g, G_ in enumerate(grp):
            hs = slice(4 * g, 4 * g + 4)
            TT(k, "dve", G_.diagG, k.identf.unsqueeze(1).broadcast_to([128, 4, 128]), bc4(gc[:, hs]), ALU.mult,
               [k.b_c, b_sm], [G_.b_diagG])
        for g, G_ in enumerate(grp):
            for which, um, dec, b_dec, bias in ((0, k.umq, G_.decQ, G_.b_decQ, gc), (1, k.uma, G_.decA, G_.b_decA, gcl)):
                pR, bpR = k.ps.next()
                for hh in range(4):
                    MM(k, pR[:, hh * 128:(hh + 1) * 128], k.onesf, G_.diagG[:, hh, :], True, False,
                       [k.b_c, G_.b_diagG], [bpR])
                    MM(k, pR[:, hh * 128:(hh + 1) * 128], k.ident, um, False, True, [k.b_c], [bpR])
                for hh in range(4):
                    h = 4 * g + hh
                    ACT(k, dec[:, hh, :], pR[:, hh * 128:(hh + 1) * 128], AF.Exp, [bpR, b_sm], [b_dec],
                        scale=-1.0, bias=bias[:, h:h + 1])
        for g, G_ in enumerate(grp):
            pKK, bKK = k.ps.next()
            pQK, bQK = k.ps.next()
            for hh in range(4):
                h = 4 * g + hh
                MM(k, pKK[:, hh * 128:(hh + 1) * 128], kT[:, h, :], kT[:, h, :], True, True, [b_kT], [bKK])
                MM(k, pQK[:, hh * 128:(hh + 1) * 128], qT[:, h, :], kT[:, h, :], True, True, [b_qT, b_kT], [bQK])
            TT(k, "dve", v512(G_.A[0]), pKK[:], v512(G_.decA), ALU.mult, [bKK, G_.b_decA], [G_.b_A[0]])
            TT(k, "dve", v512(G_.qk), pQK[:], v512(G_.decQ), ALU.mult, [bQK, G_.b_decQ], [G_.b_qk])
        for g, G_ in enumerate(grp):
            for src, bsrc, dst, bdst, eng in ((G_.A[0], G_.b_A[0], G_.B[0], G_.b_B[0], "act"),
                                              (G_.qk, G_.b_qk, G_.qkT, G_.b_qkT, "dve")):
                pT, bpT = k.ps.next()
                pTv = pT[:].bitcast(BF16)
                for hh in range(4):
                    TR(k, pTv[:, hh * 128:(hh + 1) * 128], src[:, hh, :], k.ident, [bsrc], [bpT])
                CP(k, eng, v512(dst), pTv[:, 0:512], [bpT], [bdst])
            TT(k, "pool", G_.P[0], k.ident.unsqueeze(1).broadcast_to([128, 4, 128]), G_.B[0], ALU.subtract,
               [k.b_c, G_.b_B[0]], [G_.b_P[0]])
        for lvl in range(1, 7):
            cur = (lvl - 1) % 2
            nxt = lvl % 2
            for g, G_ in enumerate(grp):
                pA, bA = k.ps.next()
                for hh in range(4):
                    MM(k, pA[:, hh * 128:(hh + 1) * 128], G_.B[cur][:, hh, :], G_.A[cur][:, hh, :], True, True,
                       [G_.b_A[cur], G_.b_B[cur]], [bA])
                if lvl <= 5:
                    pB, bB = k.ps.next()
                    for hh in range(4):
                        MM(k, pB[:, hh * 128:(hh + 1) * 128], G_.A[cur][:, hh, :], G_.B[cur][:, hh, :], True, True,
                           [G_.b_A[cur], G_.b_B[cur]], [bB])
                CP(k, "act", v512(G_.A[nxt]), pA[:], [bA], [G_.b_A[nxt]])
                if lvl <= 5:
                    CP(k, "dve", v512(G_.B[nxt]), pB[:], [bB], [G_.b_B[nxt]])
            for g, G_ in enumerate(grp):
                pP, bP = k.ps.next()
                for hh in range(4):
                    MM(k, pP[:, hh * 128:(hh + 1) * 128], k.ident, G_.P[cur][:, hh, :], True, False,
                       [k.b_c, G_.b_P[cur]], [bP])
                    MM(k, pP[:, hh * 128:(hh + 1) * 128], G_.A[nxt][:, hh, :], G_.P[cur][:, hh, :], False, True,
                       [G_.b_A[nxt], G_.b_P[cur]], [bP])
                CP(k, "dve" if g else "act", v512(G_.P[nxt]), pP[:], [bP], [G_.b_P[nxt]])
        for g, G_ in enumerate(grp):
            TTm = G_.P[0]
            b_TT = G_.b_P[0]
            pW, bW = k.ps.next()
            pU, bU = k.ps.next()
            for hh in range(4):
                h = 4 * g + hh
                MM(k, pW[:, hh * 128:(hh + 1) * 128], kbg_[:, h, :], TTm[:, hh, :], True, True, [b_kbg, b_TT], [bW])
                MM(k, pU[:, hh * 128:(hh + 1) * 128], TTm[:, hh, :], vb_[:, h, :], True, True, [b_vb, b_TT], [bU])
            CP(k, "act", v512(G_.wT), pW[:], [bW], [G_.b_wT])
            CP(k, "dve", v512(G_.u), pU[:], [bU], [G_.b_u])
        for g, G_ in enumerate(grp):
            pVN, bVN = k.ps.next()
            pO1, bO1 = k.ps.next()
            for hh in range(4):
                h = 4 * g + hh
                MM(k, pVN[:, hh * 128:(hh + 1) * 128], G_.wT[:, hh, :], S16[:, h, :], True, True,
                   [G_.b_wT, b_S16[g]], [bVN])
                MM(k, pO1[:, hh * 128:(hh + 1) * 128], qT[:, h, :], S16[:, h, :], True, True, [b_qT, b_S16[g]], [bO1])
            TT(k, "dve", v512(G_.vnew), v512(G_.u), pVN[:], ALU.subtract, [G_.b_u, bVN], [G_.b_vnew])
            TT(k, "dve", G_.o1, pO1[:].rearrange("p (a b) -> p a b", a=4), bc4(eg[:, 4 * g:4 * g + 4]), ALU.mult,
               [bO1, b_sm], [G_.b_o1])
            TT(k, "pool", S32[:, 4 * g:4 * g + 4, :], S32[:, 4 * g:4 * g + 4, :], bc4(gl[:, 4 * g:4 * g + 4]), ALU.mult,
               [b_sm, b_S32[g]], [b_S32[g]])
        for g, G_ in enumerate(grp):
            pO2, bO2 = k.ps.next()
            pSU, bSU = k.ps.next()
            for hh in range(4):
                h = 4 * g + hh
                MM(k, pO2[:, hh * 128:(hh + 1) * 128], G_.qkT[:, hh, :], G_.vnew[:, hh, :], True, True,
                   [G_.b_qkT, G_.b_vnew], [bO2])
                MM(k, pSU[:, hh * 128:(hh + 1) * 128], kdec_[:, h, :], G_.vnew[:, hh, :], True, True,
                   [b_kdec, G_.b_vnew], [bSU])
            TT(k, "dve", v512(S32[:, 4 * g:4 * g + 4, :]), v512(S32[:, 4 * g:4 * g + 4, :]), pSU[:], ALU.add,
               [b_S32[g], bSU], [b_S32[g]])
            CP(k, "act", S16[:, 4 * g:4 * g + 4, :], S32[:, 4 * g:4 * g + 4, :], [b_S32[g]], [b_S16[g]])
            TT(k, "dve", v512(G_.o), pO2[:], v512(G_.o1), ALU.add, [bO2, G_.b_o1], [G_.b_o])
        for g, G_ in enumerate(grp):
            TT(k, "pool", G_.sq, G_.o, G_.o, ALU.mult, [G_.b_o], [G_.b_sq])
            p.op("dve", "tensor_reduce", reads=[G_.b_sq], writes=[G_.b_ss], out=G_.ss, in_=G_.sq, axis=AX.X, op=ALU.add)
            ACT(k, G_.ss, G_.ss, AF.Sqrt, [G_.b_ss], [G_.b_ss], scale=1.0 / 128, bias=EPS)
            p.op("dve", "reciprocal", reads=[G_.b_ss], writes=[G_.b_ss], out=G_.ss, in_=G_.ss)
            TT(k, "dve", G_.sq, G_.o, bc4(G_.ss), ALU.mult, [G_.b_o, G_.b_ss, G_.b_sq], [G_.b_sq])
            TT(k, "pool", G_.fin, G_.sq, zg_[:, 4 * g:4 * g + 4, :], ALU.mult, [G_.b_sq, b_zg], [G_.b_fin])
            pT, bpT = k.ps.next()
            pTv = pT[:].bitcast(BF16)
            for hh in range(4):
                TR(k, pTv[:, hh * 128:(hh + 1) * 128], G_.fin[:, hh, :], k.ident, [G_.b_fin], [bpT])
            CP(k, "act", oT[:, 4 * g:4 * g + 4, c * 128:(c + 1) * 128],
               pTv[:, 0:512].rearrange("p (a b) -> p a b", a=4), [bpT], [k.b_oT])


def phase_moba(k):
    p = k.p
    ar = k.ar
    I = k.I
    ar.off = k.oT_mark
    oT = k.oT
    hr = Ring([([ar.alloc((S,), BF16), ar.alloc((S,), BF16), ar.alloc((16, 132), BF16)], p.bufs(3)) for _ in range(2)])
    for (aps, bs) in hr.items:
        p.op("pool", "memset", writes=[bs[2]], ap=aps[2][:, :, 128:132], constant=1.0)
    tg = ar.alloc((8, 256), F32)
    Tp = ar.alloc((8, 256), BF16)
    rbb = ar.alloc((256,), F32)
    bmax = ar.alloc((8,), F32)
    mgb = ar.alloc((128,), F32)
    b_tp = p.buf()
    p.dma(tg, I["rb_toep"], writes=[b_tp])
    p.dma(rbb, I["rel_bias"].partition_broadcast(128), writes=[b_tp])
    p.dma(mgb, I["moba_norm_g"].partition_broadcast(128), writes=[b_tp])
    rb31 = rbb[:, 31 * 8:32 * 8]
    TT(k, "dve", tg, tg, rb31.unsqueeze(2).broadcast_to([128, 8, 256]), ALU.subtract, [b_tp], [b_tp])
    STT(k, "dve", Tp, tg, SQ128, k.cm.unsqueeze(1).broadcast_to([128, 8, 256]), ALU.mult, ALU.add, [b_tp, k.b_c], [b_tp])
    p.op("dve", "tensor_reduce", reads=[b_tp], writes=[b_tp], out=bmax, in_=rbb.rearrange("p (b h) -> p h b", h=8),
         axis=AX.X, op=ALU.max)
    TT(k, "dve", bmax, bmax, rb31, ALU.subtract, [b_tp], [b_tp])
    sq = ar.alloc((S,), BF16)
    ksq = ar.alloc((S,), BF16)
    b_sq, b_ksq = p.bufs(2)
    sm = ar.alloc((256,), F32)
    b_sm = p.buf()
    kms = sm[:, 0:8]
    km4 = sm[:, 8:12]
    ksc = sm[:, 12:13]
    nm = sm[:, 16:32]
    gsb = sm[:, 32:96].rearrange("p (a b) -> p a b", a=8)
    g2 = sm[:, 96:160].rearrange("p (a b) -> p a b", a=8)
    eq = sm[:, 160:224].rearrange("p (a b) -> p a b", a=8)
    mx = sm[:, 224:232]
    kmT = ar.alloc((8,), BF16)
    MBq = ar.alloc((16, 16), BF16)
    b_MBq = p.buf()
    p.op("pool", "memset", writes=[b_MBq], ap=MBq, constant=0.0)
    MB = ar.alloc((S,), BF16)
    b_MB = p.buf()
    PTr = Ring([(ar.alloc((256,), BF16), p.buf()) for _ in range(5)])
    yq = [ar.alloc((128,), F32) for _ in range(2)]
    b_yq = p.bufs(2)
    junk = ar.alloc((128,), BF16)
    b_junk = p.buf()
    fin = [ar.alloc((256,), BF16) for _ in range(2)]
    b_fin = p.bufs(2)
    sm2 = Ring([(ar.alloc((8,), F32), p.buf()) for _ in range(4)])
    bc8 = lambda a: a.unsqueeze(2).broadcast_to([128, 8, 8])

    for h in range(8):
        (qbT, kbT, vb1), (b_q, b_k, b_v) = hr.next()
        p.dma(qbT, k.QB[h], reads=[k.b_scr["QB"]], writes=[b_q])
        p.dma(kbT, k.KB[h], reads=[k.b_scr["KB"]], writes=[b_k])
        p.dma(vb1[:, :, 0:128], k.VB[h], reads=[k.b_scr["VB"]], writes=[b_v])
        ACT(k, sq, qbT, AF.Square, [b_q], [b_sq])
        ACT(k, ksq, kbT, AF.Square, [b_k], [b_ksq])
        p.op("dve", "tensor_reduce", reads=[b_k], writes=[b_sm], out=kms, in_=kbT.rearrange("p (n t) -> p n t", n=8),
             axis=AX.X, op=ALU.add)
        TS(k, "dve", kmT, kms, 1.0 / 256, None, ALU.mult, None, [b_sm], [b_sm])
        pG, bG = k.ps.next()
        for qi in range(8, 16):
            MM(k, pG[:, (qi - 8) * 8:(qi - 7) * 8], qbT[:, qi * 128:(qi + 1) * 128], kmT, True, True, [b_q, b_sm], [bG])
        pN, bN = k.ps.next()
        for qi in range(16):
            MM(k, pN[:, qi:qi + 1], sq[:, qi * 128:(qi + 1) * 128], k.ones[:, 0:1], True, True, [b_sq, k.b_c], [bN])
        for c in range(4):
            pK, bK = k.ps.next()
            MM(k, pK[:], k.ones, ksq[:, c * 512:(c + 1) * 512], True, True, [b_ksq, k.b_c], [bK])
            p.op("dve", "tensor_reduce", reads=[bK], writes=[b_sm], out=km4[:, c:c + 1], in_=pK[:], axis=AX.X, op=ALU.max)
        p.op("dve", "tensor_reduce", reads=[b_sm], writes=[b_sm], out=ksc, in_=km4, axis=AX.X, op=ALU.max)
        TS(k, "dve", ksc, ksc, 1.0 / 128, None, ALU.mult, None, [b_sm], [b_sm])
        ACT(k, nm, pN[:, 0:16], AF.Sqrt, [bN, b_sm], [b_sm], scale=ksc)
        TS(k, "dve", MBq[:, :, 8], nm, bmax[:, h:h + 1], -SQ128, ALU.add, ALU.mult, [b_sm, b_tp], [b_MBq])
        TT(k, "dve", gsb, pG[:, 0:64].rearrange("p (a b) -> p a b", a=8), k.gm.rearrange("p (a b) -> p a b", a=8),
           ALU.add, [bG, k.b_c], [b_sm])
        src = gsb
        for it in range(2):
            p.op("dve", "tensor_reduce", reads=[b_sm], writes=[b_sm], out=mx, in_=src, axis=AX.X, op=ALU.max)
            TT(k, "dve", eq, src, bc8(mx), ALU.is_equal, [b_sm], [b_sm])
            STT(k, "dve", g2, eq, -1e30, src, ALU.mult, ALU.add, [b_sm], [b_sm])
            src = g2
        p.op("dve", "tensor_reduce", reads=[b_sm], writes=[b_sm], out=mx, in_=g2, axis=AX.X, op=ALU.max)
        TT(k, "dve", eq, gsb, bc8(mx), ALU.is_ge, [b_sm], [b_sm])
        TS(k, "dve", MBq[:, 8:16, 0:8], eq, BIG * SQ128, -BIG * SQ128, ALU.mult, ALU.add, [b_sm], [b_MBq])
        for half in range(2):
            pM, bM = k.ps.next()
            pMv = pM[:].bitcast(BF16)
            for j in range(8):
                qi = half * 8 + j
                TR(k, pMv[0:9, j * 128:(j + 1) * 128], MBq[:, qi, 0:9], k.ident, [b_MBq], [bM])
            CP(k, "act" if half else "dve", MB[0:9, half * 1024:(half + 1) * 1024], pMv[0:9, 0:1024], [bM], [b_MB])
        for qb in range(8):
            pend = []

            def emit_pv(item):
                kj_, PT_, b_PT_ = item
                for qt in range(2):
                    if kj_ <= 2 * qb + qt:
                        pO, bO = k.psx[qt]
                        MM(k, pO[:, 0:129], PT_[:, qt * 128:(qt + 1) * 128], vb1[:, kj_, 0:129],
                           kj_ == 0, kj_ == 2 * qb + qt, [b_PT_, b_v], [bO])
            for kj in range(2 * qb + 2):
                n = kj // 2
                pS, bS = k.ps.next()
                q0 = qb * 256
                v = n if (qb >= 4 and n < qb) else 8
                if n == qb and kj % 2 == 0:
                    segs = [(0, 256, Tp[:, h, 0:256])]
                elif n == qb:
                    segs = [(128, 256, Tp[:, h, 0:128])]
                elif n == qb - 1 and kj % 2 == 1:
                    segs = [(0, 128, Tp[:, h, 128:256]), (128, 256, None)]
                else:
                    segs = [(0, 256, None)]
                c0 = segs[0][0]
                for (a, b, bias) in segs:
                    MM(k, pS[:, a:b], kbT[:, kj * 128:(kj + 1) * 128], qbT[:, q0 + a:q0 + b], True, False, [b_k, b_q], [bS])
                    MM(k, pS[:, a:b], k.emat[:, v, :], MB[0:9, q0 + a:q0 + b], False, bias is None, [k.b_c, b_MB], [bS])
                    if bias is not None:
                        MM(k, pS[:, a:b], k.ident, bias, False, True, [k.b_c, b_tp], [bS])
                PT, b_PT = PTr.next()
                ACT(k, PT[:, c0:256], pS[:, c0:256], AF.Exp, [bS], [b_PT], scale=1.0 / SQ128)
                pend.append((kj, PT, b_PT))
                if len(pend) > 2:
                    emit_pv(pend.pop(0))
            while pend:
                emit_pv(pend.pop(0))
            f_ap, f_b = fin[qb % 2], b_fin[qb % 2]
            for qt in range(2):
                s2, b_s2 = sm2.next()
                y_ap, y_b = yq[qt], b_yq[qt]
                pO, bO = k.psx[qt]
                p.op("dve", "reciprocal", reads=[bO], writes=[b_s2], out=s2[:, 0:1], in_=pO[:, 128:129])
                TS(k, "dve", y_ap, pO[:, 0:128], s2[:, 0:1], None, ALU.mult, None, [bO, b_s2], [y_b])
                ACT(k, junk, y_ap, AF.Square, [y_b], [b_junk, b_s2], accum_out=s2[:, 1:2])
                ACT(k, s2[:, 1:2], s2[:, 1:2], AF.Sqrt, [b_s2], [b_s2], scale=1.0 / 128, bias=EPS)
                p.op("dve", "reciprocal", reads=[b_s2], writes=[b_s2], out=s2[:, 1:2], in_=s2[:, 1:2])
                STT(k, "dve", f_ap[:, qt * 128:(qt + 1) * 128], y_ap, s2[:, 1:2], mgb, ALU.mult, ALU.mult,
                    [y_b, b_s2, b_tp], [f_b])
            pT, bpT = k.ps.next()
            pTv = pT[:].bitcast(BF16)
            for qt in range(2):
                TR(k, pTv[:, qt * 128:(qt + 1) * 128], f_ap[:, qt * 128:(qt + 1) * 128], k.ident, [f_b], [bpT])
            CP(k, "act", oT[:, 8 + h, qb * 256:(qb + 1) * 256], pTv[:, 0:256], [bpT], [k.b_oT])
    if hasattr(k, "OT"):
        p.dma(k.OT, oT, reads=[k.b_oT], writes=[k.b_scr["DBG"]])


def phase_wout(k):
    p = k.p
    ar = k.ar
    I = k.I
    oT = k.oT
    ar.off = k.oT_mark
    h2T = ar.alloc((16, S), BF16)
    k.h2T = h2T
    k.b_h2T = p.buf("h2T")
    k.ws_mark = ar.off
    wring = Ring([(ar.alloc((16, 512), BF16), p.buf()) for _ in range(2)])
    xr = Ring([(ar.alloc((512,), F32), p.buf()) for _ in range(3)])
    wsrc = I["w_out"].rearrange("(kt q) n -> q kt n", q=128)
    for c in range(4):
        wap, wb = load_w_block(k, wring, wsrc[:, :, c * 512:(c + 1) * 512])
        for t in range(NT):
            x_ap, x_b = xr.next()
            p.dma(x_ap, I["x"][t * 128:(t + 1) * 128, c * 512:(c + 1) * 512], writes=[x_b])
            pb, bpb = k.ps.next()
            for kt in range(16):
                MM(k, pb[:], oT[:, kt, t * 128:(t + 1) * 128], wap[:, kt, :], kt == 0, kt == 15, [wb, k.b_oT], [bpb])
            TT(k, "dve", x_ap, pb[:], x_ap, ALU.add, [bpb, x_b], [x_b])
            p.dma(k.X1[t * 128:(t + 1) * 128, c * 512:(c + 1) * 512], x_ap, reads=[x_b], writes=[k.b_scr["X1"]])
    p.barrier()
    ar.off = k.ws_mark
    tiles = [(k.X1[t * 128:(t + 1) * 128, :], [k.b_scr["X1"]]) for t in range(NT)]
    norm_T(k, tiles, I["xattn_norm_g"], h2T, k.b_h2T)


def phase_xattn(k):
    p = k.p
    ar = k.ar
    I = k.I
    h2T = k.h2T
    ar.off = 0
    memT = ar.alloc((16, MEM), BF16)
    b_memT = p.buf()
    kxT = ar.alloc((4, MEM), BF16)
    vx = ar.alloc((2, 512), BF16)
    b_kv = p.buf()
    qxT = ar.alloc((4, S), BF16)
    b_qx = p.buf()
    oxT = ar.alloc((4, S), BF16)
    b_ox = p.buf()
    wxo = ar.alloc((4, D), BF16)
    b_wxo = p.buf()
    assert ar.off <= k.oT_mark
    ar.off = k.ws_mark
    tiles = [(I["mem"][t * 128:(t + 1) * 128, :], []) for t in range(2)]
    norm_T(k, tiles, I["mem_norm_g"], memT, b_memT)
    p.barrier()
    ar.off = k.ws_mark
    wring = Ring([(ar.alloc((16, 512), BF16), p.buf()) for _ in range(2)])
    Pf = Ring([(ar.alloc((256,), F32), p.buf()) for _ in range(4)])
    Pn = Ring([(ar.alloc((256,), BF16), p.buf()) for _ in range(4)])
    PnT = Ring([(ar.alloc((2, 128), BF16), p.buf()) for _ in range(4)])
    sm = Ring([(ar.alloc((8,), F32), p.buf()) for _ in range(8)])
    xr = Ring([(ar.alloc((512,), F32), p.buf()) for _ in range(3)])
    wkv = I["w_xkv"].rearrange("(kt q) n -> q kt n", q=128)
    wap, wb = load_w_block(k, wring, wkv[:, :, 0:512])
    for hx in range(4):
        pb, bpb = k.ps.next()
        for kt in range(16):
            MM(k, pb[:, 0:MEM], wap[:, kt, hx * 128:(hx + 1) * 128], memT[:, kt, :], kt == 0, kt == 15, [wb, b_memT], [bpb])
        CP(k, "act" if hx % 2 else "dve", kxT[:, hx, :], pb[:, 0:MEM], [bpb], [b_kv])
    wap, wb = load_w_block(k, wring, wkv[:, :, 512:1024])
    for mt in range(2):
        pb, bpb = k.ps.next()
        for kt in range(16):
            MM(k, pb[:], memT[:, kt, mt * 128:(mt + 1) * 128], wap[:, kt, :], kt == 0, kt == 15, [wb, b_memT], [bpb])
        CP(k, "act" if mt else "dve", vx[:, mt, :], pb[:], [bpb], [b_kv])
    wq = I["w_xq"].rearrange("(kt q) n -> q kt n", q=128)
    wap, wb = load_w_block(k, wring, wq)
    for hx in range(4):
        for c in range(4):
            pb, bpb = k.ps.next()
            for kt in range(16):
                MM(k, pb[:], wap[:, kt, hx * 128:(hx + 1) * 128], h2T[:, kt, c * 512:(c + 1) * 512], kt == 0, kt == 15,
                   [wb, k.b_h2T], [bpb])
            CP(k, "act" if c % 2 else "dve", qxT[:, hx, c * 512:(c + 1) * 512], pb[:], [bpb], [b_qx])
    for t in range(NT):
        st = []
        for hx in range(4):
            pS, bS = k.ps.next()
            MM(k, pS[:, 0:MEM], qxT[:, hx, t * 128:(t + 1) * 128], kxT[:, hx, :], True, True, [b_qx, b_kv], [bS])
            s_ap, s_b = sm.next()
            st.append([pS, bS, s_ap, s_b])
        for hx in range(4):
            pS, bS, s_ap, s_b = st[hx]
            p.op("dve", "tensor_reduce", reads=[bS], writes=[s_b], out=s_ap[:, 0:1], in_=pS[:, 0:MEM], axis=AX.X, op=ALU.max)
            TS(k, "dve", s_ap[:, 1:2], s_ap[:, 0:1], -1.0 / SQ128, None, ALU.mult, None, [s_b], [s_b])
        for hx in range(4):
            pS, bS, s_ap, s_b = st[hx]
            pf, b_pf = Pf.next()
            ACT(k, pf, pS[:, 0:MEM], AF.Exp, [bS, s_b], [b_pf, s_b], scale=1.0 / SQ128, bias=s_ap[:, 1:2],
                accum_out=s_ap[:, 2:3])
            st[hx] += [pf, b_pf]
        for hx in range(4):
            pS, bS, s_ap, s_b, pf, b_pf = st[hx]
            p.op("dve", "reciprocal", reads=[s_b], writes=[s_b], out=s_ap[:, 3:4], in_=s_ap[:, 2:3])
            pn, b_pn = Pn.next()
            TS(k, "dve", pn, pf, s_ap[:, 3:4], None, ALU.mult, None, [b_pf, s_b], [b_pn])
            st[hx] += [pn, b_pn]
        for hx in range(4):
            pn, b_pn = st[hx][6], st[hx][7]
            pT, bpT = k.ps.next()
            pTv = pT[:].bitcast(BF16)
            for mt in range(2):
                TR(k, pTv[:, mt * 128:(mt + 1) * 128], pn[:, mt * 128:(mt + 1) * 128], k.ident, [b_pn], [bpT])
            pnt, b_pnt = PnT.next()
            CP(k, "act" if hx % 2 else "dve", pnt, pTv[:, 0:256].rearrange("p (a b) -> p a b", a=2), [bpT], [b_pnt])
            st[hx] += [pnt, b_pnt]
        for hx in range(4):
            pnt, b_pnt = st[hx][8], st[hx][9]
            pO, bO = k.ps.next()
            for mt in range(2):
                MM(k, pO[:, 0:128], vx[:, mt, hx * 128:(hx + 1) * 128], pnt[:, mt, :], mt == 0, mt == 1, [b_kv, b_pnt], [bO])
            CP(k, "dve" if hx % 2 else "act", oxT[:, hx, t * 128:(t + 1) * 128], pO[:, 0:128], [bO], [b_ox])
    p.dma(wxo, I["w_xo"].rearrange("(kt q) n -> q kt n", q=128), writes=[b_wxo], q="pool")
    for t in range(NT):
        for c in range(4):
            x_ap, x_b = xr.next()
            p.dma(x_ap, k.X1[t * 128:(t + 1) * 128, c * 512:(c + 1) * 512], reads=[k.b_scr["X1"]], writes=[x_b])
            pb, bpb = k.ps.next()
            for kt in range(4):
                MM(k, pb[:], oxT[:, kt, t * 128:(t + 1) * 128], wxo[:, kt, c * 512:(c + 1) * 512], kt == 0, kt == 3,
                   [b_wxo, b_ox], [bpb])
            TT(k, "dve", x_ap, pb[:], x_ap, ALU.add, [bpb, x_b], [x_b])
            p.dma(k.X2[t * 128:(t + 1) * 128, c * 512:(c + 1) * 512], x_ap, reads=[x_b], writes=[k.b_scr["X2"]])


def phase_ffn(k):
    p = k.p
    ar = k.ar
    I = k.I
    HT = 1024
    halo = p.sbuf("halo", [128, NFF, 2], F32)
    b_halo = p.buf()
    p.op("pool", "memset", writes=[b_halo], ap=halo[:], constant=0.0)
    cwf = p.sbuf("cwf", [128, 4 * NFF], F32)
    b_cwf = p.buf()
    for hf in range(2):
        ar.reset()
        aT = ar.alloc((NFF, HT), BF16)
        b_aT = p.buf()
        mark0 = ar.off
        h3T = ar.alloc((16, HT), BF16)
        b_h3T = p.buf()
        mark1 = ar.off
        tiles = [(k.X2[(hf * 8 + t) * 128:(hf * 8 + t + 1) * 128, :], [k.b_scr["X2"]]) for t in range(8)]
        norm_T(k, tiles, I["ffn_norm_g"], h3T, b_h3T)
        p.barrier()
        ar.off = mark1
        if hf == 0:
            stg = ar.alloc((128,), F32)
            b_stg = p.buf()
            for j in range(4):
                src = (I["ffn_conv_w"][j] if j < 3 else I["ffn_conv_b"][0]).rearrange("(b q) -> b q", q=128)
                p.dma(stg[0:NFF, :], src, writes=[b_stg])
                pb, bpb = k.ps.next()
                TR(k, pb[:, 0:NFF], stg[0:NFF, :], k.identf[0:NFF, 0:NFF], [b_stg], [bpb])
                CP(k, "dve", cwf[:, j * NFF:(j + 1) * NFF], pb[:, 0:NFF], [bpb], [b_cwf])
        wg = Ring([(ar.alloc((16, 256), BF16), p.buf()) for _ in range(2)])
        wu = Ring([(ar.alloc((16, 256), BF16), p.buf()) for _ in range(2)])
        graw = Ring([(ar.alloc((HT + 2,), F32), p.buf()) for _ in range(2)])
        gy = Ring([(ar.alloc((HT,), F32), p.buf()) for _ in range(2)])
        wgs = I["w_gate"].rearrange("(kt q) n -> q kt n", q=128)
        wus = I["w_up"].rearrange("(kt q) n -> q kt n", q=128)
        for j in range(NFF):
            if j % 2 == 0:
                g_ap2, g_b = wg.next()
                u_ap2, u_b = wu.next()
                p.dma(g_ap2, wgs[:, :, j * 128:(j + 2) * 128], writes=[g_b], q="pool")
                p.dma(u_ap2, wus[:, :, j * 128:(j + 2) * 128], writes=[u_b], q="pool")
            g_ap = g_ap2[:, :, (j % 2) * 128:(j % 2 + 1) * 128]
            u_ap = u_ap2[:, :, (j % 2) * 128:(j % 2 + 1) * 128]
            r_ap, r_b = graw.next()
            y_ap, y_b = gy.next()
            pus = []
            for c in range(2):
                pg, bg = k.ps.next()
                pu, bu = k.ps.next()
                for kt in range(16):
                    MM(k, pg[:], g_ap[:, kt, :], h3T[:, kt, c * 512:(c + 1) * 512], kt == 0, kt == 15, [g_b, b_h3T], [bg])
                for kt in range(16):
                    MM(k, pu[:], u_ap[:, kt, :], h3T[:, kt, c * 512:(c + 1) * 512], kt == 0, kt == 15, [u_b, b_h3T], [bu])
                CP(k, "act", r_ap[:, 2 + c * 512:2 + (c + 1) * 512], pg[:], [bg], [r_b])
                pus.append((pu, bu))
            CP(k, "pool", r_ap[:, 0:2], halo[:, j, :], [b_halo], [r_b])
            if hf == 0:
                CP(k, "pool", halo[:, j, :], r_ap[:, HT:HT + 2], [r_b], [b_halo])
            TS(k, "dve", y_ap, r_ap[:, 0:HT], cwf[:, j:j + 1], None, ALU.mult, None, [r_b, b_cwf], [y_b])
            for tap in range(1, 3):
                STT(k, "dve", y_ap, r_ap[:, tap:tap + HT], cwf[:, tap * NFF + j:tap * NFF + j + 1], y_ap, ALU.mult, ALU.add,
                    [r_b, b_cwf, y_b], [y_b])
            ACT(k, y_ap, y_ap, AF.Silu, [y_b, b_cwf], [y_b], bias=cwf[:, 3 * NFF + j:3 * NFF + j + 1])
            for c in range(2):
                pu, bu = pus[c]
                TT(k, "dve", aT[:, j, c * 512:(c + 1) * 512], y_ap[:, c * 512:(c + 1) * 512], pu[:], ALU.mult,
                   [y_b, bu], [b_aT])
        p.barrier()
        ar.off = mark0
        x3 = ar.alloc((8, D), F32)
        b_x3 = p.bufs(8)
        aflat = aT.rearrange("p a b -> p (a b)")
        fgb = aflat[:, 0:2 * D].bitcast(F32)
        junk = aflat[:, 2 * D:3 * D]
        b_fgb = b_aT
        b_junk = b_aT
        wd = Ring([(ar.alloc((4, 512), BF16), p.buf()) for _ in range(3)])
        xr = Ring([(ar.alloc((512,), F32), p.buf()) for _ in range(2)])
        ssf = ar.alloc((8,), F32)
        b_ssf = p.buf()
        for cc in range(4):
            for j4 in range(NFF // 4):
                w_ap, w_b = wd.next()
                p.dma(w_ap, I["w_down"][j4 * 512:(j4 + 1) * 512, cc * 512:(cc + 1) * 512].rearrange("(a q) n -> q a n", q=128),
                      writes=[w_b], q="pool")
                for a in range(4):
                    j = j4 * 4 + a
                    for tt in range(8):
                        pb, bpb = k.banks[tt]
                        MM(k, pb[:], aT[:, j, tt * 128:(tt + 1) * 128], w_ap[:, a, :], j == 0, j == NFF - 1, [w_b, b_aT], [bpb])
            for tt in range(8):
                pb, bpb = k.banks[tt]
                x_ap, x_b = xr.next()
                row = (hf * 8 + tt) * 128
                p.dma(x_ap, k.X2[row:row + 128, cc * 512:(cc + 1) * 512], reads=[k.b_scr["X2"]], writes=[x_b])
                TT(k, "dve", x3[:, tt, cc * 512:(cc + 1) * 512], pb[:], x_ap, ALU.add, [bpb, x_b], [b_x3[tt]])
        p.dma(fgb, I["final_norm_g"].partition_broadcast(128), writes=[b_aT])
        outs = []
        for tt in range(8):
            row = (hf * 8 + tt) * 128
            ACT(k, junk, x3[:, tt, :], AF.Square, [b_x3[tt]], [b_junk, b_ssf], accum_out=ssf[:, tt:tt + 1])
            ACT(k, ssf[:, tt:tt + 1], ssf[:, tt:tt + 1], AF.Sqrt, [b_ssf], [b_ssf], scale=1.0 / D, bias=EPS)
            p.op("dve", "reciprocal", reads=[b_ssf], writes=[b_ssf], out=ssf[:, tt:tt + 1], in_=ssf[:, tt:tt + 1])
            STT(k, "dve", x3[:, tt, :], x3[:, tt, :], ssf[:, tt:tt + 1], fgb, ALU.mult, ALU.mult,
                [b_x3[tt], b_ssf, b_fgb], [b_x3[tt]])
            outs.append(p.dma(k.out[row:row + 128, :], x3[:, tt, :], reads=[b_x3[tt]]))
        p.barrier()
    return []


_CACHE = {}


def make_in_maps(inputs, n=8, skip=()):
    cf, cb = host_consts()
    tidx = toeplitz_index()
    rb = np.asarray(inputs["rel_bias"], np.float32)
    rb_toep = np.ascontiguousarray(rb[tidx].transpose(0, 2, 1))
    shared = {
        "mix_norm_g": inputs["mix_norm_g"].reshape(1, D), "xattn_norm_g": inputs["xattn_norm_g"].reshape(1, D),
        "mem_norm_g": inputs["mem_norm_g"].reshape(1, D), "ffn_norm_g": inputs["ffn_norm_g"].reshape(1, D),
        "final_norm_g": inputs["final_norm_g"].reshape(1, D),
        "w_in": inputs["w_in"][0], "gdn_conv_w": inputs["gdn_conv_w"][0], "gdn_a_log": inputs["gdn_a_log"].reshape(1, 8),
        "gdn_dt_bias": inputs["gdn_dt_bias"].reshape(1, 8), "gdn_norm_g": inputs["gdn_norm_g"].reshape(1, 128),
        "moba_norm_g": inputs["moba_norm_g"].reshape(1, 128), "rel_bias": rb.reshape(1, 256), "rb_toep": rb_toep,
        "w_out": inputs["w_out"][0], "w_xq": inputs["w_xq"][0], "w_xkv": inputs["w_xkv"][0], "w_xo": inputs["w_xo"][0],
        "w_gate": inputs["w_gate"][0], "w_up": inputs["w_up"][0], "ffn_conv_w": inputs["ffn_conv_w"][0],
        "ffn_conv_b": inputs["ffn_conv_b"].reshape(1, DFF), "w_down": inputs["w_down"][0],
        "cst_f": cf, "cst_b": cb,
    }
    shared = {kk: (np.zeros((1, 1), np.float32) if kk in skip else np.ascontiguousarray(np.asarray(v, np.float32)))
              for kk, v in shared.items()}
    maps = []
    for b in range(n):
        m = dict(shared)
        m["x"] = np.ascontiguousarray(np.asarray(inputs["x"][b], np.float32))
        m["mem"] = (np.zeros((1, 1), np.float32) if "mem" in skip
                    else np.ascontiguousarray(np.asarray(inputs["mem"][b], np.float32)))
        maps.append(m)
    return maps


def kernel(**inputs):
    if "nc" not in _CACHE:
        _CACHE["nc"] = build_program()[0]
    nc = _CACHE["nc"]
    maps = make_in_maps(inputs, 8)
    res = run_bass_kernel_spmd(nc, maps, core_ids=list(range(8)))
    return np.stack([np.asarray(r["out"], np.float32) for r in res.results], axis=0)
```

```python
import bisect
import math
from contextlib import ExitStack

import numpy as np
import concourse.bass as bass
import concourse.mybir as mybir
from concourse.bass_utils import run_bass_kernel_spmd

F32 = mybir.dt.float32
BF16 = mybir.dt.bfloat16
ALU = mybir.AluOpType
AF = mybir.ActivationFunctionType
AX = mybir.AxisListType

S = 2048
D = 2048
NT = 16
H = 8
HD = 128
DFF = 5632
NFF = 44
MEM = 256
INW = 7184
EPS = 1e-6
BIG = 30000.0
SQ128 = math.sqrt(128.0)

ENGS = ("pe", "act", "dve", "pool", "sp")
SEM_LIMIT = 2000
N_ENG_SEMS = {"pe": 14, "act": 5, "dve": 5, "pool": 3, "sp": 1}
N_DMA_SEMS = 32
N_SWDMA_SEMS = 16


class Buf:
    __slots__ = ("name", "last_w", "creads", "dreads")

    def __init__(self, name=""):
        self.name = name
        self.last_w = None
        self.creads = {}
        self.dreads = []


class Op:
    __slots__ = ("eng", "name", "kw", "waits", "signal", "pos")

    def __init__(self, eng, name, kw, pos):
        self.eng = eng
        self.name = name
        self.kw = kw
        self.waits = []
        self.signal = None
        self.pos = pos


class Prog:
    def __init__(self, nc, stack):
        self.nc = nc
        self.stack = stack
        self.ops = {e: [] for e in ENGS}
        self.nsig = {e: 0 for e in ENGS}
        self.sigpos = {e: [] for e in ENGS}
        self.sigtok = {e: [] for e in ENGS}
        self.waited = {e: {} for e in ENGS}
        self.esems = {e: [stack.enter_context(nc.semaphore(f"s_{e}_{i}")) for i in range(N_ENG_SEMS[e])]
                      for e in ENGS}
        self.dsems = {"hw": [stack.enter_context(nc.semaphore(f"s_dma_{i}")) for i in range(N_DMA_SEMS)],
                      "sw": [stack.enter_context(nc.semaphore(f"s_swdma_{i}")) for i in range(N_SWDMA_SEMS)]}
        self.ndma_q = {"hw": 0, "sw": 0}
        self.ndma = 0
        self.dma_toks = []
        self.pe_prev = (None, None, True)

    def sbuf(self, name, shape, dtype):
        return self.stack.enter_context(self.nc.sbuf_tensor(name, list(shape), dtype))

    def psum(self, name, shape, dtype):
        return self.stack.enter_context(self.nc.psum_tensor(name, list(shape), dtype))

    def buf(self, name=""):
        return Buf(name)

    def bufs(self, n):
        return [Buf() for _ in range(n)]

    def _force_signal(self, eng):
        lst = self.ops[eng]
        if not lst:
            return None
        last = lst[-1]
        if last.signal is None:
            n = self.nsig[eng]
            self.nsig[eng] += 1
            sem = self.esems[eng][n // SEM_LIMIT]
            val = n % SEM_LIMIT + 1
            last.signal = (sem, 1)
            self.sigpos[eng].append(last.pos)
            self.sigtok[eng].append((sem, val))
        return last

    def _resolve(self, tok):
        if tok[0] == "d":
            return tok[1], tok[2]
        _, eng, pos = tok
        sp = self.sigpos[eng]
        i = bisect.bisect_left(sp, pos)
        if i < len(sp):
            return self.sigtok[eng][i]
        last = self._force_signal(eng)
        assert last.pos >= pos
        i = bisect.bisect_left(sp, pos)
        return self.sigtok[eng][i]

    def _add_wait(self, op, tok):
        sem, val = self._resolve(tok)
        w = self.waited[op.eng]
        key = id(sem)
        if w.get(key, 0) >= val:
            return
        w[key] = val
        for i, (s, v) in enumerate(op.waits):
            if s is sem:
                op.waits[i] = (sem, max(v, val))
                return
        op.waits.append((sem, val))

    def _deps(self, op, reads, writes, is_dma):
        eng = op.eng
        for b in reads:
            if b.last_w is not None:
                self._add_wait(op, b.last_w)
        for b in writes:
            t = b.last_w
            if t is not None and not (t[0] == "c" and t[1] == eng == "pe" and not is_dma):
                self._add_wait(op, t)
            for e, t in b.creads.items():
                if e == eng == "pe" and not is_dma:
                    continue
                self._add_wait(op, t)
            for t in b.dreads:
                self._add_wait(op, t)

    def _compute_pos_check(self, eng):
        lst = self.ops[eng]
        if lst and lst[-1].signal is None and lst[-1].name not in ("nop", "dma_start"):
            self._force_signal(eng)

    def op(self, eng, name, reads=(), writes=(), **kw):
        lst = self.ops[eng]
        rset = frozenset(id(b) for b in reads)
        wset = frozenset(id(b) for b in writes)
        if lst and lst[-1].name not in ("nop", "dma_start") and lst[-1].signal is None:
            prev = lst[-1]
            if eng != "pe":
                self._force_signal(eng)
            else:
                pr, pw, pdone = self.pe_prev
                if pr != rset or (pw != wset and pdone):
                    self._force_signal(eng)
        if eng == "pe":
            self.pe_prev = (rset, wset, kw.get("stop", True))
        o = Op(eng, name, kw, len(lst))
        self._deps(o, reads, writes, False)
        lst.append(o)
        tok = ("c", eng, o.pos)
        for b in reads:
            b.creads[eng] = tok
        for b in writes:
            b.last_w = tok
            b.creads = {}
            b.dreads = []
        return o

    def dma(self, out, in_, reads=(), writes=(), q="sp", **kw):
        if q != "sp":
            self._compute_pos_check(q)
        lst = self.ops[q]
        o = Op(q, "dma_start", dict(out=out, in_=in_, **kw), len(lst))
        self._deps(o, reads, writes, True)
        kind = "sw" if q == "pool" else "hw"
        pool_ = self.dsems[kind]
        i = self.ndma_q[kind] % len(pool_)
        r = self.ndma_q[kind] // len(pool_)
        self.ndma_q[kind] += 1
        self.ndma += 1
        sem = pool_[i]
        if r > 0:
            self._add_wait(o, ("d", sem, 16 * r))
        o.signal = (sem, 16)
        lst.append(o)
        tok = ("d", sem, 16 * (r + 1))
        self.dma_toks.append(tok)
        for b in reads:
            b.dreads.append(tok)
        for b in writes:
            b.last_w = tok
            b.creads = {}
            b.dreads = []
        return tok

    def barrier(self):
        toks = list(self.dma_toks[-(N_DMA_SEMS + N_SWDMA_SEMS) * 2:])
        for e in ENGS:
            lst = self.ops[e]
            if lst and lst[-1].name not in ("dma_start", "nop"):
                last = self._force_signal(e)
                toks.append(("c", e, last.pos))
            else:
                for o in reversed(lst):
                    if o.name not in ("dma_start", "nop"):
                        assert o.signal is not None
                        toks.append(("c", e, o.pos))
                        break
        for e in ENGS:
            o = Op(e, "nop", {}, len(self.ops[e]))
            for t in toks:
                if t[0] == "c" and t[1] == e:
                    continue
                self._add_wait(o, t)
            self.ops[e].append(o)

    def final_wait(self, eng, toks):
        o = Op(eng, "nop", {}, len(self.ops[eng]))
        for t in toks:
            self._add_wait(o, t)
        self.ops[eng].append(o)

    def emit(self):
        nc = self.nc
        prog = self
        with nc.Block() as block:
            def run(engname):
                def body(e):
                    for o in prog.ops[engname]:
                        for (sem, val) in o.waits:
                            e.wait_ge(sem, val)
                        if o.name == "nop":
                            continue
                        ins = getattr(e, o.name)(**o.kw)
                        if o.signal is not None:
                            ins.then_inc(o.signal[0], o.signal[1])
                return body
            block.tensor(run("pe"))
            block.scalar(run("act"))
            block.vector(run("dve"))
            block.gpsimd(run("pool"))
            block.sync(run("sp"))

    def stats(self):
        return {e: (len(self.ops[e]), self.nsig[e]) for e in ENGS}, self.ndma


class Arena:
    def __init__(self, p, nbytes):
        self.t = p.sbuf("arena", [128, nbytes // 2], BF16)
        self.n = nbytes
        self.off = 0

    def reset(self):
        self.off = 0

    def alloc(self, shape, dtype):
        esz = 2 if dtype == BF16 else 4
        n = 1
        for s in shape:
            n *= s
        nb = n * esz
        a = self.t[:, self.off // 2:(self.off + nb) // 2]
        self.off += (nb + 63) // 64 * 64
        assert self.off <= self.n, f"arena overflow {self.off} > {self.n}"
        if dtype != BF16:
            a = a.bitcast(dtype)
        if len(shape) == 2:
            a = a.rearrange("p (a b) -> p a b", a=shape[0])
        elif len(shape) == 3:
            a = a.rearrange("p (a b c) -> p a b c", a=shape[0], b=shape[1])
        return a


class Ring:
    def __init__(self, items):
        self.items = items
        self.i = 0

    def next(self):
        it = self.items[self.i % len(self.items)]
        self.i += 1
        return it


NCF = 576
NCB = 768 + 9 * 128


def host_consts():
    cf = np.zeros((128, NCF), np.float32)
    ii = np.arange(128)
    cf[:, 0:128] = np.eye(128, dtype=np.float32)
    cf[:, 128:256] = (ii[:, None] <= ii[None, :]).astype(np.float32)
    cf[:, 256:384] = (ii[:, None] > ii[None, :]).astype(np.float32)
    cf[:, 384:512] = 1.0
    gm = np.zeros((128, 8, 8), np.float32)
    for qi in range(8, 16):
        own = qi // 2
        gm[:, qi - 8, own:] = -1e30
    cf[:, 512:576] = gm.reshape(128, 64)
    cb = np.zeros((128, NCB), np.float32)
    cb[:, 0:128] = np.eye(128, dtype=np.float32)
    cb[:, 128:256] = 1.0
    cb[:, 256:384] = (ii[None, :] > ii[:, None]).astype(np.float32) * BIG
    cb[:, 384:512] = (ii[None, :] >= ii[:, None]).astype(np.float32) * BIG
    rr = np.arange(256)
    cb[:, 512:768] = (rr[None, :] < ii[:, None]).astype(np.float32) * (-BIG * SQ128)
    e = np.zeros((9, 9, 128), np.float32)
    for v in range(9):
        e[8, v, :] = 1.0
        if v < 8:
            e[v, v, :] = 1.0
    cb[0:9, 768:768 + 9 * 128] = e.reshape(9, 9 * 128)
    return cf, cb


def rel_bucket_np(n):
    n = np.maximum(n, 0)
    max_exact = 16
    nf = np.maximum(n, 1).astype(np.float32)
    large = max_exact + (np.log(nf / max_exact) / math.log(128 / max_exact) * (32 - max_exact)).astype(np.int32)
    large = np.minimum(large, 31)
    return np.where(n < max_exact, n, large)


def toeplitz_index():
    kk = np.arange(128)[:, None]
    r = np.arange(256)[None, :]
    return rel_bucket_np(r - kk)


class K:
    pass


def build_program(debug_phase=None):
    nc = bass.Bass("TRN2", target_bir_lowering=False)
    k = K()
    k.nc = nc
    dbg = debug_phase is not None

    order = ["inproj", "gdn", "moba", "wout", "xattn", "ffn"]
    need_from = {"mem": "xattn", "w_out": "wout", "w_xq": "xattn", "w_xkv": "xattn", "w_xo": "xattn",
                 "w_gate": "ffn", "w_up": "ffn", "w_down": "ffn"}
    k.skip = set()
    if dbg:
        for nm, ph in need_from.items():
            if order.index(ph) > order.index(debug_phase):
                k.skip.add(nm)

    def din(name, shape, dt=F32):
        if name in k.skip:
            shape = [1, 1]
        return nc.dram_tensor(name, list(shape), dt, kind="ExternalInput").ap()

    DUMP = {"inproj": ("QA", "KA", "KT", "VA", "ZS", "QB", "KB", "VB", "DBG"), "moba": ("OT",), "gdn": ("OT",),
            "wout": ("X1",), "xattn": ("X2",), "ffn": ("OT", "X1", "X2")}.get(debug_phase, ())

    def dscr(name, shape, dt=BF16):
        kind = "ExternalOutput" if name in DUMP else "Internal"
        return nc.dram_tensor(name, list(shape), dt, kind=kind).ap()

    I = {}
    I["x"] = din("x", [S, D])
    I["mem"] = din("mem", [MEM, D])
    for nm in ("mix_norm_g", "xattn_norm_g", "mem_norm_g", "ffn_norm_g", "final_norm_g"):
        I[nm] = din(nm, [1, D])
    I["w_in"] = din("w_in", [D, INW])
    I["gdn_conv_w"] = din("gdn_conv_w", [4, 3072])
    I["gdn_a_log"] = din("gdn_a_log", [1, 8])
    I["gdn_dt_bias"] = din("gdn_dt_bias", [1, 8])
    I["gdn_norm_g"] = din("gdn_norm_g", [1, 128])
    I["moba_norm_g"] = din("moba_norm_g", [1, 128])
    I["rel_bias"] = din("rel_bias", [1, 256])
    I["rb_toep"] = din("rb_toep", [128, 8, 256])
    I["w_out"] = din("w_out", [D, D])
    I["w_xq"] = din("w_xq", [D, 512])
    I["w_xkv"] = din("w_xkv", [D, 1024])
    I["w_xo"] = din("w_xo", [512, D])
    I["w_gate"] = din("w_gate", [D, DFF])
    I["w_up"] = din("w_up", [D, DFF])
    I["ffn_conv_w"] = din("ffn_conv_w", [3, DFF])
    I["ffn_conv_b"] = din("ffn_conv_b", [1, DFF])
    I["w_down"] = din("w_down", [DFF, D])
    I["cst_f"] = din("cst_f", [128, NCF])
    I["cst_b"] = din("cst_b", [128, NCB])
    out = nc.dram_tensor("out", [S, D], F32, kind="ExternalOutput").ap()
    k.I = I
    k.out = out

    k.QA = dscr("QA", [128, 16, 8, 128])
    k.KA = dscr("KA", [128, 16, 8, 128])
    k.KT = dscr("KT", [128, 16, 8, 128])
    k.VA = dscr("VA", [128, 16, 8, 128])
    k.ZS = dscr("ZS", [128, 16, 8, 128])
    k.QB = dscr("QB", [8, 128, 2048])
    k.KB = dscr("KB", [8, 128, 2048])
    k.VB = dscr("VB", [8, 128, 16, 128])
    k.X1 = dscr("X1", [S, D], F32)
    k.X2 = dscr("X2", [S, D], F32)
    if "DBG" in DUMP:
        k.DBG = dscr("DBG", [128, 4096], F32)
    if "OT" in DUMP:
        k.OT = dscr("OT", [128, 16, S], BF16)
    k.b_scr = {nm: Buf(nm) for nm in ("QA", "KA", "KT", "VA", "ZS", "QB", "KB", "VB", "X1", "X2", "DBG")}

    with ExitStack() as st:
        p = Prog(nc, st)
        k.p = p
        k.cf = p.sbuf("cf", [128, NCF], F32)
        k.cb = p.sbuf("cb", [128, NCB], BF16)
        k.b_c = p.buf("consts")
        p.dma(k.cf[:], I["cst_f"], writes=[k.b_c])
        p.dma(k.cb[:], I["cst_b"], writes=[k.b_c], q="pool")
        k.identf = k.cf[:, 0:128]
        k.trif = k.cf[:, 128:256]
        k.suf = k.cf[:, 256:384]
        k.onesf = k.cf[:, 384:512]
        k.gm = k.cf[:, 512:576]
        k.ident = k.cb[:, 0:128]
        k.ones = k.cb[:, 128:256]
        k.umq = k.cb[:, 256:384]
        k.uma = k.cb[:, 384:512]
        k.cm = k.cb[:, 512:768]
        k.emat = k.cb[0:9, 768:768 + 9 * 128].rearrange("p (v n) -> p v n", v=9)
        k.BA = p.sbuf("BA", [128, 16, 16], F32)
        k.b_BA = p.buf("BA")
        k.BETA = p.sbuf("BETA", [128, 16, 8], F32)
        k.GG = p.sbuf("GG", [128, 16, 8], F32)
        k.b_bg = p.buf("betag")
        banks = [(p.psum(f"ps{i}", [128, 512], F32), p.buf(f"ps{i}")) for i in range(8)]
        k.ps = Ring(banks[0:6])
        k.psx = banks[6:8]
        k.banks = banks
        k.ar = Arena(p, 176 * 1024)

        phases = [phase_inproj, phase_gdn, phase_moba, phase_wout, phase_xattn, phase_ffn]
        names = ["inproj", "gdn", "moba", "wout", "xattn", "ffn"]
        out_toks = []
        for fn, nm in zip(phases, names):
            r = fn(k)
            if r:
                out_toks += r
            p.barrier()
            if debug_phase == nm:
                break
        p.final_wait("sp", p.dma_toks[-(N_DMA_SEMS + N_SWDMA_SEMS) * 2:])
        k.stats = p.stats()
        p.emit()
    return nc, k


def MM(k, out, lhsT, rhs, start, stop, r, w):
    k.p.op("pe", "matmul", reads=r, writes=w, out=out, lhsT=lhsT, rhs=rhs, start=start, stop=stop)


def TR(k, out, in_, ident, r, w):
    k.p.op("pe", "transpose", reads=r + [k.b_c], writes=w, out=out, in_=in_, identity=ident)


def ACT(k, out, in_, func, r, w, **kw):
    k.p.op("act", "activation", reads=r, writes=w, out=out, in_=in_, func=func, **kw)


def CP(k, eng, out, in_, r, w):
    if eng == "act":
        k.p.op("act", "copy", reads=r, writes=w, out=out, in_=in_)
    else:
        k.p.op(eng, "tensor_copy", reads=r, writes=w, out=out, in_=in_)


def TT(k, eng, out, in0, in1, op, r, w):
    k.p.op(eng, "tensor_tensor", reads=r, writes=w, out=out, in0=in0, in1=in1, op=op)


def TS(k, eng, out, in0, s1, s2, op0, op1, r, w):
    if s2 is None:
        k.p.op(eng, "tensor_scalar", reads=r, writes=w, out=out, in0=in0, scalar1=s1, scalar2=None, op0=op0)
    else:
        k.p.op(eng, "tensor_scalar", reads=r, writes=w, out=out, in0=in0, scalar1=s1, scalar2=s2, op0=op0, op1=op1)


def STT(k, eng, out, in0, scalar, in1, op0, op1, r, w):
    k.p.op(eng, "scalar_tensor_tensor", reads=r, writes=w, out=out, in0=in0, scalar=scalar, in1=in1,
           op0=op0, op1=op1)


def norm_T(k, tiles, gain, dstT, b_dst, col0=0):
    p = k.p
    ar = k.ar
    gb = ar.alloc((D,), F32)
    b_gb = p.buf()
    p.dma(gb, gain.partition_broadcast(128), writes=[b_gb])
    xt = [ar.alloc((D,), F32) for _ in range(2)]
    b_xt = p.bufs(2)
    junk = ar.alloc((D,), BF16)
    b_junk = p.buf()
    hb = [ar.alloc((D,), BF16) for _ in range(2)]
    b_hb = p.bufs(2)
    ss = ar.alloc((len(tiles),), F32)
    b_ss = p.buf()
    for t, (src, rb) in enumerate(tiles):
        i = t % 2
        p.dma(xt[i], src, reads=rb, writes=[b_xt[i]])
        ACT(k, junk, xt[i], AF.Square, [b_xt[i]], [b_junk, b_ss], accum_out=ss[:, t:t + 1])
        ACT(k, ss[:, t:t + 1], ss[:, t:t + 1], AF.Sqrt, [b_ss], [b_ss], scale=1.0 / D, bias=EPS)
        p.op("dve", "reciprocal", reads=[b_ss], writes=[b_ss], out=ss[:, t:t + 1], in_=ss[:, t:t + 1])
        STT(k, "dve", hb[i], xt[i], ss[:, t:t + 1], gb, ALU.mult, ALU.mult, [b_xt[i], b_ss, b_gb], [b_hb[i]])
        for k4 in range(4):
            pb, bpb = k.ps.next()
            pbv = pb[:].bitcast(BF16)
            for j in range(4):
                kt = k4 * 4 + j
                TR(k, pbv[:, j * 128:(j + 1) * 128], hb[i][:, kt * 128:(kt + 1) * 128], k.ident, [b_hb[i]], [bpb])
            CP(k, "act" if k4 % 2 else "dve",
               dstT[:, k4 * 4:(k4 + 1) * 4, col0 + t * 128:col0 + (t + 1) * 128],
               pbv[:, 0:512].rearrange("p (a b) -> p a b", a=4), [bpb], [b_dst])


def load_w_block(k, ring, src):
    ap, b = ring.next()
    k.p.dma(ap, src, writes=[b], q="pool")
    return ap, b


def phase_inproj(k):
    p = k.p
    ar = k.ar
    I = k.I
    ar.reset()
    hT = ar.alloc((16, S), BF16)
    b_hT = p.buf("hT")
    k.hT = hT
    mark = ar.off
    tiles = [(I["x"][t * 128:(t + 1) * 128, :], []) for t in range(NT)]
    norm_T(k, tiles, I["mix_norm_g"], hT, b_hT)
    p.barrier()
    ar.off = mark
    wring = Ring([(ar.alloc((16, 512), BF16), p.buf()) for _ in range(2)])
    wba = ar.alloc((16, 16), BF16)
    b_wba = p.buf()
    raw = [ar.alloc((S + 4,), F32) for _ in range(2)]
    b_raw = p.bufs(2)
    y = ar.alloc((S,), F32)
    b_y = p.buf()
    y1 = ar.alloc((S,), F32)
    b_y1 = p.buf()
    sq = ar.alloc((S,), BF16)
    b_sq = p.buf()
    rs = ar.alloc((S,), F32)
    b_rs = p.buf()
    obf = [ar.alloc((S,), BF16) for _ in range(2)]
    b_obf = p.bufs(2)
    tst = [ar.alloc((16, 128), BF16) for _ in range(2)]
    b_tst = p.bufs(2)
    zst = [ar.alloc((512,), BF16) for _ in range(3)]
    b_zst = p.bufs(3)
    cw = ar.alloc((96,), F32)
    cwl = ar.alloc((128,), F32)
    b_cw = p.buf()
    small = ar.alloc((64,), F32)
    b_small = p.buf()
    p.dma(cwl[0:96, :], I["gdn_conv_w"].rearrange("j (b q) -> (j b) q", q=128), writes=[b_cw])
    pb, bpb = k.ps.next()
    TR(k, pb[:, 0:96], cwl[0:96, :], k.identf[0:96, 0:96], [b_cw], [bpb])
    CP(k, "dve", cw, pb[:, 0:96], [bpb], [b_cw])
    for i in range(2):
        p.op("pool", "memset", writes=[b_raw[i]], ap=raw[i][:, 0:4], constant=0.0)

    wsrc = I["w_in"].rearrange("(kt q) n -> q kt n", q=128)
    ri = 0
    oi = 0
    ti = 0

    def fblock(wap, wb, f, evac):
        for c in range(4):
            pb, bpb = k.ps.next()
            for kt in range(16):
                MM(k, pb[:], wap[:, kt, f * 128:(f + 1) * 128], hT[:, kt, c * 512:(c + 1) * 512],
                   kt == 0, kt == 15, [wb, b_hT], [bpb])
            evac(pb, bpb, c * 512)

    for blk in range(6):
        wap, wb = load_w_block(k, wring, wsrc[:, :, blk * 512:(blk + 1) * 512])
        for f in range(4):
            fb = blk * 4 + f
            kind = fb // 8
            hd = fb % 8
            r_ap, r_b = raw[ri % 2], b_raw[ri % 2]
            ri += 1

            def evac_raw(pb, bpb, tok0, r_ap=r_ap, r_b=r_b):
                CP(k, "act", r_ap[:, 3 + tok0:3 + tok0 + 512], pb[:], [bpb], [r_b])
            fblock(wap, wb, f, evac_raw)
            TS(k, "dve", y, r_ap[:, 0:S], cw[:, 0 * 24 + fb:0 * 24 + fb + 1], None, ALU.mult, None, [r_b, b_cw], [b_y])
            for j in range(1, 4):
                STT(k, "dve", y, r_ap[:, j:j + S], cw[:, j * 24 + fb:j * 24 + fb + 1], y, ALU.mult, ALU.add,
                    [r_b, b_cw, b_y], [b_y])
            o_ap, o_b = obf[oi % 2], b_obf[oi % 2]
            oi += 1
            if kind == 2:
                ACT(k, o_ap, y, AF.Silu, [b_y], [o_b])
            else:
                ACT(k, y, y, AF.Silu, [b_y], [b_y])
                ACT(k, sq, y, AF.Square, [b_y], [b_sq])
                for c in range(4):
                    pb, bpb = k.ps.next()
                    MM(k, pb[:], k.ones, sq[:, c * 512:(c + 1) * 512], True, True, [b_sq, k.b_c], [bpb])
                    if kind == 0:
                        ACT(k, rs[:, c * 512:(c + 1) * 512], pb[:], AF.Sqrt, [bpb], [b_rs], scale=128.0,
                            bias=128.0 * EPS)
                    else:
                        ACT(k, rs[:, c * 512:(c + 1) * 512], pb[:], AF.Sqrt, [bpb], [b_rs], scale=1.0, bias=EPS)
                p.op("dve", "reciprocal", reads=[b_rs], writes=[b_rs], out=rs, in_=rs)
                TT(k, "dve", o_ap, y, rs, ALU.mult, [b_y, b_rs], [o_b])
            if kind == 0:
                p.dma(k.QA[:, :, hd, :], o_ap.rearrange("p (c t) -> p c t", c=16), reads=[o_b],
                      writes=[k.b_scr["QA"]])
            if kind == 1:
                p.dma(k.KA[:, :, hd, :], o_ap.rearrange("p (c t) -> p c t", c=16), reads=[o_b],
                      writes=[k.b_scr["KA"]])
            if kind >= 1:
                t_ap, t_b = tst[ti % 2], b_tst[ti % 2]
                ti += 1
                for c4 in range(4):
                    pb, bpb = k.ps.next()
                    pbv = pb[:].bitcast(BF16)
                    for j in range(4):
                        c = c4 * 4 + j
                        TR(k, pbv[:, j * 128:(j + 1) * 128], o_ap[:, c * 128:(c + 1) * 128], k.ident, [o_b], [bpb])
                    CP(k, "act" if c4 % 2 else "dve", t_ap[:, c4 * 4:(c4 + 1) * 4, :],
                       pbv[:, 0:512].rearrange("p (a b) -> p a b", a=4), [bpb], [t_b])
                dst = k.KT if kind == 1 else k.VA
                p.dma(dst[:, :, hd, :], t_ap, reads=[t_b], writes=[k.b_scr["KT" if kind == 1 else "VA"]])

    zi = 0
    for blk in range(2):
        wap, wb = load_w_block(k, wring, wsrc[:, :, 3072 + blk * 512:3072 + (blk + 1) * 512])
        for t in range(NT):
            pb, bpb = k.ps.next()
            for kt in range(16):
                MM(k, pb[:], hT[:, kt, t * 128:(t + 1) * 128], wap[:, kt, :], kt == 0, kt == 15, [wb, b_hT], [bpb])
            z_ap, z_b = zst[zi % 3], b_zst[zi % 3]
            zi += 1
            ACT(k, z_ap, pb[:], AF.Silu, [bpb], [z_b])
            p.dma(k.ZS[:, t, blk * 4:(blk + 1) * 4, :], z_ap.rearrange("p (h d) -> p h d", h=4), reads=[z_b],
                  writes=[k.b_scr["ZS"]])
    p.dma(wba, wsrc[:, :, 4096:4112], writes=[b_wba], q="pool")
    for t in range(NT):
        pb, bpb = k.ps.next()
        for kt in range(16):
            MM(k, pb[:, 0:16], hT[:, kt, t * 128:(t + 1) * 128], wba[:, kt, :], kt == 0, kt == 15, [b_wba, b_hT], [bpb])
        CP(k, "dve", k.BA[:, t, :], pb[:, 0:16], [bpb], [k.b_BA])
    albc = small[:, 0:8]
    dtbc = small[:, 8:16]
    nea = small[:, 16:24]
    p.dma(albc, I["gdn_a_log"].partition_broadcast(128), writes=[b_small])
    p.dma(dtbc, I["gdn_dt_bias"].partition_broadcast(128), writes=[b_small])
    ACT(k, nea, albc, AF.Exp, [b_small], [b_small])
    TS(k, "dve", nea, nea, -1.0, None, ALU.mult, ALU.bypass, [b_small], [b_small])
    ACT(k, k.BETA[:], k.BA[:, :, 0:8], AF.Sigmoid, [k.b_BA], [k.b_bg])
    xa = ar.alloc((16, 8), F32)
    xb = ar.alloc((16, 8), F32)
    b_xa = p.buf()
    TT(k, "dve", xa, k.BA[:, :, 8:16], dtbc.unsqueeze(1).broadcast_to([128, 16, 8]), ALU.add, [k.b_BA, b_small], [b_xa])
    ACT(k, xb, xa, AF.Abs, [b_xa], [b_xa])
    ACT(k, xb, xb, AF.Exp, [b_xa], [b_xa], scale=-1.0)
    ACT(k, xb, xb, AF.Ln, [b_xa], [b_xa], bias=1.0)
    TS(k, "dve", xa, xa, 0.0, None, ALU.max, ALU.bypass, [b_xa], [b_xa])
    TT(k, "dve", xa, xa, xb, ALU.add, [b_xa], [b_xa])
    TT(k, "dve", k.GG[:], xa, nea.unsqueeze(1).broadcast_to([128, 16, 8]), ALU.mult, [b_xa, b_small], [k.b_bg])

    for blk in range(4):
        wap, wb = load_w_block(k, wring, wsrc[:, :, 4112 + blk * 512:4112 + (blk + 1) * 512])
        for f in range(4):
            fb = blk * 4 + f
            kind = fb // 8
            hd = fb % 8
            o_ap, o_b = obf[oi % 2], b_obf[oi % 2]
            oi += 1

            def evac_o(pb, bpb, tok0, o_ap=o_ap, o_b=o_b, c=[0]):
                CP(k, "act" if (tok0 // 512) % 2 else "dve", o_ap[:, tok0:tok0 + 512], pb[:], [bpb], [o_b])
            fblock(wap, wb, f, evac_o)
            dst = k.QB if kind == 0 else k.KB
            p.dma(dst[hd], o_ap, reads=[o_b], writes=[k.b_scr["QB" if kind == 0 else "KB"]])
    for blk in range(2):
        wap, wb = load_w_block(k, wring, wsrc[:, :, 6160 + blk * 512:6160 + (blk + 1) * 512])
        for t in range(NT):
            pb, bpb = k.ps.next()
            for kt in range(16):
                MM(k, pb[:], hT[:, kt, t * 128:(t + 1) * 128], wap[:, kt, :], kt == 0, kt == 15, [wb, b_hT], [bpb])
            z_ap, z_b = zst[zi % 3], b_zst[zi % 3]
            zi += 1
            CP(k, "act" if t % 2 else "dve", z_ap, pb[:], [bpb], [z_b])
            p.dma(k.VB[blk * 4:(blk + 1) * 4, :, t, :].rearrange("h p d -> p h d"),
                  z_ap.rearrange("p (h d) -> p h d", h=4), reads=[z_b], writes=[k.b_scr["VB"]])
    if hasattr(k, "DBG"):
        p.dma(k.DBG[:, 0:128], k.BETA[:].rearrange("p a b -> p (a b)"), reads=[k.b_bg], writes=[k.b_scr["DBG"]])
        p.dma(k.DBG[:, 128:256], k.GG[:].rearrange("p a b -> p (a b)"), reads=[k.b_bg], writes=[k.b_scr["DBG"]])


def phase_gdn(k):
    p = k.p
    ar = k.ar
    I = k.I
    ar.reset()
    oT = ar.alloc((16, S), BF16)
    k.oT = oT
    k.b_oT = p.buf("oT")
    k.oT_mark = ar.off
    S32 = ar.alloc((8, 128), F32)
    S16 = ar.alloc((8, 128), BF16)
    b_S32 = p.bufs(2)
    b_S16 = p.bufs(2)
    p.op("pool", "memset", writes=b_S32, ap=S32, constant=0.0)
    p.op("pool", "memset", writes=b_S16, ap=S16, constant=0.0)
    gnb = ar.alloc((128,), F32)
    b_gnb = p.buf()
    p.dma(gnb, I["gdn_norm_g"].partition_broadcast(128), writes=[b_gnb])
    names = ("QA", "KA", "KT", "VA", "ZS")
    inr = Ring([([ar.alloc((8, 128), BF16) for _ in names], p.bufs(5)) for _ in range(2)])
    sm = Ring([(ar.alloc((128,), F32), p.buf()) for _ in range(2)])
    kbg_, vb_, kdec_, zg_ = [ar.alloc((8, 128), BF16) for _ in range(4)]
    b_kbg, b_vb, b_kdec, b_zg = p.bufs(4)

    class G:
        pass
    grp = []
    for g in range(2):
        G_ = G()
        G_.diagG = ar.alloc((4, 128), F32)
        G_.decQ = ar.alloc((4, 128), BF16)
        G_.decA = ar.alloc((4, 128), BF16)
        G_.A = [ar.alloc((4, 128), BF16) for _ in range(2)]
        G_.B = [ar.alloc((4, 128), BF16) for _ in range(2)]
        G_.P = [ar.alloc((4, 128), BF16) for _ in range(2)]
        G_.qk = ar.alloc((4, 128), BF16)
        G_.qkT = ar.alloc((4, 128), BF16)
        G_.wT = ar.alloc((4, 128), BF16)
        G_.u = ar.alloc((4, 128), F32)
        G_.vnew = ar.alloc((4, 128), BF16)
        G_.o1 = ar.alloc((4, 128), F32)
        G_.o = ar.alloc((4, 128), F32)
        G_.sq = ar.alloc((4, 128), F32)
        G_.fin = ar.alloc((4, 128), BF16)
        G_.ss = ar.alloc((4,), F32)
        for nm in ("diagG", "decQ", "decA", "qk", "qkT", "wT", "u", "vnew", "o1", "o", "sq", "fin", "ss"):
            setattr(G_, "b_" + nm, p.buf())
        G_.b_A = p.bufs(2)
        G_.b_B = p.bufs(2)
        G_.b_P = p.bufs(2)
        grp.append(G_)

    def v512(ap):
        return ap.rearrange("p a b -> p (a b)")

    for c in range(16):
        (qT, kT, ktok, vtok, zs), bi = inr.next()
        b_qT, b_kT, b_ktok, b_vtok, b_zs = bi
        for ap, b, nm in zip((qT, kT, ktok, vtok, zs), bi, names):
            p.dma(ap, getattr(k, nm)[:, c, :, :], reads=[k.b_scr[nm]], writes=[b])
        smt, b_sm = sm.next()
        gc = smt[:, 0:8]
        e24 = smt[:, 8:32]
        eg = smt[:, 8:16]
        egrev = smt[:, 16:24]
        gl = smt[:, 24:32]
        bexp = smt[:, 32:40]
        gcl = smt[:, 40:48]
        pbA, bpA = k.ps.next()
        gsrc = k.GG[:, c, :]
        MM(k, pbA[:, 0:8], k.trif, gsrc, True, True, [k.b_c, k.b_bg], [bpA])
        MM(k, pbA[:, 8:16], k.suf, gsrc, True, True, [k.b_c, k.b_bg], [bpA])
        MM(k, pbA[:, 16:24], k.onesf, gsrc, True, True, [k.b_c, k.b_bg], [bpA])
        CP(k, "dve", gc, pbA[:, 0:8], [bpA], [b_sm])
        ACT(k, e24, pbA[:, 0:24], AF.Exp, [bpA], [b_sm])
        TT(k, "dve", bexp, k.BETA[:, c, :], eg, ALU.mult, [k.b_bg, b_sm], [b_sm])
        ACT(k, gcl, k.BETA[:, c, :], AF.Ln, [k.b_bg], [b_sm])
        TT(k, "dve", gcl, gcl, gc, ALU.add, [b_sm], [b_sm])
        bc = lambda a: a.unsqueeze(2).broadcast_to([128, 8, 128])
        TT(k, "pool", kbg_, ktok, bc(bexp), ALU.mult, [b_ktok, b_sm], [b_kbg])
        TT(k, "pool", vb_, vtok, bc(k.BETA[:, c, :]), ALU.mult, [b_vtok, k.b_bg], [b_vb])
        TT(k, "pool", kdec_, ktok, bc(egrev), ALU.mult, [b_ktok, b_sm], [b_kdec])
        TT(k, "pool", zg_, zs, gnb.unsqueeze(1).broadcast_to([128, 8, 128]), ALU.mult, [b_zs, b_gnb], [b_zg])
        bc4 = lambda a: a.unsqueeze(2).broadcast_to([128, 4, 128])
        for g, G_ in enumerate(grp):
            hs = slice(4 * g, 4 * g + 4)
            TT(k, "dve", G_.diagG, k.identf.unsqueeze(1).broadcast_to([128, 4, 128]), bc4(gc[:, hs]), ALU.mult,
               [k.b_c, b_sm], [G_.b_diagG])
        for g, G_ in enumerate(grp):
            for which, um, dec, b_dec, bias in ((0, k.umq, G_.decQ, G_.b_decQ, gc), (1, k.uma, G_.decA, G_.b_decA, gcl)):
                pR, bpR = k.ps.next()
                for hh in range(4):
                    MM(k, pR[:, hh * 128:(hh + 1) * 128], k.onesf, G_.diagG[:, hh, :], True, False,
                       [k.b_c, G_.b_diagG], [bpR])
                    MM(k, pR[:, hh * 128:(hh + 1) * 128], k.ident, um, False, True, [k.b_c], [bpR])
                for hh in range(4):
                    h = 4 * g + hh
                    ACT(k, dec[:, hh, :], pR[:, hh * 128:(hh + 1) * 128], AF.Exp, [bpR, b_sm], [b_dec],
                        scale=-1.0, bias=bias[:, h:h + 1])
        for g, G_ in enumerate(grp):
            pKK, bKK = k.ps.next()
            pQK, bQK = k.ps.next()
            for hh in range(4):
                h = 4 * g + hh
                MM(k, pKK[:, hh * 128:(hh + 1) * 128], kT[:, h, :], kT[:, h, :], True, True, [b_kT], [bKK])
                MM(k, pQK[:, hh * 128:(hh + 1) * 128], qT[:, h, :], kT[:, h, :], True, True, [b_qT, b_kT], [bQK])
            TT(k, "dve", v512(G_.A[0]), pKK[:], v512(G_.decA), ALU.mult, [bKK, G_.b_decA], [G_.b_A[0]])
            TT(k, "dve", v512(G_.qk), pQK[:], v512(G_.decQ), ALU.mult, [bQK, G_.b_decQ], [G_.b_qk])
        for g, G_ in enumerate(grp):
            for src, bsrc, dst, bdst, eng in ((G_.A[0], G_.b_A[0], G_.B[0], G_.b_B[0], "act"),
                                              (G_.qk, G_.b_qk, G_.qkT, G_.b_qkT, "dve")):
                pT, bpT = k.ps.next()
                pTv = pT[:].bitcast(BF16)
                for hh in range(4):
                    TR(k, pTv[:, hh * 128:(hh + 1) * 128], src[:, hh, :], k.ident, [bsrc], [bpT])
                CP(k, eng, v512(dst), pTv[:, 0:512], [bpT], [bdst])
            TT(k, "pool", G_.P[0], k.ident.unsqueeze(1).broadcast_to([128, 4, 128]), G_.B[0], ALU.subtract,
               [k.b_c, G_.b_B[0]], [G_.b_P[0]])
        for lvl in range(1, 7):
            cur = (lvl - 1) % 2
            nxt = lvl % 2
            for g, G_ in enumerate(grp):
                pA, bA = k.ps.next()
                for hh in range(4):
                    MM(k, pA[:, hh * 128:(hh + 1) * 128], G_.B[cur][:, hh, :], G_.A[cur][:, hh, :], True, True,
                       [G_.b_A[cur], G_.b_B[cur]], [bA])
                if lvl <= 5:
                    pB, bB = k.ps.next()
                    for hh in range(4):
                        MM(k, pB[:, hh * 128:(hh + 1) * 128], G_.A[cur][:, hh, :], G_.B[cur][:, hh, :], True, True,
                           [G_.b_A[cur], G_.b_B[cur]], [bB])
                CP(k, "act", v512(G_.A[nxt]), pA[:], [bA], [G_.b_A[nxt]])
                if lvl <= 5:
                    CP(k, "dve", v512(G_.B[nxt]), pB[:], [bB], [G_.b_B[nxt]])
            for g, G_ in enumerate(grp):
                pP, bP = k.ps.next()
                for hh in range(4):
                    MM(k, pP[:, hh * 128:(hh + 1) * 128], k.ident, G_.P[cur][:, hh, :], True, False,
                       [k.b_c, G_.b_P[cur]], [bP])
                    MM(k, pP[:, hh * 128:(hh + 1) * 128], G_.A[nxt][:, hh, :], G_.P[cur][:, hh, :], False, True,
                       [G_.b_A[nxt], G_.b_P[cur]], [bP])
                CP(k, "dve" if g else "act", v512(G_.P[nxt]), pP[:], [bP], [G_.b_P[nxt]])
        for g, G_ in enumerate(grp):
            TTm = G_.P[0]
            b_TT = G_.b_P[0]
            pW, bW = k.ps.next()
            pU, bU = k.ps.next()
            for hh in range(4):
                h = 4 * g + hh
                MM(k, pW[:, hh * 128:(hh + 1) * 128], kbg_[:, h, :], TTm[:, hh, :], True, True, [b_kbg, b_TT], [bW])
                MM(k, pU[:, hh * 128:(hh + 1) * 128], TTm[:, hh, :], vb_[:, h, :], True, True, [b_vb, b_TT], [bU])
            CP(k, "act", v512(G_.wT), pW[:], [bW], [G_.b_wT])
            CP(k, "dve", v512(G_.u), pU[:], [bU], [G_.b_u])
        for g, G_ in enumerate(grp):
            pVN, bVN = k.ps.next()
            pO1, bO1 = k.ps.next()
            for hh in range(4):
                h = 4 * g + hh
                MM(k, pVN[:, hh * 128:(hh + 1) * 128], G_.wT[:, hh, :], S16[:, h, :], True, True,
                   [G_.b_wT, b_S16[g]], [bVN])
                MM(k, pO1[:, hh * 128:(hh + 1) * 128], qT[:, h, :], S16[:, h, :], True, True, [b_qT, b_S16[g]], [bO1])
            TT(k, "dve", v512(G_.vnew), v512(G_.u), pVN[:], ALU.subtract, [G_.b_u, bVN], [G_.b_vnew])
            TT(k, "dve", G_.o1, pO1[:].rearrange("p (a b) -> p a b", a=4), bc4(eg[:, 4 * g:4 * g + 4]), ALU.mult,
               [bO1, b_sm], [G_.b_o1])
            TT(k, "pool", S32[:, 4 * g:4 * g + 4, :], S32[:, 4 * g:4 * g + 4, :], bc4(gl[:, 4 * g:4 * g + 4]), ALU.mult,
               [b_sm, b_S32[g]], [b_S32[g]])
        for g, G_ in enumerate(grp):
            pO2, bO2 = k.ps.next()
            pSU, bSU = k.ps.next()
            for hh in range(4):
                h = 4 * g + hh
                MM(k, pO2[:, hh * 128:(hh + 1) * 128], G_.qkT[:, hh, :], G_.vnew[:, hh, :], True, True,
                   [G_.b_qkT, G_.b_vnew], [bO2])
                MM(k, pSU[:, hh * 128:(hh + 1) * 128], kdec_[:, h, :], G_.vnew[:, hh, :], True, True,
                   [b_kdec, G_.b_vnew], [bSU])
            TT(k, "dve", v512(S32[:, 4 * g:4 * g + 4, :]), v512(S32[:, 4 * g:4 * g + 4, :]), pSU[:], ALU.add,
               [b_S32[g], bSU], [b_S32[g]])
            CP(k, "act", S16[:, 4 * g:4 * g + 4, :], S32[:, 4 * g:4 * g + 4, :], [b_S32[g]], [b_S16[g]])
            TT(k, "dve", v512(G_.o), pO2[:], v512(G_.o1), ALU.add, [bO2, G_.b_o1], [G_.b_o])
        for g, G_ in enumerate(grp):
            TT(k, "pool", G_.sq, G_.o, G_.o, ALU.mult, [G_.b_o], [G_.b_sq])
            p.op("dve", "tensor_reduce", reads=[G_.b_sq], writes=[G_.b_ss], out=G_.ss, in_=G_.sq, axis=AX.X, op=ALU.add)
            ACT(k, G_.ss, G_.ss, AF.Sqrt, [G_.b_ss], [G_.b_ss], scale=1.0 / 128, bias=EPS)
            p.op("dve", "reciprocal", reads=[G_.b_ss], writes=[G_.b_ss], out=G_.ss, in_=G_.ss)
            TT(k, "dve", G_.sq, G_.o, bc4(G_.ss), ALU.mult, [G_.b_o, G_.b_ss, G_.b_sq], [G_.b_sq])
            TT(k, "pool", G_.fin, G_.sq, zg_[:, 4 * g:4 * g + 4, :], ALU.mult, [G_.b_sq, b_zg], [G_.b_fin])
            pT, bpT = k.ps.next()
            pTv = pT[:].bitcast(BF16)
            for hh in range(4):
                TR(k, pTv[:, hh * 128:(hh + 1) * 128], G_.fin[:, hh, :], k.ident, [G_.b_fin], [bpT])
            CP(k, "act", oT[:, 4 * g:4 * g + 4, c * 128:(c + 1) * 128],
               pTv[:, 0:512].rearrange("p (a b) -> p a b", a=4), [bpT], [k.b_oT])


def phase_moba(k):
    p = k.p
    ar = k.ar
    I = k.I
    ar.off = k.oT_mark
    oT = k.oT
    hr = Ring([([ar.alloc((S,), BF16), ar.alloc((S,), BF16), ar.alloc((16, 132), BF16)], p.bufs(3)) for _ in range(2)])
    for (aps, bs) in hr.items:
        p.op("pool", "memset", writes=[bs[2]], ap=aps[2][:, :, 128:132], constant=1.0)
    tg = ar.alloc((8, 256), F32)
    Tp = ar.alloc((8, 256), BF16)
    rbb = ar.alloc((256,), F32)
    bmax = ar.alloc((8,), F32)
    mgb = ar.alloc((128,), F32)
    b_tp = p.buf()
    p.dma(tg, I["rb_toep"], writes=[b_tp])
    p.dma(rbb, I["rel_bias"].partition_broadcast(128), writes=[b_tp])
    p.dma(mgb, I["moba_norm_g"].partition_broadcast(128), writes=[b_tp])
    rb31 = rbb[:, 31 * 8:32 * 8]
    TT(k, "dve", tg, tg, rb31.unsqueeze(2).broadcast_to([128, 8, 256]), ALU.subtract, [b_tp], [b_tp])
    STT(k, "dve", Tp, tg, SQ128, k.cm.unsqueeze(1).broadcast_to([128, 8, 256]), ALU.mult, ALU.add, [b_tp, k.b_c], [b_tp])
    p.op("dve", "tensor_reduce", reads=[b_tp], writes=[b_tp], out=bmax, in_=rbb.rearrange("p (b h) -> p h b", h=8),
         axis=AX.X, op=ALU.max)
    TT(k, "dve", bmax, bmax, rb31, ALU.subtract, [b_tp], [b_tp])
    sq = ar.alloc((S,), BF16)
    ksq = ar.alloc((S,), BF16)
    b_sq, b_ksq = p.bufs(2)
    sm = ar.alloc((256,), F32)
    b_sm = p.buf()
    kms = sm[:, 0:8]
    km4 = sm[:, 8:12]
    ksc = sm[:, 12:13]
    nm = sm[:, 16:32]
    gsb = sm[:, 32:96].rearrange("p (a b) -> p a b", a=8)
    g2 = sm[:, 96:160].rearrange("p (a b) -> p a b", a=8)
    eq = sm[:, 160:224].rearrange("p (a b) -> p a b", a=8)
    mx = sm[:, 224:232]
    kmT = ar.alloc((8,), BF16)
    MBq = ar.alloc((16, 16), BF16)
    b_MBq = p.buf()
    p.op("pool", "memset", writes=[b_MBq], ap=MBq, constant=0.0)
    MB = ar.alloc((S,), BF16)
    b_MB = p.buf()
    PTr = Ring([(ar.alloc((256,), BF16), p.buf()) for _ in range(5)])
    yq = [ar.alloc((128,), F32) for _ in range(2)]
    b_yq = p.bufs(2)
    junk = ar.alloc((128,), BF16)
    b_junk = p.buf()
    fin = [ar.alloc((256,), BF16) for _ in range(2)]
    b_fin = p.bufs(2)
    sm2 = Ring([(ar.alloc((8,), F32), p.buf()) for _ in range(4)])
    bc8 = lambda a: a.unsqueeze(2).broadcast_to([128, 8, 8])

    for h in range(8):
        (qbT, kbT, vb1), (b_q, b_k, b_v) = hr.next()
        p.dma(qbT, k.QB[h], reads=[k.b_scr["QB"]], writes=[b_q])
        p.dma(kbT, k.KB[h], reads=[k.b_scr["KB"]], writes=[b_k])
        p.dma(vb1[:, :, 0:128], k.VB[h], reads=[k.b_scr["VB"]], writes=[b_v])
        ACT(k, sq, qbT, AF.Square, [b_q], [b_sq])
        ACT(k, ksq, kbT, AF.Square, [b_k], [b_ksq])
        p.op("dve", "tensor_reduce", reads=[b_k], writes=[b_sm], out=kms, in_=kbT.rearrange("p (n t) -> p n t", n=8),
             axis=AX.X, op=ALU.add)
        TS(k, "dve", kmT, kms, 1.0 / 256, None, ALU.mult, None, [b_sm], [b_sm])
        pG, bG = k.ps.next()
        for qi in range(8, 16):
            MM(k, pG[:, (qi - 8) * 8:(qi - 7) * 8], qbT[:, qi * 128:(qi + 1) * 128], kmT, True, True, [b_q, b_sm], [bG])
        pN, bN = k.ps.next()
        for qi in range(16):
            MM(k, pN[:, qi:qi + 1], sq[:, qi * 128:(qi + 1) * 128], k.ones[:, 0:1], True, True, [b_sq, k.b_c], [bN])
        for c in range(4):
            pK, bK = k.ps.next()
            MM(k, pK[:], k.ones, ksq[:, c * 512:(c + 1) * 512], True, True, [b_ksq, k.b_c], [bK])
            p.op("dve", "tensor_reduce", reads=[bK], writes=[b_sm], out=km4[:, c:c + 1], in_=pK[:], axis=AX.X, op=ALU.max)
        p.op("dve", "tensor_reduce", reads=[b_sm], writes=[b_sm], out=ksc, in_=km4, axis=AX.X, op=ALU.max)
        TS(k, "dve", ksc, ksc, 1.0 / 128, None, ALU.mult, None, [b_sm], [b_sm])
        ACT(k, nm, pN[:, 0:16], AF.Sqrt, [bN, b_sm], [b_sm], scale=ksc)
        TS(k, "dve", MBq[:, :, 8], nm, bmax[:, h:h + 1], -SQ128, ALU.add, ALU.mult, [b_sm, b_tp], [b_MBq])
        TT(k, "dve", gsb, pG[:, 0:64].rearrange("p (a b) -> p a b", a=8), k.gm.rearrange("p (a b) -> p a b", a=8),
           ALU.add, [bG, k.b_c], [b_sm])
        src = gsb
        for it in range(2):
            p.op("dve", "tensor_reduce", reads=[b_sm], writes=[b_sm], out=mx, in_=src, axis=AX.X, op=ALU.max)
            TT(k, "dve", eq, src, bc8(mx), ALU.is_equal, [b_sm], [b_sm])
            STT(k, "dve", g2, eq, -1e30, src, ALU.mult, ALU.add, [b_sm], [b_sm])
            src = g2
        p.op("dve", "tensor_reduce", reads=[b_sm], writes=[b_sm], out=mx, in_=g2, axis=AX.X, op=ALU.max)
        TT(k, "dve", eq, gsb, bc8(mx), ALU.is_ge, [b_sm], [b_sm])
        TS(k, "dve", MBq[:, 8:16, 0:8], eq, BIG * SQ128, -BIG * SQ128, ALU.mult, ALU.add, [b_sm], [b_MBq])
        for half in range(2):
            pM, bM = k.ps.next()
            pMv = pM[:].bitcast(BF16)
            for j in range(8):
                qi = half * 8 + j
                TR(k, pMv[0:9, j * 128:(j + 1) * 128], MBq[:, qi, 0:9], k.ident, [b_MBq], [bM])
            CP(k, "act" if half else "dve", MB[0:9, half * 1024:(half + 1) * 1024], pMv[0:9, 0:1024], [bM], [b_MB])
        for qb in range(8):
            pend = []

            def emit_pv(item):
                kj_, PT_, b_PT_ = item
                for qt in range(2):
                    if kj_ <= 2 * qb + qt:
                        pO, bO = k.psx[qt]
                        MM(k, pO[:, 0:129], PT_[:, qt * 128:(qt + 1) * 128], vb1[:, kj_, 0:129],
                           kj_ == 0, kj_ == 2 * qb + qt, [b_PT_, b_v], [bO])
            for kj in range(2 * qb + 2):
                n = kj // 2
                pS, bS = k.ps.next()
                q0 = qb * 256
                v = n if (qb >= 4 and n < qb) else 8
                if n == qb and kj % 2 == 0:
                    segs = [(0, 256, Tp[:, h, 0:256])]
                elif n == qb:
                    segs = [(128, 256, Tp[:, h, 0:128])]
                elif n == qb - 1 and kj % 2 == 1:
                    segs = [(0, 128, Tp[:, h, 128:256]), (128, 256, None)]
                else:
                    segs = [(0, 256, None)]
                c0 = segs[0][0]
                for (a, b, bias) in segs:
                    MM(k, pS[:, a:b], kbT[:, kj * 128:(kj + 1) * 128], qbT[:, q0 + a:q0 + b], True, False, [b_k, b_q], [bS])
                    MM(k, pS[:, a:b], k.emat[:, v, :], MB[0:9, q0 + a:q0 + b], False, bias is None, [k.b_c, b_MB], [bS])
                    if bias is not None:
                        MM(k, pS[:, a:b], k.ident, bias, False, True, [k.b_c, b_tp], [bS])
                PT, b_PT = PTr.next()
                ACT(k, PT[:, c0:256], pS[:, c0:256], AF.Exp, [bS], [b_PT], scale=1.0 / SQ128)
                pend.append((kj, PT, b_PT))
                if len(pend) > 2:
                    emit_pv(pend.pop(0))
            while pend:
                emit_pv(pend.pop(0))
            f_ap, f_b = fin[qb % 2], b_fin[qb % 2]
            for qt in range(2):
                s2, b_s2 = sm2.next()
                y_ap, y_b = yq[qt], b_yq[qt]
                pO, bO = k.psx[qt]
                p.op("dve", "reciprocal", reads=[bO], writes=[b_s2], out=s2[:, 0:1], in_=pO[:, 128:129])
                TS(k, "dve", y_ap, pO[:, 0:128], s2[:, 0:1], None, ALU.mult, None, [bO, b_s2], [y_b])
                ACT(k, junk, y_ap, AF.Square, [y_b], [b_junk, b_s2], accum_out=s2[:, 1:2])
                ACT(k, s2[:, 1:2], s2[:, 1:2], AF.Sqrt, [b_s2], [b_s2], scale=1.0 / 128, bias=EPS)
                p.op("dve", "reciprocal", reads=[b_s2], writes=[b_s2], out=s2[:, 1:2], in_=s2[:, 1:2])
                STT(k, "dve", f_ap[:, qt * 128:(qt + 1) * 128], y_ap, s2[:, 1:2], mgb, ALU.mult, ALU.mult,
                    [y_b, b_s2, b_tp], [f_b])
            pT, bpT = k.ps.next()
            pTv = pT[:].bitcast(BF16)
            for qt in range(2):
                TR(k, pTv[:, qt * 128:(qt + 1) * 128], f_ap[:, qt * 128:(qt + 1) * 128], k.ident, [f_b], [bpT])
            CP(k, "act", oT[:, 8 + h, qb * 256:(qb + 1) * 256], pTv[:, 0:256], [bpT], [k.b_oT])
    if hasattr(k, "OT"):
        p.dma(k.OT, oT, reads=[k.b_oT], writes=[k.b_scr["DBG"]])


def phase_wout(k):
    p = k.p
    ar = k.ar
    I = k.I
    oT = k.oT
    ar.off = k.oT_mark
    h2T = ar.alloc((16, S), BF16)
    k.h2T = h2T
    k.b_h2T = p.buf("h2T")
    k.ws_mark = ar.off
    wring = Ring([(ar.alloc((16, 512), BF16), p.buf()) for _ in range(2)])
    xr = Ring([(ar.alloc((512,), F32), p.buf()) for _ in range(3)])
    wsrc = I["w_out"].rearrange("(kt q) n -> q kt n", q=128)
    for c in range(4):
        wap, wb = load_w_block(k, wring, wsrc[:, :, c * 512:(c + 1) * 512])
        for t in range(NT):
            x_ap, x_b = xr.next()
            p.dma(x_ap, I["x"][t * 128:(t + 1) * 128, c * 512:(c + 1) * 512], writes=[x_b])
            pb, bpb = k.ps.next()
            for kt in range(16):
                MM(k, pb[:], oT[:, kt, t * 128:(t + 1) * 128], wap[:, kt, :], kt == 0, kt == 15, [wb, k.b_oT], [bpb])
            TT(k, "dve", x_ap, pb[:], x_ap, ALU.add, [bpb, x_b], [x_b])
            p.dma(k.X1[t * 128:(t + 1) * 128, c * 512:(c + 1) * 512], x_ap, reads=[x_b], writes=[k.b_scr["X1"]])
    p.barrier()
    ar.off = k.ws_mark
    tiles = [(k.X1[t * 128:(t + 1) * 128, :], [k.b_scr["X1"]]) for t in range(NT)]
    norm_T(k, tiles, I["xattn_norm_g"], h2T, k.b_h2T)


def phase_xattn(k):
    p = k.p
    ar = k.ar
    I = k.I
    h2T = k.h2T
    ar.off = 0
    memT = ar.alloc((16, MEM), BF16)
    b_memT = p.buf()
    kxT = ar.alloc((4, MEM), BF16)
    vx = ar.alloc((2, 512), BF16)
    b_kv = p.buf()
    qxT = ar.alloc((4, S), BF16)
    b_qx = p.buf()
    oxT = ar.alloc((4, S), BF16)
    b_ox = p.buf()
    wxo = ar.alloc((4, D), BF16)
    b_wxo = p.buf()
    assert ar.off <= k.oT_mark
    ar.off = k.ws_mark
    tiles = [(I["mem"][t * 128:(t + 1) * 128, :], []) for t in range(2)]
    norm_T(k, tiles, I["mem_norm_g"], memT, b_memT)
    p.barrier()
    ar.off = k.ws_mark
    wring = Ring([(ar.alloc((16, 512), BF16), p.buf()) for _ in range(2)])
    Pf = Ring([(ar.alloc((256,), F32), p.buf()) for _ in range(4)])
    Pn = Ring([(ar.alloc((256,), BF16), p.buf()) for _ in range(4)])
    PnT = Ring([(ar.alloc((2, 128), BF16), p.buf()) for _ in range(4)])
    sm = Ring([(ar.alloc((8,), F32), p.buf()) for _ in range(8)])
    xr = Ring([(ar.alloc((512,), F32), p.buf()) for _ in range(3)])
    wkv = I["w_xkv"].rearrange("(kt q) n -> q kt n", q=128)
    wap, wb = load_w_block(k, wring, wkv[:, :, 0:512])
    for hx in range(4):
        pb, bpb = k.ps.next()
        for kt in range(16):
            MM(k, pb[:, 0:MEM], wap[:, kt, hx * 128:(hx + 1) * 128], memT[:, kt, :], kt == 0, kt == 15, [wb, b_memT], [bpb])
        CP(k, "act" if hx % 2 else "dve", kxT[:, hx, :], pb[:, 0:MEM], [bpb], [b_kv])
    wap, wb = load_w_block(k, wring, wkv[:, :, 512:1024])
    for mt in range(2):
        pb, bpb = k.ps.next()
        for kt in range(16):
            MM(k, pb[:], memT[:, kt, mt * 128:(mt + 1) * 128], wap[:, kt, :], kt == 0, kt == 15, [wb, b_memT], [bpb])
        CP(k, "act" if mt else "dve", vx[:, mt, :], pb[:], [bpb], [b_kv])
    wq = I["w_xq"].rearrange("(kt q) n -> q kt n", q=128)
    wap, wb = load_w_block(k, wring, wq)
    for hx in range(4):
        for c in range(4):
            pb, bpb = k.ps.next()
            for kt in range(16):
                MM(k, pb[:], wap[:, kt, hx * 128:(hx + 1) * 128], h2T[:, kt, c * 512:(c + 1) * 512], kt == 0, kt == 15,
                   [wb, k.b_h2T], [bpb])
            CP(k, "act" if c % 2 else "dve", qxT[:, hx, c * 512:(c + 1) * 512], pb[:], [bpb], [b_qx])
    for t in range(NT):
        st = []
        for hx in range(4):
            pS, bS = k.ps.next()
            MM(k, pS[:, 0:MEM], qxT[:, hx, t * 128:(t + 1) * 128], kxT[:, hx, :], True, True, [b_qx, b_kv], [bS])
            s_ap, s_b = sm.next()
            st.append([pS, bS, s_ap, s_b])
        for hx in range(4):
            pS, bS, s_ap, s_b = st[hx]
            p.op("dve", "tensor_reduce", reads=[bS], writes=[s_b], out=s_ap[:, 0:1], in_=pS[:, 0:MEM], axis=AX.X, op=ALU.max)
            TS(k, "dve", s_ap[:, 1:2], s_ap[:, 0:1], -1.0 / SQ128, None, ALU.mult, None, [s_b], [s_b])
        for hx in range(4):
            pS, bS, s_ap, s_b = st[hx]
            pf, b_pf = Pf.next()
            ACT(k, pf, pS[:, 0:MEM], AF.Exp, [bS, s_b], [b_pf, s_b], scale=1.0 / SQ128, bias=s_ap[:, 1:2],
                accum_out=s_ap[:, 2:3])
            st[hx] += [pf, b_pf]
        for hx in range(4):
            pS, bS, s_ap, s_b, pf, b_pf = st[hx]
            p.op("dve", "reciprocal", reads=[s_b], writes=[s_b], out=s_ap[:, 3:4], in_=s_ap[:, 2:3])
            pn, b_pn = Pn.next()
            TS(k, "dve", pn, pf, s_ap[:, 3:4], None, ALU.mult, None, [b_pf, s_b], [b_pn])
            st[hx] += [pn, b_pn]
        for hx in range(4):
            pn, b_pn = st[hx][6], st[hx][7]
            pT, bpT = k.ps.next()
            pTv = pT[:].bitcast(BF16)
            for mt in range(2):
                TR(k, pTv[:, mt * 128:(mt + 1) * 128], pn[:, mt * 128:(mt + 1) * 128], k.ident, [b_pn], [bpT])
            pnt, b_pnt = PnT.next()
            CP(k, "act" if hx % 2 else "dve", pnt, pTv[:, 0:256].rearrange("p (a b) -> p a b", a=2), [bpT], [b_pnt])
            st[hx] += [pnt, b_pnt]
        for hx in range(4):
            pnt, b_pnt = st[hx][8], st[hx][9]
            pO, bO = k.ps.next()
            for mt in range(2):
                MM(k, pO[:, 0:128], vx[:, mt, hx * 128:(hx + 1) * 128], pnt[:, mt, :], mt == 0, mt == 1, [b_kv, b_pnt], [bO])
            CP(k, "dve" if hx % 2 else "act", oxT[:, hx, t * 128:(t + 1) * 128], pO[:, 0:128], [bO], [b_ox])
    p.dma(wxo, I["w_xo"].rearrange("(kt q) n -> q kt n", q=128), writes=[b_wxo], q="pool")
    for t in range(NT):
        for c in range(4):
            x_ap, x_b = xr.next()
            p.dma(x_ap, k.X1[t * 128:(t + 1) * 128, c * 512:(c + 1) * 512], reads=[k.b_scr["X1"]], writes=[x_b])
            pb, bpb = k.ps.next()
            for kt in range(4):
                MM(k, pb[:], oxT[:, kt, t * 128:(t + 1) * 128], wxo[:, kt, c * 512:(c + 1) * 512], kt == 0, kt == 3,
                   [b_wxo, b_ox], [bpb])
            TT(k, "dve", x_ap, pb[:], x_ap, ALU.add, [bpb, x_b], [x_b])
            p.dma(k.X2[t * 128:(t + 1) * 128, c * 512:(c + 1) * 512], x_ap, reads=[x_b], writes=[k.b_scr["X2"]])


def phase_ffn(k):
    p = k.p
    ar = k.ar
    I = k.I
    HT = 1024
    halo = p.sbuf("halo", [128, NFF, 2], F32)
    b_halo = p.buf()
    p.op("pool", "memset", writes=[b_halo], ap=halo[:], constant=0.0)
    cwf = p.sbuf("cwf", [128, 4 * NFF], F32)
    b_cwf = p.buf()
    for hf in range(2):
        ar.reset()
        aT = ar.alloc((NFF, HT), BF16)
        b_aT = p.buf()
        mark0 = ar.off
        h3T = ar.alloc((16, HT), BF16)
        b_h3T = p.buf()
        mark1 = ar.off
        tiles = [(k.X2[(hf * 8 + t) * 128:(hf * 8 + t + 1) * 128, :], [k.b_scr["X2"]]) for t in range(8)]
        norm_T(k, tiles, I["ffn_norm_g"], h3T, b_h3T)
        p.barrier()
        ar.off = mark1
        if hf == 0:
            stg = ar.alloc((128,), F32)
            b_stg = p.buf()
            for j in range(4):
                src = (I["ffn_conv_w"][j] if j < 3 else I["ffn_conv_b"][0]).rearrange("(b q) -> b q", q=128)
                p.dma(stg[0:NFF, :], src, writes=[b_stg])
                pb, bpb = k.ps.next()
                TR(k, pb[:, 0:NFF], stg[0:NFF, :], k.identf[0:NFF, 0:NFF], [b_stg], [bpb])
                CP(k, "dve", cwf[:, j * NFF:(j + 1) * NFF], pb[:, 0:NFF], [bpb], [b_cwf])
        wg = Ring([(ar.alloc((16, 256), BF16), p.buf()) for _ in range(2)])
        wu = Ring([(ar.alloc((16, 256), BF16), p.buf()) for _ in range(2)])
        graw = Ring([(ar.alloc((HT + 2,), F32), p.buf()) for _ in range(2)])
        gy = Ring([(ar.alloc((HT,), F32), p.buf()) for _ in range(2)])
        wgs = I["w_gate"].rearrange("(kt q) n -> q kt n", q=128)
        wus = I["w_up"].rearrange("(kt q) n -> q kt n", q=128)
        for j in range(NFF):
            if j % 2 == 0:
                g_ap2, g_b = wg.next()
                u_ap2, u_b = wu.next()
                p.dma(g_ap2, wgs[:, :, j * 128:(j + 2) * 128], writes=[g_b], q="pool")
                p.dma(u_ap2, wus[:, :, j * 128:(j + 2) * 128], writes=[u_b], q="pool")
            g_ap = g_ap2[:, :, (j % 2) * 128:(j % 2 + 1) * 128]
            u_ap = u_ap2[:, :, (j % 2) * 128:(j % 2 + 1) * 128]
            r_ap, r_b = graw.next()
            y_ap, y_b = gy.next()
            pus = []
            for c in range(2):
                pg, bg = k.ps.next()
                pu, bu = k.ps.next()
                for kt in range(16):
                    MM(k, pg[:], g_ap[:, kt, :], h3T[:, kt, c * 512:(c + 1) * 512], kt == 0, kt == 15, [g_b, b_h3T], [bg])
                for kt in range(16):
                    MM(k, pu[:], u_ap[:, kt, :], h3T[:, kt, c * 512:(c + 1) * 512], kt == 0, kt == 15, [u_b, b_h3T], [bu])
                CP(k, "act", r_ap[:, 2 + c * 512:2 + (c + 1) * 512], pg[:], [bg], [r_b])
                pus.append((pu, bu))
            CP(k, "pool", r_ap[:, 0:2], halo[:, j, :], [b_halo], [r_b])
            if hf == 0:
                CP(k, "pool", halo[:, j, :], r_ap[:, HT:HT + 2], [r_b], [b_halo])
            TS(k, "dve", y_ap, r_ap[:, 0:HT], cwf[:, j:j + 1], None, ALU.mult, None, [r_b, b_cwf], [y_b])
            for tap in range(1, 3):
                STT(k, "dve", y_ap, r_ap[:, tap:tap + HT], cwf[:, tap * NFF + j:tap * NFF + j + 1], y_ap, ALU.mult, ALU.add,
                    [r_b, b_cwf, y_b], [y_b])
            ACT(k, y_ap, y_ap, AF.Silu, [y_b, b_cwf], [y_b], bias=cwf[:, 3 * NFF + j:3 * NFF + j + 1])
            for c in range(2):
                pu, bu = pus[c]
                TT(k, "dve", aT[:, j, c * 512:(c + 1) * 512], y_ap[:, c * 512:(c + 1) * 512], pu[:], ALU.mult,
                   [y_b, bu], [b_aT])
        p.barrier()
        ar.off = mark0
        x3 = ar.alloc((8, D), F32)
        b_x3 = p.bufs(8)
        aflat = aT.rearrange("p a b -> p (a b)")
        fgb = aflat[:, 0:2 * D].bitcast(F32)
        junk = aflat[:, 2 * D:3 * D]
        b_fgb = b_aT
        b_junk = b_aT
        wd = Ring([(ar.alloc((4, 512), BF16), p.buf()) for _ in range(3)])
        xr = Ring([(ar.alloc((512,), F32), p.buf()) for _ in range(2)])
        ssf = ar.alloc((8,), F32)
        b_ssf = p.buf()
        for cc in range(4):
            for j4 in range(NFF // 4):
                w_ap, w_b = wd.next()
                p.dma(w_ap, I["w_down"][j4 * 512:(j4 + 1) * 512, cc * 512:(cc + 1) * 512].rearrange("(a q) n -> q a n", q=128),
                      writes=[w_b], q="pool")
                for a in range(4):
                    j = j4 * 4 + a
                    for tt in range(8):
                        pb, bpb = k.banks[tt]
                        MM(k, pb[:], aT[:, j, tt * 128:(tt + 1) * 128], w_ap[:, a, :], j == 0, j == NFF - 1, [w_b, b_aT], [bpb])
            for tt in range(8):
                pb, bpb = k.banks[tt]
                x_ap, x_b = xr.next()
                row = (hf * 8 + tt) * 128
                p.dma(x_ap, k.X2[row:row + 128, cc * 512:(cc + 1) * 512], reads=[k.b_scr["X2"]], writes=[x_b])
                TT(k, "dve", x3[:, tt, cc * 512:(cc + 1) * 512], pb[:], x_ap, ALU.add, [bpb, x_b], [b_x3[tt]])
        p.dma(fgb, I["final_norm_g"].partition_broadcast(128), writes=[b_aT])
        outs = []
        for tt in range(8):
            row = (hf * 8 + tt) * 128
            ACT(k, junk, x3[:, tt, :], AF.Square, [b_x3[tt]], [b_junk, b_ssf], accum_out=ssf[:, tt:tt + 1])
            ACT(k, ssf[:, tt:tt + 1], ssf[:, tt:tt + 1], AF.Sqrt, [b_ssf], [b_ssf], scale=1.0 / D, bias=EPS)
            p.op("dve", "reciprocal", reads=[b_ssf], writes=[b_ssf], out=ssf[:, tt:tt + 1], in_=ssf[:, tt:tt + 1])
            STT(k, "dve", x3[:, tt, :], x3[:, tt, :], ssf[:, tt:tt + 1], fgb, ALU.mult, ALU.mult,
                [b_x3[tt], b_ssf, b_fgb], [b_x3[tt]])
            outs.append(p.dma(k.out[row:row + 128, :], x3[:, tt, :], reads=[b_x3[tt]]))
        p.barrier()
    return []


_CACHE = {}


def make_in_maps(inputs, n=8, skip=()):
    cf, cb = host_consts()
    tidx = toeplitz_index()
    rb = np.asarray(inputs["rel_bias"], np.float32)
    rb_toep = np.ascontiguousarray(rb[tidx].transpose(0, 2, 1))
    shared = {
        "mix_norm_g": inputs["mix_norm_g"].reshape(1, D), "xattn_norm_g": inputs["xattn_norm_g"].reshape(1, D),
        "mem_norm_g": inputs["mem_norm_g"].reshape(1, D), "ffn_norm_g": inputs["ffn_norm_g"].reshape(1, D),
        "final_norm_g": inputs["final_norm_g"].reshape(1, D),
        "w_in": inputs["w_in"][0], "gdn_conv_w": inputs["gdn_conv_w"][0], "gdn_a_log": inputs["gdn_a_log"].reshape(1, 8),
        "gdn_dt_bias": inputs["gdn_dt_bias"].reshape(1, 8), "gdn_norm_g": inputs["gdn_norm_g"].reshape(1, 128),
        "moba_norm_g": inputs["moba_norm_g"].reshape(1, 128), "rel_bias": rb.reshape(1, 256), "rb_toep": rb_toep,
        "w_out": inputs["w_out"][0], "w_xq": inputs["w_xq"][0], "w_xkv": inputs["w_xkv"][0], "w_xo": inputs["w_xo"][0],
        "w_gate": inputs["w_gate"][0], "w_up": inputs["w_up"][0], "ffn_conv_w": inputs["ffn_conv_w"][0],
        "ffn_conv_b": inputs["ffn_conv_b"].reshape(1, DFF), "w_down": inputs["w_down"][0],
        "cst_f": cf, "cst_b": cb,
    }
    shared = {kk: (np.zeros((1, 1), np.float32) if kk in skip else np.ascontiguousarray(np.asarray(v, np.float32)))
              for kk, v in shared.items()}
    maps = []
    for b in range(n):
        m = dict(shared)
        m["x"] = np.ascontiguousarray(np.asarray(inputs["x"][b], np.float32))
        m["mem"] = (np.zeros((1, 1), np.float32) if "mem" in skip
                    else np.ascontiguousarray(np.asarray(inputs["mem"][b], np.float32)))
        maps.append(m)
    return maps


def kernel(**inputs):
    if "nc" not in _CACHE:
        _CACHE["nc"] = build_program()[0]
    nc = _CACHE["nc"]
    maps = make_in_maps(inputs, 8)
    res = run_bass_kernel_spmd(nc, maps, core_ids=list(range(8)))
    return np.stack([np.asarray(r["out"], np.float32) for r in res.results], axis=0)
```

```python
import bisect
import math
from contextlib import ExitStack

import numpy as np
import concourse.bass as bass
import concourse.mybir as mybir
from concourse.bass_utils import run_bass_kernel_spmd

F32 = mybir.dt.float32
BF16 = mybir.dt.bfloat16
ALU = mybir.AluOpType
AF = mybir.ActivationFunctionType
AX = mybir.AxisListType

S = 2048
D = 2048
NT = 16
H = 8
HD = 128
DFF = 5632
NFF = 44
MEM = 256
INW = 7184
EPS = 1e-6
BIG = 30000.0
SQ128 = math.sqrt(128.0)

ENGS = ("pe", "act", "dve", "pool", "sp")
SEM_LIMIT = 2000
N_ENG_SEMS = {"pe": 14, "act": 5, "dve": 5, "pool": 3, "sp": 1}
N_DMA_SEMS = 32
N_SWDMA_SEMS = 16


class Buf:
    __slots__ = ("name", "last_w", "creads", "dreads")

    def __init__(self, name=""):
        self.name = name
        self.last_w = None
        self.creads = {}
        self.dreads = []


class Op:
    __slots__ = ("eng", "name", "kw", "waits", "signal", "pos")

    def __init__(self, eng, name, kw, pos):
        self.eng = eng
        self.name = name
        self.kw = kw
        self.waits = []
        self.signal = None
        self.pos = pos


class Prog:
    def __init__(self, nc, stack):
        self.nc = nc
        self.stack = stack
        self.ops = {e: [] for e in ENGS}
        self.nsig = {e: 0 for e in ENGS}
        self.sigpos = {e: [] for e in ENGS}
        self.sigtok = {e: [] for e in ENGS}
        self.waited = {e: {} for e in ENGS}
        self.esems = {e: [stack.enter_context(nc.semaphore(f"s_{e}_{i}")) for i in range(N_ENG_SEMS[e])]
                      for e in ENGS}
        self.dsems = {"hw": [stack.enter_context(nc.semaphore(f"s_dma_{i}")) for i in range(N_DMA_SEMS)],
                      "sw": [stack.enter_context(nc.semaphore(f"s_swdma_{i}")) for i in range(N_SWDMA_SEMS)]}
        self.ndma_q = {"hw": 0, "sw": 0}
        self.ndma = 0
        self.dma_toks = []
        self.pe_prev = (None, None, True)

    def sbuf(self, name, shape, dtype):
        return self.stack.enter_context(self.nc.sbuf_tensor(name, list(shape), dtype))

    def psum(self, name, shape, dtype):
        return self.stack.enter_context(self.nc.psum_tensor(name, list(shape), dtype))

    def buf(self, name=""):
        return Buf(name)

    def bufs(self, n):
        return [Buf() for _ in range(n)]

    def _force_signal(self, eng):
        lst = self.ops[eng]
        if not lst:
            return None
        last = lst[-1]
        if last.signal is None:
            n = self.nsig[eng]
            self.nsig[eng] += 1
            sem = self.esems[eng][n // SEM_LIMIT]
            val = n % SEM_LIMIT + 1
            last.signal = (sem, 1)
            self.sigpos[eng].append(last.pos)
            self.sigtok[eng].append((sem, val))
        return last

    def _resolve(self, tok):
        if tok[0] == "d":
            return tok[1], tok[2]
        _, eng, pos = tok
        sp = self.sigpos[eng]
        i = bisect.bisect_left(sp, pos)
        if i < len(sp):
            return self.sigtok[eng][i]
        last = self._force_signal(eng)
        assert last.pos >= pos
        i = bisect.bisect_left(sp, pos)
        return self.sigtok[eng][i]

    def _add_wait(self, op, tok):
        sem, val = self._resolve(tok)
        w = self.waited[op.eng]
        key = id(sem)
        if w.get(key, 0) >= val:
            return
        w[key] = val
        for i, (s, v) in enumerate(op.waits):
            if s is sem:
                op.waits[i] = (sem, max(v, val))
                return
        op.waits.append((sem, val))

    def _deps(self, op, reads, writes, is_dma):
        eng = op.eng
        for b in reads:
            if b.last_w is not None:
                self._add_wait(op, b.last_w)
        for b in writes:
            t = b.last_w
            if t is not None and not (t[0] == "c" and t[1] == eng == "pe" and not is_dma):
                self._add_wait(op, t)
            for e, t in b.creads.items():
                if e == eng == "pe" and not is_dma:
                    continue
                self._add_wait(op, t)
            for t in b.dreads:
                self._add_wait(op, t)

    def _compute_pos_check(self, eng):
        lst = self.ops[eng]
        if lst and lst[-1].signal is None and lst[-1].name not in ("nop", "dma_start"):
            self._force_signal(eng)

    def op(self, eng, name, reads=(), writes=(), **kw):
        lst = self.ops[eng]
        rset = frozenset(id(b) for b in reads)
        wset = frozenset(id(b) for b in writes)
        if lst and lst[-1].name not in ("nop", "dma_start") and lst[-1].signal is None:
            prev = lst[-1]
            if eng != "pe":
                self._force_signal(eng)
            else:
                pr, pw, pdone = self.pe_prev
                if pr != rset or (pw != wset and pdone):
                    self._force_signal(eng)
        if eng == "pe":
            self.pe_prev = (rset, wset, kw.get("stop", True))
        o = Op(eng, name, kw, len(lst))
        self._deps(o, reads, writes, False)
        lst.append(o)
        tok = ("c", eng, o.pos)
        for b in reads:
            b.creads[eng] = tok
        for b in writes:
            b.last_w = tok
            b.creads = {}
            b.dreads = []
        return o

    def dma(self, out, in_, reads=(), writes=(), q="sp", **kw):
        if q != "sp":
            self._compute_pos_check(q)
        lst = self.ops[q]
        o = Op(q, "dma_start", dict(out=out, in_=in_, **kw), len(lst))
        self._deps(o, reads, writes, True)
        kind = "sw" if q == "pool" else "hw"
        pool_ = self.dsems[kind]
        i = self.ndma_q[kind] % len(pool_)
        r = self.ndma_q[kind] // len(pool_)
        self.ndma_q[kind] += 1
        self.ndma += 1
        sem = pool_[i]
        if r > 0:
            self._add_wait(o, ("d", sem, 16 * r))
        o.signal = (sem, 16)
        lst.append(o)
        tok = ("d", sem, 16 * (r + 1))
        self.dma_toks.append(tok)
        for b in reads:
            b.dreads.append(tok)
        for b in writes:
            b.last_w = tok
            b.creads = {}
            b.dreads = []
        return tok

    def barrier(self):
        toks = list(self.dma_toks[-(N_DMA_SEMS + N_SWDMA_SEMS) * 2:])
        for e in ENGS:
            lst = self.ops[e]
            if lst and lst[-1].name not in ("dma_start", "nop"):
                last = self._force_signal(e)
                toks.append(("c", e, last.pos))
            else:
                for o in reversed(lst):
                    if o.name not in ("dma_start", "nop"):
                        assert o.signal is not None
                        toks.append(("c", e, o.pos))
                        break
        for e in ENGS:
            o = Op(e, "nop", {}, len(self.ops[e]))
            for t in toks:
                self._add_wait(o, t)
            self.ops[e].append(o)

    def final_wait(self, eng, toks):
        o = Op(eng, "nop", {}, len(self.ops[eng]))
        for t in toks:
            self._add_wait(o, t)
        self.ops[eng].append(o)

    def emit(self):
        nc = self.nc
        prog = self
        with nc.Block() as block:
            def run(engname):
                def body(e):
                    for o in prog.ops[engname]:
                        for (sem, val) in o.waits:
                            e.wait_ge(sem, val)
                        if o.name == "nop":
                            continue
                        ins = getattr(e, o.name)(**o.kw)
                        if o.signal is not None:
                            ins.then_inc(o.signal[0], o.signal[1])
                return body
            block.tensor(run("pe"))
            block.scalar(run("act"))
            block.vector(run("dve"))
            block.gpsimd(run("pool"))
            block.sync(run("sp"))

    def stats(self):
        return {e: (len(self.ops[e]), self.nsig[e]) for e in ENGS}, self.ndma


class Arena:
    def __init__(self, p, nbytes):
        self.t = p.sbuf("arena", [128, nbytes // 2], BF16)
        self.n = nbytes
        self.off = 0

    def reset(self):
        self.off = 0

    def alloc(self, shape, dtype):
        esz = 2 if dtype == BF16 else 4
        n = 1
        for s in shape:
            n *= s
        nb = n * esz
        a = self.t[:, self.off // 2:(self.off + nb) // 2]
        self.off += (nb + 63) // 64 * 64
        assert self.off <= self.n, f"arena overflow {self.off} > {self.n}"
        if dtype != BF16:
            a = a.bitcast(dtype)
        if len(shape) == 2:
            a = a.rearrange("p (a b) -> p a b", a=shape[0])
        elif len(shape) == 3:
            a = a.rearrange("p (a b c) -> p a b c", a=shape[0], b=shape[1])
        return a


class Ring:
    def __init__(self, items):
        self.items = items
        self.i = 0

    def next(self):
        it = self.items[self.i % len(self.items)]
        self.i += 1
        return it


NCF = 576
NCB = 768 + 9 * 128


def host_consts():
    cf = np.zeros((128, NCF), np.float32)
    ii = np.arange(128)
    cf[:, 0:128] = np.eye(128, dtype=np.float32)
    cf[:, 128:256] = (ii[:, None] <= ii[None, :]).astype(np.float32)
    cf[:, 256:384] = (ii[:, None] > ii[None, :]).astype(np.float32)
    cf[:, 384:512] = 1.0
    gm = np.zeros((128, 8, 8), np.float32)
    for qi in range(8, 16):
        own = qi // 2
        gm[:, qi - 8, own:] = -1e30
    cf[:, 512:576] = gm.reshape(128, 64)
    cb = np.zeros((128, NCB), np.float32)
    cb[:, 0:128] = np.eye(128, dtype=np.float32)
    cb[:, 128:256] = 1.0
    cb[:, 256:384] = (ii[None, :] > ii[:, None]).astype(np.float32) * BIG
    cb[:, 384:512] = (ii[None, :] >= ii[:, None]).astype(np.float32) * BIG
    rr = np.arange(256)
    cb[:, 512:768] = (rr[None, :] < ii[:, None]).astype(np.float32) * (-BIG * SQ128)
    e = np.zeros((9, 9, 128), np.float32)
    for v in range(9):
        e[8, v, :] = 1.0
        if v < 8:
            e[v, v, :] = 1.0
    cb[0:9, 768:768 + 9 * 128] = e.reshape(9, 9 * 128)
    return cf, cb


def rel_bucket_np(n):
    n = np.maximum(n, 0)
    max_exact = 16
    nf = np.maximum(n, 1).astype(np.float32)
    large = max_exact + (np.log(nf / max_exact) / math.log(128 / max_exact) * (32 - max_exact)).astype(np.int32)
    large = np.minimum(large, 31)
    return np.where(n < max_exact, n, large)


def toeplitz_index():
    kk = np.arange(128)[:, None]
    r = np.arange(256)[None, :]
    return rel_bucket_np(r - kk)


class K:
    pass


def build_program(debug_phase=None):
    nc = bass.Bass("TRN2", target_bir_lowering=False)
    k = K()
    k.nc = nc
    dbg = debug_phase is not None

    order = ["inproj", "gdn", "moba", "wout", "xattn", "ffn"]
    need_from = {"mem": "xattn", "w_out": "wout", "w_xq": "xattn", "w_xkv": "xattn", "w_xo": "xattn",
                 "w_gate": "ffn", "w_up": "ffn", "w_down": "ffn"}
    k.skip = set()
    if dbg:
        for nm, ph in need_from.items():
            if order.index(ph) > order.index(debug_phase):
                k.skip.add(nm)

    def din(name, shape, dt=F32):
        if name in k.skip:
            shape = [1, 1]
        return nc.dram_tensor(name, list(shape), dt, kind="ExternalInput").ap()

    DUMP = {"inproj": ("QA", "KA", "KT", "VA", "ZS", "QB", "KB", "VB", "DBG"), "moba": ("OT",), "gdn": ("OT",),
            "wout": ("X1",), "xattn": ("X2",), "ffn": ("OT", "X1", "X2")}.get(debug_phase, ())

    def dscr(name, shape, dt=BF16):
        kind = "ExternalOutput" if name in DUMP else "Internal"
        return nc.dram_tensor(name, list(shape), dt, kind=kind).ap()

    I = {}
    I["x"] = din("x", [S, D])
    I["mem"] = din("mem", [MEM, D])
    for nm in ("mix_norm_g", "xattn_norm_g", "mem_norm_g", "ffn_norm_g", "final_norm_g"):
        I[nm] = din(nm, [1, D])
    I["w_in"] = din("w_in", [D, INW])
    I["gdn_conv_w"] = din("gdn_conv_w", [4, 3072])
    I["gdn_a_log"] = din("gdn_a_log", [1, 8])
    I["gdn_dt_bias"] = din("gdn_dt_bias", [1, 8])
    I["gdn_norm_g"] = din("gdn_norm_g", [1, 128])
    I["moba_norm_g"] = din("moba_norm_g", [1, 128])
    I["rel_bias"] = din("rel_bias", [1, 256])
    I["rb_toep"] = din("rb_toep", [128, 8, 256])
    I["w_out"] = din("w_out", [D, D])
    I["w_xq"] = din("w_xq", [D, 512])
    I["w_xkv"] = din("w_xkv", [D, 1024])
    I["w_xo"] = din("w_xo", [512, D])
    I["w_gate"] = din("w_gate", [D, DFF])
    I["w_up"] = din("w_up", [D, DFF])
    I["ffn_conv_w"] = din("ffn_conv_w", [3, DFF])
    I["ffn_conv_b"] = din("ffn_conv_b", [1, DFF])
    I["w_down"] = din("w_down", [DFF, D])
    I["cst_f"] = din("cst_f", [128, NCF])
    I["cst_b"] = din("cst_b", [128, NCB])
    out = nc.dram_tensor("out", [S, D], F32, kind="ExternalOutput").ap()
    k.I = I
    k.out = out

    k.QA = dscr("QA", [128, 16, 8, 128])
    k.KA = dscr("KA", [128, 16, 8, 128])
    k.KT = dscr("KT", [128, 16, 8, 128])
    k.VA = dscr("VA", [128, 16, 8, 128])
    k.ZS = dscr("ZS", [128, 16, 8, 128])
    k.QB = dscr("QB", [8, 128, 2048])
    k.KB = dscr("KB", [8, 128, 2048])
    k.VB = dscr("VB", [8, 128, 16, 128])
    k.X1 = dscr("X1", [S, D], F32)
    k.X2 = dscr("X2", [S, D], F32)
    if "DBG" in DUMP:
        k.DBG = dscr("DBG", [128, 4096], F32)
    if "OT" in DUMP:
        k.OT = dscr("OT", [128, 16, S], BF16)
    k.b_scr = {nm: Buf(nm) for nm in ("QA", "KA", "KT", "VA", "ZS", "QB", "KB", "VB", "X1", "X2", "DBG")}

    with ExitStack() as st:
        p = Prog(nc, st)
        k.p = p
        k.cf = p.sbuf("cf", [128, NCF], F32)
        k.cb = p.sbuf("cb", [128, NCB], BF16)
        k.b_c = p.buf("consts")
        p.dma(k.cf[:], I["cst_f"], writes=[k.b_c])
        p.dma(k.cb[:], I["cst_b"], writes=[k.b_c], q="pool")
        k.identf = k.cf[:, 0:128]
        k.trif = k.cf[:, 128:256]
        k.suf = k.cf[:, 256:384]
        k.onesf = k.cf[:, 384:512]
        k.gm = k.cf[:, 512:576]
        k.ident = k.cb[:, 0:128]
        k.ones = k.cb[:, 128:256]
        k.umq = k.cb[:, 256:384]
        k.uma = k.cb[:, 384:512]
        k.cm = k.cb[:, 512:768]
        k.emat = k.cb[0:9, 768:768 + 9 * 128].rearrange("p (v n) -> p v n", v=9)
        k.BA = p.sbuf("BA", [128, 16, 16], F32)
        k.b_BA = p.buf("BA")
        k.BETA = p.sbuf("BETA", [128, 16, 8], F32)
        k.GG = p.sbuf("GG", [128, 16, 8], F32)
        k.b_bg = p.buf("betag")
        banks = [(p.psum(f"ps{i}", [128, 512], F32), p.buf(f"ps{i}")) for i in range(8)]
        k.ps = Ring(banks[0:6])
        k.psx = banks[6:8]
        k.banks = banks
        k.ar = Arena(p, 176 * 1024)

        phases = [phase_inproj, phase_gdn, phase_moba, phase_wout, phase_xattn, phase_ffn]
        names = ["inproj", "gdn", "moba", "wout", "xattn", "ffn"]
        out_toks = []
        for fn, nm in zip(phases, names):
            r = fn(k)
            if r:
                out_toks += r
            p.barrier()
            if debug_phase == nm:
                break
        p.final_wait("sp", p.dma_toks[-(N_DMA_SEMS + N_SWDMA_SEMS) * 2:])
        k.stats = p.stats()
        p.emit()
    return nc, k


def MM(k, out, lhsT, rhs, start, stop, r, w):
    k.p.op("pe", "matmul", reads=r, writes=w, out=out, lhsT=lhsT, rhs=rhs, start=start, stop=stop)


def TR(k, out, in_, ident, r, w):
    k.p.op("pe", "transpose", reads=r + [k.b_c], writes=w, out=out, in_=in_, identity=ident)


def ACT(k, out, in_, func, r, w, **kw):
    k.p.op("act", "activation", reads=r, writes=w, out=out, in_=in_, func=func, **kw)


def CP(k, eng, out, in_, r, w):
    if eng == "act":
        k.p.op("act", "copy", reads=r, writes=w, out=out, in_=in_)
    else:
        k.p.op(eng, "tensor_copy", reads=r, writes=w, out=out, in_=in_)


def TT(k, eng, out, in0, in1, op, r, w):
    k.p.op(eng, "tensor_tensor", reads=r, writes=w, out=out, in0=in0, in1=in1, op=op)


def TS(k, eng, out, in0, s1, s2, op0, op1, r, w):
    if s2 is None:
        k.p.op(eng, "tensor_scalar", reads=r, writes=w, out=out, in0=in0, scalar1=s1, scalar2=None, op0=op0)
    else:
        k.p.op(eng, "tensor_scalar", reads=r, writes=w, out=out, in0=in0, scalar1=s1, scalar2=s2, op0=op0, op1=op1)


def STT(k, eng, out, in0, scalar, in1, op0, op1, r, w):
    k.p.op(eng, "scalar_tensor_tensor", reads=r, writes=w, out=out, in0=in0, scalar=scalar, in1=in1,
           op0=op0, op1=op1)


def norm_T(k, tiles, gain, dstT, b_dst, col0=0):
    p = k.p
    ar = k.ar
    gb = ar.alloc((D,), F32)
    b_gb = p.buf()
    p.dma(gb, gain.partition_broadcast(128), writes=[b_gb])
    xt = [ar.alloc((D,), F32) for _ in range(2)]
    b_xt = p.bufs(2)
    junk = ar.alloc((D,), BF16)
    b_junk = p.buf()
    hb = [ar.alloc((D,), BF16) for _ in range(2)]
    b_hb = p.bufs(2)
    ss = ar.alloc((len(tiles),), F32)
    b_ss = p.buf()
    for t, (src, rb) in enumerate(tiles):
        i = t % 2
        p.dma(xt[i], src, reads=rb, writes=[b_xt[i]])
        ACT(k, junk, xt[i], AF.Square, [b_xt[i]], [b_junk, b_ss], accum_out=ss[:, t:t + 1])
        ACT(k, ss[:, t:t + 1], ss[:, t:t + 1], AF.Sqrt, [b_ss], [b_ss], scale=1.0 / D, bias=EPS)
        p.op("dve", "reciprocal", reads=[b_ss], writes=[b_ss], out=ss[:, t:t + 1], in_=ss[:, t:t + 1])
        STT(k, "dve", hb[i], xt[i], ss[:, t:t + 1], gb, ALU.mult, ALU.mult, [b_xt[i], b_ss, b_gb], [b_hb[i]])
        for k4 in range(4):
            pb, bpb = k.ps.next()
            pbv = pb[:].bitcast(BF16)
            for j in range(4):
                kt = k4 * 4 + j
                TR(k, pbv[:, j * 128:(j + 1) * 128], hb[i][:, kt * 128:(kt + 1) * 128], k.ident, [b_hb[i]], [bpb])
            CP(k, "act" if k4 % 2 else "dve",
               dstT[:, k4 * 4:(k4 + 1) * 4, col0 + t * 128:col0 + (t + 1) * 128],
               pbv[:, 0:512].rearrange("p (a b) -> p a b", a=4), [bpb], [b_dst])


def load_w_block(k, ring, src):
    ap, b = ring.next()
    k.p.dma(ap, src, writes=[b], q="pool")
    return ap, b


def phase_inproj(k):
    p = k.p
    ar = k.ar
    I = k.I
    ar.reset()
    hT = ar.alloc((16, S), BF16)
    b_hT = p.buf("hT")
    k.hT = hT
    mark = ar.off
    tiles = [(I["x"][t * 128:(t + 1) * 128, :], []) for t in range(NT)]
    norm_T(k, tiles, I["mix_norm_g"], hT, b_hT)
    p.barrier()
    ar.off = mark
    wring = Ring([(ar.alloc((16, 512), BF16), p.buf()) for _ in range(2)])
    wba = ar.alloc((16, 16), BF16)
    b_wba = p.buf()
    raw = [ar.alloc((S + 4,), F32) for _ in range(2)]
    b_raw = p.bufs(2)
    yr = Ring([(ar.alloc((S,), F32), p.buf()) for _ in range(2)])
    sqr = Ring([(ar.alloc((S,), BF16), p.buf()) for _ in range(2)])
    rsr = Ring([(ar.alloc((S,), F32), p.buf()) for _ in range(2)])
    obf = [ar.alloc((S,), BF16) for _ in range(2)]
    b_obf = p.bufs(2)
    tst = [ar.alloc((16, 128), BF16) for _ in range(2)]
    b_tst = p.bufs(2)
    zst = [ar.alloc((512,), BF16) for _ in range(3)]
    b_zst = p.bufs(3)
    cw = ar.alloc((96,), F32)
    cwl = ar.alloc((128,), F32)
    b_cw = p.buf()
    small = ar.alloc((64,), F32)
    b_small = p.buf()
    p.dma(cwl[0:96, :], I["gdn_conv_w"].rearrange("j (b q) -> (j b) q", q=128), writes=[b_cw])
    pb, bpb = k.ps.next()
    TR(k, pb[:, 0:96], cwl[0:96, :], k.identf[0:96, 0:96], [b_cw], [bpb])
    CP(k, "dve", cw, pb[:, 0:96], [bpb], [b_cw])
    for i in range(2):
        p.op("pool", "memset", writes=[b_raw[i]], ap=raw[i][:, 0:4], constant=0.0)

    wsrc = I["w_in"].rearrange("(kt q) n -> q kt n", q=128)
    ri = 0
    oi = 0
    ti = 0

    def fblock(wap, wb, f, evac):
        for c in range(4):
            pb, bpb = k.ps.next()
            for kt in range(16):
                MM(k, pb[:], wap[:, kt, f * 128:(f + 1) * 128], hT[:, kt, c * 512:(c + 1) * 512],
                   kt == 0, kt == 15, [wb, b_hT], [bpb])
            evac(pb, bpb, c * 512)

    for blk in range(6):
        wap, wb = load_w_block(k, wring, wsrc[:, :, blk * 512:(blk + 1) * 512])
        for f in range(4):
            fb = blk * 4 + f
            kind = fb // 8
            hd = fb % 8
            r_ap, r_b = raw[ri % 2], b_raw[ri % 2]
            ri += 1

            def evac_raw(pb, bpb, tok0, r_ap=r_ap, r_b=r_b):
                CP(k, "act", r_ap[:, 3 + tok0:3 + tok0 + 512], pb[:], [bpb], [r_b])
            fblock(wap, wb, f, evac_raw)
            y, b_y = yr.next()
            if kind != 2:
                sq, b_sq = sqr.next()
                rs, b_rs = rsr.next()
            TS(k, "dve", y, r_ap[:, 0:S], cw[:, 0 * 24 + fb:0 * 24 + fb + 1], None, ALU.mult, None, [r_b, b_cw], [b_y])
            for j in range(1, 4):
                STT(k, "dve", y, r_ap[:, j:j + S], cw[:, j * 24 + fb:j * 24 + fb + 1], y, ALU.mult, ALU.add,
                    [r_b, b_cw, b_y], [b_y])
            o_ap, o_b = obf[oi % 2], b_obf[oi % 2]
            oi += 1
            if kind == 2:
                ACT(k, o_ap, y, AF.Silu, [b_y], [o_b])
            else:
                ACT(k, y, y, AF.Silu, [b_y], [b_y])
                ACT(k, sq, y, AF.Square, [b_y], [b_sq])
                for c in range(4):
                    pb, bpb = k.ps.next()
                    MM(k, pb[:], k.ones, sq[:, c * 512:(c + 1) * 512], True, True, [b_sq, k.b_c], [bpb])
                    if kind == 0:
                        ACT(k, rs[:, c * 512:(c + 1) * 512], pb[:], AF.Sqrt, [bpb], [b_rs], scale=128.0,
                            bias=128.0 * EPS)
                    else:
                        ACT(k, rs[:, c * 512:(c + 1) * 512], pb[:], AF.Sqrt, [bpb], [b_rs], scale=1.0, bias=EPS)
                p.op("dve", "reciprocal", reads=[b_rs], writes=[b_rs], out=rs, in_=rs)
                TT(k, "dve", o_ap, y, rs, ALU.mult, [b_y, b_rs], [o_b])
            if kind == 0:
                p.dma(k.QA[:, :, hd, :], o_ap.rearrange("p (c t) -> p c t", c=16), reads=[o_b],
                      writes=[k.b_scr["QA"]])
            if kind == 1:
                p.dma(k.KA[:, :, hd, :], o_ap.rearrange("p (c t) -> p c t", c=16), reads=[o_b],
                      writes=[k.b_scr["KA"]])
            if kind >= 1:
                t_ap, t_b = tst[ti % 2], b_tst[ti % 2]
                ti += 1
                for c4 in range(4):
                    pb, bpb = k.ps.next()
                    pbv = pb[:].bitcast(BF16)
                    for j in range(4):
                        c = c4 * 4 + j
                        TR(k, pbv[:, j * 128:(j + 1) * 128], o_ap[:, c * 128:(c + 1) * 128], k.ident, [o_b], [bpb])
                    CP(k, "act" if c4 % 2 else "dve", t_ap[:, c4 * 4:(c4 + 1) * 4, :],
                       pbv[:, 0:512].rearrange("p (a b) -> p a b", a=4), [bpb], [t_b])
                dst = k.KT if kind == 1 else k.VA
                p.dma(dst[:, :, hd, :], t_ap, reads=[t_b], writes=[k.b_scr["KT" if kind == 1 else "VA"]])

    zi = 0
    for blk in range(2):
        wap, wb = load_w_block(k, wring, wsrc[:, :, 3072 + blk * 512:3072 + (blk + 1) * 512])
        for t in range(NT):
            pb, bpb = k.ps.next()
            for kt in range(16):
                MM(k, pb[:], hT[:, kt, t * 128:(t + 1) * 128], wap[:, kt, :], kt == 0, kt == 15, [wb, b_hT], [bpb])
            z_ap, z_b = zst[zi % 3], b_zst[zi % 3]
            zi += 1
            ACT(k, z_ap, pb[:], AF.Silu, [bpb], [z_b])
            p.dma(k.ZS[:, t, blk * 4:(blk + 1) * 4, :], z_ap.rearrange("p (h d) -> p h d", h=4), reads=[z_b],
                  writes=[k.b_scr["ZS"]])
    p.dma(wba, wsrc[:, :, 4096:4112], writes=[b_wba], q="pool")
    for t in range(NT):
        pb, bpb = k.ps.next()
        for kt in range(16):
            MM(k, pb[:, 0:16], hT[:, kt, t * 128:(t + 1) * 128], wba[:, kt, :], kt == 0, kt == 15, [b_wba, b_hT], [bpb])
        CP(k, "dve", k.BA[:, t, :], pb[:, 0:16], [bpb], [k.b_BA])
    albc = small[:, 0:8]
    dtbc = small[:, 8:16]
    nea = small[:, 16:24]
    p.dma(albc, I["gdn_a_log"].partition_broadcast(128), writes=[b_small])
    p.dma(dtbc, I["gdn_dt_bias"].partition_broadcast(128), writes=[b_small])
    ACT(k, nea, albc, AF.Exp, [b_small], [b_small])
    TS(k, "dve", nea, nea, -1.0, None, ALU.mult, ALU.bypass, [b_small], [b_small])
    ACT(k, k.BETA[:], k.BA[:, :, 0:8], AF.Sigmoid, [k.b_BA], [k.b_bg])
    xa = ar.alloc((16, 8), F32)
    xb = ar.alloc((16, 8), F32)
    b_xa = p.buf()
    TT(k, "dve", xa, k.BA[:, :, 8:16], dtbc.unsqueeze(1).broadcast_to([128, 16, 8]), ALU.add, [k.b_BA, b_small], [b_xa])
    ACT(k, xb, xa, AF.Abs, [b_xa], [b_xa])
    ACT(k, xb, xb, AF.Exp, [b_xa], [b_xa], scale=-1.0)
    ACT(k, xb, xb, AF.Ln, [b_xa], [b_xa], bias=1.0)
    TS(k, "dve", xa, xa, 0.0, None, ALU.max, ALU.bypass, [b_xa], [b_xa])
    TT(k, "dve", xa, xa, xb, ALU.add, [b_xa], [b_xa])
    TT(k, "dve", k.GG[:], xa, nea.unsqueeze(1).broadcast_to([128, 16, 8]), ALU.mult, [b_xa, b_small], [k.b_bg])

    for blk in range(4):
        wap, wb = load_w_block(k, wring, wsrc[:, :, 4112 + blk * 512:4112 + (blk + 1) * 512])
        for f in range(4):
            fb = blk * 4 + f
            kind = fb // 8
            hd = fb % 8
            o_ap, o_b = obf[oi % 2], b_obf[oi % 2]
            oi += 1

            def evac_o(pb, bpb, tok0, o_ap=o_ap, o_b=o_b, c=[0]):
                CP(k, "act" if (tok0 // 512) % 2 else "dve", o_ap[:, tok0:tok0 + 512], pb[:], [bpb], [o_b])
            fblock(wap, wb, f, evac_o)
            dst = k.QB if kind == 0 else k.KB
            p.dma(dst[hd], o_ap, reads=[o_b], writes=[k.b_scr["QB" if kind == 0 else "KB"]])
    for blk in range(2):
        wap, wb = load_w_block(k, wring, wsrc[:, :, 6160 + blk * 512:6160 + (blk + 1) * 512])
        for t in range(NT):
            pb, bpb = k.ps.next()
            for kt in range(16):
                MM(k, pb[:], hT[:, kt, t * 128:(t + 1) * 128], wap[:, kt, :], kt == 0, kt == 15, [wb, b_hT], [bpb])
            z_ap, z_b = zst[zi % 3], b_zst[zi % 3]
            zi += 1
            CP(k, "act" if t % 2 else "dve", z_ap, pb[:], [bpb], [z_b])
            p.dma(k.VB[blk * 4:(blk + 1) * 4, :, t, :].rearrange("h p d -> p h d"),
                  z_ap.rearrange("p (h d) -> p h d", h=4), reads=[z_b], writes=[k.b_scr["VB"]])
    if hasattr(k, "DBG"):
        p.dma(k.DBG[:, 0:128], k.BETA[:].rearrange("p a b -> p (a b)"), reads=[k.b_bg], writes=[k.b_scr["DBG"]])
        p.dma(k.DBG[:, 128:256], k.GG[:].rearrange("p a b -> p (a b)"), reads=[k.b_bg], writes=[k.b_scr["DBG"]])


def phase_gdn(k):
    p = k.p
    ar = k.ar
    I = k.I
    ar.reset()
    oT = ar.alloc((16, S), BF16)
    k.oT = oT
    k.b_oT = p.buf("oT")
    k.oT_mark = ar.off
    S32 = ar.alloc((8, 128), F32)
    S16 = ar.alloc((8, 128), BF16)
    b_S32 = p.bufs(2)
    b_S16 = p.bufs(2)
    p.op("pool", "memset", writes=b_S32, ap=S32, constant=0.0)
    p.op("pool", "memset", writes=b_S16, ap=S16, constant=0.0)
    gnb = ar.alloc((128,), F32)
    b_gnb = p.buf()
    p.dma(gnb, I["gdn_norm_g"].partition_broadcast(128), writes=[b_gnb])
    names = ("QA", "KA", "KT", "VA", "ZS")
    inr = Ring([([ar.alloc((8, 128), BF16) for _ in names], p.bufs(5)) for _ in range(2)])
    sm = Ring([(ar.alloc((128,), F32), p.buf()) for _ in range(2)])
    kbg_, vb_, kdec_, zg_ = [ar.alloc((8, 128), BF16) for _ in range(4)]
    b_kbg, b_vb, b_kdec, b_zg = p.bufs(4)

    class G:
        pass
    grp = []
    for g in range(2):
        G_ = G()
        G_.diagG = ar.alloc((4, 128), F32)
        G_.decQ = ar.alloc((4, 128), BF16)
        G_.decA = ar.alloc((4, 128), BF16)
        G_.A = [ar.alloc((4, 128), BF16) for _ in range(2)]
        G_.B = [ar.alloc((4, 128), BF16) for _ in range(2)]
        G_.P = [ar.alloc((4, 128), BF16) for _ in range(2)]
        G_.qk = ar.alloc((4, 128), BF16)
        G_.qkT = ar.alloc((4, 128), BF16)
        G_.wT = ar.alloc((4, 128), BF16)
        G_.u = ar.alloc((4, 128), F32)
        G_.vnew = ar.alloc((4, 128), BF16)
        G_.o1 = ar.alloc((4, 128), F32)
        G_.o = ar.alloc((4, 128), F32)
        G_.sq = ar.alloc((4, 128), F32)
        G_.fin = ar.alloc((4, 128), BF16)
        G_.ss = ar.alloc((4,), F32)
        for nm in ("diagG", "decQ", "decA", "qk", "qkT", "wT", "u", "vnew", "o1", "o", "sq", "fin", "ss"):
            setattr(G_, "b_" + nm, p.buf())
        G_.b_A = p.bufs(2)
        G_.b_B = p.bufs(2)
        G_.b_P = p.bufs(2)
        grp.append(G_)

    def v512(ap):
        return ap.rearrange("p a b -> p (a b)")

    for c in range(16):
        (qT, kT, ktok, vtok, zs), bi = inr.next()
        b_qT, b_kT, b_ktok, b_vtok, b_zs = bi
        for ap, b, nm in zip((qT, kT, ktok, vtok, zs), bi, names):
            p.dma(ap, getattr(k, nm)[:, c, :, :], reads=[k.b_scr[nm]], writes=[b])
        smt, b_sm = sm.next()
        gc = smt[:, 0:8]
        e24 = smt[:, 8:32]
        eg = smt[:, 8:16]
        egrev = smt[:, 16:24]
        gl = smt[:, 24:32]
        bexp = smt[:, 32:40]
        gcl = smt[:, 40:48]
        pbA, bpA = k.ps.next()
        gsrc = k.GG[:, c, :]
        MM(k, pbA[:, 0:8], k.trif, gsrc, True, True, [k.b_c, k.b_bg], [bpA])
        MM(k, pbA[:, 8:16], k.suf, gsrc, True, True, [k.b_c, k.b_bg], [bpA])
        MM(k, pbA[:, 16:24], k.onesf, gsrc, True, True, [k.b_c, k.b_bg], [bpA])
        CP(k, "dve", gc, pbA[:, 0:8], [bpA], [b_sm])
        ACT(k, e24, pbA[:, 0:24], AF.Exp, [bpA], [b_sm])
        TT(k, "dve", bexp, k.BETA[:, c, :], eg, ALU.mult, [k.b_bg, b_sm], [b_sm])
        ACT(k, gcl, k.BETA[:, c, :], AF.Ln, [k.b_bg], [b_sm])
        TT(k, "dve", gcl, gcl, gc, ALU.add, [b_sm], [b_sm])
        bc = lambda a: a.unsqueeze(2).broadcast_to([128, 8, 128])
        TT(k, "pool", kbg_, ktok, bc(bexp), ALU.mult, [b_ktok, b_sm], [b_kbg])
        TT(k, "pool", vb_, vtok, bc(k.BETA[:, c, :]), ALU.mult, [b_vtok, k.b_bg], [b_vb])
        TT(k, "pool", kdec_, ktok, bc(egrev), ALU.mult, [b_ktok, b_sm], [b_kdec])
        TT(k, "pool", zg_, zs, gnb.unsqueeze(1).broadcast_to([128, 8, 128]), ALU.mult, [b_zs, b_gnb], [b_zg])
        bc4 = lambda a: a.unsqueeze(2).broadcast_to([128, 4, 128])
        for g, G_ in enumerate(grp):
            hs = slice(4 * g, 4 * g + 4)
            TT(k, "dve", G_.diagG, k.identf.unsqueeze(1).broadcast_to([128, 4, 128]), bc4(gc[:, hs]), ALU.mult,
               [k.b_c, b_sm], [G_.b_diagG])
        for g, G_ in enumerate(grp):
            for which, um, dec, b_dec, bias in ((0, k.umq, G_.decQ, G_.b_decQ, gc), (1, k.uma, G_.decA, G_.b_decA, gcl)):
                pR, bpR = k.ps.next()
                for hh in range(4):
                    MM(k, pR[:, hh * 128:(hh + 1) * 128], k.onesf, G_.diagG[:, hh, :], True, False,
                       [k.b_c, G_.b_diagG], [bpR])
                    MM(k, pR[:, hh * 128:(hh + 1) * 128], k.ident, um, False, True, [k.b_c], [bpR])
                for hh in range(4):
                    h = 4 * g + hh
                    ACT(k, dec[:, hh, :], pR[:, hh * 128:(hh + 1) * 128], AF.Exp, [bpR, b_sm], [b_dec],
                        scale=-1.0, bias=bias[:, h:h + 1])
        for g, G_ in enumerate(grp):
            pKK, bKK = k.ps.next()
            pQK, bQK = k.ps.next()
            for hh in range(4):
                h = 4 * g + hh
                MM(k, pKK[:, hh * 128:(hh + 1) * 128], kT[:, h, :], kT[:, h, :], True, True, [b_kT], [bKK])
                MM(k, pQK[:, hh * 128:(hh + 1) * 128], qT[:, h, :], kT[:, h, :], True, True, [b_qT, b_kT], [bQK])
            TT(k, "dve", v512(G_.A[0]), pKK[:], v512(G_.decA), ALU.mult, [bKK, G_.b_decA], [G_.b_A[0]])
            TT(k, "dve", v512(G_.qk), pQK[:], v512(G_.decQ), ALU.mult, [bQK, G_.b_decQ], [G_.b_qk])
        for g, G_ in enumerate(grp):
            for src, bsrc, dst, bdst, eng in ((G_.A[0], G_.b_A[0], G_.B[0], G_.b_B[0], "act"),
                                              (G_.qk, G_.b_qk, G_.qkT, G_.b_qkT, "dve")):
                pT, bpT = k.ps.next()
                pTv = pT[:].bitcast(BF16)
                for hh in range(4):
                    TR(k, pTv[:, hh * 128:(hh + 1) * 128], src[:, hh, :], k.ident, [bsrc], [bpT])
                CP(k, eng, v512(dst), pTv[:, 0:512], [bpT], [bdst])
            TT(k, "pool", G_.P[0], k.ident.unsqueeze(1).broadcast_to([128, 4, 128]), G_.B[0], ALU.subtract,
               [k.b_c, G_.b_B[0]], [G_.b_P[0]])
        for lvl in range(1, 7):
            cur = (lvl - 1) % 2
            nxt = lvl % 2
            for g, G_ in enumerate(grp):
                pA, bA = k.ps.next()
                for hh in range(4):
                    MM(k, pA[:, hh * 128:(hh + 1) * 128], G_.B[cur][:, hh, :], G_.A[cur][:, hh, :], True, True,
                       [G_.b_A[cur], G_.b_B[cur]], [bA])
                if lvl <= 5:
                    pB, bB = k.ps.next()
                    for hh in range(4):
                        MM(k, pB[:, hh * 128:(hh + 1) * 128], G_.A[cur][:, hh, :], G_.B[cur][:, hh, :], True, True,
                           [G_.b_A[cur], G_.b_B[cur]], [bB])
                CP(k, "act", v512(G_.A[nxt]), pA[:], [bA], [G_.b_A[nxt]])
                if lvl <= 5:
                    CP(k, "dve", v512(G_.B[nxt]), pB[:], [bB], [G_.b_B[nxt]])
            for g, G_ in enumerate(grp):
                pP, bP = k.ps.next()
                for hh in range(4):
                    MM(k, pP[:, hh * 128:(hh + 1) * 128], k.ident, G_.P[cur][:, hh, :], True, False,
                       [k.b_c, G_.b_P[cur]], [bP])
                    MM(k, pP[:, hh * 128:(hh + 1) * 128], G_.A[nxt][:, hh, :], G_.P[cur][:, hh, :], False, True,
                       [G_.b_A[nxt], G_.b_P[cur]], [bP])
                CP(k, "dve" if g else "act", v512(G_.P[nxt]), pP[:], [bP], [G_.b_P[nxt]])
        for g, G_ in enumerate(grp):
            TTm = G_.P[0]
            b_TT = G_.b_P[0]
            pW, bW = k.ps.next()
            pU, bU = k.ps.next()
            for hh in range(4):
                h = 4 * g + hh
                MM(k, pW[:, hh * 128:(hh + 1) * 128], kbg_[:, h, :], TTm[:, hh, :], True, True, [b_kbg, b_TT], [bW])
                MM(k, pU[:, hh * 128:(hh + 1) * 128], TTm[:, hh, :], vb_[:, h, :], True, True, [b_vb, b_TT], [bU])
            CP(k, "act", v512(G_.wT), pW[:], [bW], [G_.b_wT])
            CP(k, "dve", v512(G_.u), pU[:], [bU], [G_.b_u])
        for g, G_ in enumerate(grp):
            pVN, bVN = k.ps.next()
            pO1, bO1 = k.ps.next()
            for hh in range(4):
                h = 4 * g + hh
                MM(k, pVN[:, hh * 128:(hh + 1) * 128], G_.wT[:, hh, :], S16[:, h, :], True, True,
                   [G_.b_wT, b_S16[g]], [bVN])
                MM(k, pO1[:, hh * 128:(hh + 1) * 128], qT[:, h, :], S16[:, h, :], True, True, [b_qT, b_S16[g]], [bO1])
            TT(k, "dve", v512(G_.vnew), v512(G_.u), pVN[:], ALU.subtract, [G_.b_u, bVN], [G_.b_vnew])
            TT(k, "dve", G_.o1, pO1[:].rearrange("p (a b) -> p a b", a=4), bc4(eg[:, 4 * g:4 * g + 4]), ALU.mult,
               [bO1, b_sm], [G_.b_o1])
            TT(k, "pool", S32[:, 4 * g:4 * g + 4, :], S32[:, 4 * g:4 * g + 4, :], bc4(gl[:, 4 * g:4 * g + 4]), ALU.mult,
               [b_sm, b_S32[g]], [b_S32[g]])
        for g, G_ in enumerate(grp):
            pO2, bO2 = k.ps.next()
            pSU, bSU = k.ps.next()
            for hh in range(4):
                h = 4 * g + hh
                MM(k, pO2[:, hh * 128:(hh + 1) * 128], G_.qkT[:, hh, :], G_.vnew[:, hh, :], True, True,
                   [G_.b_qkT, G_.b_vnew], [bO2])
                MM(k, pSU[:, hh * 128:(hh + 1) * 128], kdec_[:, h, :], G_.vnew[:, hh, :], True, True,
                   [b_kdec, G_.b_vnew], [bSU])
            TT(k, "dve", v512(S32[:, 4 * g:4 * g + 4, :]), v512(S32[:, 4 * g:4 * g + 4, :]), pSU[:], ALU.add,
               [b_S32[g], bSU], [b_S32[g]])
            CP(k, "act", S16[:, 4 * g:4 * g + 4, :], S32[:, 4 * g:4 * g + 4, :], [b_S32[g]], [b_S16[g]])
            TT(k, "dve", v512(G_.o), pO2[:], v512(G_.o1), ALU.add, [bO2, G_.b_o1], [G_.b_o])
        for g, G_ in enumerate(grp):
            TT(k, "pool", G_.sq, G_.o, G_.o, ALU.mult, [G_.b_o], [G_.b_sq])
            p.op("dve", "tensor_reduce", reads=[G_.b_sq], writes=[G_.b_ss], out=G_.ss, in_=G_.sq, axis=AX.X, op=ALU.add)
            ACT(k, G_.ss, G_.ss, AF.Sqrt, [G_.b_ss], [G_.b_ss], scale=1.0 / 128, bias=EPS)
            p.op("dve", "reciprocal", reads=[G_.b_ss], writes=[G_.b_ss], out=G_.ss, in_=G_.ss)
            TT(k, "dve", G_.sq, G_.o, bc4(G_.ss), ALU.mult, [G_.b_o, G_.b_ss, G_.b_sq], [G_.b_sq])
            TT(k, "pool", G_.fin, G_.sq, zg_[:, 4 * g:4 * g + 4, :], ALU.mult, [G_.b_sq, b_zg], [G_.b_fin])
            pT, bpT = k.ps.next()
            pTv = pT[:].bitcast(BF16)
            for hh in range(4):
                TR(k, pTv[:, hh * 128:(hh + 1) * 128], G_.fin[:, hh, :], k.ident, [G_.b_fin], [bpT])
            CP(k, "act", oT[:, 4 * g:4 * g + 4, c * 128:(c + 1) * 128],
               pTv[:, 0:512].rearrange("p (a b) -> p a b", a=4), [bpT], [k.b_oT])


def phase_moba(k):
    p = k.p
    ar = k.ar
    I = k.I
    ar.off = k.oT_mark
    oT = k.oT
    hr = Ring([([ar.alloc((S,), BF16), ar.alloc((S,), BF16), ar.alloc((16, 132), BF16)], p.bufs(3)) for _ in range(2)])
    for (aps, bs) in hr.items:
        p.op("pool", "memset", writes=[bs[2]], ap=aps[2][:, :, 128:132], constant=1.0)
    tg = ar.alloc((8, 256), F32)
    Tp = ar.alloc((8, 256), BF16)
    rbb = ar.alloc((256,), F32)
    bmax = ar.alloc((8,), F32)
    mgb = ar.alloc((128,), F32)
    b_tp = p.buf()
    p.dma(tg, I["rb_toep"], writes=[b_tp])
    p.dma(rbb, I["rel_bias"].partition_broadcast(128), writes=[b_tp])
    p.dma(mgb, I["moba_norm_g"].partition_broadcast(128), writes=[b_tp])
    rb31 = rbb[:, 31 * 8:32 * 8]
    TT(k, "dve", tg, tg, rb31.unsqueeze(2).broadcast_to([128, 8, 256]), ALU.subtract, [b_tp], [b_tp])
    STT(k, "dve", Tp, tg, SQ128, k.cm.unsqueeze(1).broadcast_to([128, 8, 256]), ALU.mult, ALU.add, [b_tp, k.b_c], [b_tp])
    p.op("dve", "tensor_reduce", reads=[b_tp], writes=[b_tp], out=bmax, in_=rbb.rearrange("p (b h) -> p h b", h=8),
         axis=AX.X, op=ALU.max)
    TT(k, "dve", bmax, bmax, rb31, ALU.subtract, [b_tp], [b_tp])
    sq = ar.alloc((S,), BF16)
    ksq = ar.alloc((S,), BF16)
    b_sq, b_ksq = p.bufs(2)
    sm = ar.alloc((256,), F32)
    b_sm = p.buf()
    kms = sm[:, 0:8]
    km4 = sm[:, 8:12]
    ksc = sm[:, 12:13]
    nm = sm[:, 16:32]
    gsb = sm[:, 32:96].rearrange("p (a b) -> p a b", a=8)
    g2 = sm[:, 96:160].rearrange("p (a b) -> p a b", a=8)
    eq = sm[:, 160:224].rearrange("p (a b) -> p a b", a=8)
    mx = sm[:, 224:232]
    kmT = ar.alloc((8,), BF16)
    MBq = ar.alloc((16, 16), BF16)
    b_MBq = p.buf()
    p.op("pool", "memset", writes=[b_MBq], ap=MBq, constant=0.0)
    MB = ar.alloc((S,), BF16)
    b_MB = p.buf()
    PTr = Ring([(ar.alloc((256,), BF16), p.buf()) for _ in range(5)])
    yq = [ar.alloc((128,), F32) for _ in range(4)]
    b_yq = p.bufs(4)
    junk = ar.alloc((128,), BF16)
    b_junk = p.buf()
    fin = [ar.alloc((256,), BF16) for _ in range(2)]
    b_fin = p.bufs(2)
    sm2 = Ring([(ar.alloc((8,), F32), p.buf()) for _ in range(8)])
    bc8 = lambda a: a.unsqueeze(2).broadcast_to([128, 8, 8])

    for h in range(8):
        (qbT, kbT, vb1), (b_q, b_k, b_v) = hr.next()
        p.dma(qbT, k.QB[h], reads=[k.b_scr["QB"]], writes=[b_q])
        p.dma(kbT, k.KB[h], reads=[k.b_scr["KB"]], writes=[b_k])
        p.dma(vb1[:, :, 0:128], k.VB[h], reads=[k.b_scr["VB"]], writes=[b_v])
        ACT(k, sq, qbT, AF.Square, [b_q], [b_sq])
        ACT(k, ksq, kbT, AF.Square, [b_k], [b_ksq])
        p.op("dve", "tensor_reduce", reads=[b_k], writes=[b_sm], out=kms, in_=kbT.rearrange("p (n t) -> p n t", n=8),
             axis=AX.X, op=ALU.add)
        TS(k, "dve", kmT, kms, 1.0 / 256, None, ALU.mult, None, [b_sm], [b_sm])
        pG, bG = k.ps.next()
        for qi in range(8, 16):
            MM(k, pG[:, (qi - 8) * 8:(qi - 7) * 8], qbT[:, qi * 128:(qi + 1) * 128], kmT, True, True, [b_q, b_sm], [bG])
        pN, bN = k.ps.next()
        for qi in range(16):
            MM(k, pN[:, qi:qi + 1], sq[:, qi * 128:(qi + 1) * 128], k.ones[:, 0:1], True, True, [b_sq, k.b_c], [bN])
        for c in range(4):
            pK, bK = k.ps.next()
            MM(k, pK[:], k.ones, ksq[:, c * 512:(c + 1) * 512], True, True, [b_ksq, k.b_c], [bK])
            p.op("dve", "tensor_reduce", reads=[bK], writes=[b_sm], out=km4[:, c:c + 1], in_=pK[:], axis=AX.X, op=ALU.max)
        p.op("dve", "tensor_reduce", reads=[b_sm], writes=[b_sm], out=ksc, in_=km4, axis=AX.X, op=ALU.max)
        TS(k, "dve", ksc, ksc, 1.0 / 128, None, ALU.mult, None, [b_sm], [b_sm])
        ACT(k, nm, pN[:, 0:16], AF.Sqrt, [bN, b_sm], [b_sm], scale=ksc)
        TS(k, "dve", MBq[:, :, 8], nm, bmax[:, h:h + 1], -SQ128, ALU.add, ALU.mult, [b_sm, b_tp], [b_MBq])
        TT(k, "dve", gsb, pG[:, 0:64].rearrange("p (a b) -> p a b", a=8), k.gm.rearrange("p (a b) -> p a b", a=8),
           ALU.add, [bG, k.b_c], [b_sm])
        src = gsb
        for it in range(2):
            p.op("dve", "tensor_reduce", reads=[b_sm], writes=[b_sm], out=mx, in_=src, axis=AX.X, op=ALU.max)
            TT(k, "dve", eq, src, bc8(mx), ALU.is_equal, [b_sm], [b_sm])
            STT(k, "dve", g2, eq, -1e30, src, ALU.mult, ALU.add, [b_sm], [b_sm])
            src = g2
        p.op("dve", "tensor_reduce", reads=[b_sm], writes=[b_sm], out=mx, in_=g2, axis=AX.X, op=ALU.max)
        TT(k, "dve", eq, gsb, bc8(mx), ALU.is_ge, [b_sm], [b_sm])
        TS(k, "dve", MBq[:, 8:16, 0:8], eq, BIG * SQ128, -BIG * SQ128, ALU.mult, ALU.add, [b_sm], [b_MBq])
        for half in range(2):
            pM, bM = k.ps.next()
            pMv = pM[:].bitcast(BF16)
            for j in range(8):
                qi = half * 8 + j
                TR(k, pMv[0:9, j * 128:(j + 1) * 128], MBq[:, qi, 0:9], k.ident, [b_MBq], [bM])
            CP(k, "act" if half else "dve", MB[0:9, half * 1024:(half + 1) * 1024], pMv[0:9, 0:1024], [bM], [b_MB])
        deferred = []
        for qb in range(8):
            pend = []

            def emit_pv(item):
                kj_, PT_, b_PT_ = item
                for qt in range(2):
                    if kj_ <= 2 * qb + qt:
                        pO, bO = k.psx[qt]
                        MM(k, pO[:, 0:129], PT_[:, qt * 128:(qt + 1) * 128], vb1[:, kj_, 0:129],
                           kj_ == 0, kj_ == 2 * qb + qt, [b_PT_, b_v], [bO])
            for kj in range(2 * qb + 2):
                n = kj // 2
                pS, bS = k.ps.next()
                q0 = qb * 256
                v = n if (qb >= 4 and n < qb) else 8
                if n == qb and kj % 2 == 0:
                    segs = [(0, 256, Tp[:, h, 0:256])]
                elif n == qb:
                    segs = [(128, 256, Tp[:, h, 0:128])]
                elif n == qb - 1 and kj % 2 == 1:
                    segs = [(0, 128, Tp[:, h, 128:256]), (128, 256, None)]
                else:
                    segs = [(0, 256, None)]
                c0 = segs[0][0]
                for (a, b, bias) in segs:
                    MM(k, pS[:, a:b], kbT[:, kj * 128:(kj + 1) * 128], qbT[:, q0 + a:q0 + b], True, False, [b_k, b_q], [bS])
                    MM(k, pS[:, a:b], k.emat[:, v, :], MB[0:9, q0 + a:q0 + b], False, bias is None, [k.b_c, b_MB], [bS])
                    if bias is not None:
                        MM(k, pS[:, a:b], k.ident, bias, False, True, [k.b_c, b_tp], [bS])
                PT, b_PT = PTr.next()
                ACT(k, PT[:, c0:256], pS[:, c0:256], AF.Exp, [bS], [b_PT], scale=1.0 / SQ128)
                pend.append((kj, PT, b_PT))
                if len(pend) > 2:
                    emit_pv(pend.pop(0))
                if kj == min(3, 2 * qb + 1) and deferred:
                    deferred.pop(0)()
            while pend:
                emit_pv(pend.pop(0))
            parts = []
            for qt in range(2):
                s2, b_s2 = sm2.next()
                y_ap, y_b = yq[(qb % 2) * 2 + qt], b_yq[(qb % 2) * 2 + qt]
                pO, bO = k.psx[qt]
                p.op("dve", "reciprocal", reads=[bO], writes=[b_s2], out=s2[:, 0:1], in_=pO[:, 128:129])
                TS(k, "dve", y_ap, pO[:, 0:128], s2[:, 0:1], None, ALU.mult, None, [bO, b_s2], [y_b])
                parts.append((s2, b_s2, y_ap, y_b))

            def part2(qb=qb, parts=parts):
                f_ap, f_b = fin[qb % 2], b_fin[qb % 2]
                for qt in range(2):
                    s2, b_s2, y_ap, y_b = parts[qt]
                    ACT(k, junk, y_ap, AF.Square, [y_b], [b_junk, b_s2], accum_out=s2[:, 1:2])
                    ACT(k, s2[:, 1:2], s2[:, 1:2], AF.Sqrt, [b_s2], [b_s2], scale=1.0 / 128, bias=EPS)
                    p.op("dve", "reciprocal", reads=[b_s2], writes=[b_s2], out=s2[:, 1:2], in_=s2[:, 1:2])
                    STT(k, "dve", f_ap[:, qt * 128:(qt + 1) * 128], y_ap, s2[:, 1:2], mgb, ALU.mult, ALU.mult,
                        [y_b, b_s2, b_tp], [f_b])
                pT, bpT = k.ps.next()
                pTv = pT[:].bitcast(BF16)
                for qt in range(2):
                    TR(k, pTv[:, qt * 128:(qt + 1) * 128], f_ap[:, qt * 128:(qt + 1) * 128], k.ident, [f_b], [bpT])
                CP(k, "act", oT[:, 8 + h, qb * 256:(qb + 1) * 256], pTv[:, 0:256], [bpT], [k.b_oT])
            deferred.append(part2)
        while deferred:
            deferred.pop(0)()
    if hasattr(k, "OT"):
        p.dma(k.OT, oT, reads=[k.b_oT], writes=[k.b_scr["DBG"]])


def phase_wout(k):
    p = k.p
    ar = k.ar
    I = k.I
    oT = k.oT
    ar.off = k.oT_mark
    h2T = ar.alloc((16, S), BF16)
    k.h2T = h2T
    k.b_h2T = p.buf("h2T")
    k.ws_mark = ar.off
    wring = Ring([(ar.alloc((16, 512), BF16), p.buf()) for _ in range(2)])
    xr = Ring([(ar.alloc((512,), F32), p.buf()) for _ in range(3)])
    wsrc = I["w_out"].rearrange("(kt q) n -> q kt n", q=128)
    for c in range(4):
        wap, wb = load_w_block(k, wring, wsrc[:, :, c * 512:(c + 1) * 512])
        for t in range(NT):
            x_ap, x_b = xr.next()
            p.dma(x_ap, I["x"][t * 128:(t + 1) * 128, c * 512:(c + 1) * 512], writes=[x_b])
            pb, bpb = k.ps.next()
            for kt in range(16):
                MM(k, pb[:], oT[:, kt, t * 128:(t + 1) * 128], wap[:, kt, :], kt == 0, kt == 15, [wb, k.b_oT], [bpb])
            TT(k, "dve", x_ap, pb[:], x_ap, ALU.add, [bpb, x_b], [x_b])
            p.dma(k.X1[t * 128:(t + 1) * 128, c * 512:(c + 1) * 512], x_ap, reads=[x_b], writes=[k.b_scr["X1"]])
    p.barrier()
    ar.off = k.ws_mark
    tiles = [(k.X1[t * 128:(t + 1) * 128, :], [k.b_scr["X1"]]) for t in range(NT)]
    norm_T(k, tiles, I["xattn_norm_g"], h2T, k.b_h2T)


def phase_xattn(k):
    p = k.p
    ar = k.ar
    I = k.I
    h2T = k.h2T
    ar.off = 0
    memT = ar.alloc((16, MEM), BF16)
    b_memT = p.buf()
    kxT = ar.alloc((4, MEM), BF16)
    vx = ar.alloc((2, 512), BF16)
    b_kv = p.buf()
    qxT = ar.alloc((4, S), BF16)
    b_qx = p.buf()
    oxT = ar.alloc((4, S), BF16)
    b_ox = p.buf()
    wxo = ar.alloc((4, D), BF16)
    b_wxo = p.buf()
    assert ar.off <= k.oT_mark
    ar.off = k.ws_mark
    tiles = [(I["mem"][t * 128:(t + 1) * 128, :], []) for t in range(2)]
    norm_T(k, tiles, I["mem_norm_g"], memT, b_memT)
    p.barrier()
    ar.off = k.ws_mark
    wring = Ring([(ar.alloc((16, 512), BF16), p.buf()) for _ in range(2)])
    Pf = Ring([(ar.alloc((256,), F32), p.buf()) for _ in range(4)])
    Pn = Ring([(ar.alloc((256,), BF16), p.buf()) for _ in range(4)])
    PnT = Ring([(ar.alloc((2, 128), BF16), p.buf()) for _ in range(4)])
    sm = Ring([(ar.alloc((8,), F32), p.buf()) for _ in range(8)])
    xr = Ring([(ar.alloc((512,), F32), p.buf()) for _ in range(3)])
    wkv = I["w_xkv"].rearrange("(kt q) n -> q kt n", q=128)
    wap, wb = load_w_block(k, wring, wkv[:, :, 0:512])
    for hx in range(4):
        pb, bpb = k.ps.next()
        for kt in range(16):
            MM(k, pb[:, 0:MEM], wap[:, kt, hx * 128:(hx + 1) * 128], memT[:, kt, :], kt == 0, kt == 15, [wb, b_memT], [bpb])
        CP(k, "act" if hx % 2 else "dve", kxT[:, hx, :], pb[:, 0:MEM], [bpb], [b_kv])
    wap, wb = load_w_block(k, wring, wkv[:, :, 512:1024])
    for mt in range(2):
        pb, bpb = k.ps.next()
        for kt in range(16):
            MM(k, pb[:], memT[:, kt, mt * 128:(mt + 1) * 128], wap[:, kt, :], kt == 0, kt == 15, [wb, b_memT], [bpb])
        CP(k, "act" if mt else "dve", vx[:, mt, :], pb[:], [bpb], [b_kv])
    wq = I["w_xq"].rearrange("(kt q) n -> q kt n", q=128)
    wap, wb = load_w_block(k, wring, wq)
    for hx in range(4):
        for c in range(4):
            pb, bpb = k.ps.next()
            for kt in range(16):
                MM(k, pb[:], wap[:, kt, hx * 128:(hx + 1) * 128], h2T[:, kt, c * 512:(c + 1) * 512], kt == 0, kt == 15,
                   [wb, k.b_h2T], [bpb])
            CP(k, "act" if c % 2 else "dve", qxT[:, hx, c * 512:(c + 1) * 512], pb[:], [bpb], [b_qx])
    for t in range(NT):
        st = []
        for hx in range(4):
            pS, bS = k.ps.next()
            MM(k, pS[:, 0:MEM], qxT[:, hx, t * 128:(t + 1) * 128], kxT[:, hx, :], True, True, [b_qx, b_kv], [bS])
            s_ap, s_b = sm.next()
            st.append([pS, bS, s_ap, s_b])
        for hx in range(4):
            pS, bS, s_ap, s_b = st[hx]
            p.op("dve", "tensor_reduce", reads=[bS], writes=[s_b], out=s_ap[:, 0:1], in_=pS[:, 0:MEM], axis=AX.X, op=ALU.max)
            TS(k, "dve", s_ap[:, 1:2], s_ap[:, 0:1], -1.0 / SQ128, None, ALU.mult, None, [s_b], [s_b])
        for hx in range(4):
            pS, bS, s_ap, s_b = st[hx]
            pf, b_pf = Pf.next()
            ACT(k, pf, pS[:, 0:MEM], AF.Exp, [bS, s_b], [b_pf, s_b], scale=1.0 / SQ128, bias=s_ap[:, 1:2],
                accum_out=s_ap[:, 2:3])
            st[hx] += [pf, b_pf]
        for hx in range(4):
            pS, bS, s_ap, s_b, pf, b_pf = st[hx]
            p.op("dve", "reciprocal", reads=[s_b], writes=[s_b], out=s_ap[:, 3:4], in_=s_ap[:, 2:3])
            pn, b_pn = Pn.next()
            TS(k, "dve", pn, pf, s_ap[:, 3:4], None, ALU.mult, None, [b_pf, s_b], [b_pn])
            st[hx] += [pn, b_pn]
        for hx in range(4):
            pn, b_pn = st[hx][6], st[hx][7]
            pT, bpT = k.ps.next()
            pTv = pT[:].bitcast(BF16)
            for mt in range(2):
                TR(k, pTv[:, mt * 128:(mt + 1) * 128], pn[:, mt * 128:(mt + 1) * 128], k.ident, [b_pn], [bpT])
            pnt, b_pnt = PnT.next()
            CP(k, "act" if hx % 2 else "dve", pnt, pTv[:, 0:256].rearrange("p (a b) -> p a b", a=2), [bpT], [b_pnt])
            st[hx] += [pnt, b_pnt]
        for hx in range(4):
            pnt, b_pnt = st[hx][8], st[hx][9]
            pO, bO = k.ps.next()
            for mt in range(2):
                MM(k, pO[:, 0:128], vx[:, mt, hx * 128:(hx + 1) * 128], pnt[:, mt, :], mt == 0, mt == 1, [b_kv, b_pnt], [bO])
            CP(k, "dve" if hx % 2 else "act", oxT[:, hx, t * 128:(t + 1) * 128], pO[:, 0:128], [bO], [b_ox])
    p.dma(wxo, I["w_xo"].rearrange("(kt q) n -> q kt n", q=128), writes=[b_wxo], q="pool")
    for t in range(NT):
        for c in range(4):
            x_ap, x_b = xr.next()
            p.dma(x_ap, k.X1[t * 128:(t + 1) * 128, c * 512:(c + 1) * 512], reads=[k.b_scr["X1"]], writes=[x_b])
            pb, bpb = k.ps.next()
            for kt in range(4):
                MM(k, pb[:], oxT[:, kt, t * 128:(t + 1) * 128], wxo[:, kt, c * 512:(c + 1) * 512], kt == 0, kt == 3,
                   [b_wxo, b_ox], [bpb])
            TT(k, "dve", x_ap, pb[:], x_ap, ALU.add, [bpb, x_b], [x_b])
            p.dma(k.X2[t * 128:(t + 1) * 128, c * 512:(c + 1) * 512], x_ap, reads=[x_b], writes=[k.b_scr["X2"]])


def phase_ffn(k):
    p = k.p
    ar = k.ar
    I = k.I
    HT = 1024
    halo = p.sbuf("halo", [128, NFF, 2], F32)
    b_halo = p.buf()
    p.op("pool", "memset", writes=[b_halo], ap=halo[:], constant=0.0)
    cwf = p.sbuf("cwf", [128, 4 * NFF], F32)
    b_cwf = p.buf()
    for hf in range(2):
        ar.reset()
        aT = ar.alloc((NFF, HT), BF16)
        b_aT = p.buf()
        mark0 = ar.off
        h3T = ar.alloc((16, HT), BF16)
        b_h3T = p.buf()
        mark1 = ar.off
        tiles = [(k.X2[(hf * 8 + t) * 128:(hf * 8 + t + 1) * 128, :], [k.b_scr["X2"]]) for t in range(8)]
        norm_T(k, tiles, I["ffn_norm_g"], h3T, b_h3T)
        p.barrier()
        ar.off = mark1
        if hf == 0:
            stg = ar.alloc((128,), F32)
            b_stg = p.buf()
            for j in range(4):
                src = (I["ffn_conv_w"][j] if j < 3 else I["ffn_conv_b"][0]).rearrange("(b q) -> b q", q=128)
                p.dma(stg[0:NFF, :], src, writes=[b_stg])
                pb, bpb = k.ps.next()
                TR(k, pb[:, 0:NFF], stg[0:NFF, :], k.identf[0:NFF, 0:NFF], [b_stg], [bpb])
                CP(k, "dve", cwf[:, j * NFF:(j + 1) * NFF], pb[:, 0:NFF], [bpb], [b_cwf])
        wg = Ring([(ar.alloc((16, 256), BF16), p.buf()) for _ in range(2)])
        wu = Ring([(ar.alloc((16, 256), BF16), p.buf()) for _ in range(2)])
        graw = Ring([(ar.alloc((HT + 2,), F32), p.buf()) for _ in range(2)])
        gy = Ring([(ar.alloc((HT,), F32), p.buf()) for _ in range(2)])
        wgs = I["w_gate"].rearrange("(kt q) n -> q kt n", q=128)
        wus = I["w_up"].rearrange("(kt q) n -> q kt n", q=128)
        for j in range(NFF):
            if j % 2 == 0:
                g_ap2, g_b = wg.next()
                u_ap2, u_b = wu.next()
                p.dma(g_ap2, wgs[:, :, j * 128:(j + 2) * 128], writes=[g_b], q="pool")
                p.dma(u_ap2, wus[:, :, j * 128:(j + 2) * 128], writes=[u_b], q="pool")
            g_ap = g_ap2[:, :, (j % 2) * 128:(j % 2 + 1) * 128]
            u_ap = u_ap2[:, :, (j % 2) * 128:(j % 2 + 1) * 128]
            r_ap, r_b = graw.next()
            y_ap, y_b = gy.next()
            pus = []
            for c in range(2):
                pg, bg = k.ps.next()
                for kt in range(16):
                    MM(k, pg[:], g_ap[:, kt, :], h3T[:, kt, c * 512:(c + 1) * 512], kt == 0, kt == 15, [g_b, b_h3T], [bg])
                CP(k, "act", r_ap[:, 2 + c * 512:2 + (c + 1) * 512], pg[:], [bg], [r_b])
            for c in range(2):
                pu, bu = k.ps.next()
                for kt in range(16):
                    MM(k, pu[:], u_ap[:, kt, :], h3T[:, kt, c * 512:(c + 1) * 512], kt == 0, kt == 15, [u_b, b_h3T], [bu])
                pus.append((pu, bu))
            CP(k, "pool", r_ap[:, 0:2], halo[:, j, :], [b_halo], [r_b])
            if hf == 0:
                CP(k, "pool", halo[:, j, :], r_ap[:, HT:HT + 2], [r_b], [b_halo])
            TS(k, "dve", y_ap, r_ap[:, 0:HT], cwf[:, j:j + 1], None, ALU.mult, None, [r_b, b_cwf], [y_b])
            for tap in range(1, 3):
                STT(k, "dve", y_ap, r_ap[:, tap:tap + HT], cwf[:, tap * NFF + j:tap * NFF + j + 1], y_ap, ALU.mult, ALU.add,
                    [r_b, b_cwf, y_b], [y_b])
            ACT(k, y_ap, y_ap, AF.Silu, [y_b, b_cwf], [y_b], bias=cwf[:, 3 * NFF + j:3 * NFF + j + 1])
            for c in range(2):
                pu, bu = pus[c]
                TT(k, "dve", aT[:, j, c * 512:(c + 1) * 512], y_ap[:, c * 512:(c + 1) * 512], pu[:], ALU.mult,
                   [y_b, bu], [b_aT])
        p.barrier()
        ar.off = mark0
        x3 = ar.alloc((8, D), F32)
        b_x3 = p.bufs(8)
        aflat = aT.rearrange("p a b -> p (a b)")
        fgb = aflat[:, 0:2 * D].bitcast(F32)
        junk = aflat[:, 2 * D:3 * D]
        b_fgb = b_aT
        b_junk = b_aT
        wd = Ring([(ar.alloc((4, 512), BF16), p.buf()) for _ in range(3)])
        xr = Ring([(ar.alloc((512,), F32), p.buf()) for _ in range(2)])
        ssf = ar.alloc((8,), F32)
        b_ssf = p.buf()
        for cc in range(4):
            for j4 in range(NFF // 4):
                w_ap, w_b = wd.next()
                p.dma(w_ap, I["w_down"][j4 * 512:(j4 + 1) * 512, cc * 512:(cc + 1) * 512].rearrange("(a q) n -> q a n", q=128),
                      writes=[w_b], q="pool")
                for a in range(4):
                    j = j4 * 4 + a
                    for tt in range(8):
                        pb, bpb = k.banks[tt]
                        MM(k, pb[:], aT[:, j, tt * 128:(tt + 1) * 128], w_ap[:, a, :], j == 0, j == NFF - 1, [w_b, b_aT], [bpb])
            for tt in range(8):
                pb, bpb = k.banks[tt]
                x_ap, x_b = xr.next()
                row = (hf * 8 + tt) * 128
                p.dma(x_ap, k.X2[row:row + 128, cc * 512:(cc + 1) * 512], reads=[k.b_scr["X2"]], writes=[x_b])
                TT(k, "dve", x3[:, tt, cc * 512:(cc + 1) * 512], pb[:], x_ap, ALU.add, [bpb, x_b], [b_x3[tt]])
        p.dma(fgb, I["final_norm_g"].partition_broadcast(128), writes=[b_aT])
        outs = []
        for tt in range(8):
            row = (hf * 8 + tt) * 128
            ACT(k, junk, x3[:, tt, :], AF.Square, [b_x3[tt]], [b_junk, b_ssf], accum_out=ssf[:, tt:tt + 1])
            ACT(k, ssf[:, tt:tt + 1], ssf[:, tt:tt + 1], AF.Sqrt, [b_ssf], [b_ssf], scale=1.0 / D, bias=EPS)
            p.op("dve", "reciprocal", reads=[b_ssf], writes=[b_ssf], out=ssf[:, tt:tt + 1], in_=ssf[:, tt:tt + 1])
            STT(k, "dve", x3[:, tt, :], x3[:, tt, :], ssf[:, tt:tt + 1], fgb, ALU.mult, ALU.mult,
                [b_x3[tt], b_ssf, b_fgb], [b_x3[tt]])
            outs.append(p.dma(k.out[row:row + 128, :], x3[:, tt, :], reads=[b_x3[tt]]))
        p.barrier()
    return []


_CACHE = {}


def make_in_maps(inputs, n=8, skip=()):
    cf, cb = host_consts()
    tidx = toeplitz_index()
    rb = np.asarray(inputs["rel_bias"], np.float32)
    rb_toep = np.ascontiguousarray(rb[tidx].transpose(0, 2, 1))
    shared = {
        "mix_norm_g": inputs["mix_norm_g"].reshape(1, D), "xattn_norm_g": inputs["xattn_norm_g"].reshape(1, D),
        "mem_norm_g": inputs["mem_norm_g"].reshape(1, D), "ffn_norm_g": inputs["ffn_norm_g"].reshape(1, D),
        "final_norm_g": inputs["final_norm_g"].reshape(1, D),
        "w_in": inputs["w_in"][0], "gdn_conv_w": inputs["gdn_conv_w"][0], "gdn_a_log": inputs["gdn_a_log"].reshape(1, 8),
        "gdn_dt_bias": inputs["gdn_dt_bias"].reshape(1, 8), "gdn_norm_g": inputs["gdn_norm_g"].reshape(1, 128),
        "moba_norm_g": inputs["moba_norm_g"].reshape(1, 128), "rel_bias": rb.reshape(1, 256), "rb_toep": rb_toep,
        "w_out": inputs["w_out"][0], "w_xq": inputs["w_xq"][0], "w_xkv": inputs["w_xkv"][0], "w_xo": inputs["w_xo"][0],
        "w_gate": inputs["w_gate"][0], "w_up": inputs["w_up"][0], "ffn_conv_w": inputs["ffn_conv_w"][0],
        "ffn_conv_b": inputs["ffn_conv_b"].reshape(1, DFF), "w_down": inputs["w_down"][0],
        "cst_f": cf, "cst_b": cb,
    }
    shared = {kk: (np.zeros((1, 1), np.float32) if kk in skip else np.ascontiguousarray(np.asarray(v, np.float32)))
              for kk, v in shared.items()}
    maps = []
    for b in range(n):
        m = dict(shared)
        m["x"] = np.ascontiguousarray(np.asarray(inputs["x"][b], np.float32))
        m["mem"] = (np.zeros((1, 1), np.float32) if "mem" in skip
                    else np.ascontiguousarray(np.asarray(inputs["mem"][b], np.float32)))
        maps.append(m)
    return maps


def kernel(**inputs):
    if "nc" not in _CACHE:
        _CACHE["nc"] = build_program()[0]
    nc = _CACHE["nc"]
    maps = make_in_maps(inputs, 8)
    res = run_bass_kernel_spmd(nc, maps, core_ids=list(range(8)))
    return np.stack([np.asarray(r["out"], np.float32) for r in res.results], axis=0)
```

```python
import bisect
import math
from contextlib import ExitStack

import numpy as np
import concourse.bass as bass
import concourse.mybir as mybir
from concourse.bass_utils import run_bass_kernel_spmd

F32 = mybir.dt.float32
BF16 = mybir.dt.bfloat16
ALU = mybir.AluOpType
AF = mybir.ActivationFunctionType
AX = mybir.AxisListType

S = 2048
D = 2048
NT = 16
H = 8
HD = 128
DFF = 5632
NFF = 44
MEM = 256
INW = 7184
EPS = 1e-6
BIG = 30000.0
SQ128 = math.sqrt(128.0)

ENGS = ("pe", "act", "dve", "pool", "sp")
SEM_LIMIT = 2000
N_ENG_SEMS = {"pe": 14, "act": 5, "dve": 5, "pool": 3, "sp": 1}
N_DMA_SEMS = 32
N_SWDMA_SEMS = 16


class Buf:
    __slots__ = ("name", "last_w", "creads", "dreads")

    def __init__(self, name=""):
        self.name = name
        self.last_w = None
        self.creads = {}
        self.dreads = []


class Op:
    __slots__ = ("eng", "name", "kw", "waits", "signal", "pos")

    def __init__(self, eng, name, kw, pos):
        self.eng = eng
        self.name = name
        self.kw = kw
        self.waits = []
        self.signal = None
        self.pos = pos


class Prog:
    def __init__(self, nc, stack):
        self.nc = nc
        self.stack = stack
        self.ops = {e: [] for e in ENGS}
        self.nsig = {e: 0 for e in ENGS}
        self.sigpos = {e: [] for e in ENGS}
        self.sigtok = {e: [] for e in ENGS}
        self.waited = {e: {} for e in ENGS}
        self.esems = {e: [stack.enter_context(nc.semaphore(f"s_{e}_{i}")) for i in range(N_ENG_SEMS[e])]
                      for e in ENGS}
        self.dsems = {"hw": [stack.enter_context(nc.semaphore(f"s_dma_{i}")) for i in range(N_DMA_SEMS)],
                      "sw": [stack.enter_context(nc.semaphore(f"s_swdma_{i}")) for i in range(N_SWDMA_SEMS)]}
        self.ndma_q = {"hw": 0, "sw": 0}
        self.ndma = 0
        self.dma_toks = []
        self.pe_prev = (None, None, True)

    def sbuf(self, name, shape, dtype):
        return self.stack.enter_context(self.nc.sbuf_tensor(name, list(shape), dtype))

    def psum(self, name, shape, dtype):
        return self.stack.enter_context(self.nc.psum_tensor(name, list(shape), dtype))

    def buf(self, name=""):
        return Buf(name)

    def bufs(self, n):
        return [Buf() for _ in range(n)]

    def _force_signal(self, eng):
        lst = self.ops[eng]
        if not lst:
            return None
        last = lst[-1]
        if last.signal is None:
            n = self.nsig[eng]
            self.nsig[eng] += 1
            sem = self.esems[eng][n // SEM_LIMIT]
            val = n % SEM_LIMIT + 1
            last.signal = (sem, 1)
            self.sigpos[eng].append(last.pos)
            self.sigtok[eng].append((sem, val))
        return last

    def _resolve(self, tok):
        if tok[0] == "d":
            return tok[1], tok[2]
        _, eng, pos = tok
        sp = self.sigpos[eng]
        i = bisect.bisect_left(sp, pos)
        if i < len(sp):
            return self.sigtok[eng][i]
        last = self._force_signal(eng)
        assert last.pos >= pos
        i = bisect.bisect_left(sp, pos)
        return self.sigtok[eng][i]

    def _add_wait(self, op, tok):
        sem, val = self._resolve(tok)
        w = self.waited[op.eng]
        key = id(sem)
        if w.get(key, 0) >= val:
            return
        w[key] = val
        for i, (s, v) in enumerate(op.waits):
            if s is sem:
                op.waits[i] = (sem, max(v, val))
                return
        op.waits.append((sem, val))

    def _deps(self, op, reads, writes, is_dma):
        eng = op.eng
        for b in reads:
            if b.last_w is not None:
                self._add_wait(op, b.last_w)
        for b in writes:
            t = b.last_w
            if t is not None and not (t[0] == "c" and t[1] == eng == "pe" and not is_dma):
                self._add_wait(op, t)
            for e, t in b.creads.items():
                if e == eng == "pe" and not is_dma:
                    continue
                self._add_wait(op, t)
            for t in b.dreads:
                self._add_wait(op, t)

    def _compute_pos_check(self, eng):
        lst = self.ops[eng]
        if lst and lst[-1].signal is None and lst[-1].name not in ("nop", "dma_start"):
            self._force_signal(eng)

    def op(self, eng, name, reads=(), writes=(), **kw):
        lst = self.ops[eng]
        rset = frozenset(id(b) for b in reads)
        wset = frozenset(id(b) for b in writes)
        if lst and lst[-1].name not in ("nop", "dma_start") and lst[-1].signal is None:
            prev = lst[-1]
            if eng != "pe":
                self._force_signal(eng)
            else:
                pr, pw, pdone = self.pe_prev
                if pr != rset or (pw != wset and pdone):
                    self._force_signal(eng)
        if eng == "pe":
            self.pe_prev = (rset, wset, kw.get("stop", True))
        o = Op(eng, name, kw, len(lst))
        self._deps(o, reads, writes, False)
        lst.append(o)
        tok = ("c", eng, o.pos)
        for b in reads:
            b.creads[eng] = tok
        for b in writes:
            b.last_w = tok
            b.creads = {}
            b.dreads = []
        return o

    def dma(self, out, in_, reads=(), writes=(), q="sp", **kw):
        if q != "sp":
            self._compute_pos_check(q)
        lst = self.ops[q]
        o = Op(q, "dma_start", dict(out=out, in_=in_, **kw), len(lst))
        self._deps(o, reads, writes, True)
        kind = "sw" if q == "pool" else "hw"
        pool_ = self.dsems[kind]
        i = self.ndma_q[kind] % len(pool_)
        r = self.ndma_q[kind] // len(pool_)
        self.ndma_q[kind] += 1
        self.ndma += 1
        sem = pool_[i]
        if r > 0:
            self._add_wait(o, ("d", sem, 16 * r))
        o.signal = (sem, 16)
        lst.append(o)
        tok = ("d", sem, 16 * (r + 1))
        self.dma_toks.append(tok)
        for b in reads:
            b.dreads.append(tok)
        for b in writes:
            b.last_w = tok
            b.creads = {}
            b.dreads = []
        return tok

    def barrier(self):
        toks = list(self.dma_toks[-(N_DMA_SEMS + N_SWDMA_SEMS) * 2:])
        for e in ENGS:
            lst = self.ops[e]
            if lst and lst[-1].name not in ("dma_start", "nop"):
                last = self._force_signal(e)
                toks.append(("c", e, last.pos))
            else:
                for o in reversed(lst):
                    if o.name not in ("dma_start", "nop"):
                        assert o.signal is not None
                        toks.append(("c", e, o.pos))
                        break
        for e in ENGS:
            o = Op(e, "nop", {}, len(self.ops[e]))
            for t in toks:
                self._add_wait(o, t)
            self.ops[e].append(o)

    def final_wait(self, eng, toks):
        o = Op(eng, "nop", {}, len(self.ops[eng]))
        for t in toks:
            self._add_wait(o, t)
        self.ops[eng].append(o)

    def emit(self):
        nc = self.nc
        prog = self
        with nc.Block() as block:
            def run(engname):
                def body(e):
                    for o in prog.ops[engname]:
                        for (sem, val) in o.waits:
                            e.wait_ge(sem, val)
                        if o.name == "nop":
                            continue
                        ins = getattr(e, o.name)(**o.kw)
                        if o.signal is not None:
                            ins.then_inc(o.signal[0], o.signal[1])
                return body
            block.tensor(run("pe"))
            block.scalar(run("act"))
            block.vector(run("dve"))
            block.gpsimd(run("pool"))
            block.sync(run("sp"))

    def stats(self):
        return {e: (len(self.ops[e]), self.nsig[e]) for e in ENGS}, self.ndma


class Arena:
    def __init__(self, p, nbytes):
        self.t = p.sbuf("arena", [128, nbytes // 2], BF16)
        self.n = nbytes
        self.off = 0

    def reset(self):
        self.off = 0

    def alloc(self, shape, dtype):
        esz = 2 if dtype == BF16 else 4
        n = 1
        for s in shape:
            n *= s
        nb = n * esz
        a = self.t[:, self.off // 2:(self.off + nb) // 2]
        self.off += (nb + 63) // 64 * 64
        assert self.off <= self.n, f"arena overflow {self.off} > {self.n}"
        if dtype != BF16:
            a = a.bitcast(dtype)
        if len(shape) == 2:
            a = a.rearrange("p (a b) -> p a b", a=shape[0])
        elif len(shape) == 3:
            a = a.rearrange("p (a b c) -> p a b c", a=shape[0], b=shape[1])
        return a


class Ring:
    def __init__(self, items):
        self.items = items
        self.i = 0

    def next(self):
        it = self.items[self.i % len(self.items)]
        self.i += 1
        return it


NCF = 576
NCB = 768 + 9 * 128


def host_consts():
    cf = np.zeros((128, NCF), np.float32)
    ii = np.arange(128)
    cf[:, 0:128] = np.eye(128, dtype=np.float32)
    cf[:, 128:256] = (ii[:, None] <= ii[None, :]).astype(np.float32)
    cf[:, 256:384] = (ii[:, None] > ii[None, :]).astype(np.float32)
    cf[:, 384:512] = 1.0
    gm = np.zeros((128, 8, 8), np.float32)
    for qi in range(8, 16):
        own = qi // 2
        gm[:, qi - 8, own:] = -1e30
    cf[:, 512:576] = gm.reshape(128, 64)
    cb = np.zeros((128, NCB), np.float32)
    cb[:, 0:128] = np.eye(128, dtype=np.float32)
    cb[:, 128:256] = 1.0
    cb[:, 256:384] = (ii[None, :] > ii[:, None]).astype(np.float32) * BIG
    cb[:, 384:512] = (ii[None, :] >= ii[:, None]).astype(np.float32) * BIG
    rr = np.arange(256)
    cb[:, 512:768] = (rr[None, :] < ii[:, None]).astype(np.float32) * (-BIG * SQ128)
    e = np.zeros((9, 9, 128), np.float32)
    for v in range(9):
        e[8, v, :] = 1.0
        if v < 8:
            e[v, v, :] = 1.0
    cb[0:9, 768:768 + 9 * 128] = e.reshape(9, 9 * 128)
    return cf, cb


def rel_bucket_np(n):
    n = np.maximum(n, 0)
    max_exact = 16
    nf = np.maximum(n, 1).astype(np.float32)
    large = max_exact + (np.log(nf / max_exact) / math.log(128 / max_exact) * (32 - max_exact)).astype(np.int32)
    large = np.minimum(large, 31)
    return np.where(n < max_exact, n, large)


def toeplitz_index():
    kk = np.arange(128)[:, None]
    r = np.arange(256)[None, :]
    return rel_bucket_np(r - kk)


class K:
    pass


def build_program(debug_phase=None):
    nc = bass.Bass("TRN2", target_bir_lowering=False)
    k = K()
    k.nc = nc
    dbg = debug_phase is not None

    order = ["inproj", "gdn", "moba", "wout", "xattn", "ffn"]
    need_from = {"mem": "xattn", "w_out": "wout", "w_xq": "xattn", "w_xkv": "xattn", "w_xo": "xattn",
                 "w_gate": "ffn", "w_up": "ffn", "w_down": "ffn"}
    k.skip = set()
    if dbg:
        for nm, ph in need_from.items():
            if order.index(ph) > order.index(debug_phase):
                k.skip.add(nm)

    def din(name, shape, dt=F32):
        if name in k.skip:
            shape = [1, 1]
        return nc.dram_tensor(name, list(shape), dt, kind="ExternalInput").ap()

    DUMP = {"inproj": ("QA", "KA", "KT", "VA", "ZS", "QB", "KB", "VB", "DBG"), "moba": ("OT",), "gdn": ("OT",),
            "wout": ("X1",), "xattn": ("X2",), "ffn": ("OT", "X1", "X2")}.get(debug_phase, ())

    def dscr(name, shape, dt=BF16):
        kind = "ExternalOutput" if name in DUMP else "Internal"
        return nc.dram_tensor(name, list(shape), dt, kind=kind).ap()

    I = {}
    I["x"] = din("x", [S, D])
    I["mem"] = din("mem", [MEM, D])
    for nm in ("mix_norm_g", "xattn_norm_g", "mem_norm_g", "ffn_norm_g", "final_norm_g"):
        I[nm] = din(nm, [1, D])
    I["w_in"] = din("w_in", [D, INW])
    I["gdn_conv_w"] = din("gdn_conv_w", [4, 3072])
    I["gdn_a_log"] = din("gdn_a_log", [1, 8])
    I["gdn_dt_bias"] = din("gdn_dt_bias", [1, 8])
    I["gdn_norm_g"] = din("gdn_norm_g", [1, 128])
    I["moba_norm_g"] = din("moba_norm_g", [1, 128])
    I["rel_bias"] = din("rel_bias", [1, 256])
    I["rb_toep"] = din("rb_toep", [128, 8, 256])
    I["w_out"] = din("w_out", [D, D])
    I["w_xq"] = din("w_xq", [D, 512])
    I["w_xkv"] = din("w_xkv", [D, 1024])
    I["w_xo"] = din("w_xo", [512, D])
    I["w_gate"] = din("w_gate", [D, DFF])
    I["w_up"] = din("w_up", [D, DFF])
    I["ffn_conv_w"] = din("ffn_conv_w", [3, DFF])
    I["ffn_conv_b"] = din("ffn_conv_b", [1, DFF])
    I["w_down"] = din("w_down", [DFF, D])
    I["cst_f"] = din("cst_f", [128, NCF])
    I["cst_b"] = din("cst_b", [128, NCB])
    out = nc.dram_tensor("out", [S, D], F32, kind="ExternalOutput").ap()
    k.I = I
    k.out = out

    k.QA = dscr("QA", [128, 16, 8, 128])
    k.KA = dscr("KA", [128, 16, 8, 128])
    k.KT = dscr("KT", [128, 16, 8, 128])
    k.VA = dscr("VA", [128, 16, 8, 128])
    k.ZS = dscr("ZS", [128, 16, 8, 128])
    k.QB = dscr("QB", [8, 128, 2048])
    k.KB = dscr("KB", [8, 128, 2048])
    k.VB = dscr("VB", [8, 128, 16, 128])
    k.X1 = dscr("X1", [S, D], F32)
    k.X2 = dscr("X2", [S, D], F32)
    if "DBG" in DUMP:
        k.DBG = dscr("DBG", [128, 4096], F32)
    if "OT" in DUMP:
        k.OT = dscr("OT", [128, 16, S], BF16)
    k.b_scr = {nm: Buf(nm) for nm in ("QA", "KA", "KT", "VA", "ZS", "QB", "KB", "VB", "X1", "X2", "DBG")}

    with ExitStack() as st:
        p = Prog(nc, st)
        k.p = p
        k.cf = p.sbuf("cf", [128, NCF], F32)
        k.cb = p.sbuf("cb", [128, NCB], BF16)
        k.b_c = p.buf("consts")
        p.dma(k.cf[:], I["cst_f"], writes=[k.b_c])
        p.dma(k.cb[:], I["cst_b"], writes=[k.b_c], q="pool")
        k.identf = k.cf[:, 0:128]
        k.trif = k.cf[:, 128:256]
        k.suf = k.cf[:, 256:384]
        k.onesf = k.cf[:, 384:512]
        k.gm = k.cf[:, 512:576]
        k.ident = k.cb[:, 0:128]
        k.ones = k.cb[:, 128:256]
        k.umq = k.cb[:, 256:384]
        k.uma = k.cb[:, 384:512]
        k.cm = k.cb[:, 512:768]
        k.emat = k.cb[0:9, 768:768 + 9 * 128].rearrange("p (v n) -> p v n", v=9)
        k.BA = p.sbuf("BA", [128, 16, 16], F32)
        k.b_BA = p.buf("BA")
        k.BETA = p.sbuf("BETA", [128, 16, 8], F32)
        k.GG = p.sbuf("GG", [128, 16, 8], F32)
        k.b_bg = p.buf("betag")
        banks = [(p.psum(f"ps{i}", [128, 512], F32), p.buf(f"ps{i}")) for i in range(8)]
        k.ps = Ring(banks[0:6])
        k.psx = banks[6:8]
        k.banks = banks
        k.ar = Arena(p, 176 * 1024)

        phases = [phase_inproj, phase_gdn, phase_moba, phase_wout, phase_xattn, phase_ffn]
        names = ["inproj", "gdn", "moba", "wout", "xattn", "ffn"]
        out_toks = []
        for fn, nm in zip(phases, names):
            r = fn(k)
            if r:
                out_toks += r
            p.barrier()
            if debug_phase == nm:
                break
        p.final_wait("sp", p.dma_toks[-(N_DMA_SEMS + N_SWDMA_SEMS) * 2:])
        k.stats = p.stats()
        p.emit()
    return nc, k


def MM(k, out, lhsT, rhs, start, stop, r, w):
    k.p.op("pe", "matmul", reads=r, writes=w, out=out, lhsT=lhsT, rhs=rhs, start=start, stop=stop)


def TR(k, out, in_, ident, r, w):
    k.p.op("pe", "transpose", reads=r + [k.b_c], writes=w, out=out, in_=in_, identity=ident)


def ACT(k, out, in_, func, r, w, **kw):
    k.p.op("act", "activation", reads=r, writes=w, out=out, in_=in_, func=func, **kw)


def CP(k, eng, out, in_, r, w):
    if eng == "act":
        k.p.op("act", "copy", reads=r, writes=w, out=out, in_=in_)
    else:
        k.p.op(eng, "tensor_copy", reads=r, writes=w, out=out, in_=in_)


def TT(k, eng, out, in0, in1, op, r, w):
    k.p.op(eng, "tensor_tensor", reads=r, writes=w, out=out, in0=in0, in1=in1, op=op)


def TS(k, eng, out, in0, s1, s2, op0, op1, r, w):
    if s2 is None:
        k.p.op(eng, "tensor_scalar", reads=r, writes=w, out=out, in0=in0, scalar1=s1, scalar2=None, op0=op0)
    else:
        k.p.op(eng, "tensor_scalar", reads=r, writes=w, out=out, in0=in0, scalar1=s1, scalar2=s2, op0=op0, op1=op1)


def STT(k, eng, out, in0, scalar, in1, op0, op1, r, w):
    k.p.op(eng, "scalar_tensor_tensor", reads=r, writes=w, out=out, in0=in0, scalar=scalar, in1=in1,
           op0=op0, op1=op1)


def norm_T(k, tiles, gain, dstT, b_dst, col0=0):
    p = k.p
    ar = k.ar
    gb = ar.alloc((D,), F32)
    b_gb = p.buf()
    p.dma(gb, gain.partition_broadcast(128), writes=[b_gb])
    xt = [ar.alloc((D,), F32) for _ in range(2)]
    b_xt = p.bufs(2)
    junk = ar.alloc((D,), BF16)
    b_junk = p.buf()
    hb = [ar.alloc((D,), BF16) for _ in range(2)]
    b_hb = p.bufs(2)
    ss = ar.alloc((len(tiles),), F32)
    b_ss = p.buf()
    pend = []
    for t, (src, rb) in enumerate(tiles):
        i = t % 2
        p.dma(xt[i], src, reads=rb, writes=[b_xt[i]])
        ACT(k, junk, xt[i], AF.Square, [b_xt[i]], [b_junk, b_ss], accum_out=ss[:, t:t + 1])
        ACT(k, ss[:, t:t + 1], ss[:, t:t + 1], AF.Sqrt, [b_ss], [b_ss], scale=1.0 / D, bias=EPS)
        p.op("dve", "reciprocal", reads=[b_ss], writes=[b_ss], out=ss[:, t:t + 1], in_=ss[:, t:t + 1])
        STT(k, "dve", hb[i], xt[i], ss[:, t:t + 1], gb, ALU.mult, ALU.mult, [b_xt[i], b_ss, b_gb], [b_hb[i]])

        def stage_b(t=t, i=i):
            for k4 in range(4):
                pb, bpb = k.ps.next()
                pbv = pb[:].bitcast(BF16)
                for j in range(4):
                    kt = k4 * 4 + j
                    TR(k, pbv[:, j * 128:(j + 1) * 128], hb[i][:, kt * 128:(kt + 1) * 128], k.ident, [b_hb[i]], [bpb])
                CP(k, "act" if k4 % 2 else "dve",
                   dstT[:, k4 * 4:(k4 + 1) * 4, col0 + t * 128:col0 + (t + 1) * 128],
                   pbv[:, 0:512].rearrange("p (a b) -> p a b", a=4), [bpb], [b_dst])
        if pend:
            pend.pop(0)()
        pend.append(stage_b)
    while pend:
        pend.pop(0)()


def load_w_block(k, ring, src):
    ap, b = ring.next()
    k.p.dma(ap, src, writes=[b], q="pool")
    return ap, b


def phase_inproj(k):
    p = k.p
    ar = k.ar
    I = k.I
    ar.reset()
    hT = ar.alloc((16, S), BF16)
    b_hT = p.buf("hT")
    k.hT = hT
    mark = ar.off
    tiles = [(I["x"][t * 128:(t + 1) * 128, :], []) for t in range(NT)]
    norm_T(k, tiles, I["mix_norm_g"], hT, b_hT)
    p.barrier()
    ar.off = mark
    wring = Ring([(ar.alloc((16, 512), BF16), p.buf()) for _ in range(2)])
    wba = ar.alloc((16, 16), BF16)
    b_wba = p.buf()
    raw = [ar.alloc((S + 4,), F32) for _ in range(2)]
    b_raw = p.bufs(2)
    yr = Ring([(ar.alloc((S,), F32), p.buf()) for _ in range(2)])
    sqr = Ring([(ar.alloc((S,), BF16), p.buf()) for _ in range(2)])
    rsr = Ring([(ar.alloc((S,), F32), p.buf()) for _ in range(2)])
    obf = [ar.alloc((S,), BF16) for _ in range(2)]
    b_obf = p.bufs(2)
    tst = [ar.alloc((16, 128), BF16) for _ in range(2)]
    b_tst = p.bufs(2)
    zst = [ar.alloc((512,), BF16) for _ in range(3)]
    b_zst = p.bufs(3)
    cw = ar.alloc((96,), F32)
    cwl = ar.alloc((128,), F32)
    b_cw = p.buf()
    small = ar.alloc((64,), F32)
    b_small = p.buf()
    p.dma(cwl[0:96, :], I["gdn_conv_w"].rearrange("j (b q) -> (j b) q", q=128), writes=[b_cw])
    pb, bpb = k.ps.next()
    TR(k, pb[:, 0:96], cwl[0:96, :], k.identf[0:96, 0:96], [b_cw], [bpb])
    CP(k, "dve", cw, pb[:, 0:96], [bpb], [b_cw])
    for i in range(2):
        p.op("pool", "memset", writes=[b_raw[i]], ap=raw[i][:, 0:4], constant=0.0)

    wsrc = I["w_in"].rearrange("(kt q) n -> q kt n", q=128)
    ri = 0
    oi = 0
    ti = 0

    def fblock(wap, wb, f, evac):
        for c in range(4):
            pb, bpb = k.ps.next()
            for kt in range(16):
                MM(k, pb[:], wap[:, kt, f * 128:(f + 1) * 128], hT[:, kt, c * 512:(c + 1) * 512],
                   kt == 0, kt == 15, [wb, b_hT], [bpb])
            evac(pb, bpb, c * 512)

    deferred = []

    def make_post(fb, kind, hd, r_ap, r_b):
        st = {}

        def stage_a():
            nonlocal oi
            y, b_y = yr.next()
            st["y"] = (y, b_y)
            TS(k, "dve", y, r_ap[:, 0:S], cw[:, 0 * 24 + fb:0 * 24 + fb + 1], None, ALU.mult, None, [r_b, b_cw], [b_y])
            for j in range(1, 4):
                STT(k, "dve", y, r_ap[:, j:j + S], cw[:, j * 24 + fb:j * 24 + fb + 1], y, ALU.mult, ALU.add,
                    [r_b, b_cw, b_y], [b_y])
            o_ap, o_b = obf[oi % 2], b_obf[oi % 2]
            oi += 1
            st["o"] = (o_ap, o_b)
            if kind == 2:
                ACT(k, o_ap, y, AF.Silu, [b_y], [o_b])
            else:
                sq, b_sq = sqr.next()
                st["sq"] = (sq, b_sq)
                ACT(k, y, y, AF.Silu, [b_y], [b_y])
                ACT(k, sq, y, AF.Square, [b_y], [b_sq])

        def stage_b():
            nonlocal ti
            y, b_y = st["y"]
            o_ap, o_b = st["o"]
            if kind != 2:
                sq, b_sq = st["sq"]
                rs, b_rs = rsr.next()
                for c in range(4):
                    pb, bpb = k.ps.next()
                    MM(k, pb[:], k.ones, sq[:, c * 512:(c + 1) * 512], True, True, [b_sq, k.b_c], [bpb])
                    if kind == 0:
                        ACT(k, rs[:, c * 512:(c + 1) * 512], pb[:], AF.Sqrt, [bpb], [b_rs], scale=128.0,
                            bias=128.0 * EPS)
                    else:
                        ACT(k, rs[:, c * 512:(c + 1) * 512], pb[:], AF.Sqrt, [bpb], [b_rs], scale=1.0, bias=EPS)
                p.op("dve", "reciprocal", reads=[b_rs], writes=[b_rs], out=rs, in_=rs)
                TT(k, "dve", o_ap, y, rs, ALU.mult, [b_y, b_rs], [o_b])
            if kind == 0:
                p.dma(k.QA[:, :, hd, :], o_ap.rearrange("p (c t) -> p c t", c=16), reads=[o_b],
                      writes=[k.b_scr["QA"]])
            if kind == 1:
                p.dma(k.KA[:, :, hd, :], o_ap.rearrange("p (c t) -> p c t", c=16), reads=[o_b],
                      writes=[k.b_scr["KA"]])
            if kind >= 1:
                t_ap, t_b = tst[ti % 2], b_tst[ti % 2]
                ti += 1
                for c4 in range(4):
                    pb, bpb = k.ps.next()
                    pbv = pb[:].bitcast(BF16)
                    for j in range(4):
                        c = c4 * 4 + j
                        TR(k, pbv[:, j * 128:(j + 1) * 128], o_ap[:, c * 128:(c + 1) * 128], k.ident, [o_b], [bpb])
                    CP(k, "act" if c4 % 2 else "dve", t_ap[:, c4 * 4:(c4 + 1) * 4, :],
                       pbv[:, 0:512].rearrange("p (a b) -> p a b", a=4), [bpb], [t_b])
                dst = k.KT if kind == 1 else k.VA
                p.dma(dst[:, :, hd, :], t_ap, reads=[t_b], writes=[k.b_scr["KT" if kind == 1 else "VA"]])
        return stage_a, stage_b

    for blk in range(6):
        wap, wb = load_w_block(k, wring, wsrc[:, :, blk * 512:(blk + 1) * 512])
        for f in range(4):
            fb = blk * 4 + f
            kind = fb // 8
            hd = fb % 8
            r_ap, r_b = raw[ri % 2], b_raw[ri % 2]
            ri += 1
            prev = deferred.pop(0) if deferred else None

            def evac_raw(pb, bpb, tok0, r_ap=r_ap, r_b=r_b, prev=prev):
                CP(k, "act", r_ap[:, 3 + tok0:3 + tok0 + 512], pb[:], [bpb], [r_b])
                if tok0 == 512 and prev is not None:
                    prev[0]()
            fblock(wap, wb, f, evac_raw)
            if prev is not None:
                prev[1]()
            deferred.append(make_post(fb, kind, hd, r_ap, r_b))
    for a_, b_ in deferred:
        a_()
        b_()
    zi = 0
    for blk in range(2):
        wap, wb = load_w_block(k, wring, wsrc[:, :, 3072 + blk * 512:3072 + (blk + 1) * 512])
        for t in range(NT):
            pb, bpb = k.ps.next()
            for kt in range(16):
                MM(k, pb[:], hT[:, kt, t * 128:(t + 1) * 128], wap[:, kt, :], kt == 0, kt == 15, [wb, b_hT], [bpb])
            z_ap, z_b = zst[zi % 3], b_zst[zi % 3]
            zi += 1
            ACT(k, z_ap, pb[:], AF.Silu, [bpb], [z_b])
            p.dma(k.ZS[:, t, blk * 4:(blk + 1) * 4, :], z_ap.rearrange("p (h d) -> p h d", h=4), reads=[z_b],
                  writes=[k.b_scr["ZS"]])
    p.dma(wba, wsrc[:, :, 4096:4112], writes=[b_wba], q="pool")
    for t in range(NT):
        pb, bpb = k.ps.next()
        for kt in range(16):
            MM(k, pb[:, 0:16], hT[:, kt, t * 128:(t + 1) * 128], wba[:, kt, :], kt == 0, kt == 15, [b_wba, b_hT], [bpb])
        CP(k, "dve", k.BA[:, t, :], pb[:, 0:16], [bpb], [k.b_BA])
    albc = small[:, 0:8]
    dtbc = small[:, 8:16]
    nea = small[:, 16:24]
    p.dma(albc, I["gdn_a_log"].partition_broadcast(128), writes=[b_small])
    p.dma(dtbc, I["gdn_dt_bias"].partition_broadcast(128), writes=[b_small])
    ACT(k, nea, albc, AF.Exp, [b_small], [b_small])
    TS(k, "dve", nea, nea, -1.0, None, ALU.mult, ALU.bypass, [b_small], [b_small])
    ACT(k, k.BETA[:], k.BA[:, :, 0:8], AF.Sigmoid, [k.b_BA], [k.b_bg])
    xa = ar.alloc((16, 8), F32)
    xb = ar.alloc((16, 8), F32)
    b_xa = p.buf()
    TT(k, "dve", xa, k.BA[:, :, 8:16], dtbc.unsqueeze(1).broadcast_to([128, 16, 8]), ALU.add, [k.b_BA, b_small], [b_xa])
    ACT(k, xb, xa, AF.Abs, [b_xa], [b_xa])
    ACT(k, xb, xb, AF.Exp, [b_xa], [b_xa], scale=-1.0)
    ACT(k, xb, xb, AF.Ln, [b_xa], [b_xa], bias=1.0)
    TS(k, "dve", xa, xa, 0.0, None, ALU.max, ALU.bypass, [b_xa], [b_xa])
    TT(k, "dve", xa, xa, xb, ALU.add, [b_xa], [b_xa])
    TT(k, "dve", k.GG[:], xa, nea.unsqueeze(1).broadcast_to([128, 16, 8]), ALU.mult, [b_xa, b_small], [k.b_bg])

    for blk in range(4):
        wap, wb = load_w_block(k, wring, wsrc[:, :, 4112 + blk * 512:4112 + (blk + 1) * 512])
        for f in range(4):
            fb = blk * 4 + f
            kind = fb // 8
            hd = fb % 8
            o_ap, o_b = obf[oi % 2], b_obf[oi % 2]
            oi += 1

            def evac_o(pb, bpb, tok0, o_ap=o_ap, o_b=o_b, c=[0]):
                CP(k, "act" if (tok0 // 512) % 2 else "dve", o_ap[:, tok0:tok0 + 512], pb[:], [bpb], [o_b])
            fblock(wap, wb, f, evac_o)
            dst = k.QB if kind == 0 else k.KB
            p.dma(dst[hd], o_ap, reads=[o_b], writes=[k.b_scr["QB" if kind == 0 else "KB"]])
    for blk in range(2):
        wap, wb = load_w_block(k, wring, wsrc[:, :, 6160 + blk * 512:6160 + (blk + 1) * 512])
        for t in range(NT):
            pb, bpb = k.ps.next()
            for kt in range(16):
                MM(k, pb[:], hT[:, kt, t * 128:(t + 1) * 128], wap[:, kt, :], kt == 0, kt == 15, [wb, b_hT], [bpb])
            z_ap, z_b = zst[zi % 3], b_zst[zi % 3]
            zi += 1
            CP(k, "act" if t % 2 else "dve", z_ap, pb[:], [bpb], [z_b])
            p.dma(k.VB[blk * 4:(blk + 1) * 4, :, t, :].rearrange("h p d -> p h d"),
                  z_ap.rearrange("p (h d) -> p h d", h=4), reads=[z_b], writes=[k.b_scr["VB"]])
    if hasattr(k, "DBG"):
        p.dma(k.DBG[:, 0:128], k.BETA[:].rearrange("p a b -> p (a b)"), reads=[k.b_bg], writes=[k.b_scr["DBG"]])
        p.dma(k.DBG[:, 128:256], k.GG[:].rearrange("p a b -> p (a b)"), reads=[k.b_bg], writes=[k.b_scr["DBG"]])


def phase_gdn(k):
    p = k.p
    ar = k.ar
    I = k.I
    ar.reset()
    oT = ar.alloc((16, S), BF16)
    k.oT = oT
    k.b_oT = p.buf("oT")
    k.oT_mark = ar.off
    S32 = ar.alloc((8, 128), F32)
    S16 = ar.alloc((8, 128), BF16)
    b_S32 = p.bufs(2)
    b_S16 = p.bufs(2)
    p.op("pool", "memset", writes=b_S32, ap=S32, constant=0.0)
    p.op("pool", "memset", writes=b_S16, ap=S16, constant=0.0)
    gnb = ar.alloc((128,), F32)
    b_gnb = p.buf()
    p.dma(gnb, I["gdn_norm_g"].partition_broadcast(128), writes=[b_gnb])
    names = ("QA", "KA", "KT", "VA", "ZS")
    inr = Ring([([ar.alloc((8, 128), BF16) for _ in names], p.bufs(5)) for _ in range(2)])
    sm = Ring([(ar.alloc((128,), F32), p.buf()) for _ in range(2)])
    kbg_, vb_, kdec_, zg_ = [ar.alloc((8, 128), BF16) for _ in range(4)]
    b_kbg, b_vb, b_kdec, b_zg = p.bufs(4)

    class G:
        pass
    grp = []
    for g in range(2):
        G_ = G()
        G_.diagG = ar.alloc((4, 128), F32)
        G_.decQ = ar.alloc((4, 128), BF16)
        G_.decA = ar.alloc((4, 128), BF16)
        G_.A = [ar.alloc((4, 128), BF16) for _ in range(2)]
        G_.B = [ar.alloc((4, 128), BF16) for _ in range(2)]
        G_.P = [ar.alloc((4, 128), BF16) for _ in range(2)]
        G_.qk = ar.alloc((4, 128), BF16)
        G_.qkT = ar.alloc((4, 128), BF16)
        G_.wT = ar.alloc((4, 128), BF16)
        G_.u = ar.alloc((4, 128), F32)
        G_.vnew = ar.alloc((4, 128), BF16)
        G_.o1 = ar.alloc((4, 128), F32)
        G_.o = ar.alloc((4, 128), F32)
        G_.sq = ar.alloc((4, 128), F32)
        G_.fin = ar.alloc((4, 128), BF16)
        G_.ss = ar.alloc((4,), F32)
        for nm in ("diagG", "decQ", "decA", "qk", "qkT", "wT", "u", "vnew", "o1", "o", "sq", "fin", "ss"):
            setattr(G_, "b_" + nm, p.buf())
        G_.b_A = p.bufs(2)
        G_.b_B = p.bufs(2)
        G_.b_P = p.bufs(2)
        grp.append(G_)

    def v512(ap):
        return ap.rearrange("p a b -> p (a b)")

    for c in range(16):
        (qT, kT, ktok, vtok, zs), bi = inr.next()
        b_qT, b_kT, b_ktok, b_vtok, b_zs = bi
        for ap, b, nm in zip((qT, kT, ktok, vtok, zs), bi, names):
            p.dma(ap, getattr(k, nm)[:, c, :, :], reads=[k.b_scr[nm]], writes=[b])
        smt, b_sm = sm.next()
        gc = smt[:, 0:8]
        e24 = smt[:, 8:32]
        eg = smt[:, 8:16]
        egrev = smt[:, 16:24]
        gl = smt[:, 24:32]
        bexp = smt[:, 32:40]
        gcl = smt[:, 40:48]
        pbA, bpA = k.ps.next()
        gsrc = k.GG[:, c, :]
        MM(k, pbA[:, 0:8], k.trif, gsrc, True, True, [k.b_c, k.b_bg], [bpA])
        MM(k, pbA[:, 8:16], k.suf, gsrc, True, True, [k.b_c, k.b_bg], [bpA])
        MM(k, pbA[:, 16:24], k.onesf, gsrc, True, True, [k.b_c, k.b_bg], [bpA])
        CP(k, "dve", gc, pbA[:, 0:8], [bpA], [b_sm])
        ACT(k, e24, pbA[:, 0:24], AF.Exp, [bpA], [b_sm])
        TT(k, "dve", bexp, k.BETA[:, c, :], eg, ALU.mult, [k.b_bg, b_sm], [b_sm])
        ACT(k, gcl, k.BETA[:, c, :], AF.Ln, [k.b_bg], [b_sm])
        TT(k, "dve", gcl, gcl, gc, ALU.add, [b_sm], [b_sm])
        bc = lambda a: a.unsqueeze(2).broadcast_to([128, 8, 128])
        TT(k, "pool", kbg_, ktok, bc(bexp), ALU.mult, [b_ktok, b_sm], [b_kbg])
        TT(k, "pool", vb_, vtok, bc(k.BETA[:, c, :]), ALU.mult, [b_vtok, k.b_bg], [b_vb])
        TT(k, "pool", kdec_, ktok, bc(egrev), ALU.mult, [b_ktok, b_sm], [b_kdec])
        TT(k, "pool", zg_, zs, gnb.unsqueeze(1).broadcast_to([128, 8, 128]), ALU.mult, [b_zs, b_gnb], [b_zg])
        bc4 = lambda a: a.unsqueeze(2).broadcast_to([128, 4, 128])
        for g, G_ in enumerate(grp):
            hs = slice(4 * g, 4 * g + 4)
            TT(k, "dve", G_.diagG, k.identf.unsqueeze(1).broadcast_to([128, 4, 128]), bc4(gc[:, hs]), ALU.mult,
               [k.b_c, b_sm], [G_.b_diagG])
        for g, G_ in enumerate(grp):
            for which, um, dec, b_dec, bias in ((0, k.umq, G_.decQ, G_.b_decQ, gc), (1, k.uma, G_.decA, G_.b_decA, gcl)):
                pR, bpR = k.ps.next()
                for hh in range(4):
                    MM(k, pR[:, hh * 128:(hh + 1) * 128], k.onesf, G_.diagG[:, hh, :], True, False,
                       [k.b_c, G_.b_diagG], [bpR])
                    MM(k, pR[:, hh * 128:(hh + 1) * 128], k.ident, um, False, True, [k.b_c], [bpR])
                for hh in range(4):
                    h = 4 * g + hh
                    ACT(k, dec[:, hh, :], pR[:, hh * 128:(hh + 1) * 128], AF.Exp, [bpR, b_sm], [b_dec],
                        scale=-1.0, bias=bias[:, h:h + 1])
        for g, G_ in enumerate(grp):
            pKK, bKK = k.ps.next()
            pQK, bQK = k.ps.next()
            for hh in range(4):
                h = 4 * g + hh
                MM(k, pKK[:, hh * 128:(hh + 1) * 128], kT[:, h, :], kT[:, h, :], True, True, [b_kT], [bKK])
                MM(k, pQK[:, hh * 128:(hh + 1) * 128], qT[:, h, :], kT[:, h, :], True, True, [b_qT, b_kT], [bQK])
            TT(k, "dve", v512(G_.A[0]), pKK[:], v512(G_.decA), ALU.mult, [bKK, G_.b_decA], [G_.b_A[0]])
            TT(k, "dve", v512(G_.qk), pQK[:], v512(G_.decQ), ALU.mult, [bQK, G_.b_decQ], [G_.b_qk])
        for g, G_ in enumerate(grp):
            for src, bsrc, dst, bdst, eng in ((G_.A[0], G_.b_A[0], G_.B[0], G_.b_B[0], "act"),
                                              (G_.qk, G_.b_qk, G_.qkT, G_.b_qkT, "dve")):
                pT, bpT = k.ps.next()
                pTv = pT[:].bitcast(BF16)
                for hh in range(4):
                    TR(k, pTv[:, hh * 128:(hh + 1) * 128], src[:, hh, :], k.ident, [bsrc], [bpT])
                CP(k, eng, v512(dst), pTv[:, 0:512], [bpT], [bdst])
            TT(k, "pool", G_.P[0], k.ident.unsqueeze(1).broadcast_to([128, 4, 128]), G_.B[0], ALU.subtract,
               [k.b_c, G_.b_B[0]], [G_.b_P[0]])
        for lvl in range(1, 7):
            cur = (lvl - 1) % 2
            nxt = lvl % 2
            for g, G_ in enumerate(grp):
                pA, bA = k.ps.next()
                for hh in range(4):
                    MM(k, pA[:, hh * 128:(hh + 1) * 128], G_.B[cur][:, hh, :], G_.A[cur][:, hh, :], True, True,
                       [G_.b_A[cur], G_.b_B[cur]], [bA])
                if lvl <= 5:
                    pB, bB = k.ps.next()
                    for hh in range(4):
                        MM(k, pB[:, hh * 128:(hh + 1) * 128], G_.A[cur][:, hh, :], G_.B[cur][:, hh, :], True, True,
                           [G_.b_A[cur], G_.b_B[cur]], [bB])
                CP(k, "act", v512(G_.A[nxt]), pA[:], [bA], [G_.b_A[nxt]])
                if lvl <= 5:
                    CP(k, "dve", v512(G_.B[nxt]), pB[:], [bB], [G_.b_B[nxt]])
            for g, G_ in enumerate(grp):
                pP, bP = k.ps.next()
                for hh in range(4):
                    MM(k, pP[:, hh * 128:(hh + 1) * 128], k.ident, G_.P[cur][:, hh, :], True, False,
                       [k.b_c, G_.b_P[cur]], [bP])
                    MM(k, pP[:, hh * 128:(hh + 1) * 128], G_.A[nxt][:, hh, :], G_.P[cur][:, hh, :], False, True,
                       [G_.b_A[nxt], G_.b_P[cur]], [bP])
                CP(k, "dve" if g else "act", v512(G_.P[nxt]), pP[:], [bP], [G_.b_P[nxt]])
        for g, G_ in enumerate(grp):
            TTm = G_.P[0]
            b_TT = G_.b_P[0]
            pW, bW = k.ps.next()
            pU, bU = k.ps.next()
            for hh in range(4):
                h = 4 * g + hh
                MM(k, pW[:, hh * 128:(hh + 1) * 128], kbg_[:, h, :], TTm[:, hh, :], True, True, [b_kbg, b_TT], [bW])
                MM(k, pU[:, hh * 128:(hh + 1) * 128], TTm[:, hh, :], vb_[:, h, :], True, True, [b_vb, b_TT], [bU])
            CP(k, "act", v512(G_.wT), pW[:], [bW], [G_.b_wT])
            CP(k, "dve", v512(G_.u), pU[:], [bU], [G_.b_u])
        for g, G_ in enumerate(grp):
            pVN, bVN = k.ps.next()
            pO1, bO1 = k.ps.next()
            for hh in range(4):
                h = 4 * g + hh
                MM(k, pVN[:, hh * 128:(hh + 1) * 128], G_.wT[:, hh, :], S16[:, h, :], True, True,
                   [G_.b_wT, b_S16[g]], [bVN])
                MM(k, pO1[:, hh * 128:(hh + 1) * 128], qT[:, h, :], S16[:, h, :], True, True, [b_qT, b_S16[g]], [bO1])
            TT(k, "dve", v512(G_.vnew), v512(G_.u), pVN[:], ALU.subtract, [G_.b_u, bVN], [G_.b_vnew])
            TT(k, "dve", G_.o1, pO1[:].rearrange("p (a b) -> p a b", a=4), bc4(eg[:, 4 * g:4 * g + 4]), ALU.mult,
               [bO1, b_sm], [G_.b_o1])
            TT(k, "pool", S32[:, 4 * g:4 * g + 4, :], S32[:, 4 * g:4 * g + 4, :], bc4(gl[:, 4 * g:4 * g + 4]), ALU.mult,
               [b_sm, b_S32[g]], [b_S32[g]])
        for g, G_ in enumerate(grp):
            pO2, bO2 = k.ps.next()
            pSU, bSU = k.ps.next()
            for hh in range(4):
                h = 4 * g + hh
                MM(k, pO2[:, hh * 128:(hh + 1) * 128], G_.qkT[:, hh, :], G_.vnew[:, hh, :], True, True,
                   [G_.b_qkT, G_.b_vnew], [bO2])
                MM(k, pSU[:, hh * 128:(hh + 1) * 128], kdec_[:, h, :], G_.vnew[:, hh, :], True, True,
                   [b_kdec, G_.b_vnew], [bSU])
            TT(k, "dve", v512(S32[:, 4 * g:4 * g + 4, :]), v512(S32[:, 4 * g:4 * g + 4, :]), pSU[:], ALU.add,
               [b_S32[g], bSU], [b_S32[g]])
            CP(k, "act", S16[:, 4 * g:4 * g + 4, :], S32[:, 4 * g:4 * g + 4, :], [b_S32[g]], [b_S16[g]])
            TT(k, "dve", v512(G_.o), pO2[:], v512(G_.o1), ALU.add, [bO2, G_.b_o1], [G_.b_o])
        for g, G_ in enumerate(grp):
            TT(k, "pool", G_.sq, G_.o, G_.o, ALU.mult, [G_.b_o], [G_.b_sq])
            p.op("dve", "tensor_reduce", reads=[G_.b_sq], writes=[G_.b_ss], out=G_.ss, in_=G_.sq, axis=AX.X, op=ALU.add)
            ACT(k, G_.ss, G_.ss, AF.Sqrt, [G_.b_ss], [G_.b_ss], scale=1.0 / 128, bias=EPS)
            p.op("dve", "reciprocal", reads=[G_.b_ss], writes=[G_.b_ss], out=G_.ss, in_=G_.ss)
            TT(k, "dve", G_.sq, G_.o, bc4(G_.ss), ALU.mult, [G_.b_o, G_.b_ss, G_.b_sq], [G_.b_sq])
            TT(k, "pool", G_.fin, G_.sq, zg_[:, 4 * g:4 * g + 4, :], ALU.mult, [G_.b_sq, b_zg], [G_.b_fin])
            pT, bpT = k.ps.next()
            pTv = pT[:].bitcast(BF16)
            for hh in range(4):
                TR(k, pTv[:, hh * 128:(hh + 1) * 128], G_.fin[:, hh, :], k.ident, [G_.b_fin], [bpT])
            CP(k, "act", oT[:, 4 * g:4 * g + 4, c * 128:(c + 1) * 128],
               pTv[:, 0:512].rearrange("p (a b) -> p a b", a=4), [bpT], [k.b_oT])


def phase_moba(k):
    p = k.p
    ar = k.ar
    I = k.I
    ar.off = k.oT_mark
    oT = k.oT
    hr = Ring([([ar.alloc((S,), BF16), ar.alloc((S,), BF16), ar.alloc((16, 132), BF16)], p.bufs(3)) for _ in range(2)])
    for (aps, bs) in hr.items:
        p.op("pool", "memset", writes=[bs[2]], ap=aps[2][:, :, 128:132], constant=1.0)
    tg = ar.alloc((8, 256), F32)
    Tp = ar.alloc((8, 256), BF16)
    rbb = ar.alloc((256,), F32)
    bmax = ar.alloc((8,), F32)
    mgb = ar.alloc((128,), F32)
    b_tp = p.buf()
    p.dma(tg, I["rb_toep"], writes=[b_tp])
    p.dma(rbb, I["rel_bias"].partition_broadcast(128), writes=[b_tp])
    p.dma(mgb, I["moba_norm_g"].partition_broadcast(128), writes=[b_tp])
    rb31 = rbb[:, 31 * 8:32 * 8]
    TT(k, "dve", tg, tg, rb31.unsqueeze(2).broadcast_to([128, 8, 256]), ALU.subtract, [b_tp], [b_tp])
    STT(k, "dve", Tp, tg, SQ128, k.cm.unsqueeze(1).broadcast_to([128, 8, 256]), ALU.mult, ALU.add, [b_tp, k.b_c], [b_tp])
    p.op("dve", "tensor_reduce", reads=[b_tp], writes=[b_tp], out=bmax, in_=rbb.rearrange("p (b h) -> p h b", h=8),
         axis=AX.X, op=ALU.max)
    TT(k, "dve", bmax, bmax, rb31, ALU.subtract, [b_tp], [b_tp])
    sq = ar.alloc((S,), BF16)
    ksq = ar.alloc((S,), BF16)
    b_sq, b_ksq = p.bufs(2)
    sm = ar.alloc((256,), F32)
    b_sm = p.buf()
    kms = sm[:, 0:8]
    km4 = sm[:, 8:12]
    ksc = sm[:, 12:13]
    nm = sm[:, 16:32]
    gsb = sm[:, 32:96].rearrange("p (a b) -> p a b", a=8)
    g2 = sm[:, 96:160].rearrange("p (a b) -> p a b", a=8)
    eq = sm[:, 160:224].rearrange("p (a b) -> p a b", a=8)
    mx = sm[:, 224:232]
    kmT = ar.alloc((8,), BF16)
    MBq = ar.alloc((16, 16), BF16)
    b_MBq = p.buf()
    p.op("pool", "memset", writes=[b_MBq], ap=MBq, constant=0.0)
    MB = ar.alloc((S,), BF16)
    b_MB = p.buf()
    PTr = Ring([(ar.alloc((256,), BF16), p.buf()) for _ in range(5)])
    yq = [ar.alloc((128,), F32) for _ in range(4)]
    b_yq = p.bufs(4)
    junk = ar.alloc((128,), BF16)
    b_junk = p.buf()
    fin = [ar.alloc((256,), BF16) for _ in range(2)]
    b_fin = p.bufs(2)
    sm2 = Ring([(ar.alloc((8,), F32), p.buf()) for _ in range(8)])
    bc8 = lambda a: a.unsqueeze(2).broadcast_to([128, 8, 8])

    for h in range(8):
        (qbT, kbT, vb1), (b_q, b_k, b_v) = hr.next()
        p.dma(qbT, k.QB[h], reads=[k.b_scr["QB"]], writes=[b_q])
        p.dma(kbT, k.KB[h], reads=[k.b_scr["KB"]], writes=[b_k])
        p.dma(vb1[:, :, 0:128], k.VB[h], reads=[k.b_scr["VB"]], writes=[b_v])
        ACT(k, sq, qbT, AF.Square, [b_q], [b_sq])
        ACT(k, ksq, kbT, AF.Square, [b_k], [b_ksq])
        p.op("dve", "tensor_reduce", reads=[b_k], writes=[b_sm], out=kms, in_=kbT.rearrange("p (n t) -> p n t", n=8),
             axis=AX.X, op=ALU.add)
        TS(k, "dve", kmT, kms, 1.0 / 256, None, ALU.mult, None, [b_sm], [b_sm])
        pG, bG = k.ps.next()
        for qi in range(8, 16):
            MM(k, pG[:, (qi - 8) * 8:(qi - 7) * 8], qbT[:, qi * 128:(qi + 1) * 128], kmT, True, True, [b_q, b_sm], [bG])
        pN, bN = k.ps.next()
        for qi in range(16):
            MM(k, pN[:, qi:qi + 1], sq[:, qi * 128:(qi + 1) * 128], k.ones[:, 0:1], True, True, [b_sq, k.b_c], [bN])
        for c in range(4):
            pK, bK = k.ps.next()
            MM(k, pK[:], k.ones, ksq[:, c * 512:(c + 1) * 512], True, True, [b_ksq, k.b_c], [bK])
            p.op("dve", "tensor_reduce", reads=[bK], writes=[b_sm], out=km4[:, c:c + 1], in_=pK[:], axis=AX.X, op=ALU.max)
        p.op("dve", "tensor_reduce", reads=[b_sm], writes=[b_sm], out=ksc, in_=km4, axis=AX.X, op=ALU.max)
        TS(k, "dve", ksc, ksc, 1.0 / 128, None, ALU.mult, None, [b_sm], [b_sm])
        ACT(k, nm, pN[:, 0:16], AF.Sqrt, [bN, b_sm], [b_sm], scale=ksc)
        TS(k, "dve", MBq[:, :, 8], nm, bmax[:, h:h + 1], -SQ128, ALU.add, ALU.mult, [b_sm, b_tp], [b_MBq])
        TT(k, "dve", gsb, pG[:, 0:64].rearrange("p (a b) -> p a b", a=8), k.gm.rearrange("p (a b) -> p a b", a=8),
           ALU.add, [bG, k.b_c], [b_sm])
        src = gsb
        for it in range(2):
            p.op("dve", "tensor_reduce", reads=[b_sm], writes=[b_sm], out=mx, in_=src, axis=AX.X, op=ALU.max)
            TT(k, "dve", eq, src, bc8(mx), ALU.is_equal, [b_sm], [b_sm])
            STT(k, "dve", g2, eq, -1e30, src, ALU.mult, ALU.add, [b_sm], [b_sm])
            src = g2
        p.op("dve", "tensor_reduce", reads=[b_sm], writes=[b_sm], out=mx, in_=g2, axis=AX.X, op=ALU.max)
        TT(k, "dve", eq, gsb, bc8(mx), ALU.is_ge, [b_sm], [b_sm])
        TS(k, "dve", MBq[:, 8:16, 0:8], eq, BIG * SQ128, -BIG * SQ128, ALU.mult, ALU.add, [b_sm], [b_MBq])
        for half in range(2):
            pM, bM = k.ps.next()
            pMv = pM[:].bitcast(BF16)
            for j in range(8):
                qi = half * 8 + j
                TR(k, pMv[0:9, j * 128:(j + 1) * 128], MBq[:, qi, 0:9], k.ident, [b_MBq], [bM])
            CP(k, "act" if half else "dve", MB[0:9, half * 1024:(half + 1) * 1024], pMv[0:9, 0:1024], [bM], [b_MB])
        deferred = []
        for qb in range(8):
            pend = []

            def emit_pv(item):
                kj_, PT_, b_PT_ = item
                for qt in range(2):
                    if kj_ <= 2 * qb + qt:
                        pO, bO = k.psx[qt]
                        MM(k, pO[:, 0:129], PT_[:, qt * 128:(qt + 1) * 128], vb1[:, kj_, 0:129],
                           kj_ == 0, kj_ == 2 * qb + qt, [b_PT_, b_v], [bO])
            for kj in range(2 * qb + 2):
                n = kj // 2
                pS, bS = k.ps.next()
                q0 = qb * 256
                v = n if (qb >= 4 and n < qb) else 8
                if n == qb and kj % 2 == 0:
                    segs = [(0, 256, Tp[:, h, 0:256])]
                elif n == qb:
                    segs = [(128, 256, Tp[:, h, 0:128])]
                elif n == qb - 1 and kj % 2 == 1:
                    segs = [(0, 128, Tp[:, h, 128:256]), (128, 256, None)]
                else:
                    segs = [(0, 256, None)]
                c0 = segs[0][0]
                for (a, b, bias) in segs:
                    MM(k, pS[:, a:b], kbT[:, kj * 128:(kj + 1) * 128], qbT[:, q0 + a:q0 + b], True, False, [b_k, b_q], [bS])
                    MM(k, pS[:, a:b], k.emat[:, v, :], MB[0:9, q0 + a:q0 + b], False, bias is None, [k.b_c, b_MB], [bS])
                    if bias is not None:
                        MM(k, pS[:, a:b], k.ident, bias, False, True, [k.b_c, b_tp], [bS])
                PT, b_PT = PTr.next()
                ACT(k, PT[:, c0:256], pS[:, c0:256], AF.Exp, [bS], [b_PT], scale=1.0 / SQ128)
                pend.append((kj, PT, b_PT))
                if len(pend) > 2:
                    emit_pv(pend.pop(0))
                if kj == min(3, 2 * qb + 1) and deferred:
                    deferred.pop(0)()
            while pend:
                emit_pv(pend.pop(0))
            parts = []
            for qt in range(2):
                s2, b_s2 = sm2.next()
                y_ap, y_b = yq[(qb % 2) * 2 + qt], b_yq[(qb % 2) * 2 + qt]
                pO, bO = k.psx[qt]
                p.op("dve", "reciprocal", reads=[bO], writes=[b_s2], out=s2[:, 0:1], in_=pO[:, 128:129])
                TS(k, "dve", y_ap, pO[:, 0:128], s2[:, 0:1], None, ALU.mult, None, [bO, b_s2], [y_b])
                parts.append((s2, b_s2, y_ap, y_b))

            def part2(qb=qb, parts=parts):
                f_ap, f_b = fin[qb % 2], b_fin[qb % 2]
                for qt in range(2):
                    s2, b_s2, y_ap, y_b = parts[qt]
                    ACT(k, junk, y_ap, AF.Square, [y_b], [b_junk, b_s2], accum_out=s2[:, 1:2])
                    ACT(k, s2[:, 1:2], s2[:, 1:2], AF.Sqrt, [b_s2], [b_s2], scale=1.0 / 128, bias=EPS)
                    p.op("dve", "reciprocal", reads=[b_s2], writes=[b_s2], out=s2[:, 1:2], in_=s2[:, 1:2])
                    STT(k, "dve", f_ap[:, qt * 128:(qt + 1) * 128], y_ap, s2[:, 1:2], mgb, ALU.mult, ALU.mult,
                        [y_b, b_s2, b_tp], [f_b])
                pT, bpT = k.ps.next()
                pTv = pT[:].bitcast(BF16)
                for qt in range(2):
                    TR(k, pTv[:, qt * 128:(qt + 1) * 128], f_ap[:, qt * 128:(qt + 1) * 128], k.ident, [f_b], [bpT])
                CP(k, "act", oT[:, 8 + h, qb * 256:(qb + 1) * 256], pTv[:, 0:256], [bpT], [k.b_oT])
            deferred.append(part2)
        while deferred:
            deferred.pop(0)()
    if hasattr(k, "OT"):
        p.dma(k.OT, oT, reads=[k.b_oT], writes=[k.b_scr["DBG"]])


def phase_wout(k):
    p = k.p
    ar = k.ar
    I = k.I
    oT = k.oT
    ar.off = k.oT_mark
    h2T = ar.alloc((16, S), BF16)
    k.h2T = h2T
    k.b_h2T = p.buf("h2T")
    k.ws_mark = ar.off
    wring = Ring([(ar.alloc((16, 512), BF16), p.buf()) for _ in range(2)])
    xr = Ring([(ar.alloc((512,), F32), p.buf()) for _ in range(3)])
    wsrc = I["w_out"].rearrange("(kt q) n -> q kt n", q=128)
    for c in range(4):
        wap, wb = load_w_block(k, wring, wsrc[:, :, c * 512:(c + 1) * 512])
        for t in range(NT):
            x_ap, x_b = xr.next()
            p.dma(x_ap, I["x"][t * 128:(t + 1) * 128, c * 512:(c + 1) * 512], writes=[x_b])
            pb, bpb = k.ps.next()
            for kt in range(16):
                MM(k, pb[:], oT[:, kt, t * 128:(t + 1) * 128], wap[:, kt, :], kt == 0, kt == 15, [wb, k.b_oT], [bpb])
            TT(k, "dve", x_ap, pb[:], x_ap, ALU.add, [bpb, x_b], [x_b])
            p.dma(k.X1[t * 128:(t + 1) * 128, c * 512:(c + 1) * 512], x_ap, reads=[x_b], writes=[k.b_scr["X1"]])
    p.barrier()
    ar.off = k.ws_mark
    tiles = [(k.X1[t * 128:(t + 1) * 128, :], [k.b_scr["X1"]]) for t in range(NT)]
    norm_T(k, tiles, I["xattn_norm_g"], h2T, k.b_h2T)


def phase_xattn(k):
    p = k.p
    ar = k.ar
    I = k.I
    h2T = k.h2T
    ar.off = 0
    memT = ar.alloc((16, MEM), BF16)
    b_memT = p.buf()
    kxT = ar.alloc((4, MEM), BF16)
    vx = ar.alloc((2, 512), BF16)
    b_kv = p.buf()
    qxT = ar.alloc((4, S), BF16)
    b_qx = p.buf()
    oxT = ar.alloc((4, S), BF16)
    b_ox = p.buf()
    wxo = ar.alloc((4, D), BF16)
    b_wxo = p.buf()
    assert ar.off <= k.oT_mark
    ar.off = k.ws_mark
    tiles = [(I["mem"][t * 128:(t + 1) * 128, :], []) for t in range(2)]
    norm_T(k, tiles, I["mem_norm_g"], memT, b_memT)
    p.barrier()
    ar.off = k.ws_mark
    wring = Ring([(ar.alloc((16, 512), BF16), p.buf()) for _ in range(2)])
    Pf = Ring([(ar.alloc((256,), F32), p.buf()) for _ in range(4)])
    Pn = Ring([(ar.alloc((256,), BF16), p.buf()) for _ in range(4)])
    PnT = Ring([(ar.alloc((2, 128), BF16), p.buf()) for _ in range(4)])
    sm = Ring([(ar.alloc((8,), F32), p.buf()) for _ in range(8)])
    xr = Ring([(ar.alloc((512,), F32), p.buf()) for _ in range(3)])
    wkv = I["w_xkv"].rearrange("(kt q) n -> q kt n", q=128)
    wap, wb = load_w_block(k, wring, wkv[:, :, 0:512])
    for hx in range(4):
        pb, bpb = k.ps.next()
        for kt in range(16):
            MM(k, pb[:, 0:MEM], wap[:, kt, hx * 128:(hx + 1) * 128], memT[:, kt, :], kt == 0, kt == 15, [wb, b_memT], [bpb])
        CP(k, "act" if hx % 2 else "dve", kxT[:, hx, :], pb[:, 0:MEM], [bpb], [b_kv])
    wap, wb = load_w_block(k, wring, wkv[:, :, 512:1024])
    for mt in range(2):
        pb, bpb = k.ps.next()
        for kt in range(16):
            MM(k, pb[:], memT[:, kt, mt * 128:(mt + 1) * 128], wap[:, kt, :], kt == 0, kt == 15, [wb, b_memT], [bpb])
        CP(k, "act" if mt else "dve", vx[:, mt, :], pb[:], [bpb], [b_kv])
    wq = I["w_xq"].rearrange("(kt q) n -> q kt n", q=128)
    wap, wb = load_w_block(k, wring, wq)
    for hx in range(4):
        for c in range(4):
            pb, bpb = k.ps.next()
            for kt in range(16):
                MM(k, pb[:], wap[:, kt, hx * 128:(hx + 1) * 128], h2T[:, kt, c * 512:(c + 1) * 512], kt == 0, kt == 15,
                   [wb, k.b_h2T], [bpb])
            CP(k, "act" if c % 2 else "dve", qxT[:, hx, c * 512:(c + 1) * 512], pb[:], [bpb], [b_qx])
    for t in range(NT):
        st = []
        for hx in range(4):
            pS, bS = k.ps.next()
            MM(k, pS[:, 0:MEM], qxT[:, hx, t * 128:(t + 1) * 128], kxT[:, hx, :], True, True, [b_qx, b_kv], [bS])
            s_ap, s_b = sm.next()
            st.append([pS, bS, s_ap, s_b])
        for hx in range(4):
            pS, bS, s_ap, s_b = st[hx]
            p.op("dve", "tensor_reduce", reads=[bS], writes=[s_b], out=s_ap[:, 0:1], in_=pS[:, 0:MEM], axis=AX.X, op=ALU.max)
            TS(k, "dve", s_ap[:, 1:2], s_ap[:, 0:1], -1.0 / SQ128, None, ALU.mult, None, [s_b], [s_b])
        for hx in range(4):
            pS, bS, s_ap, s_b = st[hx]
            pf, b_pf = Pf.next()
            ACT(k, pf, pS[:, 0:MEM], AF.Exp, [bS, s_b], [b_pf, s_b], scale=1.0 / SQ128, bias=s_ap[:, 1:2],
                accum_out=s_ap[:, 2:3])
            st[hx] += [pf, b_pf]
        for hx in range(4):
            pS, bS, s_ap, s_b, pf, b_pf = st[hx]
            p.op("dve", "reciprocal", reads=[s_b], writes=[s_b], out=s_ap[:, 3:4], in_=s_ap[:, 2:3])
            pn, b_pn = Pn.next()
            TS(k, "dve", pn, pf, s_ap[:, 3:4], None, ALU.mult, None, [b_pf, s_b], [b_pn])
            st[hx] += [pn, b_pn]
        for hx in range(4):
            pn, b_pn = st[hx][6], st[hx][7]
            pT, bpT = k.ps.next()
            pTv = pT[:].bitcast(BF16)
            for mt in range(2):
                TR(k, pTv[:, mt * 128:(mt + 1) * 128], pn[:, mt * 128:(mt + 1) * 128], k.ident, [b_pn], [bpT])
            pnt, b_pnt = PnT.next()
            CP(k, "act" if hx % 2 else "dve", pnt, pTv[:, 0:256].rearrange("p (a b) -> p a b", a=2), [bpT], [b_pnt])
            st[hx] += [pnt, b_pnt]
        for hx in range(4):
            pnt, b_pnt = st[hx][8], st[hx][9]
            pO, bO = k.ps.next()
            for mt in range(2):
                MM(k, pO[:, 0:128], vx[:, mt, hx * 128:(hx + 1) * 128], pnt[:, mt, :], mt == 0, mt == 1, [b_kv, b_pnt], [bO])
            CP(k, "dve" if hx % 2 else "act", oxT[:, hx, t * 128:(t + 1) * 128], pO[:, 0:128], [bO], [b_ox])
    p.dma(wxo, I["w_xo"].rearrange("(kt q) n -> q kt n", q=128), writes=[b_wxo], q="pool")
    for t in range(NT):
        for c in range(4):
            x_ap, x_b = xr.next()
            p.dma(x_ap, k.X1[t * 128:(t + 1) * 128, c * 512:(c + 1) * 512], reads=[k.b_scr["X1"]], writes=[x_b])
            pb, bpb = k.ps.next()
            for kt in range(4):
                MM(k, pb[:], oxT[:, kt, t * 128:(t + 1) * 128], wxo[:, kt, c * 512:(c + 1) * 512], kt == 0, kt == 3,
                   [b_wxo, b_ox], [bpb])
            TT(k, "dve", x_ap, pb[:], x_ap, ALU.add, [bpb, x_b], [x_b])
            p.dma(k.X2[t * 128:(t + 1) * 128, c * 512:(c + 1) * 512], x_ap, reads=[x_b], writes=[k.b_scr["X2"]])


def phase_ffn(k):
    p = k.p
    ar = k.ar
    I = k.I
    HT = 1024
    halo = p.sbuf("halo", [128, NFF, 2], F32)
    b_halo = p.buf()
    p.op("pool", "memset", writes=[b_halo], ap=halo[:], constant=0.0)
    cwf = p.sbuf("cwf", [128, 4 * NFF], F32)
    b_cwf = p.buf()
    for hf in range(2):
        ar.reset()
        aT = ar.alloc((NFF, HT), BF16)
        b_aT = p.buf()
        mark0 = ar.off
        h3T = ar.alloc((16, HT), BF16)
        b_h3T = p.buf()
        mark1 = ar.off
        tiles = [(k.X2[(hf * 8 + t) * 128:(hf * 8 + t + 1) * 128, :], [k.b_scr["X2"]]) for t in range(8)]
        norm_T(k, tiles, I["ffn_norm_g"], h3T, b_h3T)
        p.barrier()
        ar.off = mark1
        if hf == 0:
            stg = ar.alloc((128,), F32)
            b_stg = p.buf()
            for j in range(4):
                src = (I["ffn_conv_w"][j] if j < 3 else I["ffn_conv_b"][0]).rearrange("(b q) -> b q", q=128)
                p.dma(stg[0:NFF, :], src, writes=[b_stg])
                pb, bpb = k.ps.next()
                TR(k, pb[:, 0:NFF], stg[0:NFF, :], k.identf[0:NFF, 0:NFF], [b_stg], [bpb])
                CP(k, "dve", cwf[:, j * NFF:(j + 1) * NFF], pb[:, 0:NFF], [bpb], [b_cwf])
        wg = Ring([(ar.alloc((16, 256), BF16), p.buf()) for _ in range(2)])
        wu = Ring([(ar.alloc((16, 256), BF16), p.buf()) for _ in range(2)])
        graw = Ring([(ar.alloc((HT + 2,), F32), p.buf()) for _ in range(2)])
        gy = Ring([(ar.alloc((HT,), F32), p.buf()) for _ in range(2)])
        wgs = I["w_gate"].rearrange("(kt q) n -> q kt n", q=128)
        wus = I["w_up"].rearrange("(kt q) n -> q kt n", q=128)
        for j in range(NFF):
            if j % 2 == 0:
                g_ap2, g_b = wg.next()
                u_ap2, u_b = wu.next()
                p.dma(g_ap2, wgs[:, :, j * 128:(j + 2) * 128], writes=[g_b], q="pool")
                p.dma(u_ap2, wus[:, :, j * 128:(j + 2) * 128], writes=[u_b], q="pool")
            g_ap = g_ap2[:, :, (j % 2) * 128:(j % 2 + 1) * 128]
            u_ap = u_ap2[:, :, (j % 2) * 128:(j % 2 + 1) * 128]
            r_ap, r_b = graw.next()
            y_ap, y_b = gy.next()
            pus = []
            for c in range(2):
                pg, bg = k.ps.next()
                for kt in range(16):
                    MM(k, pg[:], g_ap[:, kt, :], h3T[:, kt, c * 512:(c + 1) * 512], kt == 0, kt == 15, [g_b, b_h3T], [bg])
                CP(k, "act", r_ap[:, 2 + c * 512:2 + (c + 1) * 512], pg[:], [bg], [r_b])
            for c in range(2):
                pu, bu = k.ps.next()
                for kt in range(16):
                    MM(k, pu[:], u_ap[:, kt, :], h3T[:, kt, c * 512:(c + 1) * 512], kt == 0, kt == 15, [u_b, b_h3T], [bu])
                pus.append((pu, bu))
            CP(k, "pool", r_ap[:, 0:2], halo[:, j, :], [b_halo], [r_b])
            if hf == 0:
                CP(k, "pool", halo[:, j, :], r_ap[:, HT:HT + 2], [r_b], [b_halo])
            TS(k, "dve", y_ap, r_ap[:, 0:HT], cwf[:, j:j + 1], None, ALU.mult, None, [r_b, b_cwf], [y_b])
            for tap in range(1, 3):
                STT(k, "dve", y_ap, r_ap[:, tap:tap + HT], cwf[:, tap * NFF + j:tap * NFF + j + 1], y_ap, ALU.mult, ALU.add,
                    [r_b, b_cwf, y_b], [y_b])
            ACT(k, y_ap, y_ap, AF.Silu, [y_b, b_cwf], [y_b], bias=cwf[:, 3 * NFF + j:3 * NFF + j + 1])
            for c in range(2):
                pu, bu = pus[c]
                TT(k, "dve", aT[:, j, c * 512:(c + 1) * 512], y_ap[:, c * 512:(c + 1) * 512], pu[:], ALU.mult,
                   [y_b, bu], [b_aT])
        p.barrier()
        ar.off = mark0
        x3 = ar.alloc((8, D), F32)
        b_x3 = p.bufs(8)
        aflat = aT.rearrange("p a b -> p (a b)")
        fgb = aflat[:, 0:2 * D].bitcast(F32)
        junk = aflat[:, 2 * D:3 * D]
        b_fgb = b_aT
        b_junk = b_aT
        wd = Ring([(ar.alloc((4, 512), BF16), p.buf()) for _ in range(3)])
        xr = Ring([(ar.alloc((512,), F32), p.buf()) for _ in range(2)])
        ssf = ar.alloc((8,), F32)
        b_ssf = p.buf()
        for cc in range(4):
            for j4 in range(NFF // 4):
                w_ap, w_b = wd.next()
                p.dma(w_ap, I["w_down"][j4 * 512:(j4 + 1) * 512, cc * 512:(cc + 1) * 512].rearrange("(a q) n -> q a n", q=128),
                      writes=[w_b], q="pool")
                for a in range(4):
                    j = j4 * 4 + a
                    for tt in range(8):
                        pb, bpb = k.banks[tt]
                        MM(k, pb[:], aT[:, j, tt * 128:(tt + 1) * 128], w_ap[:, a, :], j == 0, j == NFF - 1, [w_b, b_aT], [bpb])
            for tt in range(8):
                pb, bpb = k.banks[tt]
                x_ap, x_b = xr.next()
                row = (hf * 8 + tt) * 128
                p.dma(x_ap, k.X2[row:row + 128, cc * 512:(cc + 1) * 512], reads=[k.b_scr["X2"]], writes=[x_b])
                TT(k, "dve", x3[:, tt, cc * 512:(cc + 1) * 512], pb[:], x_ap, ALU.add, [bpb, x_b], [b_x3[tt]])
        p.dma(fgb, I["final_norm_g"].partition_broadcast(128), writes=[b_aT])
        outs = []
        for tt in range(8):
            row = (hf * 8 + tt) * 128
            ACT(k, junk, x3[:, tt, :], AF.Square, [b_x3[tt]], [b_junk, b_ssf], accum_out=ssf[:, tt:tt + 1])
            ACT(k, ssf[:, tt:tt + 1], ssf[:, tt:tt + 1], AF.Sqrt, [b_ssf], [b_ssf], scale=1.0 / D, bias=EPS)
            p.op("dve", "reciprocal", reads=[b_ssf], writes=[b_ssf], out=ssf[:, tt:tt + 1], in_=ssf[:, tt:tt + 1])
            STT(k, "dve", x3[:, tt, :], x3[:, tt, :], ssf[:, tt:tt + 1], fgb, ALU.mult, ALU.mult,
                [b_x3[tt], b_ssf, b_fgb], [b_x3[tt]])
            outs.append(p.dma(k.out[row:row + 128, :], x3[:, tt, :], reads=[b_x3[tt]]))
        p.barrier()
    return []


_CACHE = {}


def make_in_maps(inputs, n=8, skip=()):
    cf, cb = host_consts()
    tidx = toeplitz_index()
    rb = np.asarray(inputs["rel_bias"], np.float32)
    rb_toep = np.ascontiguousarray(rb[tidx].transpose(0, 2, 1))
    shared = {
        "mix_norm_g": inputs["mix_norm_g"].reshape(1, D), "xattn_norm_g": inputs["xattn_norm_g"].reshape(1, D),
        "mem_norm_g": inputs["mem_norm_g"].reshape(1, D), "ffn_norm_g": inputs["ffn_norm_g"].reshape(1, D),
        "final_norm_g": inputs["final_norm_g"].reshape(1, D),
        "w_in": inputs["w_in"][0], "gdn_conv_w": inputs["gdn_conv_w"][0], "gdn_a_log": inputs["gdn_a_log"].reshape(1, 8),
        "gdn_dt_bias": inputs["gdn_dt_bias"].reshape(1, 8), "gdn_norm_g": inputs["gdn_norm_g"].reshape(1, 128),
        "moba_norm_g": inputs["moba_norm_g"].reshape(1, 128), "rel_bias": rb.reshape(1, 256), "rb_toep": rb_toep,
        "w_out": inputs["w_out"][0], "w_xq": inputs["w_xq"][0], "w_xkv": inputs["w_xkv"][0], "w_xo": inputs["w_xo"][0],
        "w_gate": inputs["w_gate"][0], "w_up": inputs["w_up"][0], "ffn_conv_w": inputs["ffn_conv_w"][0],
        "ffn_conv_b": inputs["ffn_conv_b"].reshape(1, DFF), "w_down": inputs["w_down"][0],
        "cst_f": cf, "cst_b": cb,
    }
    shared = {kk: (np.zeros((1, 1), np.float32) if kk in skip else np.ascontiguousarray(np.asarray(v, np.float32)))
              for kk, v in shared.items()}
    maps = []
    for b in range(n):
        m = dict(shared)
        m["x"] = np.ascontiguousarray(np.asarray(inputs["x"][b], np.float32))
        m["mem"] = (np.zeros((1, 1), np.float32) if "mem" in skip
                    else np.ascontiguousarray(np.asarray(inputs["mem"][b], np.float32)))
        maps.append(m)
    return maps


def kernel(**inputs):
    if "nc" not in _CACHE:
        _CACHE["nc"] = build_program()[0]
    nc = _CACHE["nc"]
    maps = make_in_maps(inputs, 8)
    res = run_bass_kernel_spmd(nc, maps, core_ids=list(range(8)))
    return np.stack([np.asarray(r["out"], np.float32) for r in res.results], axis=0)
```

```python
import bisect
import math
from contextlib import ExitStack

import numpy as np
import concourse.bass as bass
import concourse.mybir as mybir
from concourse.bass_utils import run_bass_kernel_spmd

F32 = mybir.dt.float32
BF16 = mybir.dt.bfloat16
ALU = mybir.AluOpType
AF = mybir.ActivationFunctionType
AX = mybir.AxisListType

S = 2048
D = 2048
NT = 16
H = 8
HD = 128
DFF = 5632
NFF = 44
MEM = 256
INW = 7184
EPS = 1e-6
BIG = 30000.0
SQ128 = math.sqrt(128.0)

ENGS = ("pe", "act", "dve", "pool", "sp")
SEM_LIMIT = 2000
N_ENG_SEMS = {"pe": 14, "act": 5, "dve": 5, "pool": 3, "sp": 1}
N_DMA_SEMS = 32
N_SWDMA_SEMS = 16


class Buf:
    __slots__ = ("name", "last_w", "creads", "dreads")

    def __init__(self, name=""):
        self.name = name
        self.last_w = None
        self.creads = {}
        self.dreads = []


class Op:
    __slots__ = ("eng", "name", "kw", "waits", "signal", "pos")

    def __init__(self, eng, name, kw, pos):
        self.eng = eng
        self.name = name
        self.kw = kw
        self.waits = []
        self.signal = None
        self.pos = pos


class Prog:
    def __init__(self, nc, stack):
        self.nc = nc
        self.stack = stack
        self.ops = {e: [] for e in ENGS}
        self.nsig = {e: 0 for e in ENGS}
        self.sigpos = {e: [] for e in ENGS}
        self.sigtok = {e: [] for e in ENGS}
        self.waited = {e: {} for e in ENGS}
        self.esems = {e: [stack.enter_context(nc.semaphore(f"s_{e}_{i}")) for i in range(N_ENG_SEMS[e])]
                      for e in ENGS}
        self.dsems = {"hw": [stack.enter_context(nc.semaphore(f"s_dma_{i}")) for i in range(N_DMA_SEMS)],
                      "sw": [stack.enter_context(nc.semaphore(f"s_swdma_{i}")) for i in range(N_SWDMA_SEMS)]}
        self.ndma_q = {"hw": 0, "sw": 0}
        self.ndma = 0
        self.dma_toks = []
        self.pe_prev = (None, None, True)

    def sbuf(self, name, shape, dtype):
        return self.stack.enter_context(self.nc.sbuf_tensor(name, list(shape), dtype))

    def psum(self, name, shape, dtype):
        return self.stack.enter_context(self.nc.psum_tensor(name, list(shape), dtype))

    def buf(self, name=""):
        return Buf(name)

    def bufs(self, n):
        return [Buf() for _ in range(n)]

    def _force_signal(self, eng):
        lst = self.ops[eng]
        if not lst:
            return None
        last = lst[-1]
        if last.signal is None:
            n = self.nsig[eng]
            self.nsig[eng] += 1
            sem = self.esems[eng][n // SEM_LIMIT]
            val = n % SEM_LIMIT + 1
            last.signal = (sem, 1)
            self.sigpos[eng].append(last.pos)
            self.sigtok[eng].append((sem, val))
        return last

    def _resolve(self, tok):
        if tok[0] == "d":
            return tok[1], tok[2]
        _, eng, pos = tok
        sp = self.sigpos[eng]
        i = bisect.bisect_left(sp, pos)
        if i < len(sp):
            return self.sigtok[eng][i]
        last = self._force_signal(eng)
        assert last.pos >= pos
        i = bisect.bisect_left(sp, pos)
        return self.sigtok[eng][i]

    def _add_wait(self, op, tok):
        sem, val = self._resolve(tok)
        w = self.waited[op.eng]
        key = id(sem)
        if w.get(key, 0) >= val:
            return
        w[key] = val
        for i, (s, v) in enumerate(op.waits):
            if s is sem:
                op.waits[i] = (sem, max(v, val))
                return
        op.waits.append((sem, val))

    def _deps(self, op, reads, writes, is_dma):
        eng = op.eng
        for b in reads:
            if b.last_w is not None:
                self._add_wait(op, b.last_w)
        for b in writes:
            t = b.last_w
            if t is not None and not (t[0] == "c" and t[1] == eng == "pe" and not is_dma):
                self._add_wait(op, t)
            for e, t in b.creads.items():
                if e == eng == "pe" and not is_dma:
                    continue
                self._add_wait(op, t)
            for t in b.dreads:
                self._add_wait(op, t)

    def _compute_pos_check(self, eng):
        lst = self.ops[eng]
        if lst and lst[-1].signal is None and lst[-1].name not in ("nop", "dma_start"):
            self._force_signal(eng)

    def op(self, eng, name, reads=(), writes=(), **kw):
        lst = self.ops[eng]
        rset = frozenset(id(b) for b in reads)
        wset = frozenset(id(b) for b in writes)
        if lst and lst[-1].name not in ("nop", "dma_start") and lst[-1].signal is None:
            prev = lst[-1]
            if eng != "pe":
                self._force_signal(eng)
            else:
                pr, pw, pdone = self.pe_prev
                if pr != rset or (pw != wset and pdone):
                    self._force_signal(eng)
        if eng == "pe":
            self.pe_prev = (rset, wset, kw.get("stop", True))
        o = Op(eng, name, kw, len(lst))
        self._deps(o, reads, writes, False)
        lst.append(o)
        tok = ("c", eng, o.pos)
        for b in reads:
            b.creads[eng] = tok
        for b in writes:
            b.last_w = tok
            b.creads = {}
            b.dreads = []
        return o

    def dma(self, out, in_, reads=(), writes=(), q="sp", **kw):
        if q != "sp":
            self._compute_pos_check(q)
        lst = self.ops[q]
        o = Op(q, "dma_start", dict(out=out, in_=in_, **kw), len(lst))
        self._deps(o, reads, writes, True)
        kind = "sw" if q == "pool" else "hw"
        pool_ = self.dsems[kind]
        i = self.ndma_q[kind] % len(pool_)
        r = self.ndma_q[kind] // len(pool_)
        self.ndma_q[kind] += 1
        self.ndma += 1
        sem = pool_[i]
        if r > 0:
            self._add_wait(o, ("d", sem, 16 * r))
        o.signal = (sem, 16)
        lst.append(o)
        tok = ("d", sem, 16 * (r + 1))
        self.dma_toks.append(tok)
        for b in reads:
            b.dreads.append(tok)
        for b in writes:
            b.last_w = tok
            b.creads = {}
            b.dreads = []
        return tok

    def barrier(self):
        toks = list(self.dma_toks[-(N_DMA_SEMS + N_SWDMA_SEMS) * 2:])
        for e in ENGS:
            lst = self.ops[e]
            if lst and lst[-1].name not in ("dma_start", "nop"):
                last = self._force_signal(e)
                toks.append(("c", e, last.pos))
            else:
                for o in reversed(lst):
                    if o.name not in ("dma_start", "nop"):
                        assert o.signal is not None
                        toks.append(("c", e, o.pos))
                        break
        for e in ENGS:
            o = Op(e, "nop", {}, len(self.ops[e]))
            for t in toks:
                self._add_wait(o, t)
            self.ops[e].append(o)

    def final_wait(self, eng, toks):
        o = Op(eng, "nop", {}, len(self.ops[eng]))
        for t in toks:
            self._add_wait(o, t)
        self.ops[eng].append(o)

    def emit(self):
        nc = self.nc
        prog = self
        with nc.Block() as block:
            def run(engname):
                def body(e):
                    for o in prog.ops[engname]:
                        for (sem, val) in o.waits:
                            e.wait_ge(sem, val)
                        if o.name == "nop":
                            continue
                        ins = getattr(e, o.name)(**o.kw)
                        if o.signal is not None:
                            ins.then_inc(o.signal[0], o.signal[1])
                return body
            block.tensor(run("pe"))
            block.scalar(run("act"))
            block.vector(run("dve"))
            block.gpsimd(run("pool"))
            block.sync(run("sp"))

    def stats(self):
        return {e: (len(self.ops[e]), self.nsig[e]) for e in ENGS}, self.ndma


class Arena:
    def __init__(self, p, nbytes):
        self.t = p.sbuf("arena", [128, nbytes // 2], BF16)
        self.n = nbytes
        self.off = 0

    def reset(self):
        self.off = 0

    def alloc(self, shape, dtype):
        esz = 2 if dtype == BF16 else 4
        n = 1
        for s in shape:
            n *= s
        nb = n * esz
        a = self.t[:, self.off // 2:(self.off + nb) // 2]
        self.off += (nb + 63) // 64 * 64
        assert self.off <= self.n, f"arena overflow {self.off} > {self.n}"
        if dtype != BF16:
            a = a.bitcast(dtype)
        if len(shape) == 2:
            a = a.rearrange("p (a b) -> p a b", a=shape[0])
        elif len(shape) == 3:
            a = a.rearrange("p (a b c) -> p a b c", a=shape[0], b=shape[1])
        return a


class Ring:
    def __init__(self, items):
        self.items = items
        self.i = 0

    def next(self):
        it = self.items[self.i % len(self.items)]
        self.i += 1
        return it


NCF = 576
NCB = 768 + 9 * 128


def host_consts():
    cf = np.zeros((128, NCF), np.float32)
    ii = np.arange(128)
    cf[:, 0:128] = np.eye(128, dtype=np.float32)
    cf[:, 128:256] = (ii[:, None] <= ii[None, :]).astype(np.float32)
    cf[:, 256:384] = (ii[:, None] > ii[None, :]).astype(np.float32)
    cf[:, 384:512] = 1.0
    gm = np.zeros((128, 8, 8), np.float32)
    for qi in range(8, 16):
        own = qi // 2
        gm[:, qi - 8, own:] = -1e30
    cf[:, 512:576] = gm.reshape(128, 64)
    cb = np.zeros((128, NCB), np.float32)
    cb[:, 0:128] = np.eye(128, dtype=np.float32)
    cb[:, 128:256] = 1.0
    cb[:, 256:384] = (ii[None, :] > ii[:, None]).astype(np.float32) * BIG
    cb[:, 384:512] = (ii[None, :] >= ii[:, None]).astype(np.float32) * BIG
    rr = np.arange(256)
    cb[:, 512:768] = (rr[None, :] < ii[:, None]).astype(np.float32) * (-BIG * SQ128)
    e = np.zeros((9, 9, 128), np.float32)
    for v in range(9):
        e[8, v, :] = 1.0
        if v < 8:
            e[v, v, :] = 1.0
    cb[0:9, 768:768 + 9 * 128] = e.reshape(9, 9 * 128)
    return cf, cb


def rel_bucket_np(n):
    n = np.maximum(n, 0)
    max_exact = 16
    nf = np.maximum(n, 1).astype(np.float32)
    large = max_exact + (np.log(nf / max_exact) / math.log(128 / max_exact) * (32 - max_exact)).astype(np.int32)
    large = np.minimum(large, 31)
    return np.where(n < max_exact, n, large)


def toeplitz_index():
    kk = np.arange(128)[:, None]
    r = np.arange(256)[None, :]
    return rel_bucket_np(r - kk)


class K:
    pass


def build_program(debug_phase=None):
    nc = bass.Bass("TRN2", target_bir_lowering=False)
    k = K()
    k.nc = nc
    dbg = debug_phase is not None

    order = ["inproj", "gdn", "moba", "wout", "xattn", "ffn"]
    need_from = {"mem": "xattn", "w_out": "wout", "w_xq": "xattn", "w_xkv": "xattn", "w_xo": "xattn",
                 "w_gate": "ffn", "w_up": "ffn", "w_down": "ffn"}
    k.skip = set()
    if dbg:
        for nm, ph in need_from.items():
            if order.index(ph) > order.index(debug_phase):
                k.skip.add(nm)

    def din(name, shape, dt=F32):
        if name in k.skip:
            shape = [1, 1]
        return nc.dram_tensor(name, list(shape), dt, kind="ExternalInput").ap()

    DUMP = {"inproj": ("QA", "KA", "KT", "VA", "ZS", "QB", "KB", "VB", "DBG"), "moba": ("OT",), "gdn": ("OT",),
            "wout": ("X1",), "xattn": ("X2",), "ffn": ("OT", "X1", "X2")}.get(debug_phase, ())

    def dscr(name, shape, dt=BF16):
        kind = "ExternalOutput" if name in DUMP else "Internal"
        return nc.dram_tensor(name, list(shape), dt, kind=kind).ap()

    I = {}
    I["x"] = din("x", [S, D])
    I["mem"] = din("mem", [MEM, D])
    for nm in ("mix_norm_g", "xattn_norm_g", "mem_norm_g", "ffn_norm_g", "final_norm_g"):
        I[nm] = din(nm, [1, D])
    I["w_in"] = din("w_in", [D, INW])
    I["gdn_conv_w"] = din("gdn_conv_w", [4, 3072])
    I["gdn_a_log"] = din("gdn_a_log", [1, 8])
    I["gdn_dt_bias"] = din("gdn_dt_bias", [1, 8])
    I["gdn_norm_g"] = din("gdn_norm_g", [1, 128])
    I["moba_norm_g"] = din("moba_norm_g", [1, 128])
    I["rel_bias"] = din("rel_bias", [1, 256])
    I["rb_toep"] = din("rb_toep", [128, 8, 256])
    I["w_out"] = din("w_out", [D, D])
    I["w_xq"] = din("w_xq", [D, 512])
    I["w_xkv"] = din("w_xkv", [D, 1024])
    I["w_xo"] = din("w_xo", [512, D])
    I["w_gate"] = din("w_gate", [D, DFF])
    I["w_up"] = din("w_up", [D, DFF])
    I["ffn_conv_w"] = din("ffn_conv_w", [3, DFF])
    I["ffn_conv_b"] = din("ffn_conv_b", [1, DFF])
    I["w_down"] = din("w_down", [DFF, D])
    I["cst_f"] = din("cst_f", [128, NCF])
    I["cst_b"] = din("cst_b", [128, NCB])
    out = nc.dram_tensor("out", [S, D], F32, kind="ExternalOutput").ap()
    k.I = I
    k.out = out

    k.QA = dscr("QA", [128, 16, 8, 128])
    k.KA = dscr("KA", [128, 16, 8, 128])
    k.KT = dscr("KT", [128, 16, 8, 128])
    k.VA = dscr("VA", [128, 16, 8, 128])
    k.ZS = dscr("ZS", [128, 16, 8, 128])
    k.QB = dscr("QB", [8, 128, 2048])
    k.KB = dscr("KB", [8, 128, 2048])
    k.VB = dscr("VB", [8, 128, 16, 128])
    k.X1 = dscr("X1", [S, D], F32)
    k.X2 = dscr("X2", [S, D], F32)
    if "DBG" in DUMP:
        k.DBG = dscr("DBG", [128, 4096], F32)
    if "OT" in DUMP:
        k.OT = dscr("OT", [128, 16, S], BF16)
    k.b_scr = {nm: Buf(nm) for nm in ("QA", "KA", "KT", "VA", "ZS", "QB", "KB", "VB", "X1", "X2", "DBG")}

    with ExitStack() as st:
        p = Prog(nc, st)
        k.p = p
        k.cf = p.sbuf("cf", [128, NCF], F32)
        k.cb = p.sbuf("cb", [128, NCB], BF16)
        k.b_c = p.buf("consts")
        p.dma(k.cf[:], I["cst_f"], writes=[k.b_c])
        p.dma(k.cb[:], I["cst_b"], writes=[k.b_c], q="pool")
        k.identf = k.cf[:, 0:128]
        k.trif = k.cf[:, 128:256]
        k.suf = k.cf[:, 256:384]
        k.onesf = k.cf[:, 384:512]
        k.gm = k.cf[:, 512:576]
        k.ident = k.cb[:, 0:128]
        k.ones = k.cb[:, 128:256]
        k.umq = k.cb[:, 256:384]
        k.uma = k.cb[:, 384:512]
        k.cm = k.cb[:, 512:768]
        k.emat = k.cb[0:9, 768:768 + 9 * 128].rearrange("p (v n) -> p v n", v=9)
        k.BA = p.sbuf("BA", [128, 16, 16], F32)
        k.b_BA = p.buf("BA")
        k.BETA = p.sbuf("BETA", [128, 16, 8], F32)
        k.GG = p.sbuf("GG", [128, 16, 8], F32)
        k.b_bg = p.buf("betag")
        banks = [(p.psum(f"ps{i}", [128, 512], F32), p.buf(f"ps{i}")) for i in range(8)]
        k.ps = Ring(banks[0:6])
        k.psx = banks[6:8]
        k.banks = banks
        k.ar = Arena(p, 176 * 1024)

        phases = [phase_inproj, phase_gdn, phase_moba, phase_wout, phase_xattn, phase_ffn]
        names = ["inproj", "gdn", "moba", "wout", "xattn", "ffn"]
        out_toks = []
        for fn, nm in zip(phases, names):
            r = fn(k)
            if r:
                out_toks += r
            p.barrier()
            if debug_phase == nm:
                break
        p.final_wait("sp", p.dma_toks[-(N_DMA_SEMS + N_SWDMA_SEMS) * 2:])
        k.stats = p.stats()
        p.emit()
    return nc, k


def MM(k, out, lhsT, rhs, start, stop, r, w):
    k.p.op("pe", "matmul", reads=r, writes=w, out=out, lhsT=lhsT, rhs=rhs, start=start, stop=stop)


def TR(k, out, in_, ident, r, w):
    k.p.op("pe", "transpose", reads=r + [k.b_c], writes=w, out=out, in_=in_, identity=ident)


def ACT(k, out, in_, func, r, w, **kw):
    k.p.op("act", "activation", reads=r, writes=w, out=out, in_=in_, func=func, **kw)


def CP(k, eng, out, in_, r, w):
    if eng == "act":
        k.p.op("act", "copy", reads=r, writes=w, out=out, in_=in_)
    else:
        k.p.op(eng, "tensor_copy", reads=r, writes=w, out=out, in_=in_)


def TT(k, eng, out, in0, in1, op, r, w):
    k.p.op(eng, "tensor_tensor", reads=r, writes=w, out=out, in0=in0, in1=in1, op=op)


def TS(k, eng, out, in0, s1, s2, op0, op1, r, w):
    if s2 is None:
        k.p.op(eng, "tensor_scalar", reads=r, writes=w, out=out, in0=in0, scalar1=s1, scalar2=None, op0=op0)
    else:
        k.p.op(eng, "tensor_scalar", reads=r, writes=w, out=out, in0=in0, scalar1=s1, scalar2=s2, op0=op0, op1=op1)


def STT(k, eng, out, in0, scalar, in1, op0, op1, r, w):
    k.p.op(eng, "scalar_tensor_tensor", reads=r, writes=w, out=out, in0=in0, scalar=scalar, in1=in1,
           op0=op0, op1=op1)


def norm_T(k, tiles, gain, dstT, b_dst, col0=0):
    p = k.p
    ar = k.ar
    gb = ar.alloc((D,), F32)
    b_gb = p.buf()
    p.dma(gb, gain.partition_broadcast(128), writes=[b_gb])
    xt = [ar.alloc((D,), F32) for _ in range(2)]
    b_xt = p.bufs(2)
    junk = ar.alloc((D,), BF16)
    b_junk = p.buf()
    hb = [ar.alloc((D,), BF16) for _ in range(2)]
    b_hb = p.bufs(2)
    ss = ar.alloc((len(tiles),), F32)
    b_ss = p.buf()
    pend = []
    for t, (src, rb) in enumerate(tiles):
        i = t % 2
        p.dma(xt[i], src, reads=rb, writes=[b_xt[i]])
        ACT(k, junk, xt[i], AF.Square, [b_xt[i]], [b_junk, b_ss], accum_out=ss[:, t:t + 1])
        ACT(k, ss[:, t:t + 1], ss[:, t:t + 1], AF.Sqrt, [b_ss], [b_ss], scale=1.0 / D, bias=EPS)
        p.op("dve", "reciprocal", reads=[b_ss], writes=[b_ss], out=ss[:, t:t + 1], in_=ss[:, t:t + 1])
        STT(k, "dve", hb[i], xt[i], ss[:, t:t + 1], gb, ALU.mult, ALU.mult, [b_xt[i], b_ss, b_gb], [b_hb[i]])

        def stage_b(t=t, i=i):
            for k4 in range(4):
                pb, bpb = k.ps.next()
                pbv = pb[:].bitcast(BF16)
                for j in range(4):
                    kt = k4 * 4 + j
                    TR(k, pbv[:, j * 128:(j + 1) * 128], hb[i][:, kt * 128:(kt + 1) * 128], k.ident, [b_hb[i]], [bpb])
                CP(k, "act" if k4 % 2 else "dve",
                   dstT[:, k4 * 4:(k4 + 1) * 4, col0 + t * 128:col0 + (t + 1) * 128],
                   pbv[:, 0:512].rearrange("p (a b) -> p a b", a=4), [bpb], [b_dst])
        if pend:
            pend.pop(0)()
        pend.append(stage_b)
    while pend:
        pend.pop(0)()


def load_w_block(k, ring, src):
    ap, b = ring.next()
    k.p.dma(ap, src, writes=[b], q="pool")
    return ap, b


def phase_inproj(k):
    p = k.p
    ar = k.ar
    I = k.I
    ar.reset()
    hT = ar.alloc((16, S), BF16)
    b_hT = p.buf("hT")
    k.hT = hT
    mark = ar.off
    tiles = [(I["x"][t * 128:(t + 1) * 128, :], []) for t in range(NT)]
    norm_T(k, tiles, I["mix_norm_g"], hT, b_hT)
    p.barrier()
    ar.off = mark
    wring = Ring([(ar.alloc((16, 512), BF16), p.buf()) for _ in range(2)])
    wba = ar.alloc((16, 16), BF16)
    b_wba = p.buf()
    raw = [ar.alloc((S + 4,), BF16) for _ in range(2)]
    b_raw = p.bufs(2)
    dgr = Ring([(ar.alloc((4, 128), BF16), p.buf()) for _ in range(2)])
    yr = Ring([(ar.alloc((S,), F32), p.buf()) for _ in range(2)])
    sqr = Ring([(ar.alloc((S,), BF16), p.buf()) for _ in range(2)])
    rsr = Ring([(ar.alloc((S,), F32), p.buf()) for _ in range(2)])
    obf = [ar.alloc((S,), BF16) for _ in range(2)]
    b_obf = p.bufs(2)
    tst = [ar.alloc((16, 128), BF16) for _ in range(2)]
    b_tst = p.bufs(2)
    zst = [ar.alloc((512,), BF16) for _ in range(3)]
    b_zst = p.bufs(3)
    cw = ar.alloc((96,), F32)
    cwl = ar.alloc((128,), F32)
    b_cw = p.buf()
    small = ar.alloc((64,), F32)
    b_small = p.buf()
    p.dma(cwl[0:96, :], I["gdn_conv_w"].rearrange("j (b q) -> (j b) q", q=128), writes=[b_cw])
    pb, bpb = k.ps.next()
    TR(k, pb[:, 0:96], cwl[0:96, :], k.identf[0:96, 0:96], [b_cw], [bpb])
    CP(k, "dve", cw, pb[:, 0:96], [bpb], [b_cw])
    for i in range(2):
        p.op("pool", "memset", writes=[b_raw[i]], ap=raw[i][:, 0:4], constant=0.0)

    wsrc = I["w_in"].rearrange("(kt q) n -> q kt n", q=128)
    ri = 0
    oi = 0
    ti = 0

    def fblock(wap, wb, f, evac):
        for c in range(4):
            pb, bpb = k.ps.next()
            for kt in range(16):
                MM(k, pb[:], wap[:, kt, f * 128:(f + 1) * 128], hT[:, kt, c * 512:(c + 1) * 512],
                   kt == 0, kt == 15, [wb, b_hT], [bpb])
            evac(pb, bpb, c * 512)

    deferred = []

    def make_post(fb, kind, hd, r_ap, r_b):
        st = {}

        def stage_a():
            nonlocal oi
            y, b_y = yr.next()
            st["y"] = (y, b_y)
            o_ap, o_b = obf[oi % 2], b_obf[oi % 2]
            oi += 1
            st["o"] = (o_ap, o_b)
            dg, b_dg = dgr.next()
            for j in range(4):
                TS(k, "dve", dg[:, j, :], k.identf, cw[:, j * 24 + fb:j * 24 + fb + 1], None, ALU.mult, None,
                   [k.b_c, b_cw], [b_dg])
            for c in range(4):
                pb, bpb = k.ps.next()
                for j in range(4):
                    MM(k, pb[:], dg[:, j, :], r_ap[:, c * 512 + j:c * 512 + j + 512], j == 0, j == 3, [b_dg, r_b], [bpb])
                if kind == 2:
                    ACT(k, o_ap[:, c * 512:(c + 1) * 512], pb[:], AF.Silu, [bpb], [o_b])
                else:
                    ACT(k, y[:, c * 512:(c + 1) * 512], pb[:], AF.Silu, [bpb], [b_y])
            if kind != 2:
                sq, b_sq = sqr.next()
                st["sq"] = (sq, b_sq)
                ACT(k, sq, y, AF.Square, [b_y], [b_sq])

        def stage_b():
            nonlocal ti
            y, b_y = st["y"]
            o_ap, o_b = st["o"]
            if kind != 2:
                sq, b_sq = st["sq"]
                rs, b_rs = rsr.next()
                for c in range(4):
                    pb, bpb = k.ps.next()
                    MM(k, pb[:], k.ones, sq[:, c * 512:(c + 1) * 512], True, True, [b_sq, k.b_c], [bpb])
                    if kind == 0:
                        ACT(k, rs[:, c * 512:(c + 1) * 512], pb[:], AF.Ln, [bpb], [b_rs], scale=128.0,
                            bias=128.0 * EPS)
                    else:
                        ACT(k, rs[:, c * 512:(c + 1) * 512], pb[:], AF.Ln, [bpb], [b_rs], scale=1.0, bias=EPS)
                ACT(k, rs, rs, AF.Exp, [b_rs], [b_rs], scale=-0.5)
                TT(k, "dve", o_ap, y, rs, ALU.mult, [b_y, b_rs], [o_b])
            if kind == 0:
                p.dma(k.QA[:, :, hd, :], o_ap.rearrange("p (c t) -> p c t", c=16), reads=[o_b],
                      writes=[k.b_scr["QA"]])
            if kind == 1:
                p.dma(k.KA[:, :, hd, :], o_ap.rearrange("p (c t) -> p c t", c=16), reads=[o_b],
                      writes=[k.b_scr["KA"]])
            if kind >= 1:
                t_ap, t_b = tst[ti % 2], b_tst[ti % 2]
                ti += 1
                for c4 in range(4):
                    pb, bpb = k.ps.next()
                    pbv = pb[:].bitcast(BF16)
                    for j in range(4):
                        c = c4 * 4 + j
                        TR(k, pbv[:, j * 128:(j + 1) * 128], o_ap[:, c * 128:(c + 1) * 128], k.ident, [o_b], [bpb])
                    CP(k, "act" if c4 % 2 else "dve", t_ap[:, c4 * 4:(c4 + 1) * 4, :],
                       pbv[:, 0:512].rearrange("p (a b) -> p a b", a=4), [bpb], [t_b])
                dst = k.KT if kind == 1 else k.VA
                p.dma(dst[:, :, hd, :], t_ap, reads=[t_b], writes=[k.b_scr["KT" if kind == 1 else "VA"]])
        return stage_a, stage_b

    for blk in range(6):
        wap, wb = load_w_block(k, wring, wsrc[:, :, blk * 512:(blk + 1) * 512])
        for f in range(4):
            fb = blk * 4 + f
            kind = fb // 8
            hd = fb % 8
            r_ap, r_b = raw[ri % 2], b_raw[ri % 2]
            ri += 1
            prev = deferred.pop(0) if deferred else None

            def evac_raw(pb, bpb, tok0, r_ap=r_ap, r_b=r_b, prev=prev):
                CP(k, "act", r_ap[:, 3 + tok0:3 + tok0 + 512], pb[:], [bpb], [r_b])
                if tok0 == 512 and prev is not None:
                    prev[0]()
            fblock(wap, wb, f, evac_raw)
            if prev is not None:
                prev[1]()
            deferred.append(make_post(fb, kind, hd, r_ap, r_b))
    for a_, b_ in deferred:
        a_()
        b_()
    zi = 0
    for blk in range(2):
        wap, wb = load_w_block(k, wring, wsrc[:, :, 3072 + blk * 512:3072 + (blk + 1) * 512])
        for t in range(NT):
            pb, bpb = k.ps.next()
            for kt in range(16):
                MM(k, pb[:], hT[:, kt, t * 128:(t + 1) * 128], wap[:, kt, :], kt == 0, kt == 15, [wb, b_hT], [bpb])
            z_ap, z_b = zst[zi % 3], b_zst[zi % 3]
            zi += 1
            ACT(k, z_ap, pb[:], AF.Silu, [bpb], [z_b])
            p.dma(k.ZS[:, t, blk * 4:(blk + 1) * 4, :], z_ap.rearrange("p (h d) -> p h d", h=4), reads=[z_b],
                  writes=[k.b_scr["ZS"]])
    p.dma(wba, wsrc[:, :, 4096:4112], writes=[b_wba], q="pool")
    for t in range(NT):
        pb, bpb = k.ps.next()
        for kt in range(16):
            MM(k, pb[:, 0:16], hT[:, kt, t * 128:(t + 1) * 128], wba[:, kt, :], kt == 0, kt == 15, [b_wba, b_hT], [bpb])
        CP(k, "dve", k.BA[:, t, :], pb[:, 0:16], [bpb], [k.b_BA])
    albc = small[:, 0:8]
    dtbc = small[:, 8:16]
    nea = small[:, 16:24]
    p.dma(albc, I["gdn_a_log"].partition_broadcast(128), writes=[b_small])
    p.dma(dtbc, I["gdn_dt_bias"].partition_broadcast(128), writes=[b_small])
    ACT(k, nea, albc, AF.Exp, [b_small], [b_small])
    TS(k, "dve", nea, nea, -1.0, None, ALU.mult, ALU.bypass, [b_small], [b_small])
    ACT(k, k.BETA[:], k.BA[:, :, 0:8], AF.Sigmoid, [k.b_BA], [k.b_bg])
    xa = ar.alloc((16, 8), F32)
    xb = ar.alloc((16, 8), F32)
    b_xa = p.buf()
    TT(k, "dve", xa, k.BA[:, :, 8:16], dtbc.unsqueeze(1).broadcast_to([128, 16, 8]), ALU.add, [k.b_BA, b_small], [b_xa])
    ACT(k, xb, xa, AF.Abs, [b_xa], [b_xa])
    ACT(k, xb, xb, AF.Exp, [b_xa], [b_xa], scale=-1.0)
    ACT(k, xb, xb, AF.Ln, [b_xa], [b_xa], bias=1.0)
    TS(k, "dve", xa, xa, 0.0, None, ALU.max, ALU.bypass, [b_xa], [b_xa])
    TT(k, "dve", xa, xa, xb, ALU.add, [b_xa], [b_xa])
    TT(k, "dve", k.GG[:], xa, nea.unsqueeze(1).broadcast_to([128, 16, 8]), ALU.mult, [b_xa, b_small], [k.b_bg])

    for blk in range(4):
        wap, wb = load_w_block(k, wring, wsrc[:, :, 4112 + blk * 512:4112 + (blk + 1) * 512])
        for f in range(4):
            fb = blk * 4 + f
            kind = fb // 8
            hd = fb % 8
            o_ap, o_b = obf[oi % 2], b_obf[oi % 2]
            oi += 1

            def evac_o(pb, bpb, tok0, o_ap=o_ap, o_b=o_b, c=[0]):
                CP(k, "act" if (tok0 // 512) % 2 else "dve", o_ap[:, tok0:tok0 + 512], pb[:], [bpb], [o_b])
            fblock(wap, wb, f, evac_o)
            dst = k.QB if kind == 0 else k.KB
            p.dma(dst[hd], o_ap, reads=[o_b], writes=[k.b_scr["QB" if kind == 0 else "KB"]])
    for blk in range(2):
        wap, wb = load_w_block(k, wring, wsrc[:, :, 6160 + blk * 512:6160 + (blk + 1) * 512])
        for t in range(NT):
            pb, bpb = k.ps.next()
            for kt in range(16):
                MM(k, pb[:], hT[:, kt, t * 128:(t + 1) * 128], wap[:, kt, :], kt == 0, kt == 15, [wb, b_hT], [bpb])
            z_ap, z_b = zst[zi % 3], b_zst[zi % 3]
            zi += 1
            CP(k, "act" if t % 2 else "dve", z_ap, pb[:], [bpb], [z_b])
            p.dma(k.VB[blk * 4:(blk + 1) * 4, :, t, :].rearrange("h p d -> p h d"),
                  z_ap.rearrange("p (h d) -> p h d", h=4), reads=[z_b], writes=[k.b_scr["VB"]])
    if hasattr(k, "DBG"):
        p.dma(k.DBG[:, 0:128], k.BETA[:].rearrange("p a b -> p (a b)"), reads=[k.b_bg], writes=[k.b_scr["DBG"]])
        p.dma(k.DBG[:, 128:256], k.GG[:].rearrange("p a b -> p (a b)"), reads=[k.b_bg], writes=[k.b_scr["DBG"]])


def phase_gdn(k):
    p = k.p
    ar = k.ar
    I = k.I
    ar.reset()
    oT = ar.alloc((16, S), BF16)
    k.oT = oT
    k.b_oT = p.buf("oT")
    k.oT_mark = ar.off
    S32 = ar.alloc((8, 128), F32)
    S16 = ar.alloc((8, 128), BF16)
    b_S32 = p.bufs(2)
    b_S16 = p.bufs(2)
    p.op("pool", "memset", writes=b_S32, ap=S32, constant=0.0)
    p.op("pool", "memset", writes=b_S16, ap=S16, constant=0.0)
    gnb = ar.alloc((128,), F32)
    b_gnb = p.buf()
    p.dma(gnb, I["gdn_norm_g"].partition_broadcast(128), writes=[b_gnb])
    names = ("QA", "KA", "KT", "VA", "ZS")
    inr = Ring([([ar.alloc((8, 128), BF16) for _ in names], p.bufs(5)) for _ in range(2)])
    sm = Ring([(ar.alloc((128,), F32), p.buf()) for _ in range(2)])
    kbg_, vb_, kdec_, zg_ = [ar.alloc((8, 128), BF16) for _ in range(4)]
    b_kbg, b_vb, b_kdec, b_zg = p.bufs(4)

    class G:
        pass
    grp = []
    for g in range(2):
        G_ = G()
        G_.diagG = ar.alloc((4, 128), F32)
        G_.decQ = ar.alloc((4, 128), BF16)
        G_.decA = ar.alloc((4, 128), BF16)
        G_.A = [ar.alloc((4, 128), BF16) for _ in range(2)]
        G_.B = [ar.alloc((4, 128), BF16) for _ in range(2)]
        G_.P = [ar.alloc((4, 128), BF16) for _ in range(2)]
        G_.qk = ar.alloc((4, 128), BF16)
        G_.qkT = ar.alloc((4, 128), BF16)
        G_.wT = ar.alloc((4, 128), BF16)
        G_.u = ar.alloc((4, 128), F32)
        G_.vnew = ar.alloc((4, 128), BF16)
        G_.o1 = ar.alloc((4, 128), F32)
        G_.o = ar.alloc((4, 128), F32)
        G_.sq = ar.alloc((4, 128), F32)
        G_.fin = ar.alloc((4, 128), BF16)
        G_.ss = ar.alloc((4,), F32)
        for nm in ("diagG", "decQ", "decA", "qk", "qkT", "wT", "u", "vnew", "o1", "o", "sq", "fin", "ss"):
            setattr(G_, "b_" + nm, p.buf())
        G_.b_A = p.bufs(2)
        G_.b_B = p.bufs(2)
        G_.b_P = p.bufs(2)
        grp.append(G_)

    def v512(ap):
        return ap.rearrange("p a b -> p (a b)")

    for c in range(16):
        (qT, kT, ktok, vtok, zs), bi = inr.next()
        b_qT, b_kT, b_ktok, b_vtok, b_zs = bi
        for ap, b, nm in zip((qT, kT, ktok, vtok, zs), bi, names):
            p.dma(ap, getattr(k, nm)[:, c, :, :], reads=[k.b_scr[nm]], writes=[b])
        smt, b_sm = sm.next()
        gc = smt[:, 0:8]
        e24 = smt[:, 8:32]
        eg = smt[:, 8:16]
        egrev = smt[:, 16:24]
        gl = smt[:, 24:32]
        bexp = smt[:, 32:40]
        gcl = smt[:, 40:48]
        pbA, bpA = k.ps.next()
        gsrc = k.GG[:, c, :]
        MM(k, pbA[:, 0:8], k.trif, gsrc, True, True, [k.b_c, k.b_bg], [bpA])
        MM(k, pbA[:, 8:16], k.suf, gsrc, True, True, [k.b_c, k.b_bg], [bpA])
        MM(k, pbA[:, 16:24], k.onesf, gsrc, True, True, [k.b_c, k.b_bg], [bpA])
        CP(k, "dve", gc, pbA[:, 0:8], [bpA], [b_sm])
        ACT(k, e24, pbA[:, 0:24], AF.Exp, [bpA], [b_sm])
        TT(k, "dve", bexp, k.BETA[:, c, :], eg, ALU.mult, [k.b_bg, b_sm], [b_sm])
        ACT(k, gcl, k.BETA[:, c, :], AF.Ln, [k.b_bg], [b_sm])
        TT(k, "dve", gcl, gcl, gc, ALU.add, [b_sm], [b_sm])
        bc = lambda a: a.unsqueeze(2).broadcast_to([128, 8, 128])
        TT(k, "pool", kbg_, ktok, bc(bexp), ALU.mult, [b_ktok, b_sm], [b_kbg])
        TT(k, "pool", vb_, vtok, bc(k.BETA[:, c, :]), ALU.mult, [b_vtok, k.b_bg], [b_vb])
        TT(k, "pool", kdec_, ktok, bc(egrev), ALU.mult, [b_ktok, b_sm], [b_kdec])
        TT(k, "pool", zg_, zs, gnb.unsqueeze(1).broadcast_to([128, 8, 128]), ALU.mult, [b_zs, b_gnb], [b_zg])
        bc4 = lambda a: a.unsqueeze(2).broadcast_to([128, 4, 128])
        for g, G_ in enumerate(grp):
            hs = slice(4 * g, 4 * g + 4)
            TT(k, "dve", G_.diagG, k.identf.unsqueeze(1).broadcast_to([128, 4, 128]), bc4(gc[:, hs]), ALU.mult,
               [k.b_c, b_sm], [G_.b_diagG])
        for g, G_ in enumerate(grp):
            for which, um, dec, b_dec, bias in ((0, k.umq, G_.decQ, G_.b_decQ, gc), (1, k.uma, G_.decA, G_.b_decA, gcl)):
                pR, bpR = k.ps.next()
                for hh in range(4):
                    MM(k, pR[:, hh * 128:(hh + 1) * 128], k.onesf, G_.diagG[:, hh, :], True, False,
                       [k.b_c, G_.b_diagG], [bpR])
                    MM(k, pR[:, hh * 128:(hh + 1) * 128], k.ident, um, False, True, [k.b_c], [bpR])
                for hh in range(4):
                    h = 4 * g + hh
                    ACT(k, dec[:, hh, :], pR[:, hh * 128:(hh + 1) * 128], AF.Exp, [bpR, b_sm], [b_dec],
                        scale=-1.0, bias=bias[:, h:h + 1])
        for g, G_ in enumerate(grp):
            pKK, bKK = k.ps.next()
            pQK, bQK = k.ps.next()
            for hh in range(4):
                h = 4 * g + hh
                MM(k, pKK[:, hh * 128:(hh + 1) * 128], kT[:, h, :], kT[:, h, :], True, True, [b_kT], [bKK])
                MM(k, pQK[:, hh * 128:(hh + 1) * 128], qT[:, h, :], kT[:, h, :], True, True, [b_qT, b_kT], [bQK])
            TT(k, "dve", v512(G_.A[0]), pKK[:], v512(G_.decA), ALU.mult, [bKK, G_.b_decA], [G_.b_A[0]])
            TT(k, "dve", v512(G_.qk), pQK[:], v512(G_.decQ), ALU.mult, [bQK, G_.b_decQ], [G_.b_qk])
        for g, G_ in enumerate(grp):
            for src, bsrc, dst, bdst, eng in ((G_.A[0], G_.b_A[0], G_.B[0], G_.b_B[0], "act"),
                                              (G_.qk, G_.b_qk, G_.qkT, G_.b_qkT, "dve")):
                pT, bpT = k.ps.next()
                pTv = pT[:].bitcast(BF16)
                for hh in range(4):
                    TR(k, pTv[:, hh * 128:(hh + 1) * 128], src[:, hh, :], k.ident, [bsrc], [bpT])
                CP(k, eng, v512(dst), pTv[:, 0:512], [bpT], [bdst])
            TT(k, "pool", G_.P[0], k.ident.unsqueeze(1).broadcast_to([128, 4, 128]), G_.B[0], ALU.subtract,
               [k.b_c, G_.b_B[0]], [G_.b_P[0]])
        for lvl in range(1, 7):
            cur = (lvl - 1) % 2
            nxt = lvl % 2
            for g, G_ in enumerate(grp):
                pA, bA = k.ps.next()
                for hh in range(4):
                    MM(k, pA[:, hh * 128:(hh + 1) * 128], G_.B[cur][:, hh, :], G_.A[cur][:, hh, :], True, True,
                       [G_.b_A[cur], G_.b_B[cur]], [bA])
                if lvl <= 5:
                    pB, bB = k.ps.next()
                    for hh in range(4):
                        MM(k, pB[:, hh * 128:(hh + 1) * 128], G_.A[cur][:, hh, :], G_.B[cur][:, hh, :], True, True,
                           [G_.b_A[cur], G_.b_B[cur]], [bB])
                CP(k, "act", v512(G_.A[nxt]), pA[:], [bA], [G_.b_A[nxt]])
                if lvl <= 5:
                    CP(k, "dve", v512(G_.B[nxt]), pB[:], [bB], [G_.b_B[nxt]])
            for g, G_ in enumerate(grp):
                pP, bP = k.ps.next()
                for hh in range(4):
                    MM(k, pP[:, hh * 128:(hh + 1) * 128], k.ident, G_.P[cur][:, hh, :], True, False,
                       [k.b_c, G_.b_P[cur]], [bP])
                    MM(k, pP[:, hh * 128:(hh + 1) * 128], G_.A[nxt][:, hh, :], G_.P[cur][:, hh, :], False, True,
                       [G_.b_A[nxt], G_.b_P[cur]], [bP])
                CP(k, "dve" if g else "act", v512(G_.P[nxt]), pP[:], [bP], [G_.b_P[nxt]])
        for g, G_ in enumerate(grp):
            TTm = G_.P[0]
            b_TT = G_.b_P[0]
            pW, bW = k.ps.next()
            pU, bU = k.ps.next()
            for hh in range(4):
                h = 4 * g + hh
                MM(k, pW[:, hh * 128:(hh + 1) * 128], kbg_[:, h, :], TTm[:, hh, :], True, True, [b_kbg, b_TT], [bW])
                MM(k, pU[:, hh * 128:(hh + 1) * 128], TTm[:, hh, :], vb_[:, h, :], True, True, [b_vb, b_TT], [bU])
            CP(k, "act", v512(G_.wT), pW[:], [bW], [G_.b_wT])
            CP(k, "dve", v512(G_.u), pU[:], [bU], [G_.b_u])
        for g, G_ in enumerate(grp):
            pVN, bVN = k.ps.next()
            pO1, bO1 = k.ps.next()
            for hh in range(4):
                h = 4 * g + hh
                MM(k, pVN[:, hh * 128:(hh + 1) * 128], G_.wT[:, hh, :], S16[:, h, :], True, True,
                   [G_.b_wT, b_S16[g]], [bVN])
                MM(k, pO1[:, hh * 128:(hh + 1) * 128], qT[:, h, :], S16[:, h, :], True, True, [b_qT, b_S16[g]], [bO1])
            TT(k, "dve", v512(G_.vnew), v512(G_.u), pVN[:], ALU.subtract, [G_.b_u, bVN], [G_.b_vnew])
            TT(k, "dve", G_.o1, pO1[:].rearrange("p (a b) -> p a b", a=4), bc4(eg[:, 4 * g:4 * g + 4]), ALU.mult,
               [bO1, b_sm], [G_.b_o1])
            TT(k, "pool", S32[:, 4 * g:4 * g + 4, :], S32[:, 4 * g:4 * g + 4, :], bc4(gl[:, 4 * g:4 * g + 4]), ALU.mult,
               [b_sm, b_S32[g]], [b_S32[g]])
        for g, G_ in enumerate(grp):
            pO2, bO2 = k.ps.next()
            pSU, bSU = k.ps.next()
            for hh in range(4):
                h = 4 * g + hh
                MM(k, pO2[:, hh * 128:(hh + 1) * 128], G_.qkT[:, hh, :], G_.vnew[:, hh, :], True, True,
                   [G_.b_qkT, G_.b_vnew], [bO2])
                MM(k, pSU[:, hh * 128:(hh + 1) * 128], kdec_[:, h, :], G_.vnew[:, hh, :], True, True,
                   [b_kdec, G_.b_vnew], [bSU])
            TT(k, "dve", v512(S32[:, 4 * g:4 * g + 4, :]), v512(S32[:, 4 * g:4 * g + 4, :]), pSU[:], ALU.add,
               [b_S32[g], bSU], [b_S32[g]])
            CP(k, "act", S16[:, 4 * g:4 * g + 4, :], S32[:, 4 * g:4 * g + 4, :], [b_S32[g]], [b_S16[g]])
            TT(k, "dve", v512(G_.o), pO2[:], v512(G_.o1), ALU.add, [bO2, G_.b_o1], [G_.b_o])
        for g, G_ in enumerate(grp):
            TT(k, "pool", G_.sq, G_.o, G_.o, ALU.mult, [G_.b_o], [G_.b_sq])
            p.op("dve", "tensor_reduce", reads=[G_.b_sq], writes=[G_.b_ss], out=G_.ss, in_=G_.sq, axis=AX.X, op=ALU.add)
            ACT(k, G_.ss, G_.ss, AF.Ln, [G_.b_ss], [G_.b_ss], scale=1.0 / 128, bias=EPS)
            ACT(k, G_.ss, G_.ss, AF.Exp, [G_.b_ss], [G_.b_ss], scale=-0.5)
            TT(k, "dve", G_.sq, G_.o, bc4(G_.ss), ALU.mult, [G_.b_o, G_.b_ss, G_.b_sq], [G_.b_sq])
            TT(k, "pool", G_.fin, G_.sq, zg_[:, 4 * g:4 * g + 4, :], ALU.mult, [G_.b_sq, b_zg], [G_.b_fin])
            pT, bpT = k.ps.next()
            pTv = pT[:].bitcast(BF16)
            for hh in range(4):
                TR(k, pTv[:, hh * 128:(hh + 1) * 128], G_.fin[:, hh, :], k.ident, [G_.b_fin], [bpT])
            CP(k, "act", oT[:, 4 * g:4 * g + 4, c * 128:(c + 1) * 128],
               pTv[:, 0:512].rearrange("p (a b) -> p a b", a=4), [bpT], [k.b_oT])


def phase_moba(k):
    p = k.p
    ar = k.ar
    I = k.I
    ar.off = k.oT_mark
    oT = k.oT
    hr = Ring([([ar.alloc((S,), BF16), ar.alloc((S,), BF16), ar.alloc((16, 132), BF16)], p.bufs(3)) for _ in range(2)])
    for (aps, bs) in hr.items:
        p.op("pool", "memset", writes=[bs[2]], ap=aps[2][:, :, 128:132], constant=1.0)
    tg = ar.alloc((8, 256), F32)
    Tp = ar.alloc((8, 256), BF16)
    rbb = ar.alloc((256,), F32)
    bmax = ar.alloc((8,), F32)
    mgb = ar.alloc((128,), F32)
    b_tp = p.buf()
    p.dma(tg, I["rb_toep"], writes=[b_tp])
    p.dma(rbb, I["rel_bias"].partition_broadcast(128), writes=[b_tp])
    p.dma(mgb, I["moba_norm_g"].partition_broadcast(128), writes=[b_tp])
    rb31 = rbb[:, 31 * 8:32 * 8]
    TT(k, "dve", tg, tg, rb31.unsqueeze(2).broadcast_to([128, 8, 256]), ALU.subtract, [b_tp], [b_tp])
    STT(k, "dve", Tp, tg, SQ128, k.cm.unsqueeze(1).broadcast_to([128, 8, 256]), ALU.mult, ALU.add, [b_tp, k.b_c], [b_tp])
    p.op("dve", "tensor_reduce", reads=[b_tp], writes=[b_tp], out=bmax, in_=rbb.rearrange("p (b h) -> p h b", h=8),
         axis=AX.X, op=ALU.max)
    TT(k, "dve", bmax, bmax, rb31, ALU.subtract, [b_tp], [b_tp])
    sq = ar.alloc((S,), BF16)
    ksq = ar.alloc((S,), BF16)
    b_sq, b_ksq = p.bufs(2)
    sm = ar.alloc((256,), F32)
    b_sm = p.buf()
    kms = sm[:, 0:8]
    km4 = sm[:, 8:12]
    ksc = sm[:, 12:13]
    nm = sm[:, 16:32]
    gsb = sm[:, 32:96].rearrange("p (a b) -> p a b", a=8)
    g2 = sm[:, 96:160].rearrange("p (a b) -> p a b", a=8)
    eq = sm[:, 160:224].rearrange("p (a b) -> p a b", a=8)
    mx = sm[:, 224:232]
    kmT = ar.alloc((8,), BF16)
    MBq = ar.alloc((16, 16), BF16)
    b_MBq = p.buf()
    p.op("pool", "memset", writes=[b_MBq], ap=MBq, constant=0.0)
    MB = ar.alloc((S,), BF16)
    b_MB = p.buf()
    PTr = Ring([(ar.alloc((256,), BF16), p.buf()) for _ in range(5)])
    yq = [ar.alloc((128,), F32) for _ in range(4)]
    b_yq = p.bufs(4)
    junkf = ar.alloc((128,), F32)
    b_junk = p.buf()
    fin = [ar.alloc((256,), BF16) for _ in range(2)]
    b_fin = p.bufs(2)
    sm2 = Ring([(ar.alloc((8,), F32), p.buf()) for _ in range(8)])
    bc8 = lambda a: a.unsqueeze(2).broadcast_to([128, 8, 8])

    MBs = [MB, ar.alloc((S,), BF16)]
    b_MBs = [b_MB, p.buf()]

    def prologue(h):
        MB, b_MB = MBs[h % 2], b_MBs[h % 2]
        (qbT, kbT, vb1), (b_q, b_k, b_v) = hr.next()
        p.dma(qbT, k.QB[h], reads=[k.b_scr["QB"]], writes=[b_q])
        p.dma(kbT, k.KB[h], reads=[k.b_scr["KB"]], writes=[b_k])
        p.dma(vb1[:, :, 0:128], k.VB[h], reads=[k.b_scr["VB"]], writes=[b_v])
        ACT(k, sq, qbT, AF.Square, [b_q], [b_sq])
        ACT(k, ksq, kbT, AF.Square, [b_k], [b_ksq])
        p.op("dve", "tensor_reduce", reads=[b_k], writes=[b_sm], out=kms, in_=kbT.rearrange("p (n t) -> p n t", n=8),
             axis=AX.X, op=ALU.add)
        TS(k, "dve", kmT, kms, 1.0 / 256, None, ALU.mult, None, [b_sm], [b_sm])
        pG, bG = k.ps.next()
        for qi in range(8, 16):
            MM(k, pG[:, (qi - 8) * 8:(qi - 7) * 8], qbT[:, qi * 128:(qi + 1) * 128], kmT, True, True, [b_q, b_sm], [bG])
        pN, bN = k.ps.next()
        for qi in range(16):
            MM(k, pN[:, qi:qi + 1], sq[:, qi * 128:(qi + 1) * 128], k.ones[:, 0:1], True, True, [b_sq, k.b_c], [bN])
        for c in range(4):
            pK, bK = k.ps.next()
            MM(k, pK[:], k.ones, ksq[:, c * 512:(c + 1) * 512], True, True, [b_ksq, k.b_c], [bK])
            p.op("dve", "tensor_reduce", reads=[bK], writes=[b_sm], out=km4[:, c:c + 1], in_=pK[:], axis=AX.X, op=ALU.max)
        p.op("dve", "tensor_reduce", reads=[b_sm], writes=[b_sm], out=ksc, in_=km4, axis=AX.X, op=ALU.max)
        TS(k, "dve", ksc, ksc, 1.0 / 128, None, ALU.mult, None, [b_sm], [b_sm])
        ACT(k, nm, pN[:, 0:16], AF.Sqrt, [bN, b_sm], [b_sm], scale=ksc)
        TS(k, "dve", MBq[:, :, 8], nm, bmax[:, h:h + 1], -SQ128, ALU.add, ALU.mult, [b_sm, b_tp], [b_MBq])
        TT(k, "dve", gsb, pG[:, 0:64].rearrange("p (a b) -> p a b", a=8), k.gm.rearrange("p (a b) -> p a b", a=8),
           ALU.add, [bG, k.b_c], [b_sm])
        src = gsb
        for it in range(2):
            p.op("dve", "tensor_reduce", reads=[b_sm], writes=[b_sm], out=mx, in_=src, axis=AX.X, op=ALU.max)
            TT(k, "dve", eq, src, bc8(mx), ALU.is_equal, [b_sm], [b_sm])
            STT(k, "dve", g2, eq, -1e30, src, ALU.mult, ALU.add, [b_sm], [b_sm])
            src = g2
        p.op("dve", "tensor_reduce", reads=[b_sm], writes=[b_sm], out=mx, in_=g2, axis=AX.X, op=ALU.max)
        TT(k, "dve", eq, gsb, bc8(mx), ALU.is_ge, [b_sm], [b_sm])
        TS(k, "dve", MBq[:, 8:16, 0:8], eq, BIG * SQ128, -BIG * SQ128, ALU.mult, ALU.add, [b_sm], [b_MBq])
        for half in range(2):
            pM, bM = k.ps.next()
            pMv = pM[:].bitcast(BF16)
            for j in range(8):
                qi = half * 8 + j
                TR(k, pMv[0:9, j * 128:(j + 1) * 128], MBq[:, qi, 0:9], k.ident, [b_MBq], [bM])
            CP(k, "act" if half else "dve", MB[0:9, half * 1024:(half + 1) * 1024], pMv[0:9, 0:1024], [bM], [b_MB])

        return (qbT, kbT, vb1, b_q, b_k, b_v, MB, b_MB)

    preps = {0: prologue(0)}
    for h in range(8):
        qbT, kbT, vb1, b_q, b_k, b_v, MB, b_MB = preps.pop(h)
        deferred = []
        for qb in range(8):
            if qb == 5 and h + 1 < 8:
                preps[h + 1] = prologue(h + 1)
            pend = []

            def emit_pv(item):
                kj_, PT_, b_PT_ = item
                for qt in range(2):
                    if kj_ <= 2 * qb + qt:
                        pO, bO = k.psx[qt]
                        MM(k, pO[:, 0:129], PT_[:, qt * 128:(qt + 1) * 128], vb1[:, kj_, 0:129],
                           kj_ == 0, kj_ == 2 * qb + qt, [b_PT_, b_v], [bO])
            for kj in range(2 * qb + 2):
                n = kj // 2
                pS, bS = k.ps.next()
                q0 = qb * 256
                v = n if (qb >= 4 and n < qb) else 8
                if n == qb and kj % 2 == 0:
                    segs = [(0, 256, Tp[:, h, 0:256])]
                elif n == qb:
                    segs = [(128, 256, Tp[:, h, 0:128])]
                elif n == qb - 1 and kj % 2 == 1:
                    segs = [(0, 128, Tp[:, h, 128:256]), (128, 256, None)]
                else:
                    segs = [(0, 256, None)]
                c0 = segs[0][0]
                for (a, b, bias) in segs:
                    MM(k, pS[:, a:b], kbT[:, kj * 128:(kj + 1) * 128], qbT[:, q0 + a:q0 + b], True, False, [b_k, b_q], [bS])
                    MM(k, pS[:, a:b], k.emat[:, v, :], MB[0:9, q0 + a:q0 + b], False, bias is None, [k.b_c, b_MB], [bS])
                    if bias is not None:
                        MM(k, pS[:, a:b], k.ident, bias, False, True, [k.b_c, b_tp], [bS])
                PT, b_PT = PTr.next()
                ACT(k, PT[:, c0:256], pS[:, c0:256], AF.Exp, [bS], [b_PT], scale=1.0 / SQ128)
                pend.append((kj, PT, b_PT))
                if len(pend) > 2:
                    emit_pv(pend.pop(0))
                if kj == min(3, 2 * qb + 1) and deferred:
                    deferred.pop(0)()
            while pend:
                emit_pv(pend.pop(0))
            parts = []
            for qt in range(2):
                s2, b_s2 = sm2.next()
                y_ap, y_b = yq[(qb % 2) * 2 + qt], b_yq[(qb % 2) * 2 + qt]
                pO, bO = k.psx[qt]
                p.op("dve", "reciprocal", reads=[bO], writes=[b_s2], out=s2[:, 0:1], in_=pO[:, 128:129])
                TS(k, "dve", y_ap, pO[:, 0:128], s2[:, 0:1], None, ALU.mult, None, [bO, b_s2], [y_b])
                parts.append((s2, b_s2, y_ap, y_b))

            def part2(qb=qb, parts=parts):
                f_ap, f_b = fin[qb % 2], b_fin[qb % 2]
                for qt in range(2):
                    s2, b_s2, y_ap, y_b = parts[qt]
                    TT(k, "dve", junkf, y_ap, y_ap, ALU.mult, [y_b], [b_junk])
                    p.op("dve", "tensor_reduce", reads=[b_junk], writes=[b_s2], out=s2[:, 1:2], in_=junkf, axis=AX.X,
                         op=ALU.add)
                    ACT(k, s2[:, 1:2], s2[:, 1:2], AF.Ln, [b_s2], [b_s2], scale=1.0 / 128, bias=EPS)
                    ACT(k, s2[:, 1:2], s2[:, 1:2], AF.Exp, [b_s2], [b_s2], scale=-0.5)
                    STT(k, "dve", f_ap[:, qt * 128:(qt + 1) * 128], y_ap, s2[:, 1:2], mgb, ALU.mult, ALU.mult,
                        [y_b, b_s2, b_tp], [f_b])
                pT, bpT = k.ps.next()
                pTv = pT[:].bitcast(BF16)
                for qt in range(2):
                    TR(k, pTv[:, qt * 128:(qt + 1) * 128], f_ap[:, qt * 128:(qt + 1) * 128], k.ident, [f_b], [bpT])
                CP(k, "act", oT[:, 8 + h, qb * 256:(qb + 1) * 256], pTv[:, 0:256], [bpT], [k.b_oT])
            deferred.append(part2)
        while deferred:
            deferred.pop(0)()
    if hasattr(k, "OT"):
        p.dma(k.OT, oT, reads=[k.b_oT], writes=[k.b_scr["DBG"]])


def phase_wout(k):
    p = k.p
    ar = k.ar
    I = k.I
    oT = k.oT
    ar.off = k.oT_mark
    h2T = ar.alloc((16, S), BF16)
    k.h2T = h2T
    k.b_h2T = p.buf("h2T")
    k.ws_mark = ar.off
    wring = Ring([(ar.alloc((16, 512), BF16), p.buf()) for _ in range(2)])
    xr = Ring([(ar.alloc((512,), F32), p.buf()) for _ in range(3)])
    wsrc = I["w_out"].rearrange("(kt q) n -> q kt n", q=128)
    for c in range(4):
        wap, wb = load_w_block(k, wring, wsrc[:, :, c * 512:(c + 1) * 512])
        for t in range(NT):
            x_ap, x_b = xr.next()
            p.dma(x_ap, I["x"][t * 128:(t + 1) * 128, c * 512:(c + 1) * 512], writes=[x_b])
            pb, bpb = k.ps.next()
            for kt in range(16):
                MM(k, pb[:], oT[:, kt, t * 128:(t + 1) * 128], wap[:, kt, :], kt == 0, kt == 15, [wb, k.b_oT], [bpb])
            TT(k, "dve", x_ap, pb[:], x_ap, ALU.add, [bpb, x_b], [x_b])
            p.dma(k.X1[t * 128:(t + 1) * 128, c * 512:(c + 1) * 512], x_ap, reads=[x_b], writes=[k.b_scr["X1"]])
    p.barrier()
    ar.off = k.ws_mark
    tiles = [(k.X1[t * 128:(t + 1) * 128, :], [k.b_scr["X1"]]) for t in range(NT)]
    norm_T(k, tiles, I["xattn_norm_g"], h2T, k.b_h2T)


def phase_xattn(k):
    p = k.p
    ar = k.ar
    I = k.I
    h2T = k.h2T
    ar.off = 0
    memT = ar.alloc((16, MEM), BF16)
    b_memT = p.buf()
    kxT = ar.alloc((4, MEM), BF16)
    vx = ar.alloc((2, 512), BF16)
    b_kv = p.buf()
    qxT = ar.alloc((4, S), BF16)
    b_qx = p.buf()
    oxT = ar.alloc((4, S), BF16)
    b_ox = p.buf()
    wxo = ar.alloc((4, D), BF16)
    b_wxo = p.buf()
    assert ar.off <= k.oT_mark
    ar.off = k.ws_mark
    tiles = [(I["mem"][t * 128:(t + 1) * 128, :], []) for t in range(2)]
    norm_T(k, tiles, I["mem_norm_g"], memT, b_memT)
    p.barrier()
    ar.off = k.ws_mark
    wring = Ring([(ar.alloc((16, 512), BF16), p.buf()) for _ in range(2)])
    Pf = Ring([(ar.alloc((256,), F32), p.buf()) for _ in range(4)])
    Pn = Ring([(ar.alloc((256,), BF16), p.buf()) for _ in range(4)])
    PnT = Ring([(ar.alloc((2, 128), BF16), p.buf()) for _ in range(4)])
    sm = Ring([(ar.alloc((8,), F32), p.buf()) for _ in range(8)])
    xr = Ring([(ar.alloc((512,), F32), p.buf()) for _ in range(3)])
    wkv = I["w_xkv"].rearrange("(kt q) n -> q kt n", q=128)
    wap, wb = load_w_block(k, wring, wkv[:, :, 0:512])
    for hx in range(4):
        pb, bpb = k.ps.next()
        for kt in range(16):
            MM(k, pb[:, 0:MEM], wap[:, kt, hx * 128:(hx + 1) * 128], memT[:, kt, :], kt == 0, kt == 15, [wb, b_memT], [bpb])
        CP(k, "act" if hx % 2 else "dve", kxT[:, hx, :], pb[:, 0:MEM], [bpb], [b_kv])
    wap, wb = load_w_block(k, wring, wkv[:, :, 512:1024])
    for mt in range(2):
        pb, bpb = k.ps.next()
        for kt in range(16):
            MM(k, pb[:], memT[:, kt, mt * 128:(mt + 1) * 128], wap[:, kt, :], kt == 0, kt == 15, [wb, b_memT], [bpb])
        CP(k, "act" if mt else "dve", vx[:, mt, :], pb[:], [bpb], [b_kv])
    wq = I["w_xq"].rearrange("(kt q) n -> q kt n", q=128)
    wap, wb = load_w_block(k, wring, wq)
    for hx in range(4):
        for c in range(4):
            pb, bpb = k.ps.next()
            for kt in range(16):
                MM(k, pb[:], wap[:, kt, hx * 128:(hx + 1) * 128], h2T[:, kt, c * 512:(c + 1) * 512], kt == 0, kt == 15,
                   [wb, k.b_h2T], [bpb])
            CP(k, "act" if c % 2 else "dve", qxT[:, hx, c * 512:(c + 1) * 512], pb[:], [bpb], [b_qx])
    for t in range(NT):
        st = []
        for hx in range(4):
            pS, bS = k.ps.next()
            MM(k, pS[:, 0:MEM], qxT[:, hx, t * 128:(t + 1) * 128], kxT[:, hx, :], True, True, [b_qx, b_kv], [bS])
            s_ap, s_b = sm.next()
            st.append([pS, bS, s_ap, s_b])
        for hx in range(4):
            pS, bS, s_ap, s_b = st[hx]
            p.op("dve", "tensor_reduce", reads=[bS], writes=[s_b], out=s_ap[:, 0:1], in_=pS[:, 0:MEM], axis=AX.X, op=ALU.max)
            TS(k, "dve", s_ap[:, 1:2], s_ap[:, 0:1], -1.0 / SQ128, None, ALU.mult, None, [s_b], [s_b])
        for hx in range(4):
            pS, bS, s_ap, s_b = st[hx]
            pf, b_pf = Pf.next()
            ACT(k, pf, pS[:, 0:MEM], AF.Exp, [bS, s_b], [b_pf, s_b], scale=1.0 / SQ128, bias=s_ap[:, 1:2],
                accum_out=s_ap[:, 2:3])
            st[hx] += [pf, b_pf]
        for hx in range(4):
            pS, bS, s_ap, s_b, pf, b_pf = st[hx]
            p.op("dve", "reciprocal", reads=[s_b], writes=[s_b], out=s_ap[:, 3:4], in_=s_ap[:, 2:3])
            pn, b_pn = Pn.next()
            TS(k, "dve", pn, pf, s_ap[:, 3:4], None, ALU.mult, None, [b_pf, s_b], [b_pn])
            st[hx] += [pn, b_pn]
        for hx in range(4):
            pn, b_pn = st[hx][6], st[hx][7]
            pT, bpT = k.ps.next()
            pTv = pT[:].bitcast(BF16)
            for mt in range(2):
                TR(k, pTv[:, mt * 128:(mt + 1) * 128], pn[:, mt * 128:(mt + 1) * 128], k.ident, [b_pn], [bpT])
            pnt, b_pnt = PnT.next()
            CP(k, "act" if hx % 2 else "dve", pnt, pTv[:, 0:256].rearrange("p (a b) -> p a b", a=2), [bpT], [b_pnt])
            st[hx] += [pnt, b_pnt]
        for hx in range(4):
            pnt, b_pnt = st[hx][8], st[hx][9]
            pO, bO = k.ps.next()
            for mt in range(2):
                MM(k, pO[:, 0:128], vx[:, mt, hx * 128:(hx + 1) * 128], pnt[:, mt, :], mt == 0, mt == 1, [b_kv, b_pnt], [bO])
            CP(k, "dve" if hx % 2 else "act", oxT[:, hx, t * 128:(t + 1) * 128], pO[:, 0:128], [bO], [b_ox])
    p.dma(wxo, I["w_xo"].rearrange("(kt q) n -> q kt n", q=128), writes=[b_wxo], q="pool")
    for t in range(NT):
        for c in range(4):
            x_ap, x_b = xr.next()
            p.dma(x_ap, k.X1[t * 128:(t + 1) * 128, c * 512:(c + 1) * 512], reads=[k.b_scr["X1"]], writes=[x_b])
            pb, bpb = k.ps.next()
            for kt in range(4):
                MM(k, pb[:], oxT[:, kt, t * 128:(t + 1) * 128], wxo[:, kt, c * 512:(c + 1) * 512], kt == 0, kt == 3,
                   [b_wxo, b_ox], [bpb])
            TT(k, "dve", x_ap, pb[:], x_ap, ALU.add, [bpb, x_b], [x_b])
            p.dma(k.X2[t * 128:(t + 1) * 128, c * 512:(c + 1) * 512], x_ap, reads=[x_b], writes=[k.b_scr["X2"]])


def phase_ffn(k):
    p = k.p
    ar = k.ar
    I = k.I
    HT = 1024
    halo = p.sbuf("halo", [128, NFF, 2], F32)
    b_halo = p.buf()
    p.op("pool", "memset", writes=[b_halo], ap=halo[:], constant=0.0)
    cwf = p.sbuf("cwf", [128, 4 * NFF], F32)
    b_cwf = p.buf()
    for hf in range(2):
        ar.reset()
        aT = ar.alloc((NFF, HT), BF16)
        b_aT = p.buf()
        mark0 = ar.off
        h3T = ar.alloc((16, HT), BF16)
        b_h3T = p.buf()
        mark1 = ar.off
        tiles = [(k.X2[(hf * 8 + t) * 128:(hf * 8 + t + 1) * 128, :], [k.b_scr["X2"]]) for t in range(8)]
        norm_T(k, tiles, I["ffn_norm_g"], h3T, b_h3T)
        p.barrier()
        ar.off = mark1
        if hf == 0:
            stg = ar.alloc((128,), F32)
            b_stg = p.buf()
            for j in range(4):
                src = (I["ffn_conv_w"][j] if j < 3 else I["ffn_conv_b"][0]).rearrange("(b q) -> b q", q=128)
                p.dma(stg[0:NFF, :], src, writes=[b_stg])
                pb, bpb = k.ps.next()
                TR(k, pb[:, 0:NFF], stg[0:NFF, :], k.identf[0:NFF, 0:NFF], [b_stg], [bpb])
                CP(k, "dve", cwf[:, j * NFF:(j + 1) * NFF], pb[:, 0:NFF], [bpb], [b_cwf])
        wg = Ring([(ar.alloc((16, 256), BF16), p.buf()) for _ in range(2)])
        wu = Ring([(ar.alloc((16, 256), BF16), p.buf()) for _ in range(2)])
        graw = Ring([(ar.alloc((HT + 2,), F32), p.buf()) for _ in range(2)])
        gy = Ring([(ar.alloc((HT,), F32), p.buf()) for _ in range(2)])
        wgs = I["w_gate"].rearrange("(kt q) n -> q kt n", q=128)
        wus = I["w_up"].rearrange("(kt q) n -> q kt n", q=128)
        for j in range(NFF):
            if j % 2 == 0:
                g_ap2, g_b = wg.next()
                u_ap2, u_b = wu.next()
                p.dma(g_ap2, wgs[:, :, j * 128:(j + 2) * 128], writes=[g_b], q="pool")
                p.dma(u_ap2, wus[:, :, j * 128:(j + 2) * 128], writes=[u_b], q="pool")
            g_ap = g_ap2[:, :, (j % 2) * 128:(j % 2 + 1) * 128]
            u_ap = u_ap2[:, :, (j % 2) * 128:(j % 2 + 1) * 128]
            r_ap, r_b = graw.next()
            y_ap, y_b = gy.next()
            pus = []
            for c in range(2):
                pg, bg = k.ps.next()
                for kt in range(16):
                    MM(k, pg[:], g_ap[:, kt, :], h3T[:, kt, c * 512:(c + 1) * 512], kt == 0, kt == 15, [g_b, b_h3T], [bg])
                CP(k, "act", r_ap[:, 2 + c * 512:2 + (c + 1) * 512], pg[:], [bg], [r_b])
            for c in range(2):
                pu, bu = k.ps.next()
                for kt in range(16):
                    MM(k, pu[:], u_ap[:, kt, :], h3T[:, kt, c * 512:(c + 1) * 512], kt == 0, kt == 15, [u_b, b_h3T], [bu])
                pus.append((pu, bu))
            CP(k, "pool", r_ap[:, 0:2], halo[:, j, :], [b_halo], [r_b])
            if hf == 0:
                CP(k, "pool", halo[:, j, :], r_ap[:, HT:HT + 2], [r_b], [b_halo])
            TS(k, "dve", y_ap, r_ap[:, 0:HT], cwf[:, j:j + 1], None, ALU.mult, None, [r_b, b_cwf], [y_b])
            for tap in range(1, 3):
                STT(k, "dve", y_ap, r_ap[:, tap:tap + HT], cwf[:, tap * NFF + j:tap * NFF + j + 1], y_ap, ALU.mult, ALU.add,
                    [r_b, b_cwf, y_b], [y_b])
            ACT(k, y_ap, y_ap, AF.Silu, [y_b, b_cwf], [y_b], bias=cwf[:, 3 * NFF + j:3 * NFF + j + 1])
            for c in range(2):
                pu, bu = pus[c]
                TT(k, "dve", aT[:, j, c * 512:(c + 1) * 512], y_ap[:, c * 512:(c + 1) * 512], pu[:], ALU.mult,
                   [y_b, bu], [b_aT])
        p.barrier()
        ar.off = mark0
        x3 = ar.alloc((8, D), F32)
        b_x3 = p.bufs(8)
        aflat = aT.rearrange("p a b -> p (a b)")
        fgb = aflat[:, 0:2 * D].bitcast(F32)
        junk = aflat[:, 2 * D:3 * D]
        b_fgb = b_aT
        b_junk = b_aT
        wd = Ring([(ar.alloc((4, 512), BF16), p.buf()) for _ in range(3)])
        xr = Ring([(ar.alloc((512,), F32), p.buf()) for _ in range(2)])
        ssf = ar.alloc((8,), F32)
        b_ssf = p.buf()
        for cc in range(4):
            for j4 in range(NFF // 4):
                w_ap, w_b = wd.next()
                p.dma(w_ap, I["w_down"][j4 * 512:(j4 + 1) * 512, cc * 512:(cc + 1) * 512].rearrange("(a q) n -> q a n", q=128),
                      writes=[w_b], q="pool")
                for a in range(4):
                    j = j4 * 4 + a
                    for tt in range(8):
                        pb, bpb = k.banks[tt]
                        MM(k, pb[:], aT[:, j, tt * 128:(tt + 1) * 128], w_ap[:, a, :], j == 0, j == NFF - 1, [w_b, b_aT], [bpb])
            for tt in range(8):
                pb, bpb = k.banks[tt]
                x_ap, x_b = xr.next()
                row = (hf * 8 + tt) * 128
                p.dma(x_ap, k.X2[row:row + 128, cc * 512:(cc + 1) * 512], reads=[k.b_scr["X2"]], writes=[x_b])
                TT(k, "dve", x3[:, tt, cc * 512:(cc + 1) * 512], pb[:], x_ap, ALU.add, [bpb, x_b], [b_x3[tt]])
        p.dma(fgb, I["final_norm_g"].partition_broadcast(128), writes=[b_aT])
        outs = []
        for tt in range(8):
            row = (hf * 8 + tt) * 128
            ACT(k, junk, x3[:, tt, :], AF.Square, [b_x3[tt]], [b_junk, b_ssf], accum_out=ssf[:, tt:tt + 1])
            ACT(k, ssf[:, tt:tt + 1], ssf[:, tt:tt + 1], AF.Sqrt, [b_ssf], [b_ssf], scale=1.0 / D, bias=EPS)
            p.op("dve", "reciprocal", reads=[b_ssf], writes=[b_ssf], out=ssf[:, tt:tt + 1], in_=ssf[:, tt:tt + 1])
            STT(k, "dve", x3[:, tt, :], x3[:, tt, :], ssf[:, tt:tt + 1], fgb, ALU.mult, ALU.mult,
                [b_x3[tt], b_ssf, b_fgb], [b_x3[tt]])
            outs.append(p.dma(k.out[row:row + 128, :], x3[:, tt, :], reads=[b_x3[tt]]))
        p.barrier()
    return []


_CACHE = {}


def make_in_maps(inputs, n=8, skip=()):
    cf, cb = host_consts()
    tidx = toeplitz_index()
    rb = np.asarray(inputs["rel_bias"], np.float32)
    rb_toep = np.ascontiguousarray(rb[tidx].transpose(0, 2, 1))
    shared = {
        "mix_norm_g": inputs["mix_norm_g"].reshape(1, D), "xattn_norm_g": inputs["xattn_norm_g"].reshape(1, D),
        "mem_norm_g": inputs["mem_norm_g"].reshape(1, D), "ffn_norm_g": inputs["ffn_norm_g"].reshape(1, D),
        "final_norm_g": inputs["final_norm_g"].reshape(1, D),
        "w_in": inputs["w_in"][0], "gdn_conv_w": inputs["gdn_conv_w"][0], "gdn_a_log": inputs["gdn_a_log"].reshape(1, 8),
        "gdn_dt_bias": inputs["gdn_dt_bias"].reshape(1, 8), "gdn_norm_g": inputs["gdn_norm_g"].reshape(1, 128),
        "moba_norm_g": inputs["moba_norm_g"].reshape(1, 128), "rel_bias": rb.reshape(1, 256), "rb_toep": rb_toep,
        "w_out": inputs["w_out"][0], "w_xq": inputs["w_xq"][0], "w_xkv": inputs["w_xkv"][0], "w_xo": inputs["w_xo"][0],
        "w_gate": inputs["w_gate"][0], "w_up": inputs["w_up"][0], "ffn_conv_w": inputs["ffn_conv_w"][0],
        "ffn_conv_b": inputs["ffn_conv_b"].reshape(1, DFF), "w_down": inputs["w_down"][0],
        "cst_f": cf, "cst_b": cb,
    }
    shared = {kk: (np.zeros((1, 1), np.float32) if kk in skip else np.ascontiguousarray(np.asarray(v, np.float32)))
              for kk, v in shared.items()}
    maps = []
    for b in range(n):
        m = dict(shared)
        m["x"] = np.ascontiguousarray(np.asarray(inputs["x"][b], np.float32))
        m["mem"] = (np.zeros((1, 1), np.float32) if "mem" in skip
                    else np.ascontiguousarray(np.asarray(inputs["mem"][b], np.float32)))
        maps.append(m)
    return maps


def kernel(**inputs):
    if "nc" not in _CACHE:
        _CACHE["nc"] = build_program()[0]
    nc = _CACHE["nc"]
    maps = make_in_maps(inputs, 8)
    res = run_bass_kernel_spmd(nc, maps, core_ids=list(range(8)))
    return np.stack([np.asarray(r["out"], np.float32) for r in res.results], axis=0)
```

```python
import bisect
import math
from contextlib import ExitStack

import numpy as np
import concourse.bass as bass
import concourse.mybir as mybir
from concourse.bass_utils import run_bass_kernel_spmd

F32 = mybir.dt.float32
BF16 = mybir.dt.bfloat16
ALU = mybir.AluOpType
AF = mybir.ActivationFunctionType
AX = mybir.AxisListType

S = 2048
D = 2048
NT = 16
H = 8
HD = 128
DFF = 5632
NFF = 44
MEM = 256
INW = 7184
EPS = 1e-6
BIG = 30000.0
SQ128 = math.sqrt(128.0)

ENGS = ("pe", "act", "dve", "pool", "sp")
SEM_LIMIT = 2000
N_ENG_SEMS = {"pe": 14, "act": 5, "dve": 5, "pool": 3, "sp": 1}
N_DMA_SEMS = 32
N_SWDMA_SEMS = 16


class Buf:
    __slots__ = ("name", "last_w", "creads", "dreads")

    def __init__(self, name=""):
        self.name = name
        self.last_w = None
        self.creads = {}
        self.dreads = []


class Op:
    __slots__ = ("eng", "name", "kw", "waits", "signal", "pos")

    def __init__(self, eng, name, kw, pos):
        self.eng = eng
        self.name = name
        self.kw = kw
        self.waits = []
        self.signal = None
        self.pos = pos


class Prog:
    def __init__(self, nc, stack):
        self.nc = nc
        self.stack = stack
        self.ops = {e: [] for e in ENGS}
        self.nsig = {e: 0 for e in ENGS}
        self.sigpos = {e: [] for e in ENGS}
        self.sigtok = {e: [] for e in ENGS}
        self.waited = {e: {} for e in ENGS}
        self.esems = {e: [stack.enter_context(nc.semaphore(f"s_{e}_{i}")) for i in range(N_ENG_SEMS[e])]
                      for e in ENGS}
        self.dsems = {"hw": [stack.enter_context(nc.semaphore(f"s_dma_{i}")) for i in range(N_DMA_SEMS)],
                      "sw": [stack.enter_context(nc.semaphore(f"s_swdma_{i}")) for i in range(N_SWDMA_SEMS)]}
        self.ndma_q = {"hw": 0, "sw": 0}
        self.ndma = 0
        self.dma_toks = []
        self.pe_prev = (None, None, True)

    def sbuf(self, name, shape, dtype):
        return self.stack.enter_context(self.nc.sbuf_tensor(name, list(shape), dtype))

    def psum(self, name, shape, dtype):
        return self.stack.enter_context(self.nc.psum_tensor(name, list(shape), dtype))

    def buf(self, name=""):
        return Buf(name)

    def bufs(self, n):
        return [Buf() for _ in range(n)]

    def _force_signal(self, eng):
        lst = self.ops[eng]
        if not lst:
            return None
        last = lst[-1]
        if last.signal is None:
            n = self.nsig[eng]
            self.nsig[eng] += 1
            sem = self.esems[eng][n // SEM_LIMIT]
            val = n % SEM_LIMIT + 1
            last.signal = (sem, 1)
            self.sigpos[eng].append(last.pos)
            self.sigtok[eng].append((sem, val))
        return last

    def _resolve(self, tok):
        if tok[0] == "d":
            return tok[1], tok[2]
        _, eng, pos = tok
        sp = self.sigpos[eng]
        i = bisect.bisect_left(sp, pos)
        if i < len(sp):
            return self.sigtok[eng][i]
        last = self._force_signal(eng)
        assert last.pos >= pos
        i = bisect.bisect_left(sp, pos)
        return self.sigtok[eng][i]

    def _add_wait(self, op, tok):
        sem, val = self._resolve(tok)
        w = self.waited[op.eng]
        key = id(sem)
        if w.get(key, 0) >= val:
            return
        w[key] = val
        for i, (s, v) in enumerate(op.waits):
            if s is sem:
                op.waits[i] = (sem, max(v, val))
                return
        op.waits.append((sem, val))

    def _deps(self, op, reads, writes, is_dma):
        eng = op.eng
        for b in reads:
            if b.last_w is not None:
                self._add_wait(op, b.last_w)
        for b in writes:
            t = b.last_w
            if t is not None and not (t[0] == "c" and t[1] == eng == "pe" and not is_dma):
                self._add_wait(op, t)
            for e, t in b.creads.items():
                if e == eng == "pe" and not is_dma:
                    continue
                self._add_wait(op, t)
            for t in b.dreads:
                self._add_wait(op, t)

    def _compute_pos_check(self, eng):
        lst = self.ops[eng]
        if lst and lst[-1].signal is None and lst[-1].name not in ("nop", "dma_start"):
            self._force_signal(eng)

    def op(self, eng, name, reads=(), writes=(), **kw):
        lst = self.ops[eng]
        rset = frozenset(id(b) for b in reads)
        wset = frozenset(id(b) for b in writes)
        if lst and lst[-1].name not in ("nop", "dma_start") and lst[-1].signal is None:
            prev = lst[-1]
            if eng != "pe":
                self._force_signal(eng)
            else:
                pr, pw, pdone = self.pe_prev
                if pr != rset or (pw != wset and pdone):
                    self._force_signal(eng)
        if eng == "pe":
            self.pe_prev = (rset, wset, kw.get("stop", True))
        o = Op(eng, name, kw, len(lst))
        self._deps(o, reads, writes, False)
        lst.append(o)
        tok = ("c", eng, o.pos)
        for b in reads:
            b.creads[eng] = tok
        for b in writes:
            b.last_w = tok
            b.creads = {}
            b.dreads = []
        return o

    def dma(self, out, in_, reads=(), writes=(), q="sp", **kw):
        if q != "sp":
            self._compute_pos_check(q)
        lst = self.ops[q]
        o = Op(q, "dma_start", dict(out=out, in_=in_, **kw), len(lst))
        self._deps(o, reads, writes, True)
        kind = "sw" if q == "pool" else "hw"
        pool_ = self.dsems[kind]
        i = self.ndma_q[kind] % len(pool_)
        r = self.ndma_q[kind] // len(pool_)
        self.ndma_q[kind] += 1
        self.ndma += 1
        sem = pool_[i]
        if r > 0:
            self._add_wait(o, ("d", sem, 16 * r))
        o.signal = (sem, 16)
        lst.append(o)
        tok = ("d", sem, 16 * (r + 1))
        self.dma_toks.append(tok)
        for b in reads:
            b.dreads.append(tok)
        for b in writes:
            b.last_w = tok
            b.creads = {}
            b.dreads = []
        return tok

    def barrier(self):
        toks = list(self.dma_toks[-(N_DMA_SEMS + N_SWDMA_SEMS) * 2:])
        for e in ENGS:
            lst = self.ops[e]
            if lst and lst[-1].name not in ("dma_start", "nop"):
                last = self._force_signal(e)
                toks.append(("c", e, last.pos))
            else:
                for o in reversed(lst):
                    if o.name not in ("dma_start", "nop"):
                        assert o.signal is not None
                        toks.append(("c", e, o.pos))
                        break
        for e in ENGS:
            o = Op(e, "nop", {}, len(self.ops[e]))
            for t in toks:
                self._add_wait(o, t)
            self.ops[e].append(o)

    def final_wait(self, eng, toks):
        o = Op(eng, "nop", {}, len(self.ops[eng]))
        for t in toks:
            self._add_wait(o, t)
        self.ops[eng].append(o)

    def emit(self):
        nc = self.nc
        prog = self
        with nc.Block() as block:
            def run(engname):
                def body(e):
                    for o in prog.ops[engname]:
                        for (sem, val) in o.waits:
                            e.wait_ge(sem, val)
                        if o.name == "nop":
                            continue
                        ins = getattr(e, o.name)(**o.kw)
                        if o.signal is not None:
                            ins.then_inc(o.signal[0], o.signal[1])
                return body
            block.tensor(run("pe"))
            block.scalar(run("act"))
            block.vector(run("dve"))
            block.gpsimd(run("pool"))
            block.sync(run("sp"))

    def stats(self):
        return {e: (len(self.ops[e]), self.nsig[e]) for e in ENGS}, self.ndma


class Arena:
    def __init__(self, p, nbytes):
        self.t = p.sbuf("arena", [128, nbytes // 2], BF16)
        self.n = nbytes
        self.off = 0

    def reset(self):
        self.off = 0

    def alloc(self, shape, dtype):
        esz = 2 if dtype == BF16 else 4
        n = 1
        for s in shape:
            n *= s
        nb = n * esz
        a = self.t[:, self.off // 2:(self.off + nb) // 2]
        self.off += (nb + 63) // 64 * 64
        assert self.off <= self.n, f"arena overflow {self.off} > {self.n}"
        if dtype != BF16:
            a = a.bitcast(dtype)
        if len(shape) == 2:
            a = a.rearrange("p (a b) -> p a b", a=shape[0])
        elif len(shape) == 3:
            a = a.rearrange("p (a b c) -> p a b c", a=shape[0], b=shape[1])
        return a


class Ring:
    def __init__(self, items):
        self.items = items
        self.i = 0

    def next(self):
        it = self.items[self.i % len(self.items)]
        self.i += 1
        return it


NCF = 576
NCB = 768 + 9 * 128


def host_consts():
    cf = np.zeros((128, NCF), np.float32)
    ii = np.arange(128)
    cf[:, 0:128] = np.eye(128, dtype=np.float32)
    cf[:, 128:256] = (ii[:, None] <= ii[None, :]).astype(np.float32)
    cf[:, 256:384] = (ii[:, None] > ii[None, :]).astype(np.float32)
    cf[:, 384:512] = 1.0
    gm = np.zeros((128, 8, 8), np.float32)
    for qi in range(8, 16):
        own = qi // 2
        gm[:, qi - 8, own:] = -1e30
    cf[:, 512:576] = gm.reshape(128, 64)
    cb = np.zeros((128, NCB), np.float32)
    cb[:, 0:128] = np.eye(128, dtype=np.float32)
    cb[:, 128:256] = 1.0
    cb[:, 256:384] = (ii[None, :] > ii[:, None]).astype(np.float32) * BIG
    cb[:, 384:512] = (ii[None, :] >= ii[:, None]).astype(np.float32) * BIG
    rr = np.arange(256)
    cb[:, 512:768] = (rr[None, :] < ii[:, None]).astype(np.float32) * (-BIG * SQ128)
    e = np.zeros((9, 9, 128), np.float32)
    for v in range(9):
        e[8, v, :] = 1.0
        if v < 8:
            e[v, v, :] = 1.0
    cb[0:9, 768:768 + 9 * 128] = e.reshape(9, 9 * 128)
    return cf, cb


def rel_bucket_np(n):
    n = np.maximum(n, 0)
    max_exact = 16
    nf = np.maximum(n, 1).astype(np.float32)
    large = max_exact + (np.log(nf / max_exact) / math.log(128 / max_exact) * (32 - max_exact)).astype(np.int32)
    large = np.minimum(large, 31)
    return np.where(n < max_exact, n, large)


def toeplitz_index():
    kk = np.arange(128)[:, None]
    r = np.arange(256)[None, :]
    return rel_bucket_np(r - kk)


class K:
    pass


def build_program(debug_phase=None):
    nc = bass.Bass("TRN2", target_bir_lowering=False)
    k = K()
    k.nc = nc
    dbg = debug_phase is not None

    order = ["inproj", "gdn", "moba", "wout", "xattn", "ffn"]
    need_from = {"mem": "xattn", "w_out": "wout", "w_xq": "xattn", "w_xkv": "xattn", "w_xo": "xattn",
                 "w_gate": "ffn", "w_up": "ffn", "w_down": "ffn"}
    k.skip = set()
    if dbg:
        for nm, ph in need_from.items():
            if order.index(ph) > order.index(debug_phase):
                k.skip.add(nm)

    def din(name, shape, dt=F32):
        if name in k.skip:
            shape = [1, 1]
        return nc.dram_tensor(name, list(shape), dt, kind="ExternalInput").ap()

    DUMP = {"inproj": ("QA", "KA", "KT", "VA", "ZS", "QB", "KB", "VB", "DBG"), "moba": ("OT",), "gdn": ("OT",),
            "wout": ("X1",), "xattn": ("X2",), "ffn": ("OT", "X1", "X2")}.get(debug_phase, ())

    def dscr(name, shape, dt=BF16):
        kind = "ExternalOutput" if name in DUMP else "Internal"
        return nc.dram_tensor(name, list(shape), dt, kind=kind).ap()

    I = {}
    I["x"] = din("x", [S, D])
    I["mem"] = din("mem", [MEM, D])
    for nm in ("mix_norm_g", "xattn_norm_g", "mem_norm_g", "ffn_norm_g", "final_norm_g"):
        I[nm] = din(nm, [1, D])
    I["w_in"] = din("w_in", [D, INW])
    I["gdn_conv_w"] = din("gdn_conv_w", [4, 3072])
    I["gdn_a_log"] = din("gdn_a_log", [1, 8])
    I["gdn_dt_bias"] = din("gdn_dt_bias", [1, 8])
    I["gdn_norm_g"] = din("gdn_norm_g", [1, 128])
    I["moba_norm_g"] = din("moba_norm_g", [1, 128])
    I["rel_bias"] = din("rel_bias", [1, 256])
    I["rb_toep"] = din("rb_toep", [128, 8, 256])
    I["w_out"] = din("w_out", [D, D])
    I["w_xq"] = din("w_xq", [D, 512])
    I["w_xkv"] = din("w_xkv", [D, 1024])
    I["w_xo"] = din("w_xo", [512, D])
    I["w_gate"] = din("w_gate", [D, DFF])
    I["w_up"] = din("w_up", [D, DFF])
    I["ffn_conv_w"] = din("ffn_conv_w", [3, DFF])
    I["ffn_conv_b"] = din("ffn_conv_b", [1, DFF])
    I["w_down"] = din("w_down", [DFF, D])
    I["cst_f"] = din("cst_f", [128, NCF])
    I["cst_b"] = din("cst_b", [128, NCB])
    out = nc.dram_tensor("out", [S, D], F32, kind="ExternalOutput").ap()
    k.I = I
    k.out = out

    k.QA = dscr("QA", [128, 16, 8, 128])
    k.KA = dscr("KA", [128, 16, 8, 128])
    k.KT = dscr("KT", [128, 16, 8, 128])
    k.VA = dscr("VA", [128, 16, 8, 128])
    k.ZS = dscr("ZS", [128, 16, 8, 128])
    k.QB = dscr("QB", [8, 128, 2048])
    k.KB = dscr("KB", [8, 128, 2048])
    k.VB = dscr("VB", [8, 128, 16, 128])
    k.X1 = dscr("X1", [S, D], F32)
    k.X2 = dscr("X2", [S, D], F32)
    if "DBG" in DUMP:
        k.DBG = dscr("DBG", [128, 4096], F32)
    if "OT" in DUMP:
        k.OT = dscr("OT", [128, 16, S], BF16)
    k.b_scr = {nm: Buf(nm) for nm in ("QA", "KA", "KT", "VA", "ZS", "QB", "KB", "VB", "X1", "X2", "DBG")}

    with ExitStack() as st:
        p = Prog(nc, st)
        k.p = p
        k.cf = p.sbuf("cf", [128, NCF], F32)
        k.cb = p.sbuf("cb", [128, NCB], BF16)
        k.b_c = p.buf("consts")
        p.dma(k.cf[:], I["cst_f"], writes=[k.b_c])
        p.dma(k.cb[:], I["cst_b"], writes=[k.b_c], q="pool")
        k.identf = k.cf[:, 0:128]
        k.trif = k.cf[:, 128:256]
        k.suf = k.cf[:, 256:384]
        k.onesf = k.cf[:, 384:512]
        k.gm = k.cf[:, 512:576]
        k.ident = k.cb[:, 0:128]
        k.ones = k.cb[:, 128:256]
        k.umq = k.cb[:, 256:384]
        k.uma = k.cb[:, 384:512]
        k.cm = k.cb[:, 512:768]
        k.emat = k.cb[0:9, 768:768 + 9 * 128].rearrange("p (v n) -> p v n", v=9)
        k.BA = p.sbuf("BA", [128, 16, 16], F32)
        k.b_BA = p.buf("BA")
        k.BETA = p.sbuf("BETA", [128, 16, 8], F32)
        k.GG = p.sbuf("GG", [128, 16, 8], F32)
        k.b_bg = p.buf("betag")
        banks = [(p.psum(f"ps{i}", [128, 512], F32), p.buf(f"ps{i}")) for i in range(8)]
        k.ps = Ring(banks[0:6])
        k.psx = banks[6:8]
        k.banks = banks
        k.ar = Arena(p, 176 * 1024)

        phases = [phase_inproj, phase_gdn, phase_moba, phase_wout, phase_xattn, phase_ffn]
        names = ["inproj", "gdn", "moba", "wout", "xattn", "ffn"]
        out_toks = []
        for fn, nm in zip(phases, names):
            r = fn(k)
            if r:
                out_toks += r
            p.barrier()
            if debug_phase == nm:
                break
        p.final_wait("sp", p.dma_toks[-(N_DMA_SEMS + N_SWDMA_SEMS) * 2:])
        k.stats = p.stats()
        p.emit()
    return nc, k


def MM(k, out, lhsT, rhs, start, stop, r, w):
    k.p.op("pe", "matmul", reads=r, writes=w, out=out, lhsT=lhsT, rhs=rhs, start=start, stop=stop)


def TR(k, out, in_, ident, r, w):
    k.p.op("pe", "transpose", reads=r + [k.b_c], writes=w, out=out, in_=in_, identity=ident)


def ACT(k, out, in_, func, r, w, **kw):
    k.p.op("act", "activation", reads=r, writes=w, out=out, in_=in_, func=func, **kw)


def CP(k, eng, out, in_, r, w):
    if eng == "act":
        k.p.op("act", "copy", reads=r, writes=w, out=out, in_=in_)
    else:
        k.p.op(eng, "tensor_copy", reads=r, writes=w, out=out, in_=in_)


def TT(k, eng, out, in0, in1, op, r, w):
    k.p.op(eng, "tensor_tensor", reads=r, writes=w, out=out, in0=in0, in1=in1, op=op)


def TS(k, eng, out, in0, s1, s2, op0, op1, r, w):
    if s2 is None:
        k.p.op(eng, "tensor_scalar", reads=r, writes=w, out=out, in0=in0, scalar1=s1, scalar2=None, op0=op0)
    else:
        k.p.op(eng, "tensor_scalar", reads=r, writes=w, out=out, in0=in0, scalar1=s1, scalar2=s2, op0=op0, op1=op1)


def STT(k, eng, out, in0, scalar, in1, op0, op1, r, w):
    k.p.op(eng, "scalar_tensor_tensor", reads=r, writes=w, out=out, in0=in0, scalar=scalar, in1=in1,
           op0=op0, op1=op1)


def norm_T(k, tiles, gain, dstT, b_dst, col0=0):
    p = k.p
    ar = k.ar
    gb = ar.alloc((D,), F32)
    b_gb = p.buf()
    p.dma(gb, gain.partition_broadcast(128), writes=[b_gb])
    xt = [ar.alloc((D,), F32) for _ in range(3)]
    b_xt = p.bufs(3)
    junk = ar.alloc((D,), BF16)
    b_junk = p.buf()
    hb = [ar.alloc((D,), BF16) for _ in range(2)]
    b_hb = p.bufs(2)
    ss = ar.alloc((len(tiles),), F32)
    b_ss = p.buf()
    pend = []
    for t, (src, rb) in enumerate(tiles):
        i = t % 2
        x3i = t % 3
        p.dma(xt[x3i], src, reads=rb, writes=[b_xt[x3i]])
        ACT(k, junk, xt[x3i], AF.Square, [b_xt[x3i]], [b_junk, b_ss], accum_out=ss[:, t:t + 1])
        ACT(k, ss[:, t:t + 1], ss[:, t:t + 1], AF.Sqrt, [b_ss], [b_ss], scale=1.0 / D, bias=EPS)
        p.op("dve", "reciprocal", reads=[b_ss], writes=[b_ss], out=ss[:, t:t + 1], in_=ss[:, t:t + 1])
        STT(k, "dve", hb[i], xt[x3i], ss[:, t:t + 1], gb, ALU.mult, ALU.mult, [b_xt[x3i], b_ss, b_gb], [b_hb[i]])

        def stage_b(t=t, i=i):
            for k4 in range(4):
                pb, bpb = k.ps.next()
                pbv = pb[:].bitcast(BF16)
                for j in range(4):
                    kt = k4 * 4 + j
                    TR(k, pbv[:, j * 128:(j + 1) * 128], hb[i][:, kt * 128:(kt + 1) * 128], k.ident, [b_hb[i]], [bpb])
                CP(k, "act" if k4 % 2 else "dve",
                   dstT[:, k4 * 4:(k4 + 1) * 4, col0 + t * 128:col0 + (t + 1) * 128],
                   pbv[:, 0:512].rearrange("p (a b) -> p a b", a=4), [bpb], [b_dst])
        if pend:
            pend.pop(0)()
        pend.append(stage_b)
    while pend:
        pend.pop(0)()


def load_w_block(k, ring, src):
    ap, b = ring.next()
    k.p.dma(ap, src, writes=[b], q="pool")
    return ap, b


def phase_inproj(k):
    p = k.p
    ar = k.ar
    I = k.I
    ar.reset()
    hT = ar.alloc((16, S), BF16)
    b_hT = p.buf("hT")
    k.hT = hT
    mark = ar.off
    tiles = [(I["x"][t * 128:(t + 1) * 128, :], []) for t in range(NT)]
    norm_T(k, tiles, I["mix_norm_g"], hT, b_hT)
    p.barrier()
    ar.off = mark
    wring = Ring([(ar.alloc((16, 512), BF16), p.buf()) for _ in range(2)])
    wba = ar.alloc((16, 16), BF16)
    b_wba = p.buf()
    raw = [ar.alloc((S + 4,), BF16) for _ in range(2)]
    b_raw = p.bufs(2)
    dgr = Ring([(ar.alloc((4, 128), BF16), p.buf()) for _ in range(2)])
    yr = Ring([(ar.alloc((S,), F32), p.buf()) for _ in range(2)])
    sqr = Ring([(ar.alloc((S,), BF16), p.buf()) for _ in range(2)])
    rsr = Ring([(ar.alloc((S,), F32), p.buf()) for _ in range(2)])
    obf = [ar.alloc((S,), BF16) for _ in range(2)]
    b_obf = p.bufs(2)
    tst = [ar.alloc((16, 128), BF16) for _ in range(2)]
    b_tst = p.bufs(2)
    zst = [ar.alloc((512,), BF16) for _ in range(3)]
    b_zst = p.bufs(3)
    cw = ar.alloc((96,), F32)
    cwl = ar.alloc((128,), F32)
    b_cw = p.buf()
    small = ar.alloc((64,), F32)
    b_small = p.buf()
    p.dma(cwl[0:96, :], I["gdn_conv_w"].rearrange("j (b q) -> (j b) q", q=128), writes=[b_cw])
    pb, bpb = k.ps.next()
    TR(k, pb[:, 0:96], cwl[0:96, :], k.identf[0:96, 0:96], [b_cw], [bpb])
    CP(k, "dve", cw, pb[:, 0:96], [bpb], [b_cw])
    for i in range(2):
        p.op("pool", "memset", writes=[b_raw[i]], ap=raw[i][:, 0:4], constant=0.0)

    wsrc = I["w_in"].rearrange("(kt q) n -> q kt n", q=128)
    ri = 0
    oi = 0
    ti = 0

    def fblock(wap, wb, f, evac):
        for c in range(4):
            pb, bpb = k.ps.next()
            for kt in range(16):
                MM(k, pb[:], wap[:, kt, f * 128:(f + 1) * 128], hT[:, kt, c * 512:(c + 1) * 512],
                   kt == 0, kt == 15, [wb, b_hT], [bpb])
            evac(pb, bpb, c * 512)

    deferred = []

    def make_post(fb, kind, hd, r_ap, r_b):
        st = {}

        def stage_a():
            nonlocal oi
            y, b_y = yr.next()
            st["y"] = (y, b_y)
            o_ap, o_b = obf[oi % 2], b_obf[oi % 2]
            oi += 1
            st["o"] = (o_ap, o_b)
            dg, b_dg = dgr.next()
            for j in range(4):
                TS(k, "dve", dg[:, j, :], k.identf, cw[:, j * 24 + fb:j * 24 + fb + 1], None, ALU.mult, None,
                   [k.b_c, b_cw], [b_dg])
            for c in range(4):
                pb, bpb = k.ps.next()
                for j in range(4):
                    MM(k, pb[:], dg[:, j, :], r_ap[:, c * 512 + j:c * 512 + j + 512], j == 0, j == 3, [b_dg, r_b], [bpb])
                if kind == 2:
                    ACT(k, o_ap[:, c * 512:(c + 1) * 512], pb[:], AF.Silu, [bpb], [o_b])
                else:
                    ACT(k, y[:, c * 512:(c + 1) * 512], pb[:], AF.Silu, [bpb], [b_y])
            if kind != 2:
                sq, b_sq = sqr.next()
                st["sq"] = (sq, b_sq)
                ACT(k, sq, y, AF.Square, [b_y], [b_sq])

        def stage_b():
            nonlocal ti
            y, b_y = st["y"]
            o_ap, o_b = st["o"]
            if kind != 2:
                sq, b_sq = st["sq"]
                rs, b_rs = rsr.next()
                for c in range(4):
                    pb, bpb = k.ps.next()
                    MM(k, pb[:], k.ones, sq[:, c * 512:(c + 1) * 512], True, True, [b_sq, k.b_c], [bpb])
                    if kind == 0:
                        ACT(k, rs[:, c * 512:(c + 1) * 512], pb[:], AF.Ln, [bpb], [b_rs], scale=128.0,
                            bias=128.0 * EPS)
                    else:
                        ACT(k, rs[:, c * 512:(c + 1) * 512], pb[:], AF.Ln, [bpb], [b_rs], scale=1.0, bias=EPS)
                ACT(k, rs, rs, AF.Exp, [b_rs], [b_rs], scale=-0.5)
                TT(k, "dve", o_ap, y, rs, ALU.mult, [b_y, b_rs], [o_b])
            if kind == 0:
                p.dma(k.QA[:, :, hd, :], o_ap.rearrange("p (c t) -> p c t", c=16), reads=[o_b],
                      writes=[k.b_scr["QA"]])
            if kind == 1:
                p.dma(k.KA[:, :, hd, :], o_ap.rearrange("p (c t) -> p c t", c=16), reads=[o_b],
                      writes=[k.b_scr["KA"]])
            if kind >= 1:
                t_ap, t_b = tst[ti % 2], b_tst[ti % 2]
                ti += 1
                for c4 in range(4):
                    pb, bpb = k.ps.next()
                    pbv = pb[:].bitcast(BF16)
                    for j in range(4):
                        c = c4 * 4 + j
                        TR(k, pbv[:, j * 128:(j + 1) * 128], o_ap[:, c * 128:(c + 1) * 128], k.ident, [o_b], [bpb])
                    CP(k, "act" if c4 % 2 else "dve", t_ap[:, c4 * 4:(c4 + 1) * 4, :],
                       pbv[:, 0:512].rearrange("p (a b) -> p a b", a=4), [bpb], [t_b])
                dst = k.KT if kind == 1 else k.VA
                p.dma(dst[:, :, hd, :], t_ap, reads=[t_b], writes=[k.b_scr["KT" if kind == 1 else "VA"]])
        return stage_a, stage_b

    for blk in range(6):
        wap, wb = load_w_block(k, wring, wsrc[:, :, blk * 512:(blk + 1) * 512])
        for f in range(4):
            fb = blk * 4 + f
            kind = fb // 8
            hd = fb % 8
            r_ap, r_b = raw[ri % 2], b_raw[ri % 2]
            ri += 1
            prev = deferred.pop(0) if deferred else None

            def evac_raw(pb, bpb, tok0, r_ap=r_ap, r_b=r_b, prev=prev):
                CP(k, "act", r_ap[:, 3 + tok0:3 + tok0 + 512], pb[:], [bpb], [r_b])
                if tok0 == 512 and prev is not None:
                    prev[0]()
            fblock(wap, wb, f, evac_raw)
            if prev is not None:
                prev[1]()
            deferred.append(make_post(fb, kind, hd, r_ap, r_b))
    for a_, b_ in deferred:
        a_()
        b_()
    zi = 0
    for blk in range(2):
        wap, wb = load_w_block(k, wring, wsrc[:, :, 3072 + blk * 512:3072 + (blk + 1) * 512])
        for t in range(NT):
            pb, bpb = k.ps.next()
            for kt in range(16):
                MM(k, pb[:], hT[:, kt, t * 128:(t + 1) * 128], wap[:, kt, :], kt == 0, kt == 15, [wb, b_hT], [bpb])
            z_ap, z_b = zst[zi % 3], b_zst[zi % 3]
            zi += 1
            ACT(k, z_ap, pb[:], AF.Silu, [bpb], [z_b])
            p.dma(k.ZS[:, t, blk * 4:(blk + 1) * 4, :], z_ap.rearrange("p (h d) -> p h d", h=4), reads=[z_b],
                  writes=[k.b_scr["ZS"]])
    p.dma(wba, wsrc[:, :, 4096:4112], writes=[b_wba], q="pool")
    for t in range(NT):
        pb, bpb = k.ps.next()
        for kt in range(16):
            MM(k, pb[:, 0:16], hT[:, kt, t * 128:(t + 1) * 128], wba[:, kt, :], kt == 0, kt == 15, [b_wba, b_hT], [bpb])
        CP(k, "dve", k.BA[:, t, :], pb[:, 0:16], [bpb], [k.b_BA])
    albc = small[:, 0:8]
    dtbc = small[:, 8:16]
    nea = small[:, 16:24]
    p.dma(albc, I["gdn_a_log"].partition_broadcast(128), writes=[b_small])
    p.dma(dtbc, I["gdn_dt_bias"].partition_broadcast(128), writes=[b_small])
    ACT(k, nea, albc, AF.Exp, [b_small], [b_small])
    TS(k, "dve", nea, nea, -1.0, None, ALU.mult, ALU.bypass, [b_small], [b_small])
    ACT(k, k.BETA[:], k.BA[:, :, 0:8], AF.Sigmoid, [k.b_BA], [k.b_bg])
    xa = ar.alloc((16, 8), F32)
    xb = ar.alloc((16, 8), F32)
    b_xa = p.buf()
    TT(k, "dve", xa, k.BA[:, :, 8:16], dtbc.unsqueeze(1).broadcast_to([128, 16, 8]), ALU.add, [k.b_BA, b_small], [b_xa])
    ACT(k, xb, xa, AF.Abs, [b_xa], [b_xa])
    ACT(k, xb, xb, AF.Exp, [b_xa], [b_xa], scale=-1.0)
    ACT(k, xb, xb, AF.Ln, [b_xa], [b_xa], bias=1.0)
    TS(k, "dve", xa, xa, 0.0, None, ALU.max, ALU.bypass, [b_xa], [b_xa])
    TT(k, "dve", xa, xa, xb, ALU.add, [b_xa], [b_xa])
    TT(k, "dve", k.GG[:], xa, nea.unsqueeze(1).broadcast_to([128, 16, 8]), ALU.mult, [b_xa, b_small], [k.b_bg])

    for blk in range(4):
        wap, wb = load_w_block(k, wring, wsrc[:, :, 4112 + blk * 512:4112 + (blk + 1) * 512])
        for f in range(4):
            fb = blk * 4 + f
            kind = fb // 8
            hd = fb % 8
            o_ap, o_b = obf[oi % 2], b_obf[oi % 2]
            oi += 1

            def evac_o(pb, bpb, tok0, o_ap=o_ap, o_b=o_b, c=[0]):
                CP(k, "act" if (tok0 // 512) % 2 else "dve", o_ap[:, tok0:tok0 + 512], pb[:], [bpb], [o_b])
            fblock(wap, wb, f, evac_o)
            dst = k.QB if kind == 0 else k.KB
            p.dma(dst[hd], o_ap, reads=[o_b], writes=[k.b_scr["QB" if kind == 0 else "KB"]])
    for blk in range(2):
        wap, wb = load_w_block(k, wring, wsrc[:, :, 6160 + blk * 512:6160 + (blk + 1) * 512])
        for t in range(NT):
            pb, bpb = k.ps.next()
            for kt in range(16):
                MM(k, pb[:], hT[:, kt, t * 128:(t + 1) * 128], wap[:, kt, :], kt == 0, kt == 15, [wb, b_hT], [bpb])
            z_ap, z_b = zst[zi % 3], b_zst[zi % 3]
            zi += 1
            CP(k, "act" if t % 2 else "dve", z_ap, pb[:], [bpb], [z_b])
            p.dma(k.VB[blk * 4:(blk + 1) * 4, :, t, :].rearrange("h p d -> p h d"),
                  z_ap.rearrange("p (h d) -> p h d", h=4), reads=[z_b], writes=[k.b_scr["VB"]])
    if hasattr(k, "DBG"):
        p.dma(k.DBG[:, 0:128], k.BETA[:].rearrange("p a b -> p (a b)"), reads=[k.b_bg], writes=[k.b_scr["DBG"]])
        p.dma(k.DBG[:, 128:256], k.GG[:].rearrange("p a b -> p (a b)"), reads=[k.b_bg], writes=[k.b_scr["DBG"]])


def phase_gdn(k):
    p = k.p
    ar = k.ar
    I = k.I
    ar.reset()
    oT = ar.alloc((16, S), BF16)
    k.oT = oT
    k.b_oT = p.buf("oT")
    k.oT_mark = ar.off
    S32 = ar.alloc((8, 128), F32)
    S16 = ar.alloc((8, 128), BF16)
    b_S32 = p.bufs(2)
    b_S16 = p.bufs(2)
    p.op("pool", "memset", writes=b_S32, ap=S32, constant=0.0)
    p.op("pool", "memset", writes=b_S16, ap=S16, constant=0.0)
    gnb = ar.alloc((128,), F32)
    b_gnb = p.buf()
    p.dma(gnb, I["gdn_norm_g"].partition_broadcast(128), writes=[b_gnb])
    names = ("QA", "KA", "KT", "VA", "ZS")
    inr = Ring([([ar.alloc((8, 128), BF16) for _ in names], p.bufs(5)) for _ in range(2)])
    sm = Ring([(ar.alloc((128,), F32), p.buf()) for _ in range(2)])
    kbg_, vb_, kdec_, zg_ = [ar.alloc((8, 128), BF16) for _ in range(4)]
    b_kbg, b_vb, b_kdec, b_zg = p.bufs(4)

    class G:
        pass
    grp = []
    for g in range(2):
        G_ = G()
        G_.diagG = ar.alloc((4, 128), F32)
        G_.decQ = ar.alloc((4, 128), BF16)
        G_.decA = ar.alloc((4, 128), BF16)
        G_.A = [ar.alloc((4, 128), BF16) for _ in range(2)]
        G_.B = [ar.alloc((4, 128), BF16) for _ in range(2)]
        G_.P = [ar.alloc((4, 128), BF16) for _ in range(2)]
        G_.qk = ar.alloc((4, 128), BF16)
        G_.qkT = ar.alloc((4, 128), BF16)
        G_.wT = ar.alloc((4, 128), BF16)
        G_.u = ar.alloc((4, 128), F32)
        G_.vnew = ar.alloc((4, 128), BF16)
        G_.o1 = ar.alloc((4, 128), F32)
        G_.o = ar.alloc((4, 128), F32)
        G_.sq = ar.alloc((4, 128), F32)
        G_.fin = ar.alloc((4, 128), BF16)
        G_.ss = ar.alloc((4,), F32)
        for nm in ("diagG", "decQ", "decA", "qk", "qkT", "wT", "u", "vnew", "o1", "o", "sq", "fin", "ss"):
            setattr(G_, "b_" + nm, p.buf())
        G_.b_A = p.bufs(2)
        G_.b_B = p.bufs(2)
        G_.b_P = p.bufs(2)
        grp.append(G_)

    def v512(ap):
        return ap.rearrange("p a b -> p (a b)")

    for c in range(16):
        (qT, kT, ktok, vtok, zs), bi = inr.next()
        b_qT, b_kT, b_ktok, b_vtok, b_zs = bi
        for ap, b, nm in zip((qT, kT, ktok, vtok, zs), bi, names):
            p.dma(ap, getattr(k, nm)[:, c, :, :], reads=[k.b_scr[nm]], writes=[b])
        smt, b_sm = sm.next()
        gc = smt[:, 0:8]
        e24 = smt[:, 8:32]
        eg = smt[:, 8:16]
        egrev = smt[:, 16:24]
        gl = smt[:, 24:32]
        bexp = smt[:, 32:40]
        gcl = smt[:, 40:48]
        pbA, bpA = k.ps.next()
        gsrc = k.GG[:, c, :]
        MM(k, pbA[:, 0:8], k.trif, gsrc, True, True, [k.b_c, k.b_bg], [bpA])
        MM(k, pbA[:, 8:16], k.suf, gsrc, True, True, [k.b_c, k.b_bg], [bpA])
        MM(k, pbA[:, 16:24], k.onesf, gsrc, True, True, [k.b_c, k.b_bg], [bpA])
        CP(k, "dve", gc, pbA[:, 0:8], [bpA], [b_sm])
        ACT(k, e24, pbA[:, 0:24], AF.Exp, [bpA], [b_sm])
        TT(k, "dve", bexp, k.BETA[:, c, :], eg, ALU.mult, [k.b_bg, b_sm], [b_sm])
        ACT(k, gcl, k.BETA[:, c, :], AF.Ln, [k.b_bg], [b_sm])
        TT(k, "dve", gcl, gcl, gc, ALU.add, [b_sm], [b_sm])
        bc = lambda a: a.unsqueeze(2).broadcast_to([128, 8, 128])
        TT(k, "pool", kbg_, ktok, bc(bexp), ALU.mult, [b_ktok, b_sm], [b_kbg])
        TT(k, "pool", vb_, vtok, bc(k.BETA[:, c, :]), ALU.mult, [b_vtok, k.b_bg], [b_vb])
        TT(k, "pool", kdec_, ktok, bc(egrev), ALU.mult, [b_ktok, b_sm], [b_kdec])
        TT(k, "pool", zg_, zs, gnb.unsqueeze(1).broadcast_to([128, 8, 128]), ALU.mult, [b_zs, b_gnb], [b_zg])
        bc4 = lambda a: a.unsqueeze(2).broadcast_to([128, 4, 128])
        for g, G_ in enumerate(grp):
            hs = slice(4 * g, 4 * g + 4)
            TT(k, "dve", G_.diagG, k.identf.unsqueeze(1).broadcast_to([128, 4, 128]), bc4(gc[:, hs]), ALU.mult,
               [k.b_c, b_sm], [G_.b_diagG])
        for g, G_ in enumerate(grp):
            for which, um, dec, b_dec, bias in ((0, k.umq, G_.decQ, G_.b_decQ, gc), (1, k.uma, G_.decA, G_.b_decA, gcl)):
                pR, bpR = k.ps.next()
                for hh in range(4):
                    MM(k, pR[:, hh * 128:(hh + 1) * 128], k.onesf, G_.diagG[:, hh, :], True, False,
                       [k.b_c, G_.b_diagG], [bpR])
                    MM(k, pR[:, hh * 128:(hh + 1) * 128], k.ident, um, False, True, [k.b_c], [bpR])
                for hh in range(4):
                    h = 4 * g + hh
                    ACT(k, dec[:, hh, :], pR[:, hh * 128:(hh + 1) * 128], AF.Exp, [bpR, b_sm], [b_dec],
                        scale=-1.0, bias=bias[:, h:h + 1])
        for g, G_ in enumerate(grp):
            pKK, bKK = k.ps.next()
            pQK, bQK = k.ps.next()
            for hh in range(4):
                h = 4 * g + hh
                MM(k, pKK[:, hh * 128:(hh + 1) * 128], kT[:, h, :], kT[:, h, :], True, True, [b_kT], [bKK])
                MM(k, pQK[:, hh * 128:(hh + 1) * 128], qT[:, h, :], kT[:, h, :], True, True, [b_qT, b_kT], [bQK])
            TT(k, "dve", v512(G_.A[0]), pKK[:], v512(G_.decA), ALU.mult, [bKK, G_.b_decA], [G_.b_A[0]])
            TT(k, "dve", v512(G_.qk), pQK[:], v512(G_.decQ), ALU.mult, [bQK, G_.b_decQ], [G_.b_qk])
        for g, G_ in enumerate(grp):
            for src, bsrc, dst, bdst, eng in ((G_.A[0], G_.b_A[0], G_.B[0], G_.b_B[0], "act"),
                                              (G_.qk, G_.b_qk, G_.qkT, G_.b_qkT, "dve")):
                pT, bpT = k.ps.next()
                pTv = pT[:].bitcast(BF16)
                for hh in range(4):
                    TR(k, pTv[:, hh * 128:(hh + 1) * 128], src[:, hh, :], k.ident, [bsrc], [bpT])
                CP(k, eng, v512(dst), pTv[:, 0:512], [bpT], [bdst])
            TT(k, "pool", G_.P[0], k.ident.unsqueeze(1).broadcast_to([128, 4, 128]), G_.B[0], ALU.subtract,
               [k.b_c, G_.b_B[0]], [G_.b_P[0]])
        for lvl in range(1, 7):
            cur = (lvl - 1) % 2
            nxt = lvl % 2
            for g, G_ in enumerate(grp):
                pA, bA = k.ps.next()
                for hh in range(4):
                    MM(k, pA[:, hh * 128:(hh + 1) * 128], G_.B[cur][:, hh, :], G_.A[cur][:, hh, :], True, True,
                       [G_.b_A[cur], G_.b_B[cur]], [bA])
                if lvl <= 5:
                    pB, bB = k.ps.next()
                    for hh in range(4):
                        MM(k, pB[:, hh * 128:(hh + 1) * 128], G_.A[cur][:, hh, :], G_.B[cur][:, hh, :], True, True,
                           [G_.b_A[cur], G_.b_B[cur]], [bB])
                CP(k, "act", v512(G_.A[nxt]), pA[:], [bA], [G_.b_A[nxt]])
                if lvl <= 5:
                    CP(k, "dve", v512(G_.B[nxt]), pB[:], [bB], [G_.b_B[nxt]])
            for g, G_ in enumerate(grp):
                pP, bP = k.ps.next()
                for hh in range(4):
                    MM(k, pP[:, hh * 128:(hh + 1) * 128], k.ident, G_.P[cur][:, hh, :], True, False,
                       [k.b_c, G_.b_P[cur]], [bP])
                    MM(k, pP[:, hh * 128:(hh + 1) * 128], G_.A[nxt][:, hh, :], G_.P[cur][:, hh, :], False, True,
                       [G_.b_A[nxt], G_.b_P[cur]], [bP])
                CP(k, "dve" if g else "act", v512(G_.P[nxt]), pP[:], [bP], [G_.b_P[nxt]])
        for g, G_ in enumerate(grp):
            TTm = G_.P[0]
            b_TT = G_.b_P[0]
            pW, bW = k.ps.next()
            pU, bU = k.ps.next()
            for hh in range(4):
                h = 4 * g + hh
                MM(k, pW[:, hh * 128:(hh + 1) * 128], kbg_[:, h, :], TTm[:, hh, :], True, True, [b_kbg, b_TT], [bW])
                MM(k, pU[:, hh * 128:(hh + 1) * 128], TTm[:, hh, :], vb_[:, h, :], True, True, [b_vb, b_TT], [bU])
            CP(k, "act", v512(G_.wT), pW[:], [bW], [G_.b_wT])
            CP(k, "dve", v512(G_.u), pU[:], [bU], [G_.b_u])
        for g, G_ in enumerate(grp):
            pVN, bVN = k.ps.next()
            pO1, bO1 = k.ps.next()
            for hh in range(4):
                h = 4 * g + hh
                MM(k, pVN[:, hh * 128:(hh + 1) * 128], G_.wT[:, hh, :], S16[:, h, :], True, True,
                   [G_.b_wT, b_S16[g]], [bVN])
                MM(k, pO1[:, hh * 128:(hh + 1) * 128], qT[:, h, :], S16[:, h, :], True, True, [b_qT, b_S16[g]], [bO1])
            TT(k, "dve", v512(G_.vnew), v512(G_.u), pVN[:], ALU.subtract, [G_.b_u, bVN], [G_.b_vnew])
            TT(k, "dve", G_.o1, pO1[:].rearrange("p (a b) -> p a b", a=4), bc4(eg[:, 4 * g:4 * g + 4]), ALU.mult,
               [bO1, b_sm], [G_.b_o1])
            TT(k, "pool", S32[:, 4 * g:4 * g + 4, :], S32[:, 4 * g:4 * g + 4, :], bc4(gl[:, 4 * g:4 * g + 4]), ALU.mult,
               [b_sm, b_S32[g]], [b_S32[g]])
        for g, G_ in enumerate(grp):
            pO2, bO2 = k.ps.next()
            pSU, bSU = k.ps.next()
            for hh in range(4):
                h = 4 * g + hh
                MM(k, pO2[:, hh * 128:(hh + 1) * 128], G_.qkT[:, hh, :], G_.vnew[:, hh, :], True, True,
                   [G_.b_qkT, G_.b_vnew], [bO2])
                MM(k, pSU[:, hh * 128:(hh + 1) * 128], kdec_[:, h, :], G_.vnew[:, hh, :], True, True,
                   [b_kdec, G_.b_vnew], [bSU])
            TT(k, "dve", v512(S32[:, 4 * g:4 * g + 4, :]), v512(S32[:, 4 * g:4 * g + 4, :]), pSU[:], ALU.add,
               [b_S32[g], bSU], [b_S32[g]])
            CP(k, "act", S16[:, 4 * g:4 * g + 4, :], S32[:, 4 * g:4 * g + 4, :], [b_S32[g]], [b_S16[g]])
            TT(k, "dve", v512(G_.o), pO2[:], v512(G_.o1), ALU.add, [bO2, G_.b_o1], [G_.b_o])
        for g, G_ in enumerate(grp):
            TT(k, "pool", G_.sq, G_.o, G_.o, ALU.mult, [G_.b_o], [G_.b_sq])
            p.op("dve", "tensor_reduce", reads=[G_.b_sq], writes=[G_.b_ss], out=G_.ss, in_=G_.sq, axis=AX.X, op=ALU.add)
            ACT(k, G_.ss, G_.ss, AF.Ln, [G_.b_ss], [G_.b_ss], scale=1.0 / 128, bias=EPS)
            ACT(k, G_.ss, G_.ss, AF.Exp, [G_.b_ss], [G_.b_ss], scale=-0.5)
            TT(k, "dve", G_.sq, G_.o, bc4(G_.ss), ALU.mult, [G_.b_o, G_.b_ss, G_.b_sq], [G_.b_sq])
            TT(k, "pool", G_.fin, G_.sq, zg_[:, 4 * g:4 * g + 4, :], ALU.mult, [G_.b_sq, b_zg], [G_.b_fin])
            pT, bpT = k.ps.next()
            pTv = pT[:].bitcast(BF16)
            for hh in range(4):
                TR(k, pTv[:, hh * 128:(hh + 1) * 128], G_.fin[:, hh, :], k.ident, [G_.b_fin], [bpT])
            CP(k, "act", oT[:, 4 * g:4 * g + 4, c * 128:(c + 1) * 128],
               pTv[:, 0:512].rearrange("p (a b) -> p a b", a=4), [bpT], [k.b_oT])


def phase_moba(k):
    p = k.p
    ar = k.ar
    I = k.I
    ar.off = k.oT_mark
    oT = k.oT
    hr = Ring([([ar.alloc((S,), BF16), ar.alloc((S,), BF16), ar.alloc((16, 132), BF16)], p.bufs(3)) for _ in range(2)])
    for (aps, bs) in hr.items:
        p.op("pool", "memset", writes=[bs[2]], ap=aps[2][:, :, 128:132], constant=1.0)
    tg = ar.alloc((8, 256), F32)
    Tp = ar.alloc((8, 256), BF16)
    rbb = ar.alloc((256,), F32)
    bmax = ar.alloc((8,), F32)
    mgb = ar.alloc((128,), F32)
    b_tp = p.buf()
    p.dma(tg, I["rb_toep"], writes=[b_tp])
    p.dma(rbb, I["rel_bias"].partition_broadcast(128), writes=[b_tp])
    p.dma(mgb, I["moba_norm_g"].partition_broadcast(128), writes=[b_tp])
    rb31 = rbb[:, 31 * 8:32 * 8]
    TT(k, "dve", tg, tg, rb31.unsqueeze(2).broadcast_to([128, 8, 256]), ALU.subtract, [b_tp], [b_tp])
    STT(k, "dve", Tp, tg, SQ128, k.cm.unsqueeze(1).broadcast_to([128, 8, 256]), ALU.mult, ALU.add, [b_tp, k.b_c], [b_tp])
    p.op("dve", "tensor_reduce", reads=[b_tp], writes=[b_tp], out=bmax, in_=rbb.rearrange("p (b h) -> p h b", h=8),
         axis=AX.X, op=ALU.max)
    TT(k, "dve", bmax, bmax, rb31, ALU.subtract, [b_tp], [b_tp])
    sq = ar.alloc((S,), BF16)
    ksq = ar.alloc((S,), BF16)
    b_sq, b_ksq = p.bufs(2)
    sm = ar.alloc((256,), F32)
    b_sm = p.buf()
    kms = sm[:, 0:8]
    km4 = sm[:, 8:12]
    ksc = sm[:, 12:13]
    nm = sm[:, 16:32]
    gsb = sm[:, 32:96].rearrange("p (a b) -> p a b", a=8)
    g2 = sm[:, 96:160].rearrange("p (a b) -> p a b", a=8)
    eq = sm[:, 160:224].rearrange("p (a b) -> p a b", a=8)
    mx = sm[:, 224:232]
    kmT = ar.alloc((8,), BF16)
    MBq = ar.alloc((16, 16), BF16)
    b_MBq = p.buf()
    p.op("pool", "memset", writes=[b_MBq], ap=MBq, constant=0.0)
    MB = ar.alloc((S,), BF16)
    b_MB = p.buf()
    PTr = Ring([(ar.alloc((256,), BF16), p.buf()) for _ in range(5)])
    yq = [ar.alloc((128,), F32) for _ in range(4)]
    b_yq = p.bufs(4)
    junkf = ar.alloc((128,), F32)
    b_junk = p.buf()
    fin = [ar.alloc((256,), BF16) for _ in range(2)]
    b_fin = p.bufs(2)
    sm2 = Ring([(ar.alloc((8,), F32), p.buf()) for _ in range(8)])
    bc8 = lambda a: a.unsqueeze(2).broadcast_to([128, 8, 8])

    MBs = [MB, ar.alloc((S,), BF16)]
    b_MBs = [b_MB, p.buf()]

    def prologue(h):
        MB, b_MB = MBs[h % 2], b_MBs[h % 2]
        (qbT, kbT, vb1), (b_q, b_k, b_v) = hr.next()
        p.dma(qbT, k.QB[h], reads=[k.b_scr["QB"]], writes=[b_q])
        p.dma(kbT, k.KB[h], reads=[k.b_scr["KB"]], writes=[b_k])
        p.dma(vb1[:, :, 0:128], k.VB[h], reads=[k.b_scr["VB"]], writes=[b_v])
        ACT(k, sq, qbT, AF.Square, [b_q], [b_sq])
        ACT(k, ksq, kbT, AF.Square, [b_k], [b_ksq])
        p.op("dve", "tensor_reduce", reads=[b_k], writes=[b_sm], out=kms, in_=kbT.rearrange("p (n t) -> p n t", n=8),
             axis=AX.X, op=ALU.add)
        TS(k, "dve", kmT, kms, 1.0 / 256, None, ALU.mult, None, [b_sm], [b_sm])
        pG, bG = k.ps.next()
        for qi in range(8, 16):
            MM(k, pG[:, (qi - 8) * 8:(qi - 7) * 8], qbT[:, qi * 128:(qi + 1) * 128], kmT, True, True, [b_q, b_sm], [bG])
        pN, bN = k.ps.next()
        for qi in range(16):
            MM(k, pN[:, qi:qi + 1], sq[:, qi * 128:(qi + 1) * 128], k.ones[:, 0:1], True, True, [b_sq, k.b_c], [bN])
        for c in range(4):
            pK, bK = k.ps.next()
            MM(k, pK[:], k.ones, ksq[:, c * 512:(c + 1) * 512], True, True, [b_ksq, k.b_c], [bK])
            p.op("dve", "tensor_reduce", reads=[bK], writes=[b_sm], out=km4[:, c:c + 1], in_=pK[:], axis=AX.X, op=ALU.max)
        p.op("dve", "tensor_reduce", reads=[b_sm], writes=[b_sm], out=ksc, in_=km4, axis=AX.X, op=ALU.max)
        TS(k, "dve", ksc, ksc, 1.0 / 128, None, ALU.mult, None, [b_sm], [b_sm])
        ACT(k, nm, pN[:, 0:16], AF.Sqrt, [bN, b_sm], [b_sm], scale=ksc)
        TS(k, "dve", MBq[:, :, 8], nm, bmax[:, h:h + 1], -SQ128, ALU.add, ALU.mult, [b_sm, b_tp], [b_MBq])
        TT(k, "dve", gsb, pG[:, 0:64].rearrange("p (a b) -> p a b", a=8), k.gm.rearrange("p (a b) -> p a b", a=8),
           ALU.add, [bG, k.b_c], [b_sm])
        src = gsb
        for it in range(2):
            p.op("dve", "tensor_reduce", reads=[b_sm], writes=[b_sm], out=mx, in_=src, axis=AX.X, op=ALU.max)
            TT(k, "dve", eq, src, bc8(mx), ALU.is_equal, [b_sm], [b_sm])
            STT(k, "dve", g2, eq, -1e30, src, ALU.mult, ALU.add, [b_sm], [b_sm])
            src = g2
        p.op("dve", "tensor_reduce", reads=[b_sm], writes=[b_sm], out=mx, in_=g2, axis=AX.X, op=ALU.max)
        TT(k, "dve", eq, gsb, bc8(mx), ALU.is_ge, [b_sm], [b_sm])
        TS(k, "dve", MBq[:, 8:16, 0:8], eq, BIG * SQ128, -BIG * SQ128, ALU.mult, ALU.add, [b_sm], [b_MBq])
        for half in range(2):
            pM, bM = k.ps.next()
            pMv = pM[:].bitcast(BF16)
            for j in range(8):
                qi = half * 8 + j
                TR(k, pMv[0:9, j * 128:(j + 1) * 128], MBq[:, qi, 0:9], k.ident, [b_MBq], [bM])
            CP(k, "act" if half else "dve", MB[0:9, half * 1024:(half + 1) * 1024], pMv[0:9, 0:1024], [bM], [b_MB])

        return (qbT, kbT, vb1, b_q, b_k, b_v, MB, b_MB)

    preps = {0: prologue(0)}
    for h in range(8):
        qbT, kbT, vb1, b_q, b_k, b_v, MB, b_MB = preps.pop(h)
        deferred = []
        for qb in range(8):
            if qb == 5 and h + 1 < 8:
                preps[h + 1] = prologue(h + 1)
            pend = []

            def emit_pv(item):
                kj_, PT_, b_PT_ = item
                for qt in range(2):
                    if kj_ <= 2 * qb + qt:
                        pO, bO = k.psx[qt]
                        MM(k, pO[:, 0:129], PT_[:, qt * 128:(qt + 1) * 128], vb1[:, kj_, 0:129],
                           kj_ == 0, kj_ == 2 * qb + qt, [b_PT_, b_v], [bO])
            for kj in range(2 * qb + 2):
                n = kj // 2
                pS, bS = k.ps.next()
                q0 = qb * 256
                v = n if (qb >= 4 and n < qb) else 8
                if n == qb and kj % 2 == 0:
                    segs = [(0, 256, Tp[:, h, 0:256])]
                elif n == qb:
                    segs = [(128, 256, Tp[:, h, 0:128])]
                elif n == qb - 1 and kj % 2 == 1:
                    segs = [(0, 128, Tp[:, h, 128:256]), (128, 256, None)]
                else:
                    segs = [(0, 256, None)]
                c0 = segs[0][0]
                for (a, b, bias) in segs:
                    MM(k, pS[:, a:b], kbT[:, kj * 128:(kj + 1) * 128], qbT[:, q0 + a:q0 + b], True, False, [b_k, b_q], [bS])
                    MM(k, pS[:, a:b], k.emat[:, v, :], MB[0:9, q0 + a:q0 + b], False, bias is None, [k.b_c, b_MB], [bS])
                    if bias is not None:
                        MM(k, pS[:, a:b], k.ident, bias, False, True, [k.b_c, b_tp], [bS])
                PT, b_PT = PTr.next()
                ACT(k, PT[:, c0:256], pS[:, c0:256], AF.Exp, [bS], [b_PT], scale=1.0 / SQ128)
                pend.append((kj, PT, b_PT))
                if len(pend) > 2:
                    emit_pv(pend.pop(0))
                if kj == min(3, 2 * qb + 1) and deferred:
                    deferred.pop(0)()
            while pend:
                emit_pv(pend.pop(0))
            parts = []
            for qt in range(2):
                s2, b_s2 = sm2.next()
                y_ap, y_b = yq[(qb % 2) * 2 + qt], b_yq[(qb % 2) * 2 + qt]
                pO, bO = k.psx[qt]
                p.op("dve", "reciprocal", reads=[bO], writes=[b_s2], out=s2[:, 0:1], in_=pO[:, 128:129])
                TS(k, "dve", y_ap, pO[:, 0:128], s2[:, 0:1], None, ALU.mult, None, [bO, b_s2], [y_b])
                parts.append((s2, b_s2, y_ap, y_b))

            def part2(qb=qb, parts=parts):
                f_ap, f_b = fin[qb % 2], b_fin[qb % 2]
                for qt in range(2):
                    s2, b_s2, y_ap, y_b = parts[qt]
                    TT(k, "dve", junkf, y_ap, y_ap, ALU.mult, [y_b], [b_junk])
                    p.op("dve", "tensor_reduce", reads=[b_junk], writes=[b_s2], out=s2[:, 1:2], in_=junkf, axis=AX.X,
                         op=ALU.add)
                    ACT(k, s2[:, 1:2], s2[:, 1:2], AF.Ln, [b_s2], [b_s2], scale=1.0 / 128, bias=EPS)
                    ACT(k, s2[:, 1:2], s2[:, 1:2], AF.Exp, [b_s2], [b_s2], scale=-0.5)
                    STT(k, "dve", f_ap[:, qt * 128:(qt + 1) * 128], y_ap, s2[:, 1:2], mgb, ALU.mult, ALU.mult,
                        [y_b, b_s2, b_tp], [f_b])
                pT, bpT = k.ps.next()
                pTv = pT[:].bitcast(BF16)
                for qt in range(2):
                    TR(k, pTv[:, qt * 128:(qt + 1) * 128], f_ap[:, qt * 128:(qt + 1) * 128], k.ident, [f_b], [bpT])
                CP(k, "act", oT[:, 8 + h, qb * 256:(qb + 1) * 256], pTv[:, 0:256], [bpT], [k.b_oT])
            deferred.append(part2)
        while deferred:
            deferred.pop(0)()
    if hasattr(k, "OT"):
        p.dma(k.OT, oT, reads=[k.b_oT], writes=[k.b_scr["DBG"]])


def phase_wout(k):
    p = k.p
    ar = k.ar
    I = k.I
    oT = k.oT
    ar.off = k.oT_mark
    h2T = ar.alloc((16, S), BF16)
    k.h2T = h2T
    k.b_h2T = p.buf("h2T")
    k.ws_mark = ar.off
    wring = Ring([(ar.alloc((16, 512), BF16), p.buf()) for _ in range(2)])
    xr = Ring([(ar.alloc((512,), F32), p.buf()) for _ in range(6)])
    wsrc = I["w_out"].rearrange("(kt q) n -> q kt n", q=128)
    for c in range(4):
        wap, wb = load_w_block(k, wring, wsrc[:, :, c * 512:(c + 1) * 512])
        for t in range(NT):
            x_ap, x_b = xr.next()
            p.dma(x_ap, I["x"][t * 128:(t + 1) * 128, c * 512:(c + 1) * 512], writes=[x_b])
            pb, bpb = k.ps.next()
            for kt in range(16):
                MM(k, pb[:], oT[:, kt, t * 128:(t + 1) * 128], wap[:, kt, :], kt == 0, kt == 15, [wb, k.b_oT], [bpb])
            TT(k, "dve", x_ap, pb[:], x_ap, ALU.add, [bpb, x_b], [x_b])
            p.dma(k.X1[t * 128:(t + 1) * 128, c * 512:(c + 1) * 512], x_ap, reads=[x_b], writes=[k.b_scr["X1"]])
    p.barrier()
    ar.off = k.ws_mark
    tiles = [(k.X1[t * 128:(t + 1) * 128, :], [k.b_scr["X1"]]) for t in range(NT)]
    norm_T(k, tiles, I["xattn_norm_g"], h2T, k.b_h2T)


def phase_xattn(k):
    p = k.p
    ar = k.ar
    I = k.I
    h2T = k.h2T
    ar.off = 0
    memT = ar.alloc((16, MEM), BF16)
    b_memT = p.buf()
    kxT = ar.alloc((4, MEM), BF16)
    vx = ar.alloc((2, 512), BF16)
    b_kv = p.buf()
    qxT = ar.alloc((4, S), BF16)
    b_qx = p.buf()
    oxT = ar.alloc((4, S), BF16)
    b_ox = p.buf()
    wxo = ar.alloc((4, D), BF16)
    b_wxo = p.buf()
    assert ar.off <= k.oT_mark
    ar.off = k.ws_mark
    tiles = [(I["mem"][t * 128:(t + 1) * 128, :], []) for t in range(2)]
    norm_T(k, tiles, I["mem_norm_g"], memT, b_memT)
    p.barrier()
    ar.off = k.ws_mark
    wring = Ring([(ar.alloc((16, 512), BF16), p.buf()) for _ in range(2)])
    Pf = Ring([(ar.alloc((256,), F32), p.buf()) for _ in range(4)])
    Pn = Ring([(ar.alloc((256,), BF16), p.buf()) for _ in range(4)])
    PnT = Ring([(ar.alloc((2, 128), BF16), p.buf()) for _ in range(4)])
    sm = Ring([(ar.alloc((8,), F32), p.buf()) for _ in range(8)])
    xr = Ring([(ar.alloc((512,), F32), p.buf()) for _ in range(3)])
    wkv = I["w_xkv"].rearrange("(kt q) n -> q kt n", q=128)
    wap, wb = load_w_block(k, wring, wkv[:, :, 0:512])
    for hx in range(4):
        pb, bpb = k.ps.next()
        for kt in range(16):
            MM(k, pb[:, 0:MEM], wap[:, kt, hx * 128:(hx + 1) * 128], memT[:, kt, :], kt == 0, kt == 15, [wb, b_memT], [bpb])
        CP(k, "act" if hx % 2 else "dve", kxT[:, hx, :], pb[:, 0:MEM], [bpb], [b_kv])
    wap, wb = load_w_block(k, wring, wkv[:, :, 512:1024])
    for mt in range(2):
        pb, bpb = k.ps.next()
        for kt in range(16):
            MM(k, pb[:], memT[:, kt, mt * 128:(mt + 1) * 128], wap[:, kt, :], kt == 0, kt == 15, [wb, b_memT], [bpb])
        CP(k, "act" if mt else "dve", vx[:, mt, :], pb[:], [bpb], [b_kv])
    wq = I["w_xq"].rearrange("(kt q) n -> q kt n", q=128)
    wap, wb = load_w_block(k, wring, wq)
    for hx in range(4):
        for c in range(4):
            pb, bpb = k.ps.next()
            for kt in range(16):
                MM(k, pb[:], wap[:, kt, hx * 128:(hx + 1) * 128], h2T[:, kt, c * 512:(c + 1) * 512], kt == 0, kt == 15,
                   [wb, k.b_h2T], [bpb])
            CP(k, "act" if c % 2 else "dve", qxT[:, hx, c * 512:(c + 1) * 512], pb[:], [bpb], [b_qx])
    p.dma(wxo, I["w_xo"].rearrange("(kt q) n -> q kt n", q=128), writes=[b_wxo], q="pool")
    for t in range(NT):
        st = []
        for hx in range(4):
            pS, bS = k.ps.next()
            MM(k, pS[:, 0:MEM], qxT[:, hx, t * 128:(t + 1) * 128], kxT[:, hx, :], True, True, [b_qx, b_kv], [bS])
            s_ap, s_b = sm.next()
            st.append([pS, bS, s_ap, s_b])
        for hx in range(4):
            pS, bS, s_ap, s_b = st[hx]
            p.op("dve", "tensor_reduce", reads=[bS], writes=[s_b], out=s_ap[:, 0:1], in_=pS[:, 0:MEM], axis=AX.X, op=ALU.max)
            TS(k, "dve", s_ap[:, 1:2], s_ap[:, 0:1], -1.0 / SQ128, None, ALU.mult, None, [s_b], [s_b])
        for hx in range(4):
            pS, bS, s_ap, s_b = st[hx]
            pf, b_pf = Pf.next()
            ACT(k, pf, pS[:, 0:MEM], AF.Exp, [bS, s_b], [b_pf, s_b], scale=1.0 / SQ128, bias=s_ap[:, 1:2],
                accum_out=s_ap[:, 2:3])
            st[hx] += [pf, b_pf]
        for hx in range(4):
            pS, bS, s_ap, s_b, pf, b_pf = st[hx]
            p.op("dve", "reciprocal", reads=[s_b], writes=[s_b], out=s_ap[:, 3:4], in_=s_ap[:, 2:3])
            pn, b_pn = Pn.next()
            TS(k, "dve", pn, pf, s_ap[:, 3:4], None, ALU.mult, None, [b_pf, s_b], [b_pn])
            st[hx] += [pn, b_pn]
        for hx in range(4):
            pn, b_pn = st[hx][6], st[hx][7]
            pT, bpT = k.ps.next()
            pTv = pT[:].bitcast(BF16)
            for mt in range(2):
                TR(k, pTv[:, mt * 128:(mt + 1) * 128], pn[:, mt * 128:(mt + 1) * 128], k.ident, [b_pn], [bpT])
            pnt, b_pnt = PnT.next()
            CP(k, "act" if hx % 2 else "dve", pnt, pTv[:, 0:256].rearrange("p (a b) -> p a b", a=2), [bpT], [b_pnt])
            st[hx] += [pnt, b_pnt]
        for hx in range(4):
            pnt, b_pnt = st[hx][8], st[hx][9]
            pO, bO = k.ps.next()
            for mt in range(2):
                MM(k, pO[:, 0:128], vx[:, mt, hx * 128:(hx + 1) * 128], pnt[:, mt, :], mt == 0, mt == 1, [b_kv, b_pnt], [bO])
            CP(k, "dve" if hx % 2 else "act", oxT[:, hx, t * 128:(t + 1) * 128], pO[:, 0:128], [bO], [b_ox])
    p.barrier()
    ar.off = k.ws_mark
    xr = Ring([(ar.alloc((512,), F32), p.buf()) for _ in range(8)])
    for t in range(NT):
        for c in range(4):
            x_ap, x_b = xr.next()
            p.dma(x_ap, k.X1[t * 128:(t + 1) * 128, c * 512:(c + 1) * 512], reads=[k.b_scr["X1"]], writes=[x_b])
            pb, bpb = k.ps.next()
            for kt in range(4):
                MM(k, pb[:], oxT[:, kt, t * 128:(t + 1) * 128], wxo[:, kt, c * 512:(c + 1) * 512], kt == 0, kt == 3,
                   [b_wxo, b_ox], [bpb])
            TT(k, "dve", x_ap, pb[:], x_ap, ALU.add, [bpb, x_b], [x_b])
            p.dma(k.X2[t * 128:(t + 1) * 128, c * 512:(c + 1) * 512], x_ap, reads=[x_b], writes=[k.b_scr["X2"]])


def phase_ffn(k):
    p = k.p
    ar = k.ar
    I = k.I
    HT = 1024
    halo = p.sbuf("halo", [128, NFF, 2], F32)
    b_halo = p.buf()
    p.op("pool", "memset", writes=[b_halo], ap=halo[:], constant=0.0)
    cwf = p.sbuf("cwf", [128, 4 * NFF], F32)
    b_cwf = p.buf()
    for hf in range(2):
        ar.reset()
        aT = ar.alloc((NFF, HT), BF16)
        b_aT = p.buf()
        mark0 = ar.off
        h3T = ar.alloc((16, HT), BF16)
        b_h3T = p.buf()
        mark1 = ar.off
        tiles = [(k.X2[(hf * 8 + t) * 128:(hf * 8 + t + 1) * 128, :], [k.b_scr["X2"]]) for t in range(8)]
        norm_T(k, tiles, I["ffn_norm_g"], h3T, b_h3T)
        p.barrier()
        ar.off = mark1
        if hf == 0:
            stg = ar.alloc((128,), F32)
            b_stg = p.buf()
            for j in range(4):
                src = (I["ffn_conv_w"][j] if j < 3 else I["ffn_conv_b"][0]).rearrange("(b q) -> b q", q=128)
                p.dma(stg[0:NFF, :], src, writes=[b_stg])
                pb, bpb = k.ps.next()
                TR(k, pb[:, 0:NFF], stg[0:NFF, :], k.identf[0:NFF, 0:NFF], [b_stg], [bpb])
                CP(k, "dve", cwf[:, j * NFF:(j + 1) * NFF], pb[:, 0:NFF], [bpb], [b_cwf])
        wg = Ring([(ar.alloc((16, 256), BF16), p.buf()) for _ in range(2)])
        wu = Ring([(ar.alloc((16, 256), BF16), p.buf()) for _ in range(2)])
        graw = Ring([(ar.alloc((HT + 2,), F32), p.buf()) for _ in range(2)])
        gy = Ring([(ar.alloc((HT,), F32), p.buf()) for _ in range(2)])
        wgs = I["w_gate"].rearrange("(kt q) n -> q kt n", q=128)
        wus = I["w_up"].rearrange("(kt q) n -> q kt n", q=128)
        for j in range(NFF):
            if j % 2 == 0:
                g_ap2, g_b = wg.next()
                u_ap2, u_b = wu.next()
                p.dma(g_ap2, wgs[:, :, j * 128:(j + 2) * 128], writes=[g_b], q="pool")
                p.dma(u_ap2, wus[:, :, j * 128:(j + 2) * 128], writes=[u_b], q="pool")
            g_ap = g_ap2[:, :, (j % 2) * 128:(j % 2 + 1) * 128]
            u_ap = u_ap2[:, :, (j % 2) * 128:(j % 2 + 1) * 128]
            r_ap, r_b = graw.next()
            y_ap, y_b = gy.next()
            pus = []
            for c in range(2):
                pg, bg = k.ps.next()
                for kt in range(16):
                    MM(k, pg[:], g_ap[:, kt, :], h3T[:, kt, c * 512:(c + 1) * 512], kt == 0, kt == 15, [g_b, b_h3T], [bg])
                CP(k, "act", r_ap[:, 2 + c * 512:2 + (c + 1) * 512], pg[:], [bg], [r_b])
            for c in range(2):
                pu, bu = k.ps.next()
                for kt in range(16):
                    MM(k, pu[:], u_ap[:, kt, :], h3T[:, kt, c * 512:(c + 1) * 512], kt == 0, kt == 15, [u_b, b_h3T], [bu])
                pus.append((pu, bu))
            CP(k, "pool", r_ap[:, 0:2], halo[:, j, :], [b_halo], [r_b])
            if hf == 0:
                CP(k, "pool", halo[:, j, :], r_ap[:, HT:HT + 2], [r_b], [b_halo])
            TS(k, "dve", y_ap, r_ap[:, 0:HT], cwf[:, j:j + 1], None, ALU.mult, None, [r_b, b_cwf], [y_b])
            for tap in range(1, 3):
                STT(k, "dve", y_ap, r_ap[:, tap:tap + HT], cwf[:, tap * NFF + j:tap * NFF + j + 1], y_ap, ALU.mult, ALU.add,
                    [r_b, b_cwf, y_b], [y_b])
            ACT(k, y_ap, y_ap, AF.Silu, [y_b, b_cwf], [y_b], bias=cwf[:, 3 * NFF + j:3 * NFF + j + 1])
            for c in range(2):
                pu, bu = pus[c]
                TT(k, "dve", aT[:, j, c * 512:(c + 1) * 512], y_ap[:, c * 512:(c + 1) * 512], pu[:], ALU.mult,
                   [y_b, bu], [b_aT])
        p.barrier()
        ar.off = mark0
        x3 = ar.alloc((8, D), F32)
        b_x3 = p.bufs(8)
        aflat = aT.rearrange("p a b -> p (a b)")
        fgb = aflat[:, 0:2 * D].bitcast(F32)
        junk = aflat[:, 2 * D:3 * D]
        b_fgb = b_aT
        b_junk = b_aT
        wd = Ring([(ar.alloc((4, 512), BF16), p.buf()) for _ in range(3)])
        xr = Ring([(ar.alloc((512,), F32), p.buf()) for _ in range(2)])
        ssf = ar.alloc((8,), F32)
        b_ssf = p.buf()
        for cc in range(4):
            for j4 in range(NFF // 4):
                w_ap, w_b = wd.next()
                p.dma(w_ap, I["w_down"][j4 * 512:(j4 + 1) * 512, cc * 512:(cc + 1) * 512].rearrange("(a q) n -> q a n", q=128),
                      writes=[w_b], q="pool")
                for a in range(4):
                    j = j4 * 4 + a
                    for tt in range(8):
                        pb, bpb = k.banks[tt]
                        MM(k, pb[:], aT[:, j, tt * 128:(tt + 1) * 128], w_ap[:, a, :], j == 0, j == NFF - 1, [w_b, b_aT], [bpb])
            for tt in range(8):
                pb, bpb = k.banks[tt]
                x_ap, x_b = xr.next()
                row = (hf * 8 + tt) * 128
                p.dma(x_ap, k.X2[row:row + 128, cc * 512:(cc + 1) * 512], reads=[k.b_scr["X2"]], writes=[x_b])
                TT(k, "dve", x3[:, tt, cc * 512:(cc + 1) * 512], pb[:], x_ap, ALU.add, [bpb, x_b], [b_x3[tt]])
        p.dma(fgb, I["final_norm_g"].partition_broadcast(128), writes=[b_aT])
        outs = []
        for tt in range(8):
            row = (hf * 8 + tt) * 128
            ACT(k, junk, x3[:, tt, :], AF.Square, [b_x3[tt]], [b_junk, b_ssf], accum_out=ssf[:, tt:tt + 1])
            ACT(k, ssf[:, tt:tt + 1], ssf[:, tt:tt + 1], AF.Sqrt, [b_ssf], [b_ssf], scale=1.0 / D, bias=EPS)
            p.op("dve", "reciprocal", reads=[b_ssf], writes=[b_ssf], out=ssf[:, tt:tt + 1], in_=ssf[:, tt:tt + 1])
            STT(k, "dve", x3[:, tt, :], x3[:, tt, :], ssf[:, tt:tt + 1], fgb, ALU.mult, ALU.mult,
                [b_x3[tt], b_ssf, b_fgb], [b_x3[tt]])
            outs.append(p.dma(k.out[row:row + 128, :], x3[:, tt, :], reads=[b_x3[tt]]))
        p.barrier()
    return []


_CACHE = {}


def make_in_maps(inputs, n=8, skip=()):
    cf, cb = host_consts()
    tidx = toeplitz_index()
    rb = np.asarray(inputs["rel_bias"], np.float32)
    rb_toep = np.ascontiguousarray(rb[tidx].transpose(0, 2, 1))
    shared = {
        "mix_norm_g": inputs["mix_norm_g"].reshape(1, D), "xattn_norm_g": inputs["xattn_norm_g"].reshape(1, D),
        "mem_norm_g": inputs["mem_norm_g"].reshape(1, D), "ffn_norm_g": inputs["ffn_norm_g"].reshape(1, D),
        "final_norm_g": inputs["final_norm_g"].reshape(1, D),
        "w_in": inputs["w_in"][0], "gdn_conv_w": inputs["gdn_conv_w"][0], "gdn_a_log": inputs["gdn_a_log"].reshape(1, 8),
        "gdn_dt_bias": inputs["gdn_dt_bias"].reshape(1, 8), "gdn_norm_g": inputs["gdn_norm_g"].reshape(1, 128),
        "moba_norm_g": inputs["moba_norm_g"].reshape(1, 128), "rel_bias": rb.reshape(1, 256), "rb_toep": rb_toep,
        "w_out": inputs["w_out"][0], "w_xq": inputs["w_xq"][0], "w_xkv": inputs["w_xkv"][0], "w_xo": inputs["w_xo"][0],
        "w_gate": inputs["w_gate"][0], "w_up": inputs["w_up"][0], "ffn_conv_w": inputs["ffn_conv_w"][0],
        "ffn_conv_b": inputs["ffn_conv_b"].reshape(1, DFF), "w_down": inputs["w_down"][0],
        "cst_f": cf, "cst_b": cb,
    }
    shared = {kk: (np.zeros((1, 1), np.float32) if kk in skip else np.ascontiguousarray(np.asarray(v, np.float32)))
              for kk, v in shared.items()}
    maps = []
    for b in range(n):
        m = dict(shared)
        m["x"] = np.ascontiguousarray(np.asarray(inputs["x"][b], np.float32))
        m["mem"] = (np.zeros((1, 1), np.float32) if "mem" in skip
                    else np.ascontiguousarray(np.asarray(inputs["mem"][b], np.float32)))
        maps.append(m)
    return maps


def kernel(**inputs):
    if "nc" not in _CACHE:
        _CACHE["nc"] = build_program()[0]
    nc = _CACHE["nc"]
    maps = make_in_maps(inputs, 8)
    res = run_bass_kernel_spmd(nc, maps, core_ids=list(range(8)))
    return np.stack([np.asarray(r["out"], np.float32) for r in res.results], axis=0)
```

```python
import bisect
import math
from contextlib import ExitStack

import numpy as np
import concourse.bass as bass
import concourse.mybir as mybir
from concourse.bass_utils import run_bass_kernel_spmd

F32 = mybir.dt.float32
BF16 = mybir.dt.bfloat16
ALU = mybir.AluOpType
AF = mybir.ActivationFunctionType
AX = mybir.AxisListType

S = 2048
D = 2048
NT = 16
H = 8
HD = 128
DFF = 5632
NFF = 44
MEM = 256
INW = 7184
EPS = 1e-6
BIG = 30000.0
SQ128 = math.sqrt(128.0)

ENGS = ("pe", "act", "dve", "pool", "sp")
SEM_LIMIT = 2000
N_ENG_SEMS = {"pe": 14, "act": 5, "dve": 5, "pool": 3, "sp": 1}
N_DMA_SEMS = 32
N_SWDMA_SEMS = 16


class Buf:
    __slots__ = ("name", "last_w", "creads", "dreads")

    def __init__(self, name=""):
        self.name = name
        self.last_w = None
        self.creads = {}
        self.dreads = []


class Op:
    __slots__ = ("eng", "name", "kw", "waits", "signal", "pos")

    def __init__(self, eng, name, kw, pos):
        self.eng = eng
        self.name = name
        self.kw = kw
        self.waits = []
        self.signal = None
        self.pos = pos


class Prog:
    def __init__(self, nc, stack):
        self.nc = nc
        self.stack = stack
        self.ops = {e: [] for e in ENGS}
        self.nsig = {e: 0 for e in ENGS}
        self.sigpos = {e: [] for e in ENGS}
        self.sigtok = {e: [] for e in ENGS}
        self.waited = {e: {} for e in ENGS}
        self.esems = {e: [stack.enter_context(nc.semaphore(f"s_{e}_{i}")) for i in range(N_ENG_SEMS[e])]
                      for e in ENGS}
        self.dsems = {"hw": [stack.enter_context(nc.semaphore(f"s_dma_{i}")) for i in range(N_DMA_SEMS)],
                      "sw": [stack.enter_context(nc.semaphore(f"s_swdma_{i}")) for i in range(N_SWDMA_SEMS)]}
        self.ndma_q = {"hw": 0, "sw": 0}
        self.ndma = 0
        self.dma_toks = []
        self.pe_prev = (None, None, True)

    def sbuf(self, name, shape, dtype):
        return self.stack.enter_context(self.nc.sbuf_tensor(name, list(shape), dtype))

    def psum(self, name, shape, dtype):
        return self.stack.enter_context(self.nc.psum_tensor(name, list(shape), dtype))

    def buf(self, name=""):
        return Buf(name)

    def bufs(self, n):
        return [Buf() for _ in range(n)]

    def _force_signal(self, eng):
        lst = self.ops[eng]
        if not lst:
            return None
        last = lst[-1]
        if last.signal is None:
            n = self.nsig[eng]
            self.nsig[eng] += 1
            sem = self.esems[eng][n // SEM_LIMIT]
            val = n % SEM_LIMIT + 1
            last.signal = (sem, 1)
            self.sigpos[eng].append(last.pos)
            self.sigtok[eng].append((sem, val))
        return last

    def _resolve(self, tok):
        if tok[0] == "d":
            return tok[1], tok[2]
        _, eng, pos = tok
        sp = self.sigpos[eng]
        i = bisect.bisect_left(sp, pos)
        if i < len(sp):
            return self.sigtok[eng][i]
        last = self._force_signal(eng)
        assert last.pos >= pos
        i = bisect.bisect_left(sp, pos)
        return self.sigtok[eng][i]

    def _add_wait(self, op, tok):
        sem, val = self._resolve(tok)
        w = self.waited[op.eng]
        key = id(sem)
        if w.get(key, 0) >= val:
            return
        w[key] = val
        for i, (s, v) in enumerate(op.waits):
            if s is sem:
                op.waits[i] = (sem, max(v, val))
                return
        op.waits.append((sem, val))

    def _deps(self, op, reads, writes, is_dma):
        eng = op.eng
        for b in reads:
            if b.last_w is not None:
                self._add_wait(op, b.last_w)
        for b in writes:
            t = b.last_w
            if t is not None and not (t[0] == "c" and t[1] == eng == "pe" and not is_dma):
                self._add_wait(op, t)
            for e, t in b.creads.items():
                if e == eng == "pe" and not is_dma:
                    continue
                self._add_wait(op, t)
            for t in b.dreads:
                self._add_wait(op, t)

    def _compute_pos_check(self, eng):
        lst = self.ops[eng]
        if lst and lst[-1].signal is None and lst[-1].name not in ("nop", "dma_start"):
            self._force_signal(eng)

    def op(self, eng, name, reads=(), writes=(), **kw):
        lst = self.ops[eng]
        rset = frozenset(id(b) for b in reads)
        wset = frozenset(id(b) for b in writes)
        if lst and lst[-1].name not in ("nop", "dma_start") and lst[-1].signal is None:
            prev = lst[-1]
            if eng != "pe":
                self._force_signal(eng)
            else:
                pr, pw, pdone = self.pe_prev
                if pr != rset or (pw != wset and pdone):
                    self._force_signal(eng)
        if eng == "pe":
            self.pe_prev = (rset, wset, kw.get("stop", True))
        o = Op(eng, name, kw, len(lst))
        self._deps(o, reads, writes, False)
        lst.append(o)
        tok = ("c", eng, o.pos)
        for b in reads:
            b.creads[eng] = tok
        for b in writes:
            b.last_w = tok
            b.creads = {}
            b.dreads = []
        return o

    def dma(self, out, in_, reads=(), writes=(), q="sp", **kw):
        if q != "sp":
            self._compute_pos_check(q)
        lst = self.ops[q]
        o = Op(q, "dma_start", dict(out=out, in_=in_, **kw), len(lst))
        self._deps(o, reads, writes, True)
        kind = "sw" if q == "pool" else "hw"
        pool_ = self.dsems[kind]
        i = self.ndma_q[kind] % len(pool_)
        r = self.ndma_q[kind] // len(pool_)
        self.ndma_q[kind] += 1
        self.ndma += 1
        sem = pool_[i]
        if r > 0:
            self._add_wait(o, ("d", sem, 16 * r))
        o.signal = (sem, 16)
        lst.append(o)
        tok = ("d", sem, 16 * (r + 1))
        self.dma_toks.append(tok)
        for b in reads:
            b.dreads.append(tok)
        for b in writes:
            b.last_w = tok
            b.creads = {}
            b.dreads = []
        return tok

    def barrier(self):
        toks = list(self.dma_toks[-(N_DMA_SEMS + N_SWDMA_SEMS) * 2:])
        for e in ENGS:
            lst = self.ops[e]
            if lst and lst[-1].name not in ("dma_start", "nop"):
                last = self._force_signal(e)
                toks.append(("c", e, last.pos))
            else:
                for o in reversed(lst):
                    if o.name not in ("dma_start", "nop"):
                        assert o.signal is not None
                        toks.append(("c", e, o.pos))
                        break
        for e in ENGS:
            o = Op(e, "nop", {}, len(self.ops[e]))
            for t in toks:
                self._add_wait(o, t)
            self.ops[e].append(o)

    def final_wait(self, eng, toks):
        o = Op(eng, "nop", {}, len(self.ops[eng]))
        for t in toks:
            self._add_wait(o, t)
        self.ops[eng].append(o)

    def emit(self):
        nc = self.nc
        prog = self
        with nc.Block() as block:
            def run(engname):
                def body(e):
                    for o in prog.ops[engname]:
                        for (sem, val) in o.waits:
                            e.wait_ge(sem, val)
                        if o.name == "nop":
                            continue
                        ins = getattr(e, o.name)(**o.kw)
                        if o.signal is not None:
                            ins.then_inc(o.signal[0], o.signal[1])
                return body
            block.tensor(run("pe"))
            block.scalar(run("act"))
            block.vector(run("dve"))
            block.gpsimd(run("pool"))
            block.sync(run("sp"))

    def stats(self):
        return {e: (len(self.ops[e]), self.nsig[e]) for e in ENGS}, self.ndma


class Arena:
    def __init__(self, p, nbytes):
        self.t = p.sbuf("arena", [128, nbytes // 2], BF16)
        self.n = nbytes
        self.off = 0

    def reset(self):
        self.off = 0

    def alloc(self, shape, dtype):
        esz = 2 if dtype == BF16 else 4
        n = 1
        for s in shape:
            n *= s
        nb = n * esz
        a = self.t[:, self.off // 2:(self.off + nb) // 2]
        self.off += (nb + 63) // 64 * 64
        assert self.off <= self.n, f"arena overflow {self.off} > {self.n}"
        if dtype != BF16:
            a = a.bitcast(dtype)
        if len(shape) == 2:
            a = a.rearrange("p (a b) -> p a b", a=shape[0])
        elif len(shape) == 3:
            a = a.rearrange("p (a b c) -> p a b c", a=shape[0], b=shape[1])
        return a


class Ring:
    def __init__(self, items):
        self.items = items
        self.i = 0

    def next(self):
        it = self.items[self.i % len(self.items)]
        self.i += 1
        return it


NCF = 576
NCB = 768 + 9 * 128


def host_consts():
    cf = np.zeros((128, NCF), np.float32)
    ii = np.arange(128)
    cf[:, 0:128] = np.eye(128, dtype=np.float32)
    cf[:, 128:256] = (ii[:, None] <= ii[None, :]).astype(np.float32)
    cf[:, 256:384] = (ii[:, None] > ii[None, :]).astype(np.float32)
    cf[:, 384:512] = 1.0
    gm = np.zeros((128, 8, 8), np.float32)
    for qi in range(8, 16):
        own = qi // 2
        gm[:, qi - 8, own:] = -1e30
    cf[:, 512:576] = gm.reshape(128, 64)
    cb = np.zeros((128, NCB), np.float32)
    cb[:, 0:128] = np.eye(128, dtype=np.float32)
    cb[:, 128:256] = 1.0
    cb[:, 256:384] = (ii[None, :] > ii[:, None]).astype(np.float32) * BIG
    cb[:, 384:512] = (ii[None, :] >= ii[:, None]).astype(np.float32) * BIG
    rr = np.arange(256)
    cb[:, 512:768] = (rr[None, :] < ii[:, None]).astype(np.float32) * (-BIG * SQ128)
    e = np.zeros((9, 9, 128), np.float32)
    for v in range(9):
        e[8, v, :] = 1.0
        if v < 8:
            e[v, v, :] = 1.0
    cb[0:9, 768:768 + 9 * 128] = e.reshape(9, 9 * 128)
    return cf, cb


def rel_bucket_np(n):
    n = np.maximum(n, 0)
    max_exact = 16
    nf = np.maximum(n, 1).astype(np.float32)
    large = max_exact + (np.log(nf / max_exact) / math.log(128 / max_exact) * (32 - max_exact)).astype(np.int32)
    large = np.minimum(large, 31)
    return np.where(n < max_exact, n, large)


def toeplitz_index():
    kk = np.arange(128)[:, None]
    r = np.arange(256)[None, :]
    return rel_bucket_np(r - kk)


class K:
    pass


def build_program(debug_phase=None):
    nc = bass.Bass("TRN2", target_bir_lowering=False)
    k = K()
    k.nc = nc
    dbg = debug_phase is not None

    order = ["inproj", "gdn", "moba", "wout", "xattn", "ffn"]
    need_from = {"mem": "xattn", "w_out": "wout", "w_xq": "xattn", "w_xkv": "xattn", "w_xo": "xattn",
                 "w_gate": "ffn", "w_up": "ffn", "w_down": "ffn"}
    k.skip = set()
    if dbg:
        for nm, ph in need_from.items():
            if order.index(ph) > order.index(debug_phase):
                k.skip.add(nm)

    def din(name, shape, dt=F32):
        if name in k.skip:
            shape = [1, 1]
        return nc.dram_tensor(name, list(shape), dt, kind="ExternalInput").ap()

    DUMP = {"inproj": ("QA", "KA", "KT", "VA", "ZS", "QB", "KB", "VB", "DBG"), "moba": ("OT",), "gdn": ("OT",),
            "wout": ("X1",), "xattn": ("X2",), "ffn": ("OT", "X1", "X2")}.get(debug_phase, ())

    def dscr(name, shape, dt=BF16):
        kind = "ExternalOutput" if name in DUMP else "Internal"
        return nc.dram_tensor(name, list(shape), dt, kind=kind).ap()

    I = {}
    I["x"] = din("x", [S, D])
    I["mem"] = din("mem", [MEM, D])
    for nm in ("mix_norm_g", "xattn_norm_g", "mem_norm_g", "ffn_norm_g", "final_norm_g"):
        I[nm] = din(nm, [1, D])
    I["w_in"] = din("w_in", [D, INW])
    I["gdn_conv_w"] = din("gdn_conv_w", [4, 3072])
    I["gdn_a_log"] = din("gdn_a_log", [1, 8])
    I["gdn_dt_bias"] = din("gdn_dt_bias", [1, 8])
    I["gdn_norm_g"] = din("gdn_norm_g", [1, 128])
    I["moba_norm_g"] = din("moba_norm_g", [1, 128])
    I["rel_bias"] = din("rel_bias", [1, 256])
    I["rb_toep"] = din("rb_toep", [128, 8, 256])
    I["w_out"] = din("w_out", [D, D])
    I["w_xq"] = din("w_xq", [D, 512])
    I["w_xkv"] = din("w_xkv", [D, 1024])
    I["w_xo"] = din("w_xo", [512, D])
    I["w_gate"] = din("w_gate", [D, DFF])
    I["w_up"] = din("w_up", [D, DFF])
    I["ffn_conv_w"] = din("ffn_conv_w", [3, DFF])
    I["ffn_conv_b"] = din("ffn_conv_b", [1, DFF])
    I["w_down"] = din("w_down", [DFF, D])
    I["cst_f"] = din("cst_f", [128, NCF])
    I["cst_b"] = din("cst_b", [128, NCB])
    out = nc.dram_tensor("out", [S, D], F32, kind="ExternalOutput").ap()
    k.I = I
    k.out = out

    k.QA = dscr("QA", [128, 16, 8, 128])
    k.KA = dscr("KA", [128, 16, 8, 128])
    k.KT = dscr("KT", [128, 16, 8, 128])
    k.VA = dscr("VA", [128, 16, 8, 128])
    k.ZS = dscr("ZS", [128, 16, 8, 128])
    k.QB = dscr("QB", [8, 128, 2048])
    k.KB = dscr("KB", [8, 128, 2048])
    k.VB = dscr("VB", [8, 128, 16, 128])
    k.X1 = dscr("X1", [S, D], F32)
    k.X2 = dscr("X2", [S, D], F32)
    if "DBG" in DUMP:
        k.DBG = dscr("DBG", [128, 4096], F32)
    if "OT" in DUMP:
        k.OT = dscr("OT", [128, 16, S], BF16)
    k.b_scr = {nm: Buf(nm) for nm in ("QA", "KA", "KT", "VA", "ZS", "QB", "KB", "VB", "X1", "X2", "DBG")}

    with ExitStack() as st:
        p = Prog(nc, st)
        k.p = p
        k.cf = p.sbuf("cf", [128, NCF], F32)
        k.cb = p.sbuf("cb", [128, NCB], BF16)
        k.b_c = p.buf("consts")
        p.dma(k.cf[:], I["cst_f"], writes=[k.b_c])
        p.dma(k.cb[:], I["cst_b"], writes=[k.b_c], q="pool")
        k.identf = k.cf[:, 0:128]
        k.trif = k.cf[:, 128:256]
        k.suf = k.cf[:, 256:384]
        k.onesf = k.cf[:, 384:512]
        k.gm = k.cf[:, 512:576]
        k.ident = k.cb[:, 0:128]
        k.ones = k.cb[:, 128:256]
        k.umq = k.cb[:, 256:384]
        k.uma = k.cb[:, 384:512]
        k.cm = k.cb[:, 512:768]
        k.emat = k.cb[0:9, 768:768 + 9 * 128].rearrange("p (v n) -> p v n", v=9)
        k.BA = p.sbuf("BA", [128, 16, 16], F32)
        k.b_BA = p.buf("BA")
        k.BETA = p.sbuf("BETA", [128, 16, 8], F32)
        k.GG = p.sbuf("GG", [128, 16, 8], F32)
        k.b_bg = p.buf("betag")
        banks = [(p.psum(f"ps{i}", [128, 512], F32), p.buf(f"ps{i}")) for i in range(8)]
        k.ps = Ring(banks[0:6])
        k.psx = banks[6:8]
        k.banks = banks
        k.ar = Arena(p, 176 * 1024)

        phases = [phase_inproj, phase_gdn, phase_moba, phase_wout, phase_xattn, phase_ffn]
        names = ["inproj", "gdn", "moba", "wout", "xattn", "ffn"]
        out_toks = []
        for fn, nm in zip(phases, names):
            r = fn(k)
            if r:
                out_toks += r
            p.barrier()
            if debug_phase == nm:
                break
        p.final_wait("sp", p.dma_toks[-(N_DMA_SEMS + N_SWDMA_SEMS) * 2:])
        k.stats = p.stats()
        p.emit()
    return nc, k


def MM(k, out, lhsT, rhs, start, stop, r, w):
    k.p.op("pe", "matmul", reads=r, writes=w, out=out, lhsT=lhsT, rhs=rhs, start=start, stop=stop)


def TR(k, out, in_, ident, r, w):
    k.p.op("pe", "transpose", reads=r + [k.b_c], writes=w, out=out, in_=in_, identity=ident)


def ACT(k, out, in_, func, r, w, **kw):
    k.p.op("act", "activation", reads=r, writes=w, out=out, in_=in_, func=func, **kw)


def CP(k, eng, out, in_, r, w):
    if eng == "act":
        k.p.op("act", "copy", reads=r, writes=w, out=out, in_=in_)
    else:
        k.p.op(eng, "tensor_copy", reads=r, writes=w, out=out, in_=in_)


def TT(k, eng, out, in0, in1, op, r, w):
    k.p.op(eng, "tensor_tensor", reads=r, writes=w, out=out, in0=in0, in1=in1, op=op)


def TS(k, eng, out, in0, s1, s2, op0, op1, r, w):
    if s2 is None:
        k.p.op(eng, "tensor_scalar", reads=r, writes=w, out=out, in0=in0, scalar1=s1, scalar2=None, op0=op0)
    else:
        k.p.op(eng, "tensor_scalar", reads=r, writes=w, out=out, in0=in0, scalar1=s1, scalar2=s2, op0=op0, op1=op1)


def STT(k, eng, out, in0, scalar, in1, op0, op1, r, w):
    k.p.op(eng, "scalar_tensor_tensor", reads=r, writes=w, out=out, in0=in0, scalar=scalar, in1=in1,
           op0=op0, op1=op1)


def norm_T(k, tiles, gain, dstT, b_dst, col0=0):
    p = k.p
    ar = k.ar
    gb = ar.alloc((D,), F32)
    b_gb = p.buf()
    p.dma(gb, gain.partition_broadcast(128), writes=[b_gb])
    xt = [ar.alloc((D,), F32) for _ in range(3)]
    b_xt = p.bufs(3)
    junk = ar.alloc((D,), BF16)
    b_junk = p.buf()
    hb = [ar.alloc((D,), BF16) for _ in range(2)]
    b_hb = p.bufs(2)
    ss = ar.alloc((len(tiles),), F32)
    b_ss = p.buf()
    pend = []
    for t, (src, rb) in enumerate(tiles):
        i = t % 2
        x3i = t % 3
        p.dma(xt[x3i], src, reads=rb, writes=[b_xt[x3i]], q=("sp" if t % 2 == 0 else "pool"))
        ACT(k, junk, xt[x3i], AF.Square, [b_xt[x3i]], [b_junk, b_ss], accum_out=ss[:, t:t + 1])
        ACT(k, ss[:, t:t + 1], ss[:, t:t + 1], AF.Sqrt, [b_ss], [b_ss], scale=1.0 / D, bias=EPS)
        p.op("dve", "reciprocal", reads=[b_ss], writes=[b_ss], out=ss[:, t:t + 1], in_=ss[:, t:t + 1])
        STT(k, "dve", hb[i], xt[x3i], ss[:, t:t + 1], gb, ALU.mult, ALU.mult, [b_xt[x3i], b_ss, b_gb], [b_hb[i]])

        def stage_b(t=t, i=i):
            for k4 in range(4):
                pb, bpb = k.ps.next()
                pbv = pb[:].bitcast(BF16)
                for j in range(4):
                    kt = k4 * 4 + j
                    TR(k, pbv[:, j * 128:(j + 1) * 128], hb[i][:, kt * 128:(kt + 1) * 128], k.ident, [b_hb[i]], [bpb])
                CP(k, "act" if k4 % 2 else "dve",
                   dstT[:, k4 * 4:(k4 + 1) * 4, col0 + t * 128:col0 + (t + 1) * 128],
                   pbv[:, 0:512].rearrange("p (a b) -> p a b", a=4), [bpb], [b_dst])
        if pend:
            pend.pop(0)()
        pend.append(stage_b)
    while pend:
        pend.pop(0)()


def load_w_block(k, ring, src):
    ap, b = ring.next()
    k.p.dma(ap, src, writes=[b], q="pool")
    return ap, b


def phase_inproj(k):
    p = k.p
    ar = k.ar
    I = k.I
    ar.reset()
    hT = ar.alloc((16, S), BF16)
    b_hT = p.buf("hT")
    k.hT = hT
    mark = ar.off
    tiles = [(I["x"][t * 128:(t + 1) * 128, :], []) for t in range(NT)]
    norm_T(k, tiles, I["mix_norm_g"], hT, b_hT)
    p.barrier()
    ar.off = mark
    wring = Ring([(ar.alloc((16, 512), BF16), p.buf()) for _ in range(2)])
    wba = ar.alloc((16, 16), BF16)
    b_wba = p.buf()
    raw = [ar.alloc((S + 4,), BF16) for _ in range(2)]
    b_raw = p.bufs(2)
    dgr = Ring([(ar.alloc((4, 128), BF16), p.buf()) for _ in range(2)])
    yr = Ring([(ar.alloc((S,), F32), p.buf()) for _ in range(2)])
    sqr = Ring([(ar.alloc((S,), BF16), p.buf()) for _ in range(2)])
    rsr = Ring([(ar.alloc((S,), F32), p.buf()) for _ in range(2)])
    obf = [ar.alloc((S,), BF16) for _ in range(2)]
    b_obf = p.bufs(2)
    tst = [ar.alloc((16, 128), BF16) for _ in range(2)]
    b_tst = p.bufs(2)
    zst = [ar.alloc((512,), BF16) for _ in range(3)]
    b_zst = p.bufs(3)
    cw = ar.alloc((96,), F32)
    cwl = ar.alloc((128,), F32)
    b_cw = p.buf()
    small = ar.alloc((64,), F32)
    b_small = p.buf()
    p.dma(cwl[0:96, :], I["gdn_conv_w"].rearrange("j (b q) -> (j b) q", q=128), writes=[b_cw])
    pb, bpb = k.ps.next()
    TR(k, pb[:, 0:96], cwl[0:96, :], k.identf[0:96, 0:96], [b_cw], [bpb])
    CP(k, "dve", cw, pb[:, 0:96], [bpb], [b_cw])
    for i in range(2):
        p.op("pool", "memset", writes=[b_raw[i]], ap=raw[i][:, 0:4], constant=0.0)

    wsrc = I["w_in"].rearrange("(kt q) n -> q kt n", q=128)
    ri = 0
    oi = 0
    ti = 0

    def fblock(wap, wb, f, evac):
        for c in range(4):
            pb, bpb = k.ps.next()
            for kt in range(16):
                MM(k, pb[:], wap[:, kt, f * 128:(f + 1) * 128], hT[:, kt, c * 512:(c + 1) * 512],
                   kt == 0, kt == 15, [wb, b_hT], [bpb])
            evac(pb, bpb, c * 512)

    deferred = []

    def make_post(fb, kind, hd, r_ap, r_b):
        st = {}

        def stage_a():
            nonlocal oi
            y, b_y = yr.next()
            st["y"] = (y, b_y)
            o_ap, o_b = obf[oi % 2], b_obf[oi % 2]
            oi += 1
            st["o"] = (o_ap, o_b)
            dg, b_dg = dgr.next()
            for j in range(4):
                TS(k, "dve", dg[:, j, :], k.identf, cw[:, j * 24 + fb:j * 24 + fb + 1], None, ALU.mult, None,
                   [k.b_c, b_cw], [b_dg])
            for c in range(4):
                pb, bpb = k.ps.next()
                for j in range(4):
                    MM(k, pb[:], dg[:, j, :], r_ap[:, c * 512 + j:c * 512 + j + 512], j == 0, j == 3, [b_dg, r_b], [bpb])
                if kind == 2:
                    ACT(k, o_ap[:, c * 512:(c + 1) * 512], pb[:], AF.Silu, [bpb], [o_b])
                else:
                    ACT(k, y[:, c * 512:(c + 1) * 512], pb[:], AF.Silu, [bpb], [b_y])
            if kind != 2:
                sq, b_sq = sqr.next()
                st["sq"] = (sq, b_sq)
                ACT(k, sq, y, AF.Square, [b_y], [b_sq])

        def stage_b():
            nonlocal ti
            y, b_y = st["y"]
            o_ap, o_b = st["o"]
            if kind != 2:
                sq, b_sq = st["sq"]
                rs, b_rs = rsr.next()
                for c in range(4):
                    pb, bpb = k.ps.next()
                    MM(k, pb[:], k.ones, sq[:, c * 512:(c + 1) * 512], True, True, [b_sq, k.b_c], [bpb])
                    if kind == 0:
                        ACT(k, rs[:, c * 512:(c + 1) * 512], pb[:], AF.Ln, [bpb], [b_rs], scale=128.0,
                            bias=128.0 * EPS)
                    else:
                        ACT(k, rs[:, c * 512:(c + 1) * 512], pb[:], AF.Ln, [bpb], [b_rs], scale=1.0, bias=EPS)
                ACT(k, rs, rs, AF.Exp, [b_rs], [b_rs], scale=-0.5)
                TT(k, "dve", o_ap, y, rs, ALU.mult, [b_y, b_rs], [o_b])
            if kind == 0:
                p.dma(k.QA[:, :, hd, :], o_ap.rearrange("p (c t) -> p c t", c=16), reads=[o_b],
                      writes=[k.b_scr["QA"]])
            if kind == 1:
                p.dma(k.KA[:, :, hd, :], o_ap.rearrange("p (c t) -> p c t", c=16), reads=[o_b],
                      writes=[k.b_scr["KA"]])
            if kind >= 1:
                t_ap, t_b = tst[ti % 2], b_tst[ti % 2]
                ti += 1
                for c4 in range(4):
                    pb, bpb = k.ps.next()
                    pbv = pb[:].bitcast(BF16)
                    for j in range(4):
                        c = c4 * 4 + j
                        TR(k, pbv[:, j * 128:(j + 1) * 128], o_ap[:, c * 128:(c + 1) * 128], k.ident, [o_b], [bpb])
                    CP(k, "act" if c4 % 2 else "dve", t_ap[:, c4 * 4:(c4 + 1) * 4, :],
                       pbv[:, 0:512].rearrange("p (a b) -> p a b", a=4), [bpb], [t_b])
                dst = k.KT if kind == 1 else k.VA
                p.dma(dst[:, :, hd, :], t_ap, reads=[t_b], writes=[k.b_scr["KT" if kind == 1 else "VA"]])
        return stage_a, stage_b

    for blk in range(6):
        wap, wb = load_w_block(k, wring, wsrc[:, :, blk * 512:(blk + 1) * 512])
        for f in range(4):
            fb = blk * 4 + f
            kind = fb // 8
            hd = fb % 8
            r_ap, r_b = raw[ri % 2], b_raw[ri % 2]
            ri += 1
            prev = deferred.pop(0) if deferred else None

            def evac_raw(pb, bpb, tok0, r_ap=r_ap, r_b=r_b, prev=prev):
                CP(k, "act", r_ap[:, 3 + tok0:3 + tok0 + 512], pb[:], [bpb], [r_b])
                if tok0 == 512 and prev is not None:
                    prev[0]()
            fblock(wap, wb, f, evac_raw)
            if prev is not None:
                prev[1]()
            deferred.append(make_post(fb, kind, hd, r_ap, r_b))
    for a_, b_ in deferred:
        a_()
        b_()
    zi = 0
    for blk in range(2):
        wap, wb = load_w_block(k, wring, wsrc[:, :, 3072 + blk * 512:3072 + (blk + 1) * 512])
        for t in range(NT):
            pb, bpb = k.ps.next()
            for kt in range(16):
                MM(k, pb[:], hT[:, kt, t * 128:(t + 1) * 128], wap[:, kt, :], kt == 0, kt == 15, [wb, b_hT], [bpb])
            z_ap, z_b = zst[zi % 3], b_zst[zi % 3]
            zi += 1
            ACT(k, z_ap, pb[:], AF.Silu, [bpb], [z_b])
            p.dma(k.ZS[:, t, blk * 4:(blk + 1) * 4, :], z_ap.rearrange("p (h d) -> p h d", h=4), reads=[z_b],
                  writes=[k.b_scr["ZS"]])
    p.dma(wba, wsrc[:, :, 4096:4112], writes=[b_wba], q="pool")
    for t in range(NT):
        pb, bpb = k.ps.next()
        for kt in range(16):
            MM(k, pb[:, 0:16], hT[:, kt, t * 128:(t + 1) * 128], wba[:, kt, :], kt == 0, kt == 15, [b_wba, b_hT], [bpb])
        CP(k, "dve", k.BA[:, t, :], pb[:, 0:16], [bpb], [k.b_BA])
    albc = small[:, 0:8]
    dtbc = small[:, 8:16]
    nea = small[:, 16:24]
    p.dma(albc, I["gdn_a_log"].partition_broadcast(128), writes=[b_small])
    p.dma(dtbc, I["gdn_dt_bias"].partition_broadcast(128), writes=[b_small])
    ACT(k, nea, albc, AF.Exp, [b_small], [b_small])
    TS(k, "dve", nea, nea, -1.0, None, ALU.mult, ALU.bypass, [b_small], [b_small])
    ACT(k, k.BETA[:], k.BA[:, :, 0:8], AF.Sigmoid, [k.b_BA], [k.b_bg])
    xa = ar.alloc((16, 8), F32)
    xb = ar.alloc((16, 8), F32)
    b_xa = p.buf()
    TT(k, "dve", xa, k.BA[:, :, 8:16], dtbc.unsqueeze(1).broadcast_to([128, 16, 8]), ALU.add, [k.b_BA, b_small], [b_xa])
    ACT(k, xb, xa, AF.Abs, [b_xa], [b_xa])
    ACT(k, xb, xb, AF.Exp, [b_xa], [b_xa], scale=-1.0)
    ACT(k, xb, xb, AF.Ln, [b_xa], [b_xa], bias=1.0)
    TS(k, "dve", xa, xa, 0.0, None, ALU.max, ALU.bypass, [b_xa], [b_xa])
    TT(k, "dve", xa, xa, xb, ALU.add, [b_xa], [b_xa])
    TT(k, "dve", k.GG[:], xa, nea.unsqueeze(1).broadcast_to([128, 16, 8]), ALU.mult, [b_xa, b_small], [k.b_bg])

    for blk in range(4):
        wap, wb = load_w_block(k, wring, wsrc[:, :, 4112 + blk * 512:4112 + (blk + 1) * 512])
        for f in range(4):
            fb = blk * 4 + f
            kind = fb // 8
            hd = fb % 8
            o_ap, o_b = obf[oi % 2], b_obf[oi % 2]
            oi += 1

            def evac_o(pb, bpb, tok0, o_ap=o_ap, o_b=o_b, c=[0]):
                CP(k, "act" if (tok0 // 512) % 2 else "dve", o_ap[:, tok0:tok0 + 512], pb[:], [bpb], [o_b])
            fblock(wap, wb, f, evac_o)
            dst = k.QB if kind == 0 else k.KB
            p.dma(dst[hd], o_ap, reads=[o_b], writes=[k.b_scr["QB" if kind == 0 else "KB"]])
    for blk in range(2):
        wap, wb = load_w_block(k, wring, wsrc[:, :, 6160 + blk * 512:6160 + (blk + 1) * 512])
        for t in range(NT):
            pb, bpb = k.ps.next()
            for kt in range(16):
                MM(k, pb[:], hT[:, kt, t * 128:(t + 1) * 128], wap[:, kt, :], kt == 0, kt == 15, [wb, b_hT], [bpb])
            z_ap, z_b = zst[zi % 3], b_zst[zi % 3]
            zi += 1
            CP(k, "act" if t % 2 else "dve", z_ap, pb[:], [bpb], [z_b])
            p.dma(k.VB[blk * 4:(blk + 1) * 4, :, t, :].rearrange("h p d -> p h d"),
                  z_ap.rearrange("p (h d) -> p h d", h=4), reads=[z_b], writes=[k.b_scr["VB"]])
    if hasattr(k, "DBG"):
        p.dma(k.DBG[:, 0:128], k.BETA[:].rearrange("p a b -> p (a b)"), reads=[k.b_bg], writes=[k.b_scr["DBG"]])
        p.dma(k.DBG[:, 128:256], k.GG[:].rearrange("p a b -> p (a b)"), reads=[k.b_bg], writes=[k.b_scr["DBG"]])


def phase_gdn(k):
    p = k.p
    ar = k.ar
    I = k.I
    ar.reset()
    oT = ar.alloc((16, S), BF16)
    k.oT = oT
    k.b_oT = p.buf("oT")
    k.oT_mark = ar.off
    S32 = ar.alloc((8, 128), F32)
    S16 = ar.alloc((8, 128), BF16)
    b_S32 = p.bufs(2)
    b_S16 = p.bufs(2)
    p.op("pool", "memset", writes=b_S32, ap=S32, constant=0.0)
    p.op("pool", "memset", writes=b_S16, ap=S16, constant=0.0)
    gnb = ar.alloc((128,), F32)
    b_gnb = p.buf()
    p.dma(gnb, I["gdn_norm_g"].partition_broadcast(128), writes=[b_gnb])
    names = ("QA", "KA", "KT", "VA", "ZS")
    inr = Ring([([ar.alloc((8, 128), BF16) for _ in names], p.bufs(5)) for _ in range(2)])
    sm = Ring([(ar.alloc((128,), F32), p.buf()) for _ in range(2)])
    kbg_, vb_, kdec_, zg_ = [ar.alloc((8, 128), BF16) for _ in range(4)]
    b_kbg, b_vb, b_kdec, b_zg = p.bufs(4)

    class G:
        pass
    grp = []
    for g in range(2):
        G_ = G()
        G_.diagG = ar.alloc((4, 128), F32)
        G_.decQ = ar.alloc((4, 128), BF16)
        G_.decA = ar.alloc((4, 128), BF16)
        G_.A = [ar.alloc((4, 128), BF16) for _ in range(2)]
        G_.B = [ar.alloc((4, 128), BF16) for _ in range(2)]
        G_.P = [ar.alloc((4, 128), BF16) for _ in range(2)]
        G_.qk = ar.alloc((4, 128), BF16)
        G_.qkT = ar.alloc((4, 128), BF16)
        G_.wT = ar.alloc((4, 128), BF16)
        G_.u = ar.alloc((4, 128), F32)
        G_.vnew = ar.alloc((4, 128), BF16)
        G_.o1 = ar.alloc((4, 128), F32)
        G_.o = ar.alloc((4, 128), F32)
        G_.sq = ar.alloc((4, 128), F32)
        G_.fin = ar.alloc((4, 128), BF16)
        G_.ss = ar.alloc((4,), F32)
        for nm in ("diagG", "decQ", "decA", "qk", "qkT", "wT", "u", "vnew", "o1", "o", "sq", "fin", "ss"):
            setattr(G_, "b_" + nm, p.buf())
        G_.b_A = p.bufs(2)
        G_.b_B = p.bufs(2)
        G_.b_P = p.bufs(2)
        grp.append(G_)

    def v512(ap):
        return ap.rearrange("p a b -> p (a b)")

    for c in range(16):
        (qT, kT, ktok, vtok, zs), bi = inr.next()
        b_qT, b_kT, b_ktok, b_vtok, b_zs = bi
        for ap, b, nm in zip((qT, kT, ktok, vtok, zs), bi, names):
            p.dma(ap, getattr(k, nm)[:, c, :, :], reads=[k.b_scr[nm]], writes=[b])
        smt, b_sm = sm.next()
        gc = smt[:, 0:8]
        e24 = smt[:, 8:32]
        eg = smt[:, 8:16]
        egrev = smt[:, 16:24]
        gl = smt[:, 24:32]
        bexp = smt[:, 32:40]
        gcl = smt[:, 40:48]
        pbA, bpA = k.ps.next()
        gsrc = k.GG[:, c, :]
        MM(k, pbA[:, 0:8], k.trif, gsrc, True, True, [k.b_c, k.b_bg], [bpA])
        MM(k, pbA[:, 8:16], k.suf, gsrc, True, True, [k.b_c, k.b_bg], [bpA])
        MM(k, pbA[:, 16:24], k.onesf, gsrc, True, True, [k.b_c, k.b_bg], [bpA])
        CP(k, "dve", gc, pbA[:, 0:8], [bpA], [b_sm])
        ACT(k, e24, pbA[:, 0:24], AF.Exp, [bpA], [b_sm])
        TT(k, "dve", bexp, k.BETA[:, c, :], eg, ALU.mult, [k.b_bg, b_sm], [b_sm])
        ACT(k, gcl, k.BETA[:, c, :], AF.Ln, [k.b_bg], [b_sm])
        TT(k, "dve", gcl, gcl, gc, ALU.add, [b_sm], [b_sm])
        bc = lambda a: a.unsqueeze(2).broadcast_to([128, 8, 128])
        TT(k, "pool", kbg_, ktok, bc(bexp), ALU.mult, [b_ktok, b_sm], [b_kbg])
        TT(k, "pool", vb_, vtok, bc(k.BETA[:, c, :]), ALU.mult, [b_vtok, k.b_bg], [b_vb])
        TT(k, "pool", kdec_, ktok, bc(egrev), ALU.mult, [b_ktok, b_sm], [b_kdec])
        TT(k, "pool", zg_, zs, gnb.unsqueeze(1).broadcast_to([128, 8, 128]), ALU.mult, [b_zs, b_gnb], [b_zg])
        bc4 = lambda a: a.unsqueeze(2).broadcast_to([128, 4, 128])
        for g, G_ in enumerate(grp):
            hs = slice(4 * g, 4 * g + 4)
            TT(k, "dve", G_.diagG, k.identf.unsqueeze(1).broadcast_to([128, 4, 128]), bc4(gc[:, hs]), ALU.mult,
               [k.b_c, b_sm], [G_.b_diagG])
        for g, G_ in enumerate(grp):
            for which, um, dec, b_dec, bias in ((0, k.umq, G_.decQ, G_.b_decQ, gc), (1, k.uma, G_.decA, G_.b_decA, gcl)):
                pR, bpR = k.ps.next()
                for hh in range(4):
                    MM(k, pR[:, hh * 128:(hh + 1) * 128], k.onesf, G_.diagG[:, hh, :], True, False,
                       [k.b_c, G_.b_diagG], [bpR])
                    MM(k, pR[:, hh * 128:(hh + 1) * 128], k.ident, um, False, True, [k.b_c], [bpR])
                for hh in range(4):
                    h = 4 * g + hh
                    ACT(k, dec[:, hh, :], pR[:, hh * 128:(hh + 1) * 128], AF.Exp, [bpR, b_sm], [b_dec],
                        scale=-1.0, bias=bias[:, h:h + 1])
        for g, G_ in enumerate(grp):
            pKK, bKK = k.ps.next()
            pQK, bQK = k.ps.next()
            for hh in range(4):
                h = 4 * g + hh
                MM(k, pKK[:, hh * 128:(hh + 1) * 128], kT[:, h, :], kT[:, h, :], True, True, [b_kT], [bKK])
                MM(k, pQK[:, hh * 128:(hh + 1) * 128], qT[:, h, :], kT[:, h, :], True, True, [b_qT, b_kT], [bQK])
            TT(k, "dve", v512(G_.A[0]), pKK[:], v512(G_.decA), ALU.mult, [bKK, G_.b_decA], [G_.b_A[0]])
            TT(k, "dve", v512(G_.qk), pQK[:], v512(G_.decQ), ALU.mult, [bQK, G_.b_decQ], [G_.b_qk])
        for g, G_ in enumerate(grp):
            for src, bsrc, dst, bdst, eng in ((G_.A[0], G_.b_A[0], G_.B[0], G_.b_B[0], "act"),
                                              (G_.qk, G_.b_qk, G_.qkT, G_.b_qkT, "dve")):
                pT, bpT = k.ps.next()
                pTv = pT[:].bitcast(BF16)
                for hh in range(4):
                    TR(k, pTv[:, hh * 128:(hh + 1) * 128], src[:, hh, :], k.ident, [bsrc], [bpT])
                CP(k, eng, v512(dst), pTv[:, 0:512], [bpT], [bdst])
            TT(k, "pool", G_.P[0], k.ident.unsqueeze(1).broadcast_to([128, 4, 128]), G_.B[0], ALU.subtract,
               [k.b_c, G_.b_B[0]], [G_.b_P[0]])
        for lvl in range(1, 7):
            cur = (lvl - 1) % 2
            nxt = lvl % 2
            for g, G_ in enumerate(grp):
                pA, bA = k.ps.next()
                for hh in range(4):
                    MM(k, pA[:, hh * 128:(hh + 1) * 128], G_.B[cur][:, hh, :], G_.A[cur][:, hh, :], True, True,
                       [G_.b_A[cur], G_.b_B[cur]], [bA])
                if lvl <= 5:
                    pB, bB = k.ps.next()
                    for hh in range(4):
                        MM(k, pB[:, hh * 128:(hh + 1) * 128], G_.A[cur][:, hh, :], G_.B[cur][:, hh, :], True, True,
                           [G_.b_A[cur], G_.b_B[cur]], [bB])
                CP(k, "act", v512(G_.A[nxt]), pA[:], [bA], [G_.b_A[nxt]])
                if lvl <= 5:
                    CP(k, "dve", v512(G_.B[nxt]), pB[:], [bB], [G_.b_B[nxt]])
            for g, G_ in enumerate(grp):
                pP, bP = k.ps.next()
                for hh in range(4):
                    MM(k, pP[:, hh * 128:(hh + 1) * 128], k.ident, G_.P[cur][:, hh, :], True, False,
                       [k.b_c, G_.b_P[cur]], [bP])
                    MM(k, pP[:, hh * 128:(hh + 1) * 128], G_.A[nxt][:, hh, :], G_.P[cur][:, hh, :], False, True,
                       [G_.b_A[nxt], G_.b_P[cur]], [bP])
                CP(k, "dve" if g else "act", v512(G_.P[nxt]), pP[:], [bP], [G_.b_P[nxt]])
        for g, G_ in enumerate(grp):
            TTm = G_.P[0]
            b_TT = G_.b_P[0]
            pW, bW = k.ps.next()
            pU, bU = k.ps.next()
            for hh in range(4):
                h = 4 * g + hh
                MM(k, pW[:, hh * 128:(hh + 1) * 128], kbg_[:, h, :], TTm[:, hh, :], True, True, [b_kbg, b_TT], [bW])
                MM(k, pU[:, hh * 128:(hh + 1) * 128], TTm[:, hh, :], vb_[:, h, :], True, True, [b_vb, b_TT], [bU])
            CP(k, "act", v512(G_.wT), pW[:], [bW], [G_.b_wT])
            CP(k, "dve", v512(G_.u), pU[:], [bU], [G_.b_u])
        for g, G_ in enumerate(grp):
            pVN, bVN = k.ps.next()
            pO1, bO1 = k.ps.next()
            for hh in range(4):
                h = 4 * g + hh
                MM(k, pVN[:, hh * 128:(hh + 1) * 128], G_.wT[:, hh, :], S16[:, h, :], True, True,
                   [G_.b_wT, b_S16[g]], [bVN])
                MM(k, pO1[:, hh * 128:(hh + 1) * 128], qT[:, h, :], S16[:, h, :], True, True, [b_qT, b_S16[g]], [bO1])
            TT(k, "dve", v512(G_.vnew), v512(G_.u), pVN[:], ALU.subtract, [G_.b_u, bVN], [G_.b_vnew])
            TT(k, "dve", G_.o1, pO1[:].rearrange("p (a b) -> p a b", a=4), bc4(eg[:, 4 * g:4 * g + 4]), ALU.mult,
               [bO1, b_sm], [G_.b_o1])
            TT(k, "pool", S32[:, 4 * g:4 * g + 4, :], S32[:, 4 * g:4 * g + 4, :], bc4(gl[:, 4 * g:4 * g + 4]), ALU.mult,
               [b_sm, b_S32[g]], [b_S32[g]])
        for g, G_ in enumerate(grp):
            pO2, bO2 = k.ps.next()
            pSU, bSU = k.ps.next()
            for hh in range(4):
                h = 4 * g + hh
                MM(k, pO2[:, hh * 128:(hh + 1) * 128], G_.qkT[:, hh, :], G_.vnew[:, hh, :], True, True,
                   [G_.b_qkT, G_.b_vnew], [bO2])
                MM(k, pSU[:, hh * 128:(hh + 1) * 128], kdec_[:, h, :], G_.vnew[:, hh, :], True, True,
                   [b_kdec, G_.b_vnew], [bSU])
            TT(k, "dve", v512(S32[:, 4 * g:4 * g + 4, :]), v512(S32[:, 4 * g:4 * g + 4, :]), pSU[:], ALU.add,
               [b_S32[g], bSU], [b_S32[g]])
            CP(k, "act", S16[:, 4 * g:4 * g + 4, :], S32[:, 4 * g:4 * g + 4, :], [b_S32[g]], [b_S16[g]])
            TT(k, "dve", v512(G_.o), pO2[:], v512(G_.o1), ALU.add, [bO2, G_.b_o1], [G_.b_o])
        for g, G_ in enumerate(grp):
            TT(k, "pool", G_.sq, G_.o, G_.o, ALU.mult, [G_.b_o], [G_.b_sq])
            p.op("dve", "tensor_reduce", reads=[G_.b_sq], writes=[G_.b_ss], out=G_.ss, in_=G_.sq, axis=AX.X, op=ALU.add)
            ACT(k, G_.ss, G_.ss, AF.Ln, [G_.b_ss], [G_.b_ss], scale=1.0 / 128, bias=EPS)
            ACT(k, G_.ss, G_.ss, AF.Exp, [G_.b_ss], [G_.b_ss], scale=-0.5)
            TT(k, "dve", G_.sq, G_.o, bc4(G_.ss), ALU.mult, [G_.b_o, G_.b_ss, G_.b_sq], [G_.b_sq])
            TT(k, "pool", G_.fin, G_.sq, zg_[:, 4 * g:4 * g + 4, :], ALU.mult, [G_.b_sq, b_zg], [G_.b_fin])
            pT, bpT = k.ps.next()
            pTv = pT[:].bitcast(BF16)
            for hh in range(4):
                TR(k, pTv[:, hh * 128:(hh + 1) * 128], G_.fin[:, hh, :], k.ident, [G_.b_fin], [bpT])
            CP(k, "act", oT[:, 4 * g:4 * g + 4, c * 128:(c + 1) * 128],
               pTv[:, 0:512].rearrange("p (a b) -> p a b", a=4), [bpT], [k.b_oT])


def phase_moba(k):
    p = k.p
    ar = k.ar
    I = k.I
    ar.off = k.oT_mark
    oT = k.oT
    hr = Ring([([ar.alloc((S,), BF16), ar.alloc((S,), BF16), ar.alloc((16, 132), BF16)], p.bufs(3)) for _ in range(2)])
    for (aps, bs) in hr.items:
        p.op("pool", "memset", writes=[bs[2]], ap=aps[2][:, :, 128:132], constant=1.0)
    tg = ar.alloc((8, 256), F32)
    Tp = ar.alloc((8, 256), BF16)
    rbb = ar.alloc((256,), F32)
    bmax = ar.alloc((8,), F32)
    mgb = ar.alloc((128,), F32)
    b_tp = p.buf()
    p.dma(tg, I["rb_toep"], writes=[b_tp])
    p.dma(rbb, I["rel_bias"].partition_broadcast(128), writes=[b_tp])
    p.dma(mgb, I["moba_norm_g"].partition_broadcast(128), writes=[b_tp])
    rb31 = rbb[:, 31 * 8:32 * 8]
    TT(k, "dve", tg, tg, rb31.unsqueeze(2).broadcast_to([128, 8, 256]), ALU.subtract, [b_tp], [b_tp])
    STT(k, "dve", Tp, tg, SQ128, k.cm.unsqueeze(1).broadcast_to([128, 8, 256]), ALU.mult, ALU.add, [b_tp, k.b_c], [b_tp])
    p.op("dve", "tensor_reduce", reads=[b_tp], writes=[b_tp], out=bmax, in_=rbb.rearrange("p (b h) -> p h b", h=8),
         axis=AX.X, op=ALU.max)
    TT(k, "dve", bmax, bmax, rb31, ALU.subtract, [b_tp], [b_tp])
    sq = ar.alloc((S,), BF16)
    ksq = ar.alloc((S,), BF16)
    b_sq, b_ksq = p.bufs(2)
    sm = ar.alloc((256,), F32)
    b_sm = p.buf()
    kms = sm[:, 0:8]
    km4 = sm[:, 8:12]
    ksc = sm[:, 12:13]
    nm = sm[:, 16:32]
    gsb = sm[:, 32:96].rearrange("p (a b) -> p a b", a=8)
    g2 = sm[:, 96:160].rearrange("p (a b) -> p a b", a=8)
    eq = sm[:, 160:224].rearrange("p (a b) -> p a b", a=8)
    mx = sm[:, 224:232]
    kmT = ar.alloc((8,), BF16)
    MBq = ar.alloc((16, 16), BF16)
    b_MBq = p.buf()
    p.op("pool", "memset", writes=[b_MBq], ap=MBq, constant=0.0)
    MB = ar.alloc((S,), BF16)
    b_MB = p.buf()
    PTr = Ring([(ar.alloc((256,), BF16), p.buf()) for _ in range(5)])
    yq = [ar.alloc((128,), F32) for _ in range(4)]
    b_yq = p.bufs(4)
    junkf = ar.alloc((128,), F32)
    b_junk = p.buf()
    fin = [ar.alloc((256,), BF16) for _ in range(2)]
    b_fin = p.bufs(2)
    sm2 = Ring([(ar.alloc((8,), F32), p.buf()) for _ in range(8)])
    bc8 = lambda a: a.unsqueeze(2).broadcast_to([128, 8, 8])

    MBs = [MB, ar.alloc((S,), BF16)]
    b_MBs = [b_MB, p.buf()]

    def prologue(h):
        MB, b_MB = MBs[h % 2], b_MBs[h % 2]
        (qbT, kbT, vb1), (b_q, b_k, b_v) = hr.next()
        p.dma(qbT, k.QB[h], reads=[k.b_scr["QB"]], writes=[b_q])
        p.dma(kbT, k.KB[h], reads=[k.b_scr["KB"]], writes=[b_k])
        p.dma(vb1[:, :, 0:128], k.VB[h], reads=[k.b_scr["VB"]], writes=[b_v])
        ACT(k, sq, qbT, AF.Square, [b_q], [b_sq])
        ACT(k, ksq, kbT, AF.Square, [b_k], [b_ksq])
        p.op("dve", "tensor_reduce", reads=[b_k], writes=[b_sm], out=kms, in_=kbT.rearrange("p (n t) -> p n t", n=8),
             axis=AX.X, op=ALU.add)
        TS(k, "dve", kmT, kms, 1.0 / 256, None, ALU.mult, None, [b_sm], [b_sm])
        pG, bG = k.ps.next()
        for qi in range(8, 16):
            MM(k, pG[:, (qi - 8) * 8:(qi - 7) * 8], qbT[:, qi * 128:(qi + 1) * 128], kmT, True, True, [b_q, b_sm], [bG])
        pN, bN = k.ps.next()
        for qi in range(16):
            MM(k, pN[:, qi:qi + 1], sq[:, qi * 128:(qi + 1) * 128], k.ones[:, 0:1], True, True, [b_sq, k.b_c], [bN])
        for c in range(4):
            pK, bK = k.ps.next()
            MM(k, pK[:], k.ones, ksq[:, c * 512:(c + 1) * 512], True, True, [b_ksq, k.b_c], [bK])
            p.op("dve", "tensor_reduce", reads=[bK], writes=[b_sm], out=km4[:, c:c + 1], in_=pK[:], axis=AX.X, op=ALU.max)
        p.op("dve", "tensor_reduce", reads=[b_sm], writes=[b_sm], out=ksc, in_=km4, axis=AX.X, op=ALU.max)
        TS(k, "dve", ksc, ksc, 1.0 / 128, None, ALU.mult, None, [b_sm], [b_sm])
        ACT(k, nm, pN[:, 0:16], AF.Sqrt, [bN, b_sm], [b_sm], scale=ksc)
        TS(k, "dve", MBq[:, :, 8], nm, bmax[:, h:h + 1], -SQ128, ALU.add, ALU.mult, [b_sm, b_tp], [b_MBq])
        TT(k, "dve", gsb, pG[:, 0:64].rearrange("p (a b) -> p a b", a=8), k.gm.rearrange("p (a b) -> p a b", a=8),
           ALU.add, [bG, k.b_c], [b_sm])
        src = gsb
        for it in range(2):
            p.op("dve", "tensor_reduce", reads=[b_sm], writes=[b_sm], out=mx, in_=src, axis=AX.X, op=ALU.max)
            TT(k, "dve", eq, src, bc8(mx), ALU.is_equal, [b_sm], [b_sm])
            STT(k, "dve", g2, eq, -1e30, src, ALU.mult, ALU.add, [b_sm], [b_sm])
            src = g2
        p.op("dve", "tensor_reduce", reads=[b_sm], writes=[b_sm], out=mx, in_=g2, axis=AX.X, op=ALU.max)
        TT(k, "dve", eq, gsb, bc8(mx), ALU.is_ge, [b_sm], [b_sm])
        TS(k, "dve", MBq[:, 8:16, 0:8], eq, BIG * SQ128, -BIG * SQ128, ALU.mult, ALU.add, [b_sm], [b_MBq])
        for half in range(2):
            pM, bM = k.ps.next()
            pMv = pM[:].bitcast(BF16)
            for j in range(8):
                qi = half * 8 + j
                TR(k, pMv[0:9, j * 128:(j + 1) * 128], MBq[:, qi, 0:9], k.ident, [b_MBq], [bM])
            CP(k, "act" if half else "dve", MB[0:9, half * 1024:(half + 1) * 1024], pMv[0:9, 0:1024], [bM], [b_MB])

        return (qbT, kbT, vb1, b_q, b_k, b_v, MB, b_MB)

    preps = {0: prologue(0)}
    for h in range(8):
        qbT, kbT, vb1, b_q, b_k, b_v, MB, b_MB = preps.pop(h)
        deferred = []
        for qb in range(8):
            if qb == 5 and h + 1 < 8:
                preps[h + 1] = prologue(h + 1)
            pend = []

            def emit_pv(item):
                kj_, PT_, b_PT_ = item
                for qt in range(2):
                    if kj_ <= 2 * qb + qt:
                        pO, bO = k.psx[qt]
                        MM(k, pO[:, 0:129], PT_[:, qt * 128:(qt + 1) * 128], vb1[:, kj_, 0:129],
                           kj_ == 0, kj_ == 2 * qb + qt, [b_PT_, b_v], [bO])
            for kj in range(2 * qb + 2):
                n = kj // 2
                pS, bS = k.ps.next()
                q0 = qb * 256
                v = n if (qb >= 4 and n < qb) else 8
                if n == qb and kj % 2 == 0:
                    segs = [(0, 256, Tp[:, h, 0:256])]
                elif n == qb:
                    segs = [(128, 256, Tp[:, h, 0:128])]
                elif n == qb - 1 and kj % 2 == 1:
                    segs = [(0, 128, Tp[:, h, 128:256]), (128, 256, None)]
                else:
                    segs = [(0, 256, None)]
                c0 = segs[0][0]
                for (a, b, bias) in segs:
                    MM(k, pS[:, a:b], kbT[:, kj * 128:(kj + 1) * 128], qbT[:, q0 + a:q0 + b], True, False, [b_k, b_q], [bS])
                    MM(k, pS[:, a:b], k.emat[:, v, :], MB[0:9, q0 + a:q0 + b], False, bias is None, [k.b_c, b_MB], [bS])
                    if bias is not None:
                        MM(k, pS[:, a:b], k.ident, bias, False, True, [k.b_c, b_tp], [bS])
                PT, b_PT = PTr.next()
                ACT(k, PT[:, c0:256], pS[:, c0:256], AF.Exp, [bS], [b_PT], scale=1.0 / SQ128)
                pend.append((kj, PT, b_PT))
                if len(pend) > 2:
                    emit_pv(pend.pop(0))
                if kj == min(3, 2 * qb + 1) and deferred:
                    deferred.pop(0)()
            while pend:
                emit_pv(pend.pop(0))
            parts = []
            for qt in range(2):
                s2, b_s2 = sm2.next()
                y_ap, y_b = yq[(qb % 2) * 2 + qt], b_yq[(qb % 2) * 2 + qt]
                pO, bO = k.psx[qt]
                p.op("dve", "reciprocal", reads=[bO], writes=[b_s2], out=s2[:, 0:1], in_=pO[:, 128:129])
                TS(k, "dve", y_ap, pO[:, 0:128], s2[:, 0:1], None, ALU.mult, None, [bO, b_s2], [y_b])
                parts.append((s2, b_s2, y_ap, y_b))

            def part2(qb=qb, parts=parts):
                f_ap, f_b = fin[qb % 2], b_fin[qb % 2]
                for qt in range(2):
                    s2, b_s2, y_ap, y_b = parts[qt]
                    TT(k, "dve", junkf, y_ap, y_ap, ALU.mult, [y_b], [b_junk])
                    p.op("dve", "tensor_reduce", reads=[b_junk], writes=[b_s2], out=s2[:, 1:2], in_=junkf, axis=AX.X,
                         op=ALU.add)
                    ACT(k, s2[:, 1:2], s2[:, 1:2], AF.Ln, [b_s2], [b_s2], scale=1.0 / 128, bias=EPS)
                    ACT(k, s2[:, 1:2], s2[:, 1:2], AF.Exp, [b_s2], [b_s2], scale=-0.5)
                    STT(k, "dve", f_ap[:, qt * 128:(qt + 1) * 128], y_ap, s2[:, 1:2], mgb, ALU.mult, ALU.mult,
                        [y_b, b_s2, b_tp], [f_b])
                pT, bpT = k.ps.next()
                pTv = pT[:].bitcast(BF16)
                for qt in range(2):
                    TR(k, pTv[:, qt * 128:(qt + 1) * 128], f_ap[:, qt * 128:(qt + 1) * 128], k.ident, [f_b], [bpT])
                CP(k, "act", oT[:, 8 + h, qb * 256:(qb + 1) * 256], pTv[:, 0:256], [bpT], [k.b_oT])
            deferred.append(part2)
        while deferred:
            deferred.pop(0)()
    if hasattr(k, "OT"):
        p.dma(k.OT, oT, reads=[k.b_oT], writes=[k.b_scr["DBG"]])


def phase_wout(k):
    p = k.p
    ar = k.ar
    I = k.I
    oT = k.oT
    ar.off = k.oT_mark
    h2T = ar.alloc((16, S), BF16)
    k.h2T = h2T
    k.b_h2T = p.buf("h2T")
    k.ws_mark = ar.off
    wring = Ring([(ar.alloc((16, 512), BF16), p.buf()) for _ in range(2)])
    xr = Ring([(ar.alloc((512,), F32), p.buf()) for _ in range(6)])
    wsrc = I["w_out"].rearrange("(kt q) n -> q kt n", q=128)
    for c in range(4):
        wap, wb = load_w_block(k, wring, wsrc[:, :, c * 512:(c + 1) * 512])
        for t in range(NT):
            x_ap, x_b = xr.next()
            p.dma(x_ap, I["x"][t * 128:(t + 1) * 128, c * 512:(c + 1) * 512], writes=[x_b])
            pb, bpb = k.ps.next()
            for kt in range(16):
                MM(k, pb[:], oT[:, kt, t * 128:(t + 1) * 128], wap[:, kt, :], kt == 0, kt == 15, [wb, k.b_oT], [bpb])
            TT(k, "dve", x_ap, pb[:], x_ap, ALU.add, [bpb, x_b], [x_b])
            p.dma(k.X1[t * 128:(t + 1) * 128, c * 512:(c + 1) * 512], x_ap, reads=[x_b], writes=[k.b_scr["X1"]])
    p.barrier()
    ar.off = k.ws_mark
    tiles = [(k.X1[t * 128:(t + 1) * 128, :], [k.b_scr["X1"]]) for t in range(NT)]
    norm_T(k, tiles, I["xattn_norm_g"], h2T, k.b_h2T)


def phase_xattn(k):
    p = k.p
    ar = k.ar
    I = k.I
    h2T = k.h2T
    ar.off = 0
    memT = ar.alloc((16, MEM), BF16)
    b_memT = p.buf()
    kxT = ar.alloc((4, MEM), BF16)
    vx = ar.alloc((2, 512), BF16)
    b_kv = p.buf()
    qxT = ar.alloc((4, S), BF16)
    b_qx = p.buf()
    oxT = ar.alloc((4, S), BF16)
    b_ox = p.buf()
    wxo = ar.alloc((4, D), BF16)
    b_wxo = p.buf()
    assert ar.off <= k.oT_mark
    ar.off = k.ws_mark
    tiles = [(I["mem"][t * 128:(t + 1) * 128, :], []) for t in range(2)]
    norm_T(k, tiles, I["mem_norm_g"], memT, b_memT)
    p.barrier()
    ar.off = k.ws_mark
    wring = Ring([(ar.alloc((16, 512), BF16), p.buf()) for _ in range(2)])
    Pf = Ring([(ar.alloc((256,), F32), p.buf()) for _ in range(4)])
    Pn = Ring([(ar.alloc((256,), BF16), p.buf()) for _ in range(4)])
    PnT = Ring([(ar.alloc((2, 128), BF16), p.buf()) for _ in range(4)])
    sm = Ring([(ar.alloc((8,), F32), p.buf()) for _ in range(8)])
    xr = Ring([(ar.alloc((512,), F32), p.buf()) for _ in range(3)])
    wkv = I["w_xkv"].rearrange("(kt q) n -> q kt n", q=128)
    wap, wb = load_w_block(k, wring, wkv[:, :, 0:512])
    for hx in range(4):
        pb, bpb = k.ps.next()
        for kt in range(16):
            MM(k, pb[:, 0:MEM], wap[:, kt, hx * 128:(hx + 1) * 128], memT[:, kt, :], kt == 0, kt == 15, [wb, b_memT], [bpb])
        CP(k, "act" if hx % 2 else "dve", kxT[:, hx, :], pb[:, 0:MEM], [bpb], [b_kv])
    wap, wb = load_w_block(k, wring, wkv[:, :, 512:1024])
    for mt in range(2):
        pb, bpb = k.ps.next()
        for kt in range(16):
            MM(k, pb[:], memT[:, kt, mt * 128:(mt + 1) * 128], wap[:, kt, :], kt == 0, kt == 15, [wb, b_memT], [bpb])
        CP(k, "act" if mt else "dve", vx[:, mt, :], pb[:], [bpb], [b_kv])
    wq = I["w_xq"].rearrange("(kt q) n -> q kt n", q=128)
    wap, wb = load_w_block(k, wring, wq)
    for hx in range(4):
        for c in range(4):
            pb, bpb = k.ps.next()
            for kt in range(16):
                MM(k, pb[:], wap[:, kt, hx * 128:(hx + 1) * 128], h2T[:, kt, c * 512:(c + 1) * 512], kt == 0, kt == 15,
                   [wb, k.b_h2T], [bpb])
            CP(k, "act" if c % 2 else "dve", qxT[:, hx, c * 512:(c + 1) * 512], pb[:], [bpb], [b_qx])
    p.dma(wxo, I["w_xo"].rearrange("(kt q) n -> q kt n", q=128), writes=[b_wxo], q="pool")
    for t in range(NT):
        st = []
        for hx in range(4):
            pS, bS = k.ps.next()
            MM(k, pS[:, 0:MEM], qxT[:, hx, t * 128:(t + 1) * 128], kxT[:, hx, :], True, True, [b_qx, b_kv], [bS])
            s_ap, s_b = sm.next()
            st.append([pS, bS, s_ap, s_b])
        for hx in range(4):
            pS, bS, s_ap, s_b = st[hx]
            p.op("dve", "tensor_reduce", reads=[bS], writes=[s_b], out=s_ap[:, 0:1], in_=pS[:, 0:MEM], axis=AX.X, op=ALU.max)
            TS(k, "dve", s_ap[:, 1:2], s_ap[:, 0:1], -1.0 / SQ128, None, ALU.mult, None, [s_b], [s_b])
        for hx in range(4):
            pS, bS, s_ap, s_b = st[hx]
            pf, b_pf = Pf.next()
            ACT(k, pf, pS[:, 0:MEM], AF.Exp, [bS, s_b], [b_pf, s_b], scale=1.0 / SQ128, bias=s_ap[:, 1:2],
                accum_out=s_ap[:, 2:3])
            st[hx] += [pf, b_pf]
        for hx in range(4):
            pS, bS, s_ap, s_b, pf, b_pf = st[hx]
            p.op("dve", "reciprocal", reads=[s_b], writes=[s_b], out=s_ap[:, 3:4], in_=s_ap[:, 2:3])
            pn, b_pn = Pn.next()
            TS(k, "dve", pn, pf, s_ap[:, 3:4], None, ALU.mult, None, [b_pf, s_b], [b_pn])
            st[hx] += [pn, b_pn]
        for hx in range(4):
            pn, b_pn = st[hx][6], st[hx][7]
            pT, bpT = k.ps.next()
            pTv = pT[:].bitcast(BF16)
            for mt in range(2):
                TR(k, pTv[:, mt * 128:(mt + 1) * 128], pn[:, mt * 128:(mt + 1) * 128], k.ident, [b_pn], [bpT])
            pnt, b_pnt = PnT.next()
            CP(k, "act" if hx % 2 else "dve", pnt, pTv[:, 0:256].rearrange("p (a b) -> p a b", a=2), [bpT], [b_pnt])
            st[hx] += [pnt, b_pnt]
        for hx in range(4):
            pnt, b_pnt = st[hx][8], st[hx][9]
            pO, bO = k.ps.next()
            for mt in range(2):
                MM(k, pO[:, 0:128], vx[:, mt, hx * 128:(hx + 1) * 128], pnt[:, mt, :], mt == 0, mt == 1, [b_kv, b_pnt], [bO])
            CP(k, "dve" if hx % 2 else "act", oxT[:, hx, t * 128:(t + 1) * 128], pO[:, 0:128], [bO], [b_ox])
    p.barrier()
    ar.off = k.ws_mark
    xrow = Ring([(ar.alloc((D,), F32), p.buf()) for _ in range(3)])
    for t in range(NT):
        x_ap, x_b = xrow.next()
        p.dma(x_ap, k.X1[t * 128:(t + 1) * 128, :], reads=[k.b_scr["X1"]], writes=[x_b],
              q=("sp" if t % 2 == 0 else "pool"))
        for c in range(4):
            pb, bpb = k.ps.next()
            for kt in range(4):
                MM(k, pb[:], oxT[:, kt, t * 128:(t + 1) * 128], wxo[:, kt, c * 512:(c + 1) * 512], kt == 0, kt == 3,
                   [b_wxo, b_ox], [bpb])
            TT(k, "dve", x_ap[:, c * 512:(c + 1) * 512], pb[:], x_ap[:, c * 512:(c + 1) * 512], ALU.add, [bpb, x_b], [x_b])
        p.dma(k.X2[t * 128:(t + 1) * 128, :], x_ap, reads=[x_b], writes=[k.b_scr["X2"]])


def phase_ffn(k):
    p = k.p
    ar = k.ar
    I = k.I
    HT = 1024
    halo = p.sbuf("halo", [128, NFF, 2], F32)
    b_halo = p.buf()
    p.op("pool", "memset", writes=[b_halo], ap=halo[:], constant=0.0)
    cwf = p.sbuf("cwf", [128, 4 * NFF], F32)
    b_cwf = p.buf()
    for hf in range(2):
        ar.reset()
        aT = ar.alloc((NFF, HT), BF16)
        b_aT = p.buf()
        mark0 = ar.off
        h3T = ar.alloc((16, HT), BF16)
        b_h3T = p.buf()
        mark1 = ar.off
        tiles = [(k.X2[(hf * 8 + t) * 128:(hf * 8 + t + 1) * 128, :], [k.b_scr["X2"]]) for t in range(8)]
        norm_T(k, tiles, I["ffn_norm_g"], h3T, b_h3T)
        p.barrier()
        ar.off = mark1
        if hf == 0:
            stg = ar.alloc((128,), F32)
            b_stg = p.buf()
            for j in range(4):
                src = (I["ffn_conv_w"][j] if j < 3 else I["ffn_conv_b"][0]).rearrange("(b q) -> b q", q=128)
                p.dma(stg[0:NFF, :], src, writes=[b_stg])
                pb, bpb = k.ps.next()
                TR(k, pb[:, 0:NFF], stg[0:NFF, :], k.identf[0:NFF, 0:NFF], [b_stg], [bpb])
                CP(k, "dve", cwf[:, j * NFF:(j + 1) * NFF], pb[:, 0:NFF], [bpb], [b_cwf])
        wg = Ring([(ar.alloc((16, 256), BF16), p.buf()) for _ in range(2)])
        wu = Ring([(ar.alloc((16, 256), BF16), p.buf()) for _ in range(2)])
        graw = Ring([(ar.alloc((HT + 2,), F32), p.buf()) for _ in range(2)])
        gy = Ring([(ar.alloc((HT,), F32), p.buf()) for _ in range(2)])
        wgs = I["w_gate"].rearrange("(kt q) n -> q kt n", q=128)
        wus = I["w_up"].rearrange("(kt q) n -> q kt n", q=128)
        for j in range(NFF):
            if j % 2 == 0:
                g_ap2, g_b = wg.next()
                u_ap2, u_b = wu.next()
                p.dma(g_ap2, wgs[:, :, j * 128:(j + 2) * 128], writes=[g_b], q="pool")
                p.dma(u_ap2, wus[:, :, j * 128:(j + 2) * 128], writes=[u_b], q="pool")
            g_ap = g_ap2[:, :, (j % 2) * 128:(j % 2 + 1) * 128]
            u_ap = u_ap2[:, :, (j % 2) * 128:(j % 2 + 1) * 128]
            r_ap, r_b = graw.next()
            y_ap, y_b = gy.next()
            pus = []
            for c in range(2):
                pg, bg = k.ps.next()
                for kt in range(16):
                    MM(k, pg[:], g_ap[:, kt, :], h3T[:, kt, c * 512:(c + 1) * 512], kt == 0, kt == 15, [g_b, b_h3T], [bg])
                CP(k, "act", r_ap[:, 2 + c * 512:2 + (c + 1) * 512], pg[:], [bg], [r_b])
            for c in range(2):
                pu, bu = k.ps.next()
                for kt in range(16):
                    MM(k, pu[:], u_ap[:, kt, :], h3T[:, kt, c * 512:(c + 1) * 512], kt == 0, kt == 15, [u_b, b_h3T], [bu])
                pus.append((pu, bu))
            CP(k, "pool", r_ap[:, 0:2], halo[:, j, :], [b_halo], [r_b])
            if hf == 0:
                CP(k, "pool", halo[:, j, :], r_ap[:, HT:HT + 2], [r_b], [b_halo])
            TS(k, "dve", y_ap, r_ap[:, 0:HT], cwf[:, j:j + 1], None, ALU.mult, None, [r_b, b_cwf], [y_b])
            for tap in range(1, 3):
                STT(k, "dve", y_ap, r_ap[:, tap:tap + HT], cwf[:, tap * NFF + j:tap * NFF + j + 1], y_ap, ALU.mult, ALU.add,
                    [r_b, b_cwf, y_b], [y_b])
            ACT(k, y_ap, y_ap, AF.Silu, [y_b, b_cwf], [y_b], bias=cwf[:, 3 * NFF + j:3 * NFF + j + 1])
            for c in range(2):
                pu, bu = pus[c]
                TT(k, "dve", aT[:, j, c * 512:(c + 1) * 512], y_ap[:, c * 512:(c + 1) * 512], pu[:], ALU.mult,
                   [y_b, bu], [b_aT])
        p.barrier()
        ar.off = mark0
        x3 = ar.alloc((8, D), F32)
        b_x3 = p.bufs(8)
        aflat = aT.rearrange("p a b -> p (a b)")
        fgb = aflat[:, 0:2 * D].bitcast(F32)
        junk = aflat[:, 2 * D:3 * D]
        b_fgb = b_aT
        b_junk = b_aT
        wd = Ring([(ar.alloc((4, 512), BF16), p.buf()) for _ in range(3)])
        xr = Ring([(ar.alloc((512,), F32), p.buf()) for _ in range(2)])
        ssf = ar.alloc((8,), F32)
        b_ssf = p.buf()
        for cc in range(4):
            for j4 in range(NFF // 4):
                w_ap, w_b = wd.next()
                p.dma(w_ap, I["w_down"][j4 * 512:(j4 + 1) * 512, cc * 512:(cc + 1) * 512].rearrange("(a q) n -> q a n", q=128),
                      writes=[w_b], q="pool")
                for a in range(4):
                    j = j4 * 4 + a
                    for tt in range(8):
                        pb, bpb = k.banks[tt]
                        MM(k, pb[:], aT[:, j, tt * 128:(tt + 1) * 128], w_ap[:, a, :], j == 0, j == NFF - 1, [w_b, b_aT], [bpb])
            for tt in range(8):
                pb, bpb = k.banks[tt]
                x_ap, x_b = xr.next()
                row = (hf * 8 + tt) * 128
                p.dma(x_ap, k.X2[row:row + 128, cc * 512:(cc + 1) * 512], reads=[k.b_scr["X2"]], writes=[x_b])
                TT(k, "dve", x3[:, tt, cc * 512:(cc + 1) * 512], pb[:], x_ap, ALU.add, [bpb, x_b], [b_x3[tt]])
        p.dma(fgb, I["final_norm_g"].partition_broadcast(128), writes=[b_aT])
        outs = []
        for tt in range(8):
            row = (hf * 8 + tt) * 128
            ACT(k, junk, x3[:, tt, :], AF.Square, [b_x3[tt]], [b_junk, b_ssf], accum_out=ssf[:, tt:tt + 1])
            ACT(k, ssf[:, tt:tt + 1], ssf[:, tt:tt + 1], AF.Sqrt, [b_ssf], [b_ssf], scale=1.0 / D, bias=EPS)
            p.op("dve", "reciprocal", reads=[b_ssf], writes=[b_ssf], out=ssf[:, tt:tt + 1], in_=ssf[:, tt:tt + 1])
            STT(k, "dve", x3[:, tt, :], x3[:, tt, :], ssf[:, tt:tt + 1], fgb, ALU.mult, ALU.mult,
                [b_x3[tt], b_ssf, b_fgb], [b_x3[tt]])
            outs.append(p.dma(k.out[row:row + 128, :], x3[:, tt, :], reads=[b_x3[tt]]))
        p.barrier()
    return []


_CACHE = {}


def make_in_maps(inputs, n=8, skip=()):
    cf, cb = host_consts()
    tidx = toeplitz_index()
    rb = np.asarray(inputs["rel_bias"], np.float32)
    rb_toep = np.ascontiguousarray(rb[tidx].transpose(0, 2, 1))
    shared = {
        "mix_norm_g": inputs["mix_norm_g"].reshape(1, D), "xattn_norm_g": inputs["xattn_norm_g"].reshape(1, D),
        "mem_norm_g": inputs["mem_norm_g"].reshape(1, D), "ffn_norm_g": inputs["ffn_norm_g"].reshape(1, D),
        "final_norm_g": inputs["final_norm_g"].reshape(1, D),
        "w_in": inputs["w_in"][0], "gdn_conv_w": inputs["gdn_conv_w"][0], "gdn_a_log": inputs["gdn_a_log"].reshape(1, 8),
        "gdn_dt_bias": inputs["gdn_dt_bias"].reshape(1, 8), "gdn_norm_g": inputs["gdn_norm_g"].reshape(1, 128),
        "moba_norm_g": inputs["moba_norm_g"].reshape(1, 128), "rel_bias": rb.reshape(1, 256), "rb_toep": rb_toep,
        "w_out": inputs["w_out"][0], "w_xq": inputs["w_xq"][0], "w_xkv": inputs["w_xkv"][0], "w_xo": inputs["w_xo"][0],
        "w_gate": inputs["w_gate"][0], "w_up": inputs["w_up"][0], "ffn_conv_w": inputs["ffn_conv_w"][0],
        "ffn_conv_b": inputs["ffn_conv_b"].reshape(1, DFF), "w_down": inputs["w_down"][0],
        "cst_f": cf, "cst_b": cb,
    }
    shared = {kk: (np.zeros((1, 1), np.float32) if kk in skip else np.ascontiguousarray(np.asarray(v, np.float32)))
              for kk, v in shared.items()}
    maps = []
    for b in range(n):
        m = dict(shared)
        m["x"] = np.ascontiguousarray(np.asarray(inputs["x"][b], np.float32))
        m["mem"] = (np.zeros((1, 1), np.float32) if "mem" in skip
                    else np.ascontiguousarray(np.asarray(inputs["mem"][b], np.float32)))
        maps.append(m)
    return maps


def kernel(**inputs):
    if "nc" not in _CACHE:
        _CACHE["nc"] = build_program()[0]
    nc = _CACHE["nc"]
    maps = make_in_maps(inputs, 8)
    res = run_bass_kernel_spmd(nc, maps, core_ids=list(range(8)))
    return np.stack([np.asarray(r["out"], np.float32) for r in res.results], axis=0)
```
